# Optimizing a Trainium2 kernel written in Bass

```python
import math
import jax
import jax.numpy as jnp
from jax import lax
import numpy as np

D_MODEL = 1024
BATCH = 32
SEQ = 2048
DEPTH = 2

GRID_W = 64
CTX_LEN = 256
EPS = 1e-6

ATT_HEAD_DIM = 64
ATT_Q_HEADS = (D_MODEL // 2) // ATT_HEAD_DIM
ATT_KV_HEADS = ATT_Q_HEADS // 2
ATT_GROUP = ATT_Q_HEADS // ATT_KV_HEADS
ATT_WIDTH = ATT_Q_HEADS * ATT_HEAD_DIM
ATT_KV_WIDTH = ATT_KV_HEADS * ATT_HEAD_DIM
ATT_IN = ATT_WIDTH + 2 * ATT_KV_WIDTH
Q_BLOCK = 128
ROPE_THETA = 10000.0

S5_WIDTH = D_MODEL // 4
S5_GROUP_CH = 16
S5_GROUPS = S5_WIDTH // S5_GROUP_CH
S5_STATE = 64

RWKV_WIDTH = D_MODEL // 4
RWKV_HEAD = 64
RWKV_HEADS = RWKV_WIDTH // RWKV_HEAD
DECAY_LORA = 64
AAA_LORA = 64
GATE_LORA = 128
RWKV_STREAM = 3 * RWKV_WIDTH + DECAY_LORA + AAA_LORA + GATE_LORA
SHORT_CONV = 3
GN_EPS = 64e-5

N_BRANCH = 3
OFF_S5 = ATT_IN
OFF_RWKV = OFF_S5 + S5_WIDTH
OFF_GATE = OFF_RWKV + RWKV_STREAM
IN_WIDTH = OFF_GATE + N_BRANCH * D_MODEL

N_EXPERT_GROUPS = 4
EXPERTS_PER_GROUP = 8
N_EXPERTS = N_EXPERT_GROUPS * EXPERTS_PER_GROUP
TOP_K = 2
D_EXPERT = D_MODEL // 2
MOE_BLOCK = 256

kernel_name = 'hybrid_attn_s5_rwkv7_hmoe_flow_block'


def rmsnorm(x, g):
    xf = x.astype(jnp.float32)
    y = xf * lax.rsqrt(jnp.mean(xf * xf, axis=-1, keepdims=True) + EPS)
    return (y * g.astype(jnp.float32)).astype(x.dtype)


def grid_positions(n_tokens):
    rows = n_tokens // GRID_W
    row = jnp.repeat(jnp.arange(rows, dtype=jnp.int32), GRID_W)
    col = jnp.tile(jnp.arange(GRID_W, dtype=jnp.int32), rows)
    return row, col


def rope_1d(x, pos):
    half = x.shape[-1] // 2
    inv = ROPE_THETA ** (-jnp.arange(half, dtype=jnp.float32) / half)
    ang = pos.astype(jnp.float32)[:, None] * inv[None, :]
    shape = (1, pos.shape[0]) + (1,) * (x.ndim - 3) + (half,)
    cos = jnp.cos(ang).reshape(shape).astype(x.dtype)
    sin = jnp.sin(ang).reshape(shape).astype(x.dtype)
    x1, x2 = x[..., :half], x[..., half:]
    return jnp.concatenate([x1 * cos - x2 * sin, x2 * cos + x1 * sin], axis=-1)


def axial_rope(x, row, col):
    h = x.shape[-1] // 2
    return jnp.concatenate([rope_1d(x[..., :h], row), rope_1d(x[..., h:], col)], axis=-1)


def attend(q, k, v):
    s = jnp.einsum('bqhgd,bkhd->bhgqk', q, k).astype(jnp.float32) * (ATT_HEAD_DIM ** -0.5)
    p = jax.nn.softmax(s, axis=-1).astype(v.dtype)
    return jnp.einsum('bhgqk,bkhd->bqhgd', p, v)


def attention_branch(a_lat, a_ctx, q_gain, k_gain, row, col, need_ctx):
    B, L, _ = a_lat.shape
    C = a_ctx.shape[1]

    def queries(a):
        q = a[..., :ATT_WIDTH].reshape(a.shape[:2] + (ATT_KV_HEADS, ATT_GROUP, ATT_HEAD_DIM))
        return rmsnorm(q, q_gain)

    def keys_values(a):
        k = a[..., ATT_WIDTH:ATT_WIDTH + ATT_KV_WIDTH].reshape(a.shape[:2] + (ATT_KV_HEADS, ATT_HEAD_DIM))
        v = a[..., ATT_WIDTH + ATT_KV_WIDTH:ATT_IN].reshape(a.shape[:2] + (ATT_KV_HEADS, ATT_HEAD_DIM))
        return rmsnorm(k, k_gain), v

    q_l = axial_rope(queries(a_lat), row, col)
    k_l, v_l = keys_values(a_lat)
    k_l = axial_rope(k_l, row, col)
    k_c, v_c = keys_values(a_ctx)
    k_all = jnp.concatenate([k_l, k_c], axis=1)
    v_all = jnp.concatenate([v_l, v_c], axis=1)
    n_blk = L // Q_BLOCK
    q_blocks = jnp.moveaxis(q_l.reshape((B, n_blk, Q_BLOCK) + q_l.shape[2:]), 1, 0)
    o = lax.map(lambda qb: attend(qb, k_all, v_all), q_blocks)
    o_lat = jnp.moveaxis(o, 0, 1).reshape(B, L, ATT_WIDTH)
    o_ctx = attend(queries(a_ctx), k_c, v_c).reshape(B, C, ATT_WIDTH) if need_ctx else None
    return o_lat, o_ctx


def s5_discretise(a_re, a_im, log_dt, b_re, b_im):
    dt = jnp.exp(log_dt.astype(jnp.float32))[:, None]
    lam_re = a_re.astype(jnp.float32)
    lam_im = a_im.astype(jnp.float32)
    mag = jnp.exp(lam_re * dt)
    ab_re = mag * jnp.cos(lam_im * dt)
    ab_im = mag * jnp.sin(lam_im * dt)
    den = lam_re * lam_re + lam_im * lam_im
    nr = ab_re - 1.0
    z_re = (nr * lam_re + ab_im * lam_im) / den
    z_im = (ab_im * lam_re - nr * lam_im) / den
    br = b_re.astype(jnp.float32)
    bi = b_im.astype(jnp.float32)
    bb_re = z_re[..., None] * br - z_im[..., None] * bi
    bb_im = z_re[..., None] * bi + z_im[..., None] * br
    return ab_re, ab_im, bb_re, bb_im


def complex_affine_combine(left, right):
    a1r, a1i, b1r, b1i = left
    a2r, a2i, b2r, b2i = right
    ar = a2r * a1r - a2i * a1i
    ai = a2r * a1i + a2i * a1r
    br = a2r * b1r - a2i * b1i + b2r
    bi = a2r * b1i + a2i * b1r + b2i
    return ar, ai, br, bi


def s5_scan(u, ab_re, ab_im, bb_re, bb_im, h0, reverse):
    bu_re = jnp.einsum('btgc,gnc->btgn', u, bb_re)
    bu_im = jnp.einsum('btgc,gnc->btgn', u, bb_im)
    a_re = jnp.broadcast_to(ab_re, bu_re.shape)
    a_im = jnp.broadcast_to(ab_im, bu_im.shape)
    acc_re, acc_im, h_re, h_im = lax.associative_scan(
        complex_affine_combine, (a_re, a_im, bu_re, bu_im), reverse=reverse, axis=1)
    if h0 is not None:
        h0_re, h0_im = h0[0][:, None], h0[1][:, None]
        h_re = h_re + acc_re * h0_re - acc_im * h0_im
        h_im = h_im + acc_re * h0_im + acc_im * h0_re
    return h_re, h_im


def s5_readout(h_re, h_im, c_re, c_im):
    return (jnp.einsum('btgn,gcn->btgc', h_re, c_re.astype(jnp.float32))
            - jnp.einsum('btgn,gcn->btgc', h_im, c_im.astype(jnp.float32)))


def s5_branch(s_lat, s_ctx, a_re, a_im, log_dt, b_re, b_im, c_re, c_im, d_skip, w_glu, need_ctx):
    def groups(s):
        return s.astype(jnp.float32).reshape(s.shape[:2] + (S5_GROUPS, S5_GROUP_CH))

    u_l, u_c = groups(s_lat), groups(s_ctx)
    ys_l, ys_c = [], []
    for d, rev in enumerate((False, True)):
        ab_re, ab_im, bb_re, bb_im = s5_discretise(a_re[d], a_im[d], log_dt[d], b_re[d], b_im[d])
        hc_re, hc_im = s5_scan(u_c, ab_re, ab_im, bb_re, bb_im, None, rev)
        edge = 0 if rev else -1
        hl_re, hl_im = s5_scan(u_l, ab_re, ab_im, bb_re, bb_im, (hc_re[:, edge], hc_im[:, edge]), rev)
        ys_l.append(s5_readout(hl_re, hl_im, c_re[d], c_im[d]))
        if need_ctx:
            ys_c.append(s5_readout(hc_re, hc_im, c_re[d], c_im[d]))

    def finish(ys, s):
        y = (ys[0] + ys[1]).reshape(s.shape) + d_skip.astype(jnp.float32) * s.astype(jnp.float32)
        z = jax.nn.gelu(y) @ w_glu.astype(jnp.float32)
        return (z[..., :S5_WIDTH] * jax.nn.sigmoid(z[..., S5_WIDTH:])).astype(s.dtype)

    return finish(ys_l, s_lat), (finish(ys_c, s_ctx) if need_ctx else None)


def short_conv(x, w):
    xp = jnp.pad(x, ((0, 0), (1, 1), (0, 0)))
    return xp[:, :-2] * w[0] + xp[:, 1:-1] * w[1] + xp[:, 2:] * w[2]


def rwkv_scan(r, decay, k, v, kk, kka, s0, reverse):
    def step(S, inp):
        r_t, w_t, k_t, v_t, kk_t, kka_t = inp
        sa = jnp.einsum('bhij,bhj->bhi', S, kk_t)
        S = S * w_t[:, :, None, :] - sa[..., None] * kka_t[:, :, None, :] + v_t[..., None] * k_t[:, :, None, :]
        y = None if r_t is None else jnp.einsum('bhij,bhj->bhi', S, r_t)
        return S, y

    def seq_first(t):
        return None if t is None else jnp.swapaxes(t, 0, 1)

    xs = tuple(seq_first(t) for t in (r, decay, k, v, kk, kka))
    s_fin, ys = lax.scan(step, s0, xs, reverse=reverse)
    return s_fin, seq_first(ys)


def rwkv_branch(z_lat, z_ctx, conv_w, w0, w2, a0, a2, g2, k_k, k_a, r_k, ln_w, ln_b, need_ctx):
    W = RWKV_WIDTH

    def heads(t):
        return t.reshape(t.shape[:-1] + (RWKV_HEADS, RWKV_HEAD))

    def streams(z):
        z = short_conv(z, conv_w).astype(jnp.float32)
        r, k, v = z[..., :W], z[..., W:2 * W], z[..., 2 * W:3 * W]
        o = 3 * W
        xw = jnp.tanh(z[..., o:o + DECAY_LORA])
        o += DECAY_LORA
        xa = z[..., o:o + AAA_LORA]
        o += AAA_LORA
        xg = z[..., o:]
        kk = heads(k * k_k.astype(jnp.float32))
        kk = kk * lax.rsqrt(jnp.sum(kk * kk, axis=-1, keepdims=True) + 1e-12)
        return r, k, v, xw, xa, xg, kk

    def direction_terms(k, xw, xa, d):
        wl = w0[d].astype(jnp.float32) + xw @ w2[d].astype(jnp.float32)
        decay = jnp.exp(-jnp.exp(-jax.nn.softplus(-wl) - 0.5))
        a = jax.nn.sigmoid(a0[d].astype(jnp.float32) + xa @ a2[d].astype(jnp.float32))
        kt = k * (1.0 + (a - 1.0) * k_a.astype(jnp.float32))
        return heads(decay), heads(a), heads(kt)

    r_l, k_l, v_l, xw_l, xa_l, xg_l, kk_l = streams(z_lat)
    r_c, k_c, v_c, xw_c, xa_c, xg_c, kk_c = streams(z_ctx)
    s0 = jnp.zeros((z_lat.shape[0], RWKV_HEADS, RWKV_HEAD, RWKV_HEAD), jnp.float32)
    ys_l, kts_l, ys_c, kts_c = [], [], [], []
    for d, rev in enumerate((False, True)):
        dec_c, a_c, kt_c = direction_terms(k_c, xw_c, xa_c, d)
        s_c, y_c = rwkv_scan(heads(r_c) if need_ctx else None, dec_c, kt_c, heads(v_c), kk_c, kk_c * a_c, s0, rev)
        dec_l, a_l, kt_l = direction_terms(k_l, xw_l, xa_l, d)
        _, y_l = rwkv_scan(heads(r_l), dec_l, kt_l, heads(v_l), kk_l, kk_l * a_l, s_c, rev)
        ys_l.append(y_l)
        kts_l.append(kt_l)
        if need_ctx:
            ys_c.append(y_c)
            kts_c.append(kt_c)

    def finish(ys, kts, r, v, xg, dtype):
        y = ys[0] + ys[1]
        mu = jnp.mean(y, axis=-1, keepdims=True)
        var = jnp.mean(jnp.square(y - mu), axis=-1, keepdims=True)
        y = (y - mu) * lax.rsqrt(var + GN_EPS) * heads(ln_w.astype(jnp.float32)) + heads(ln_b.astype(jnp.float32))
        bonus = jnp.sum(heads(r) * (kts[0] + kts[1]) * heads(r_k.astype(jnp.float32)), axis=-1, keepdims=True)
        y = y + bonus * heads(v)
        g = jax.nn.sigmoid(xg) @ g2.astype(jnp.float32)
        return (y.reshape(y.shape[:-2] + (W,)) * g).astype(dtype)

    o_lat = finish(ys_l, kts_l, r_l, v_l, xg_l, z_lat.dtype)
    o_ctx = finish(ys_c, kts_c, r_c, v_c, xg_c, z_ctx.dtype) if need_ctx else None
    return o_lat, o_ctx


def merge_branches(gate_logits, o_att, o_s5, o_rwkv, proj_att, proj_s5, proj_rwkv, w_out):
    g = jax.nn.sigmoid(gate_logits.astype(jnp.float32)).astype(o_att.dtype)
    g_att, g_s5, g_rw = jnp.split(g, N_BRANCH, axis=-1)
    merged = g_att * (o_att @ proj_att) + g_s5 * (o_s5 @ proj_s5) + g_rw * (o_rwkv @ proj_rwkv)
    return merged @ w_out


def moe_ffn(h, w_rg, b_rg, w_re, b_re, w_gate, w_up, w_down):
    T, D = h.shape
    hf = h.astype(jnp.float32)
    g_logits = hf @ w_rg.astype(jnp.float32) + b_rg.astype(jnp.float32)
    g_prob = jax.nn.softmax(g_logits, axis=-1)
    g_sel = jnp.argmax(g_logits, axis=-1).astype(jnp.int32)
    g_w = jnp.take_along_axis(g_prob, g_sel[:, None], axis=-1)
    e_logits = (hf @ w_re.astype(jnp.float32) + b_re.astype(jnp.float32)).reshape(T, N_EXPERT_GROUPS, EXPERTS_PER_GROUP)
    e_logits = jnp.take_along_axis(e_logits, g_sel[:, None, None], axis=1)[:, 0]
    e_prob = jax.nn.softmax(e_logits, axis=-1)
    top_p, top_i = lax.top_k(e_prob, TOP_K)
    gate = g_w * top_p / jnp.sum(top_p, axis=-1, keepdims=True)
    expert = g_sel[:, None] * EXPERTS_PER_GROUP + top_i.astype(jnp.int32)

    NK = T * TOP_K
    flat_e = expert.reshape(-1)
    flat_tok = jnp.repeat(jnp.arange(T, dtype=jnp.int32), TOP_K)
    flat_g = gate.reshape(-1)
    order = jnp.argsort(flat_e)
    se = flat_e[order]
    counts = jnp.bincount(flat_e, length=N_EXPERTS).astype(jnp.int32)
    padded = (counts + MOE_BLOCK - 1) // MOE_BLOCK * MOE_BLOCK
    pad_end = jnp.cumsum(padded)
    pad_start = pad_end - padded
    start = jnp.cumsum(counts) - counts
    dest = pad_start[se] + jnp.arange(NK, dtype=jnp.int32) - start[se]
    n_blocks = -(-NK // MOE_BLOCK) + N_EXPERTS
    P = n_blocks * MOE_BLOCK
    slot_tok = jnp.full((P,), T, jnp.int32).at[dest].set(flat_tok[order])
    slot_gate = jnp.zeros((P,), h.dtype).at[dest].set(flat_g[order].astype(h.dtype))
    blk_start = jnp.arange(n_blocks, dtype=jnp.int32) * MOE_BLOCK
    blk_expert = jnp.minimum(jnp.searchsorted(pad_end, blk_start, side='right'), N_EXPERTS - 1).astype(jnp.int32)
    h_pad = jnp.concatenate([h, jnp.zeros((1, D), h.dtype)], axis=0)

    def run_block(args):
        tok, e = args
        xb = h_pad[tok]
        return (jax.nn.silu(xb @ w_gate[e]) * (xb @ w_up[e])) @ w_down[e]

    out = lax.map(run_block, (slot_tok.reshape(n_blocks, MOE_BLOCK), blk_expert))
    y = jnp.zeros((T + 1, D), h.dtype).at[slot_tok].add(out.reshape(P, D) * slot_gate[:, None])
    return y[:T]


def setup_inputs(seed: int = 0) -> dict:
    key = jax.random.key(seed)
    keys = iter(jax.random.split(key, 64))
    f32 = jnp.float32

    def nrm(shape, scale):
        return jax.random.normal(next(keys), shape, f32) * scale

    def unif(shape, lo, hi):
        return jax.random.uniform(next(keys), shape, f32, lo, hi)

    D = D_MODEL
    G, N, CG, W = S5_GROUPS, S5_STATE, S5_GROUP_CH, RWKV_WIDTH
    E, F = N_EXPERTS, D_EXPERT
    return {
        'x': nrm((BATCH, SEQ, D), 1.0),
        'c': nrm((BATCH, D), 1.0),
        'ctx': nrm((BATCH, CTX_LEN, D), 1.0),
        'c_ctx': nrm((D,), 1.0),
        'w_mod': nrm((DEPTH, D, 6 * D), 0.5 * D ** -0.5),
        'b_mod': nrm((DEPTH, 6 * D), 0.02),
        'norm1': 1.0 + nrm((DEPTH, D), 0.05),
        'w_in': nrm((DEPTH, D, IN_WIDTH), D ** -0.5),
        'q_gain': 1.0 + nrm((DEPTH, ATT_HEAD_DIM), 0.05),
        'k_gain': 1.0 + nrm((DEPTH, ATT_HEAD_DIM), 0.05),
        's5_a_re': -0.5 + nrm((DEPTH, 2, G, N), 0.01),
        's5_a_im': math.pi * jnp.arange(N, dtype=f32) + nrm((DEPTH, 2, G, N), 0.01),
        's5_log_dt': unif((DEPTH, 2, G), math.log(1e-3), math.log(1e-1)),
        's5_b_re': nrm((DEPTH, 2, G, N, CG), (2 * CG) ** -0.5),
        's5_b_im': nrm((DEPTH, 2, G, N, CG), (2 * CG) ** -0.5),
        's5_c_re': nrm((DEPTH, 2, G, CG, N), (2 * N) ** -0.5),
        's5_c_im': nrm((DEPTH, 2, G, CG, N), (2 * N) ** -0.5),
        's5_d': nrm((DEPTH, S5_WIDTH), 1.0),
        's5_w_glu': nrm((DEPTH, S5_WIDTH, 2 * S5_WIDTH), S5_WIDTH ** -0.5),
        'rwkv_conv': jnp.array([0.25, 0.5, 0.25], f32)[None, :, None] + nrm((DEPTH, SHORT_CONV, RWKV_STREAM), 0.05),
        'rwkv_w0': unif((DEPTH, 2, W), -6.0, -1.0),
        'rwkv_w2': nrm((DEPTH, 2, DECAY_LORA, W), 0.1 * DECAY_LORA ** -0.5),
        'rwkv_a0': nrm((DEPTH, 2, W), 0.1),
        'rwkv_a2': nrm((DEPTH, 2, AAA_LORA, W), 0.1 * AAA_LORA ** -0.5),
        'rwkv_g2': nrm((DEPTH, GATE_LORA, W), GATE_LORA ** -0.5),
        'rwkv_k_k': 0.85 + nrm((DEPTH, W), 0.05),
        'rwkv_k_a': 1.0 + nrm((DEPTH, W), 0.05),
        'rwkv_r_k': nrm((DEPTH, W), 0.1),
        'rwkv_ln_w': 1.0 + nrm((DEPTH, W), 0.05),
        'rwkv_ln_b': nrm((DEPTH, W), 0.02),
        'proj_att': nrm((DEPTH, ATT_WIDTH, D), ATT_WIDTH ** -0.5),
        'proj_s5': nrm((DEPTH, S5_WIDTH, D), S5_WIDTH ** -0.5),
        'proj_rwkv': nrm((DEPTH, W, D), W ** -0.5),
        'w_out': nrm((DEPTH, D, D), D ** -0.5),
        'norm2': 1.0 + nrm((DEPTH, D), 0.05),
        'router_g_w': nrm((DEPTH, D, N_EXPERT_GROUPS), D ** -0.5),
        'router_g_b': nrm((DEPTH, N_EXPERT_GROUPS), 0.01),
        'router_e_w': nrm((DEPTH, D, E), D ** -0.5),
        'router_e_b': nrm((DEPTH, E), 0.01),
        'exp_w_gate': nrm((DEPTH, E, D, F), D ** -0.5),
        'exp_w_up': nrm((DEPTH, E, D, F), D ** -0.5),
        'exp_w_down': nrm((DEPTH, E, F, D), F ** -0.5),
    }


def reference(x, c, ctx, c_ctx, w_mod, b_mod, norm1, w_in, q_gain, k_gain,
              s5_a_re, s5_a_im, s5_log_dt, s5_b_re, s5_b_im, s5_c_re, s5_c_im, s5_d, s5_w_glu,
              rwkv_conv, rwkv_w0, rwkv_w2, rwkv_a0, rwkv_a2, rwkv_g2, rwkv_k_k, rwkv_k_a, rwkv_r_k,
              rwkv_ln_w, rwkv_ln_b, proj_att, proj_s5, proj_rwkv, w_out, norm2,
              router_g_w, router_g_b, router_e_w, router_e_b, exp_w_gate, exp_w_up, exp_w_down):
    B, L, D = x.shape
    C = ctx.shape[1]
    row, col = grid_positions(L)
    xl, xc = x, ctx
    for l in range(DEPTH):
        need_ctx = l < DEPTH - 1
        mod_lat = (jax.nn.silu(c) @ w_mod[l] + b_mod[l])[:, None, :]
        mod_ctx = jax.nn.silu(c_ctx) @ w_mod[l] + b_mod[l]
        sh1_l, sc1_l, g1_l, sh2_l, sc2_l, g2_l = jnp.split(mod_lat, 6, axis=-1)
        sh1_c, sc1_c, g1_c, sh2_c, sc2_c, g2_c = jnp.split(mod_ctx, 6, axis=-1)

        u_lat = (rmsnorm(xl, norm1[l]) * (1.0 + sc1_l) + sh1_l) @ w_in[l]
        u_ctx = (rmsnorm(xc, norm1[l]) * (1.0 + sc1_c) + sh1_c) @ w_in[l]
        o_att_l, o_att_c = attention_branch(u_lat[..., :OFF_S5], u_ctx[..., :OFF_S5],
                                            q_gain[l], k_gain[l], row, col, need_ctx)
        o_s5_l, o_s5_c = s5_branch(u_lat[..., OFF_S5:OFF_RWKV], u_ctx[..., OFF_S5:OFF_RWKV],
                                   s5_a_re[l], s5_a_im[l], s5_log_dt[l], s5_b_re[l], s5_b_im[l],
                                   s5_c_re[l], s5_c_im[l], s5_d[l], s5_w_glu[l], need_ctx)
        o_rw_l, o_rw_c = rwkv_branch(u_lat[..., OFF_RWKV:OFF_GATE], u_ctx[..., OFF_RWKV:OFF_GATE],
                                     rwkv_conv[l], rwkv_w0[l], rwkv_w2[l], rwkv_a0[l], rwkv_a2[l],
                                     rwkv_g2[l], rwkv_k_k[l], rwkv_k_a[l], rwkv_r_k[l],
                                     rwkv_ln_w[l], rwkv_ln_b[l], need_ctx)
        xl = xl + g1_l * merge_branches(u_lat[..., OFF_GATE:], o_att_l, o_s5_l, o_rw_l,
                                        proj_att[l], proj_s5[l], proj_rwkv[l], w_out[l])

        h2_l = rmsnorm(xl, norm2[l]) * (1.0 + sc2_l) + sh2_l
        if need_ctx:
            xc = xc + g1_c * merge_branches(u_ctx[..., OFF_GATE:], o_att_c, o_s5_c, o_rw_c,
                                            proj_att[l], proj_s5[l], proj_rwkv[l], w_out[l])
            h2_c = rmsnorm(xc, norm2[l]) * (1.0 + sc2_c) + sh2_c
            tokens = jnp.concatenate([h2_l.reshape(B * L, D), h2_c.reshape(B * C, D)], axis=0)
            y = moe_ffn(tokens, router_g_w[l], router_g_b[l], router_e_w[l], router_e_b[l],
                        exp_w_gate[l], exp_w_up[l], exp_w_down[l])
            xl = xl + g2_l * y[:B * L].reshape(B, L, D)
            xc = xc + g2_c * y[B * L:].reshape(B, C, D)
        else:
            y = moe_ffn(h2_l.reshape(B * L, D), router_g_w[l], router_g_b[l], router_e_w[l], router_e_b[l],
                        exp_w_gate[l], exp_w_up[l], exp_w_down[l])
            xl = xl + g2_l * y.reshape(B, L, D)
    return xl
```

```python
import contextlib
import math
import numpy as np
import concourse.bass as bass
import concourse.mybir as mybir
from concourse.bass_utils import run_bass_kernel_spmd

F32 = mybir.dt.float32
BF16 = mybir.dt.bfloat16
F32R = mybir.dt.float32r
I32 = mybir.dt.int32
U32 = mybir.dt.uint32
ALU = mybir.AluOpType
AF = mybir.ActivationFunctionType
AX = mybir.AxisListType

NCORES = 8
NBC = 4
DM = 1024
LAT = 2048
CTX = 256
T = LAT + CTX
DEPTH = 2
INW = 5376
OFF_S5 = 1024
OFF_RW = 1280
OFF_GATE = 2304
EPS = 1e-6
NEXP = 32
DEXP = 512
CAP = 1536


class Tok:
    __slots__ = ("lw", "rd", "name")

    def __init__(self, name=""):
        self.lw = None
        self.rd = {}
        self.name = name


class TT:
    def __init__(self, t, name):
        self.t = t
        self.name = name
        self.tok = Tok(name)
        self.slots = {}

    def __getitem__(self, idx):
        return self.t[idx]

    def s(self, k):
        if k not in self.slots:
            self.slots[k] = Tok(f"{self.name}.{k}")
        return self.slots[k]


def _tok(x):
    return x.tok if isinstance(x, TT) else x


class Prog:
    NDMA = 6

    def __init__(self, nc, debug=False):
        self.nc = nc
        self.debug = debug
        self.es = contextlib.ExitStack()
        self.engs = {"pe": nc.tensor, "act": nc.scalar, "dve": nc.vector, "pool": nc.gpsimd, "sp": nc.sync}
        self.sems = []
        self.vals = []
        self.esem = {}
        for n in self.engs:
            self.esem[n] = self._newsem("e_" + n)
        self.dsem = {}
        self.dnext = {}
        for q in ("sp", "act", "pool"):
            self.dsem[q] = [self._newsem(f"d_{q}{i}") for i in range(self.NDMA)]
            self.dnext[q] = 0
        self.seen = {n: {} for n in self.engs}
        self.ninstr = 0
        self.nwait = 0
        self.uid = 0

    def _newsem(self, name):
        s = self.es.enter_context(self.nc.semaphore(name))
        self.sems.append(s)
        self.vals.append(0)
        return len(self.sems) - 1

    def sb(self, name, shape, dtype=F32, stack=None):
        self.uid += 1
        t = (stack if stack is not None else self.es).enter_context(
            self.nc.sbuf_tensor(f"{name}_{self.uid}", list(shape), dtype))
        return TT(t, name)

    def ps(self, name, shape, dtype=F32, stack=None):
        self.uid += 1
        t = (stack if stack is not None else self.es).enter_context(
            self.nc.psum_tensor(f"{name}_{self.uid}", list(shape), dtype))
        return TT(t, name)

    def dram(self, name, shape, dtype=F32, kind="Internal"):
        if kind == "Internal" and self.debug:
            kind = "ExternalOutput"
        t = self.nc.dram_tensor(name, list(shape), dtype, kind=kind)
        return TT(t.ap(), name)

    def _need(self, eng, ev):
        if ev is None:
            return
        s, v = ev
        if self.seen[eng].get(s, 0) >= v:
            return
        self.engs[eng].wait_ge(self.sems[s], v)
        self.seen[eng][s] = v
        self.nwait += 1

    def _deps(self, eng, R, W):
        for x in R:
            self._need(eng, _tok(x).lw)
        for x in W:
            tk = _tok(x)
            self._need(eng, tk.lw)
            for s, v in tk.rd.items():
                self._need(eng, (s, v))

    def _mark(self, ev, R, W):
        for x in R:
            _tok(x).rd[ev[0]] = ev[1]
        for x in W:
            tk = _tok(x)
            tk.lw = ev
            tk.rd = {}

    def op(self, eng, meth, R=(), W=(), **kw):
        self._deps(eng, R, W)
        ins = getattr(self.engs[eng], meth)(**kw)
        s = self.esem[eng]
        self.vals[s] += 1
        ins.then_inc(self.sems[s], 1)
        self._mark((s, self.vals[s]), R, W)
        self.ninstr += 1
        return ins

    def V(self, meth, **kw):
        return self.op("dve", meth, **kw)

    def A(self, meth, **kw):
        return self.op("act", meth, **kw)

    def G(self, meth, **kw):
        return self.op("pool", meth, **kw)

    def PE(self, meth, **kw):
        return self.op("pe", meth, **kw)

    def mm(self, out, lhsT, rhs, start, stop, R, W):
        return self.op("pe", "matmul", R=R, W=W, out=out, lhsT=lhsT, rhs=rhs, start=start, stop=stop)

    def dma(self, q, R=(), W=(), meth="dma_start", **kw):
        self._deps(q, R, W)
        k = self.dnext[q]
        self.dnext[q] = (k + 1) % self.NDMA
        s = self.dsem[q][k]
        if self.vals[s] > 0:
            self._need(q, (s, self.vals[s]))
        ins = getattr(self.engs[q], meth)(**kw)
        self.vals[s] += 16
        ins.then_inc(self.sems[s], 16)
        self._mark((s, self.vals[s]), R, W)
        self.ninstr += 1
        return ins

    def barrier(self):
        for e in self.engs:
            for s in range(len(self.sems)):
                if self.vals[s] > 0:
                    self._need(e, (s, self.vals[s]))

    def finish(self, toks):
        for x in toks:
            self._need("sp", _tok(x).lw)

    def close(self):
        self.es.close()


class NS:
    pass


def fm(v):
    v = np.asarray(v, np.float32)
    lead = v.shape[:-1]
    n = v.shape[-1] // 128
    r = v.reshape(lead + (n, 128))
    return np.ascontiguousarray(np.moveaxis(r, -1, 0))


def make_consts():
    c = {}
    c["ident"] = np.eye(128, dtype=np.float32)
    c["ones"] = np.ones((128, 128), np.float32)
    bd = np.zeros((128, 128), np.float32)
    bd[:64, :64] = 1.0 / 64
    bd[64:, 64:] = 1.0 / 64
    c["bd64"] = bd
    bd1 = np.zeros((128, 128), np.float32)
    bd1[:64, :64] = 1.0
    bd1[64:, 64:] = 1.0
    c["bs64"] = bd1
    rot = np.zeros((128, 128), np.float32)
    for hh in range(2):
        for blk in range(2):
            base = hh * 64 + blk * 32
            for i in range(16):
                rot[base + 16 + i, base + i] = -1.0
                rot[base + i, base + 16 + i] = 1.0
    c["rot"] = rot
    pos = np.arange(LAT)
    row = pos // 64
    col = pos % 64
    inv = 10000.0 ** (-np.arange(16, dtype=np.float32) / 16.0)
    cos = np.zeros((64, LAT), np.float32)
    sin = np.zeros((64, LAT), np.float32)
    for blk, pp in enumerate((row, col)):
        ang = pp[None, :].astype(np.float32) * inv[:, None]
        cc = np.cos(ang).astype(np.float32)
        ss = np.sin(ang).astype(np.float32)
        cos[blk * 32:blk * 32 + 16] = cc
        cos[blk * 32 + 16:blk * 32 + 32] = cc
        sin[blk * 32:blk * 32 + 16] = ss
        sin[blk * 32 + 16:blk * 32 + 32] = ss
    c["cos"] = np.concatenate([cos, cos], 0)
    c["sin"] = np.concatenate([sin, sin], 0)
    sh0 = np.zeros((128, 128), np.float32)
    sh1 = np.zeros((128, 128), np.float32)
    for j in range(64):
        sh0[64 + j, j] = 1.0
        sh1[j, 64 + j] = 1.0
    c["sh0"] = sh0
    c["sh1"] = sh1
    ss = np.arange(64)[:, None]
    tt = np.arange(64)[None, :]
    for d in range(2):
        before = (ss < tt) if d == 0 else (ss > tt)
        beq = (ss <= tt) if d == 0 else (ss >= tt)
        st_ = before.astype(np.float32)
        inc = beq.astype(np.float32)
        c[f"MA{d}"] = np.tile(np.concatenate([-st_, -inc], 1), (1, 4))
        c[f"MB{d}"] = np.tile(np.concatenate([st_, inc], 1), (1, 4))
        c[f"MC{d}"] = np.tile(-(st_.T), (1, 4))
    c["YXI"] = np.tile(np.concatenate([np.zeros((64, 64), np.float32), np.eye(64, dtype=np.float32)], 1), (1, 4))
    rm = np.ones((128, T), np.float32)
    rm[:, ::64] = 0.0
    c["rmask"] = rm
    c["eoff"] = np.tile((np.arange(NEXP, dtype=np.float32) * CAP)[None, :], (128, 1))
    mg = np.zeros((128, 8), np.float32)
    for p in range(128):
        mg[p, p // 16] = 1.0
    c["maskG"] = mg
    tri = np.zeros((128, 128), np.float32)
    for k in range(128):
        tri[k, k + 1:] = 1.0
    c["tri"] = tri
    return c


def phase_mod(P, l, D, S):
    modT = S.modT
    with contextlib.ExitStack() as st:
        wm = [P.sb(f"wm{i}", [128, 8, 1024], F32, st) for i in range(2)]
        ct = P.sb("ct", [128, 8, 8], F32, st)
        sct = P.sb("sct", [128, 8, 8], F32, st)
        psA = S.psum[0]
        P.dma("sp", R=[D.cT], W=[ct], out=ct[:], in_=D.cT[:, :, :])
        P.A("activation", R=[ct], W=[sct], out=sct[:], in_=ct[:], func=AF.Silu)
        for j in range(6):
            w = wm[j % 2]
            P.dma("sp", R=[D.w_mod], W=[w], out=w[:],
                  in_=D.w_mod[l, :, j * 1024:(j + 1) * 1024].rearrange("(kt p) n -> p kt n", p=128))
            for m in range(8):
                o = (j * 8 + m) * 8
                for kt in range(8):
                    P.mm(psA[:, o:o + 8], w[:, kt, m * 128:(m + 1) * 128], sct[:, kt, :], kt == 0, kt == 7,
                         R=[w, sct], W=[psA])
        P.V("tensor_tensor", R=[psA, S.bmodT], W=[modT],
            out=modT[:], in0=psA[:, 0:384].rearrange("p (a b) -> p a b", b=8),
            in1=S.bmodT[:, l, :].unsqueeze(2).to_broadcast([128, 48, 8]), op=ALU.add)
        for j, nt in ((1, S.norm1T), (4, S.norm2T)):
            P.V("tensor_scalar", R=[modT], W=[modT], out=modT[:, j * 8:(j + 1) * 8, :], in0=modT[:, j * 8:(j + 1) * 8, :],
                scalar1=1.0, scalar2=None, op0=ALU.add)
            P.V("tensor_tensor", R=[modT, nt], W=[modT], out=modT[:, j * 8:(j + 1) * 8, :],
                in0=modT[:, j * 8:(j + 1) * 8, :],
                in1=nt[:, l, :].unsqueeze(2).to_broadcast([128, 8, 8]), op=ALU.mult)
    P.barrier()


TILES = [(0, 256)] + [(256 + i * 512, 512) for i in range(4)]


def rms_mod(P, S, x_t, n, col, jsc, jsh, out_fn, sq, rstd, ps):
    P.A("activation", R=[x_t], W=[sq], out=sq[:, :, 0:n], in_=x_t[:, :, 0:n], func=AF.Square)
    for kt in range(8):
        P.mm(ps[:, 0:n], S.ones[:], sq[:, kt, 0:n], kt == 0, kt == 7, R=[S.ones, sq], W=[ps])
    P.A("activation", R=[ps], W=[rstd], out=rstd[:, 0:n], in_=ps[:, 0:n], func=AF.Sqrt, scale=1.0 / DM,
        bias=S.epsb[:, 0:1])
    P.V("reciprocal", R=[rstd], W=[rstd], out=rstd[:, 0:n], in_=rstd[:, 0:n])
    for kt in range(8):
        P.V("tensor_tensor", R=[x_t, rstd], W=[sq], out=sq[:, kt, 0:n], in0=x_t[:, kt, 0:n], in1=rstd[:, 0:n],
            op=ALU.mult)
        o, wt = out_fn(kt)
        P.V("tensor_scalar", R=[sq, S.modT], W=wt, out=o, in0=sq[:, kt, 0:n],
            scalar1=S.modT[:, jsc * 8 + kt, col:col + 1], scalar2=S.modT[:, jsh * 8 + kt, col:col + 1],
            op0=ALU.mult, op1=ALU.add)


def phase_inproj(P, l, D, S, b, xsrc):
    with contextlib.ExitStack() as st:
        hT = P.sb("hT", [128, 8, T], BF16, st)
        xt = [P.sb(f"xt{i}", [128, 8, 512], F32, st) for i in range(2)]
        sq = P.sb("sq", [128, 8, 512], F32, st)
        rstd = P.sb("rstd", [128, 512], F32, st)
        wb = [P.sb(f"wb{i}", [128, 8, 768], BF16, st) for i in range(2)]
        stg = [P.sb(f"stg{i}", [128, T], F32, st) for i in range(2)]
        for ti, (t0, n) in enumerate(TILES):
            col = 4 if t0 < CTX else b
            x_t = xt[ti % 2]
            P.dma("sp", R=[xsrc], W=[x_t], out=x_t[:, :, 0:n],
                  in_=xsrc[b, :, t0:t0 + n].rearrange("(kt p) t -> p kt t", p=128))
            rms_mod(P, S, x_t, n, col, 1, 0, lambda kt: (hT[:, kt, t0:t0 + n], [hT.s(ti)]), sq, rstd, S.psum[0])
        hall = [hT.s(ti) for ti in range(len(TILES))]
        ci = 0
        for cb in range(7):
            w = wb[cb % 2]
            P.dma("pool", R=[D.w_in], W=[w], out=w[:],
                  in_=D.w_in[l, :, cb * 768:(cb + 1) * 768].rearrange("(kt p) n -> p kt n", p=128))
            for m in range(6):
                chunk = cb * 6 + m
                sg = stg[chunk % 2]
                for ti, (t0, n) in enumerate(TILES):
                    ps = S.psum[1 + (ci % 4)]
                    ci += 1
                    for kt in range(8):
                        P.mm(ps[:, 0:n], w[:, kt, m * 128:(m + 1) * 128], hT[:, kt, t0:t0 + n], kt == 0, kt == 7,
                             R=[w, hT.s(ti)], W=[ps])
                    if chunk * 128 >= OFF_GATE:
                        P.A("activation", R=[ps], W=[sg], out=sg[:, t0:t0 + n], in_=ps[:, 0:n], func=AF.Sigmoid)
                    elif ci % 2 == 0:
                        P.A("activation", R=[ps], W=[sg], out=sg[:, t0:t0 + n], in_=ps[:, 0:n], func=AF.Identity)
                    else:
                        P.V("tensor_copy", R=[ps], W=[sg], out=sg[:, t0:t0 + n], in_=ps[:, 0:n])
                P.dma("sp", R=[sg], W=[D.uT.s(b)], out=D.uT[b, chunk * 128:(chunk + 1) * 128, :], in_=sg[:])
    P.barrier()


def qk_norm_rope(P, S, load_fn, dst, nt, gain, stg, tmps):
    tmp, tmp2 = tmps
    for i in range(nt):
        sg = stg.s(i % 2)
        load_fn(i, i % 2)
        for ti, (t0, n) in enumerate(TILES):
            ps = S.psum[0]
            x = stg[:, i % 2, t0:t0 + n]
            d_ = dst[:, i, t0:t0 + n]
            P.A("activation", R=[sg], W=[tmp], out=tmp[:, 0:n], in_=x, func=AF.Square)
            P.mm(ps[:, 0:n], S.bd64[:], tmp[:, 0:n], True, True, R=[S.bd64, tmp], W=[ps])
            P.A("activation", R=[ps], W=[tmp], out=tmp[:, 0:n], in_=ps[:, 0:n], func=AF.Sqrt, scale=1.0,
                bias=S.epsb[:, 0:1])
            P.V("reciprocal", R=[tmp], W=[tmp], out=tmp[:, 0:n], in_=tmp[:, 0:n])
            if t0 < CTX:
                P.V("scalar_tensor_tensor", R=[sg, tmp, gain], W=[dst.s(i)], out=d_, in0=x, scalar=gain[:, 0:1], in1=tmp[:, 0:n],
                    op0=ALU.mult, op1=ALU.mult)
            else:
                P.V("scalar_tensor_tensor", R=[sg, tmp, gain], W=[sg], out=x, in0=x, scalar=gain[:, 0:1], in1=tmp[:, 0:n],
                    op0=ALU.mult, op1=ALU.mult)
                p0 = t0 - CTX
                ps2 = S.psum[1]
                P.mm(ps2[:, 0:n], S.rot[:], x, True, True, R=[S.rot, sg], W=[ps2])
                P.V("tensor_tensor", R=[ps2, S.sin], W=[tmp2], out=tmp2[:, 0:n], in0=ps2[:, 0:n],
                    in1=S.sin[:, p0:p0 + n], op=ALU.mult)
                P.V("tensor_tensor", R=[sg, S.cos], W=[tmp], out=tmp[:, 0:n], in0=x,
                    in1=S.cos[:, p0:p0 + n], op=ALU.mult)
                P.V("tensor_tensor", R=[tmp, tmp2], W=[dst.s(i)], out=d_, in0=tmp[:, 0:n],
                    in1=tmp2[:, 0:n], op=ALU.add)


def phase_attn(P, l, D, S, b, need_ctx):
    with contextlib.ExitStack() as st:
        qT = P.sb("qT", [128, 4, T], F32R, st)
        kTa = P.sb("kTa", [128, 2, T], F32R, st)
        kTb = P.sb("kTb", [128, 2, T], F32R, st)
        vaug = P.sb("vaug", [128, 18, 4, 192], BF16, st)
        oT = P.sb("oT", [128, 4, T], F32, st)
        pT = [P.sb(f"pT{i}", [128, 512], BF16, st) for i in range(3)]
        oacc = P.sb("oacc", [128, 512], F32, st)
        rden = P.sb("rden", [128, 512], F32, st)
        with contextlib.ExitStack() as st2:
            vT = P.sb("vT", [128, 2, T], F32, st2)
            stg = P.sb("qkstg", [128, 2, T], F32, st2)
            for i in range(2):
                P.dma("sp", R=[D.uT.s(b)], W=[vT.s(i)], out=vT[:, i, :], in_=D.uT[b, 768 + i * 128:768 + (i + 1) * 128, :])
            P.G("memset", W=[vaug], ap=vaug[:], constant=1.0)

            def ld_q(i, k):
                P.dma("sp", R=[D.uT.s(b)], W=[stg.s(k)], out=stg[:, k, :], in_=D.uT[b, i * 128:(i + 1) * 128, :])

            def ld_ka(i, k):
                P.dma("sp", R=[D.uT.s(b)], W=[stg.s(k)], out=stg[:, k, :], in_=D.uT[b, 512 + i * 128:512 + (i + 1) * 128, :])

            def ld_kb(i, k):
                P.dma("sp", R=[D.uT.s(b)], W=[stg.s(k)], out=stg[0:64, k, :], in_=D.uT[b, 512 + i * 128 + 64:512 + (i + 1) * 128, :])
                P.dma("sp", R=[D.uT.s(b)], W=[stg.s(k)], out=stg[64:128, k, :], in_=D.uT[b, 512 + i * 128:512 + i * 128 + 64, :])

            tmps = (P.sb("qk_tmp", [128, 512], F32, st2), P.sb("qk_tmp2", [128, 512], F32, st2))
            qk_norm_rope(P, S, ld_q, qT, 4, S.qg[l], stg, tmps)
            qk_norm_rope(P, S, ld_ka, kTa, 2, S.kg[l], stg, tmps)
            qk_norm_rope(P, S, ld_kb, kTb, 2, S.kg[l], stg, tmps)
            cnt = 0
            for i in range(2):
                for kt in range(18):
                    ps = S.psum[2 + cnt % 2]
                    cnt += 1
                    P.PE("transpose", R=[vT.s(i), S.ident], W=[ps], out=ps[:, 0:128], in_=vT[:, i, kt * 128:(kt + 1) * 128],
                         identity=S.ident[:])
                    for e in range(2):
                        P.V("tensor_copy", R=[ps], W=[vaug], out=vaug[:, kt, 2 * i + e, 64:128],
                            in_=ps[:, e * 64:(e + 1) * 64])
        P.barrier()
        qsegs = [(CTX + qc * 512, 512, list(range(18))) for qc in range(4)]
        if need_ctx:
            qsegs.append((0, CTX, [0, 1]))
        pi = 0
        for h in range(4):
            i, e = h // 2, h % 2
            for g in range(2):
                ksrc = kTa if e == g else kTb
                shm = S.sh0 if g == 0 else S.sh1
                lo = g * 64
                for (q0, qn, kts) in qsegs:
                    acc = S.psum[4 + (pi % 2)]
                    for ki, kt in enumerate(kts):
                        pss = S.psum[pi % 2 + 6]
                        pt = pT[pi % 3]
                        pi += 1
                        P.mm(pss[:, 0:qn], ksrc[lo:lo + 64, i, kt * 128:(kt + 1) * 128], qT[lo:lo + 64, h, q0:q0 + qn],
                             True, True, R=[ksrc.s(i), qT.s(h)], W=[pss])
                        P.A("activation", R=[pss], W=[pt], out=pt[:, 0:qn], in_=pss[:, 0:qn], func=AF.Exp, scale=0.125)
                        va = vaug[:, kt, h, 64:192] if g == 0 else vaug[:, kt, h, 0:128]
                        P.mm(acc[:, 0:qn], va, pt[:, 0:qn], ki == 0, ki == len(kts) - 1, R=[vaug, pt], W=[acc])
                    P.A("activation", R=[acc], W=[oacc], out=oacc[:, 0:qn], in_=acc[:, 0:qn], func=AF.Identity)
                    psd = S.psum[0]
                    P.mm(psd[:, 0:qn], shm[:], oacc[:, 0:qn], True, True, R=[shm, oacc], W=[psd])
                    P.V("reciprocal", R=[psd], W=[rden], out=rden[lo:lo + 64, 0:qn], in_=psd[lo:lo + 64, 0:qn])
                    P.V("tensor_tensor", R=[oacc, rden], W=[oT.s(h)], out=oT[lo:lo + 64, h, q0:q0 + qn],
                        in0=oacc[lo:lo + 64, 0:qn], in1=rden[lo:lo + 64, 0:qn], op=ALU.mult)
        c0 = 0 if need_ctx else CTX
        for h in range(4):
            P.dma("sp", R=[oT.s(h)], W=[D.oatt.s(b)], out=D.oatt[b, h * 128:(h + 1) * 128, c0:T], in_=oT[:, h, c0:T])
    P.barrier()


TWO_PI = 2.0 * math.pi


def _tt(P, out, a, b, op, R, W):
    P.V("tensor_tensor", R=R, W=W, out=out, in0=a, in1=b, op=op)


def _ts(P, out, a, s1, op0, R, W, s2=None, op1=None):
    if op1 is None:
        P.V("tensor_scalar", R=R, W=W, out=out, in0=a, scalar1=s1, scalar2=None, op0=op0)
    else:
        P.V("tensor_scalar", R=R, W=W, out=out, in0=a, scalar1=s1, scalar2=s2, op0=op0, op1=op1)


def sin_turns(P, st, r, out, F, name):
    ki = P.sb(name + "_ki", [128, F], I32, st)
    kf = P.sb(name + "_kf", [128, F], F32, st)
    m = P.sb(name + "_m", [128, F], F32, st)
    P.V("tensor_copy", R=[r], W=[ki], out=ki[:], in_=r[:])
    P.V("tensor_copy", R=[ki], W=[kf], out=kf[:], in_=ki[:])
    _tt(P, kf[:], r[:], kf[:], ALU.subtract, [r, kf], [kf])
    _ts(P, m[:], kf[:], 0.5, ALU.is_gt, [kf], [m])
    _tt(P, kf[:], kf[:], m[:], ALU.subtract, [kf, m], [kf])
    _ts(P, m[:], kf[:], -0.5, ALU.is_lt, [kf], [m])
    _tt(P, kf[:], kf[:], m[:], ALU.add, [kf, m], [kf])
    P.A("activation", R=[kf], W=[out], out=out[:], in_=kf[:], func=AF.Sin, scale=TWO_PI)


def s5_derive(P, st, are, aim, ldt, F, name, need_z):
    mk = lambda n: P.sb(f"{name}_{n}", [128, F], F32, st)
    dt, rho, th, r, r2, cth, sth = mk("dt"), mk("rho"), mk("th"), mk("r"), mk("r2"), mk("cth"), mk("sth")
    P.A("activation", R=[ldt], W=[dt], out=dt[:], in_=ldt[:], func=AF.Exp)
    _tt(P, rho[:], are[:], dt[:], ALU.mult, [are, dt], [rho])
    P.A("activation", R=[rho], W=[rho], out=rho[:], in_=rho[:], func=AF.Exp)
    _tt(P, th[:], aim[:], dt[:], ALU.mult, [aim, dt], [th])
    _ts(P, r[:], th[:], 1.0 / TWO_PI, ALU.mult, [th], [r])
    _ts(P, r2[:], r[:], 0.25, ALU.add, [r], [r2])
    sin_turns(P, st, r, sth, F, name + "_s")
    sin_turns(P, st, r2, cth, F, name + "_c")
    res = dict(rho=rho, cth=cth, sth=sth)
    if need_z:
        abr, abi, den, nr, zre, zim, t1 = mk("abr"), mk("abi"), mk("den"), mk("nr"), mk("zre"), mk("zim"), mk("t1")
        _tt(P, abr[:], rho[:], cth[:], ALU.mult, [rho, cth], [abr])
        _tt(P, abi[:], rho[:], sth[:], ALU.mult, [rho, sth], [abi])
        _tt(P, den[:], are[:], are[:], ALU.mult, [are], [den])
        _tt(P, t1[:], aim[:], aim[:], ALU.mult, [aim], [t1])
        _tt(P, den[:], den[:], t1[:], ALU.add, [den, t1], [den])
        P.V("reciprocal", R=[den], W=[den], out=den[:], in_=den[:])
        _ts(P, nr[:], abr[:], -1.0, ALU.add, [abr], [nr])
        _tt(P, zre[:], nr[:], are[:], ALU.mult, [nr, are], [zre])
        _tt(P, t1[:], abi[:], aim[:], ALU.mult, [abi, aim], [t1])
        _tt(P, zre[:], zre[:], t1[:], ALU.add, [zre, t1], [zre])
        _tt(P, zre[:], zre[:], den[:], ALU.mult, [zre, den], [zre])
        _tt(P, zim[:], abi[:], are[:], ALU.mult, [abi, are], [zim])
        _tt(P, t1[:], nr[:], aim[:], ALU.mult, [nr, aim], [t1])
        _tt(P, zim[:], zim[:], t1[:], ALU.subtract, [zim, t1], [zim])
        _tt(P, zim[:], zim[:], den[:], ALU.mult, [zim, den], [zim])
        res.update(zre=zre, zim=zim)
    return res


def s5_setup(P, l, D, S, st):
    X = NS()
    ld = lambda nm, src, shp, dt=F32: _load(P, st, nm, src, shp, dt)
    are = ld("s5are", D.s5_A_rep_re[:, l, :], [128, 256])
    aim = ld("s5aim", D.s5_A_rep_im[:, l, :], [128, 256])
    ldt = ld("s5ldt", D.s5_ldt_rep[:, l, :], [128, 256])
    bre = ld("s5bre", D.s5_Bt_re[:, l, :], [128, 256])
    bim = ld("s5bim", D.s5_Bt_im[:, l, :], [128, 256])
    X.BTr = P.sb("BTr", [128, 4, 8, 64], BF16, st)
    X.BTi = P.sb("BTi", [128, 4, 8, 64], BF16, st)
    with contextlib.ExitStack() as st2:
        r = s5_derive(P, st2, are, aim, ldt, 256, "dr", True)
        bbr = P.sb("bbr", [128, 256], F32, st2)
        bbi = P.sb("bbi", [128, 256], F32, st2)
        t1 = P.sb("bt1", [128, 256], F32, st2)
        _tt(P, bbr[:], r["zre"][:], bre[:], ALU.mult, [r["zre"], bre], [bbr])
        _tt(P, t1[:], r["zim"][:], bim[:], ALU.mult, [r["zim"], bim], [t1])
        _tt(P, bbr[:], bbr[:], t1[:], ALU.subtract, [bbr, t1], [bbr])
        _tt(P, bbi[:], r["zre"][:], bim[:], ALU.mult, [r["zre"], bim], [bbi])
        _tt(P, t1[:], r["zim"][:], bre[:], ALU.mult, [r["zim"], bre], [t1])
        _tt(P, bbi[:], bbi[:], t1[:], ALU.add, [bbi, t1], [bbi])
        for dst, src in ((X.BTr, bbr), (X.BTi, bbi)):
            for q in range(4):
                P.V("tensor_tensor", R=[src, S.maskG], W=[dst], out=dst[:, q, :, :],
                    in0=src[:, q * 64:(q + 1) * 64].unsqueeze(1).to_broadcast([128, 8, 64]),
                    in1=S.maskG[:, :].unsqueeze(2).to_broadcast([128, 8, 64]), op=ALU.mult)
    P.barrier()
    sare = ld("s5sare", D.s5_A_st_re[:, l, :], [128, 16])
    saim = ld("s5saim", D.s5_A_st_im[:, l, :], [128, 16])
    sldt = ld("s5sldt", D.s5_ldt_st[:, l, :], [128, 16])
    dbg(P, "sare", sare, sare[:], [128, 16])
    dbg(P, "sldt", sldt, sldt[:], [128, 16])
    r2 = s5_derive(P, st, sare, saim, sldt, 16, "ds", False)
    X.rho = r2["rho"]
    dbg(P, "rho", X.rho, X.rho[:], [128, 16])
    dbg(P, "cth", r2["cth"], r2["cth"][:], [128, 16])
    dbg(P, "sth", r2["sth"], r2["sth"][:], [128, 16])
    X.cosT = P.sb("s5cos", [128, 16, 256], F32, st)
    X.sinT = P.sb("s5sin", [128, 16, 256], F32, st)
    nsin = P.sb("s5nsin", [128, 16, 256], F32, st)
    X.nsinT = nsin
    P.V("memset", W=[X.cosT], ap=X.cosT[:, :, 0:1], constant=1.0)
    P.V("memset", W=[X.sinT], ap=X.sinT[:, :, 0:1], constant=0.0)
    P.V("tensor_copy", R=[r2["cth"]], W=[X.cosT], out=X.cosT[:, :, 1], in_=r2["cth"][:])
    P.V("tensor_copy", R=[r2["sth"]], W=[X.sinT], out=X.sinT[:, :, 1], in_=r2["sth"][:])
    tmpa = P.sb("s5tmpa", [128, 16, 128], F32, st)
    m = 2
    while m < 256:
        cm, sm = X.cosT[:, :, m], X.sinT[:, :, m]
        c1, s1 = X.cosT[:, :, 1], X.sinT[:, :, 1]
        cp, sp_ = X.cosT[:, :, m - 1], X.sinT[:, :, m - 1]
        ta = tmpa[:, :, 0]
        tb = tmpa[:, :, 1]
        _tt(P, ta, cp, c1, ALU.mult, [X.cosT], [tmpa])
        _tt(P, tb, sp_, s1, ALU.mult, [X.sinT], [tmpa])
        _tt(P, cm, ta, tb, ALU.subtract, [tmpa], [X.cosT])
        _tt(P, ta, cp, s1, ALU.mult, [X.cosT, X.sinT], [tmpa])
        _tt(P, tb, sp_, c1, ALU.mult, [X.cosT, X.sinT], [tmpa])
        _tt(P, sm, ta, tb, ALU.add, [tmpa], [X.sinT])
        n = m - 1
        cmb = X.cosT[:, :, m:m + 1].to_broadcast([128, 16, n])
        smb = X.sinT[:, :, m:m + 1].to_broadcast([128, 16, n])
        cj, sj = X.cosT[:, :, 1:m], X.sinT[:, :, 1:m]
        co, so = X.cosT[:, :, m + 1:2 * m], X.sinT[:, :, m + 1:2 * m]
        ta = tmpa[:, :, 0:n]
        _tt(P, ta, sj, smb, ALU.mult, [X.sinT], [tmpa])
        _tt(P, co, cj, cmb, ALU.mult, [X.cosT], [X.cosT])
        _tt(P, co, co, ta, ALU.subtract, [X.cosT, tmpa], [X.cosT])
        _tt(P, ta, cj, smb, ALU.mult, [X.cosT, X.sinT], [tmpa])
        _tt(P, so, sj, cmb, ALU.mult, [X.sinT, X.cosT], [X.sinT])
        _tt(P, so, so, ta, ALU.add, [X.sinT, tmpa], [X.sinT])
        m *= 2
    _ts(P, nsin[:], X.sinT[:], -1.0, ALU.mult, [X.sinT], [nsin])
    dbg(P, "cosT", X.cosT, X.cosT[:], [128, 16, 256])
    dbg(P, "sinT", X.sinT, X.sinT[:], [128, 16, 256])
    dbg(P, "BTr", X.BTr, X.BTr[:], [128, 4, 8, 64], BF16)
    X.rhoT = P.sb("s5rhoT", [128, 16, 256], F32, st)
    P.V("tensor_copy", R=[X.rho], W=[X.rhoT], out=X.rhoT[:], in_=X.rho[:].unsqueeze(2).to_broadcast([128, 16, 256]))
    cre = ld("s5cre", D.s5_C_st_re[:, l, :], [128, 16 * 64])
    cim = ld("s5cim", D.s5_C_st_im[:, l, :], [128, 16 * 64])
    X.Cr = P.sb("s5Cr", [128, 16, 64], BF16, st)
    X.Ci = P.sb("s5Ci", [128, 16, 64], BF16, st)
    P.V("tensor_copy", R=[cre], W=[X.Cr], out=X.Cr[:], in_=cre[:].rearrange("p (a b) -> p a b", b=64))
    _ts(P, X.Ci[:], cim[:].rearrange("p (a b) -> p a b", b=64), -1.0, ALU.mult, [cim], [X.Ci])
    X.dT = ld("s5dT", D.s5_dT[:, l, :], [128, 2])
    X.wglu = P.sb("s5wglu", [128, 2, 512], BF16, st)
    P.dma("pool", R=[D.s5_w_glu], W=[X.wglu], out=X.wglu[:], in_=D.s5_w_glu[l].rearrange("(kt p) n -> p kt n", p=128))
    return X


def dbg(P, name, tt, ap, shape, dt=F32):
    if not P.debug:
        return
    d = P.dram("dbg_" + name, shape, dt, kind="ExternalOutput")
    P.dma("sp", R=[tt], W=[d], out=d[tuple(slice(None) for _ in shape)], in_=ap)
    P.dbg_out = getattr(P, "dbg_out", []) + [d]


def _load(P, st, nm, src_ap, shp, dt=F32):
    t = P.sb(nm, shp, dt, st)
    P.dma("sp", R=[], W=[t], out=t[:], in_=src_ap)
    return t


S5CH = 256


def phase_s5(P, l, D, S, b, X, need_ctx):
    nch = T // S5CH
    with contextlib.ExitStack() as st:
        sT = P.sb("s5sT", [128, 2, T], F32, st)
        sT16 = P.sb("s5sT16", [128, 2, T], BF16, st)
        yacc = P.sb("s5yacc", [128, 2, T], F32, st)
        for ft in range(2):
            P.dma("sp", R=[D.uT.s(b)], W=[sT], out=sT[:, ft, :], in_=D.uT[b, OFF_S5 + ft * 128:OFF_S5 + (ft + 1) * 128, :])
        P.A("activation", R=[sT], W=[sT16], out=sT16[:], in_=sT[:], func=AF.Identity)
        for ft in range(2):
            _ts(P, yacc[:, ft, :], sT[:, ft, :], X.dT[:, ft:ft + 1], ALU.mult, [sT, X.dT], [yacc])
        mk = lambda n, dt=F32: [P.sb(f"s5{n}{i}", [128, S5CH], dt, st) for i in range(2)]
        xr_re, xr_im, g_re, g_im = mk("xrr"), mk("xri"), mk("gr"), mk("gi")
        h_re, h_im = mk("hr", BF16), mk("hi", BF16)
        hp = P.sb("s5hp", [128, 4], F32, st)
        tA = P.sb("s5tA", [128, S5CH], F32, st)
        tB = P.sb("s5tB", [128, S5CH], F32, st)
        it = 0
        for d in range(2):
            for pr in range(8):
                ft, pp = pr // 4, pr % 4
                dp = d * 8 + pr
                cT_, sT_, nsT_ = X.cosT[:, dp, :], X.sinT[:, dp, :], X.nsinT[:, dp, :]
                order = list(range(nch)) if d == 0 else [0] + list(range(nch - 1, 0, -1))
                for oi, ch in enumerate(order):
                    c0 = ch * S5CH
                    k = it % 2
                    it += 1
                    ps_r, ps_i = S.psum[2 * k], S.psum[2 * k + 1]
                    lr = X.BTr[:, d * 2 + ft, 2 * pp:2 * pp + 2, :].rearrange("p a b -> p (a b)")
                    li = X.BTi[:, d * 2 + ft, 2 * pp:2 * pp + 2, :].rearrange("p a b -> p (a b)")
                    P.mm(ps_r[:, 0:S5CH], lr, sT16[:, ft, c0:c0 + S5CH], True, True, R=[X.BTr, sT16], W=[ps_r])
                    P.mm(ps_i[:, 0:S5CH], li, sT16[:, ft, c0:c0 + S5CH], True, True, R=[X.BTi, sT16], W=[ps_i])
                    rv = (lambda ap: ap) if d == 0 else (lambda ap: ap[:, ::-1])
                    xrr, xri, gr, gi, hr, hi = xr_re[k], xr_im[k], g_re[k], g_im[k], h_re[k], h_im[k]
                    _tt(P, tA[:], rv(ps_r[:, 0:S5CH]), cT_, ALU.mult, [ps_r, X.cosT], [tA]) if d == 0 else \
                        _tt(P, rv(tA[:]), ps_r[:, 0:S5CH], rv(cT_), ALU.mult, [ps_r, X.cosT], [tA])
                    if d == 0:
                        _tt(P, tB[:], ps_i[:, 0:S5CH], sT_, ALU.mult, [ps_i, X.sinT], [tB])
                        _tt(P, xrr[:], tA[:], tB[:], ALU.add, [tA, tB], [xrr])
                        _tt(P, tA[:], ps_i[:, 0:S5CH], cT_, ALU.mult, [ps_i, X.cosT], [tA])
                        _tt(P, tB[:], ps_r[:, 0:S5CH], nsT_, ALU.mult, [ps_r, X.nsinT], [tB])
                        _tt(P, xri[:], tA[:], tB[:], ALU.add, [tA, tB], [xri])
                    else:
                        _tt(P, rv(tB[:]), ps_i[:, 0:S5CH], rv(sT_), ALU.mult, [ps_i, X.sinT], [tB])
                        _tt(P, xrr[:], tA[:], tB[:], ALU.add, [tA, tB], [xrr])
                        _tt(P, rv(tA[:]), ps_i[:, 0:S5CH], rv(cT_), ALU.mult, [ps_i, X.cosT], [tA])
                        _tt(P, rv(tB[:]), ps_r[:, 0:S5CH], rv(nsT_), ALU.mult, [ps_r, X.nsinT], [tB])
                        _tt(P, xri[:], tA[:], tB[:], ALU.add, [tA, tB], [xri])
                    if oi == 0:
                        ini_r, ini_i = 0.0, 0.0
                        Rini = []
                    else:
                        c1, s1 = X.cosT[:, dp, 1:2], X.sinT[:, dp, 1:2]
                        ns1 = X.nsinT[:, dp, 1:2]
                        _tt(P, hp[:, 2:3], hp[:, 0:1], c1, ALU.mult, [hp, X.cosT], [hp])
                        P.V("scalar_tensor_tensor", R=[hp, X.nsinT], W=[hp], out=hp[:, 2:3], in0=hp[:, 1:2], scalar=ns1,
                            in1=hp[:, 2:3], op0=ALU.mult, op1=ALU.add)
                        _tt(P, hp[:, 3:4], hp[:, 0:1], s1, ALU.mult, [hp, X.sinT], [hp])
                        P.V("scalar_tensor_tensor", R=[hp, X.cosT], W=[hp], out=hp[:, 3:4], in0=hp[:, 1:2], scalar=c1,
                            in1=hp[:, 3:4], op0=ALU.mult, op1=ALU.add)
                        ini_r, ini_i = hp[:, 2:3], hp[:, 3:4]
                        Rini = [hp]
                    P.V("tensor_tensor_scan", R=[X.rhoT, xrr] + Rini, W=[gr], out=gr[:], data0=X.rhoT[:, dp, :], data1=xrr[:],
                        initial=ini_r, op0=ALU.mult, op1=ALU.add)
                    P.V("tensor_tensor_scan", R=[X.rhoT, xri] + Rini, W=[gi], out=gi[:], data0=X.rhoT[:, dp, :], data1=xri[:],
                        initial=ini_i, op0=ALU.mult, op1=ALU.add)
                    _tt(P, tA[:], gr[:], cT_, ALU.mult, [gr, X.cosT], [tA])
                    _tt(P, tB[:], gi[:], nsT_, ALU.mult, [gi, X.nsinT], [tB])
                    _tt(P, tA[:], tA[:], tB[:], ALU.add, [tA, tB], [tA])
                    P.V("tensor_copy", R=[tA], W=[hr], out=rv(hr[:]), in_=tA[:])
                    P.V("tensor_copy", R=[tA], W=[hp], out=hp[:, 0:1], in_=tA[:, S5CH - 1:S5CH])
                    _tt(P, tA[:], gi[:], cT_, ALU.mult, [gi, X.cosT], [tA])
                    _tt(P, tB[:], gr[:], sT_, ALU.mult, [gr, X.sinT], [tB])
                    _tt(P, tA[:], tA[:], tB[:], ALU.add, [tA, tB], [tA])
                    P.V("tensor_copy", R=[tA], W=[hi], out=rv(hi[:]), in_=tA[:])
                    P.V("tensor_copy", R=[tA], W=[hp], out=hp[:, 1:2], in_=tA[:, S5CH - 1:S5CH])
                    ps_y = S.psum[4 + k]
                    pb = (pp // 2) * 64
                    P.mm(ps_y[pb:pb + 64, 0:S5CH], X.Cr[:, dp, :], hr[:], True, False, R=[X.Cr, hr], W=[ps_y])
                    P.mm(ps_y[pb:pb + 64, 0:S5CH], X.Ci[:, dp, :], hi[:], False, True, R=[X.Ci, hi], W=[ps_y])
                    _tt(P, yacc[pb:pb + 64, ft, c0:c0 + S5CH], yacc[pb:pb + 64, ft, c0:c0 + S5CH],
                        ps_y[pb:pb + 64, 0:S5CH], ALU.add, [yacc, ps_y], [yacc])
        dbg(P, f"yacc{b}", yacc, yacc[:], [128, 2, T])
        ge = P.sb("s5ge", [128, 2, T], BF16, st)
        gt = P.sb("s5gt", [128, 512], F32, st)
        for ft in range(2):
            for (t0, n) in TILES:
                y = yacc[:, ft, t0:t0 + n]
                P.A("activation", R=[yacc], W=[gt], out=gt[:, 0:n], in_=y, func=AF.Square)
                _ts(P, gt[:, 0:n], gt[:, 0:n], 0.044715, ALU.mult, [gt], [gt], 1.0, ALU.add)
                _tt(P, gt[:, 0:n], gt[:, 0:n], y, ALU.mult, [gt, yacc], [gt])
                P.A("activation", R=[gt], W=[gt], out=gt[:, 0:n], in_=gt[:, 0:n], func=AF.Tanh,
                    scale=math.sqrt(2.0 / math.pi))
                _ts(P, gt[:, 0:n], gt[:, 0:n], 1.0, ALU.add, [gt], [gt], 0.5, ALU.mult)
                _tt(P, ge[:, ft, t0:t0 + n], gt[:, 0:n], y, ALU.mult, [gt, yacc], [ge])
        osb = sT
        for m in range(2):
            for (t0, n) in TILES:
                p1, p2 = S.psum[0], S.psum[1]
                for kt in range(2):
                    P.mm(p1[:, 0:n], X.wglu[:, kt, m * 128:(m + 1) * 128], ge[:, kt, t0:t0 + n], kt == 0, kt == 1, R=[X.wglu, ge], W=[p1])
                for kt in range(2):
                    P.mm(p2[:, 0:n], X.wglu[:, kt, 256 + m * 128:256 + (m + 1) * 128], ge[:, kt, t0:t0 + n], kt == 0, kt == 1,
                         R=[X.wglu, ge], W=[p2])
                P.A("activation", R=[p2], W=[gt], out=gt[:, 0:n], in_=p2[:, 0:n], func=AF.Sigmoid)
                _tt(P, osb[:, m, t0:t0 + n], p1[:, 0:n], gt[:, 0:n], ALU.mult, [p1, gt], [osb])
        c0 = 0 if need_ctx else CTX
        for m in range(2):
            P.dma("sp", R=[osb], W=[D.os5.s(b)], out=D.os5[b, m * 128:(m + 1) * 128, c0:T], in_=osb[:, m, c0:T])
    P.barrier()


def merge_setup(P, l, D, S, st):
    X = NS()
    X.pa = P.sb("m_pa", [128, 4, DM], BF16, st)
    X.p5 = P.sb("m_p5", [128, 2, DM], BF16, st)
    X.pr = P.sb("m_pr", [128, 2, DM], BF16, st)
    X.wo = P.sb("m_wo", [128, 8, DM], BF16, st)
    for t, src in ((X.pa, D.proj_att), (X.p5, D.proj_s5), (X.pr, D.proj_rwkv), (X.wo, D.w_out)):
        P.dma("pool", R=[src], W=[t], out=t[:], in_=src[l].rearrange("(kt p) n -> p kt n", p=128))
    X.wr = P.sb("m_wr", [128, 8, 36], F32, st)
    P.dma("sp", R=[D.router_w], W=[X.wr], out=X.wr[:], in_=D.router_w[l].rearrange("(kt p) n -> p kt n", p=128))
    X.rb = P.sb("m_rb", [128, 36], F32, st)
    P.dma("sp", R=[D.router_b], W=[X.rb], out=X.rb[:], in_=D.router_b[:, l, :])
    X.carry = P.sb("m_carry", [128, NEXP], F32, st)
    P.V("memset", W=[X.carry], ap=X.carry[:], constant=0.0)
    X.eoff = P.sb("m_eoff", [128, NEXP], F32, st)
    P.dma("sp", R=[D.consts["eoff"]], W=[X.eoff], out=X.eoff[:], in_=D.consts["eoff"][:, :])
    return X


def phase_merge(P, l, D, S, b, X, need_ctx, xsrc, G):
    tiles = TILES if need_ctx else TILES[1:]
    with contextlib.ExitStack() as st:
        oa = P.sb("mg_oa", [128, 4, 512], BF16, st)
        o5 = P.sb("mg_o5", [128, 2, 512], BF16, st)
        orw = P.sb("mg_or", [128, 2, 512], BF16, st)
        gt = [P.sb(f"mg_g{i}", [128, 3, 512], F32, st) for i in range(2)]
        xt = P.sb("mg_x", [128, 8, 512], F32, st)
        mg = P.sb("mg_m", [128, 8, 512], BF16, st)
        t1 = P.sb("mg_t1", [128, 512], F32, st)
        t2 = P.sb("mg_t2", [128, 512], F32, st)
        sq = P.sb("mg_sq", [128, 8, 512], F32, st)
        rstd = P.sb("mg_rstd", [128, 512], F32, st)
        h2 = P.sb("mg_h2", [128, 8, 512], F32, st)
        htm = P.sb("mg_htm", [128, DM], F32, st)
        rt = {k: P.sb("mg_r" + k, shp, dt, st) for k, shp, dt in (
            ("lg", [128, 36], F32), ("mx", [128, 8], F32), ("ohg", [128, 4], F32), ("el", [128, 8], F32),
            ("ee", [128, 8], F32), ("t8", [128, 8], F32), ("oh1", [128, 8], F32), ("oh2", [128, 8], F32),
            ("M1", [128, 4, 8], F32), ("M2", [128, 4, 8], F32), ("M", [128, NEXP], F32), ("pos", [128, NEXP], F32),
            ("s1", [128, 4], F32), ("gs", [128, 4], F32), ("si", [128, 2], I32))}
        for (t0, n) in tiles:
            col = 4 if t0 < CTX else b
            P.dma("pool", R=[D.oatt.s(b)], W=[oa], out=oa[:, :, 0:n], in_=D.oatt[b, :, t0:t0 + n].rearrange("(k p) t -> p k t", p=128))
            P.dma("pool", R=[D.os5.s(b)], W=[o5], out=o5[:, :, 0:n], in_=D.os5[b, :, t0:t0 + n].rearrange("(k p) t -> p k t", p=128))
            P.dma("pool", R=[D.orw.s(b)], W=[orw], out=orw[:, :, 0:n], in_=D.orw[b, :, t0:t0 + n].rearrange("(k p) t -> p k t", p=128))
            P.dma("sp", R=[xsrc], W=[xt], out=xt[:, :, 0:n], in_=xsrc[b, :, t0:t0 + n].rearrange("(kt p) t -> p kt t", p=128))
            for m in range(8):
                g = gt[m % 2]
                for j in range(3):
                    r0 = OFF_GATE + j * DM + m * 128
                    P.dma("sp", R=[D.uT.s(b)], W=[g], out=g[:, j, 0:n], in_=D.uT[b, r0:r0 + 128, t0:t0 + n])
                pa_, p5_, pr_ = S.psum[1], S.psum[2], S.psum[3]
                for k in range(4):
                    P.mm(pa_[:, 0:n], X.pa[:, k, m * 128:(m + 1) * 128], oa[:, k, 0:n], k == 0, k == 3, R=[X.pa, oa], W=[pa_])
                for k in range(2):
                    P.mm(p5_[:, 0:n], X.p5[:, k, m * 128:(m + 1) * 128], o5[:, k, 0:n], k == 0, k == 1, R=[X.p5, o5], W=[p5_])
                for k in range(2):
                    P.mm(pr_[:, 0:n], X.pr[:, k, m * 128:(m + 1) * 128], orw[:, k, 0:n], k == 0, k == 1, R=[X.pr, orw], W=[pr_])
                _tt(P, t1[:, 0:n], pa_[:, 0:n], g[:, 0, 0:n], ALU.mult, [pa_, g], [t1])
                _tt(P, t2[:, 0:n], p5_[:, 0:n], g[:, 1, 0:n], ALU.mult, [p5_, g], [t2])
                _tt(P, t1[:, 0:n], t1[:, 0:n], t2[:, 0:n], ALU.add, [t1, t2], [t1])
                _tt(P, t2[:, 0:n], pr_[:, 0:n], g[:, 2, 0:n], ALU.mult, [pr_, g], [t2])
                _tt(P, mg[:, m, 0:n], t1[:, 0:n], t2[:, 0:n], ALU.add, [t1, t2], [mg])
            for m in range(8):
                po = S.psum[4 + m % 2]
                for k in range(8):
                    P.mm(po[:, 0:n], X.wo[:, k, m * 128:(m + 1) * 128], mg[:, k, 0:n], k == 0, k == 7, R=[X.wo, mg], W=[po])
                P.V("scalar_tensor_tensor", R=[po, xt, S.modT], W=[xt], out=xt[:, m, 0:n], in0=po[:, 0:n],
                    scalar=S.modT[:, 2 * 8 + m, col:col + 1], in1=xt[:, m, 0:n], op0=ALU.mult, op1=ALU.add)
            P.dma("sp", R=[xt], W=[D.x1T.s(b)], out=D.x1T[b, :, t0:t0 + n].rearrange("(kt p) t -> p kt t", p=128), in_=xt[:, :, 0:n])
            rms_mod(P, S, xt, n, col, 4, 3, lambda kt: (h2[:, kt, 0:n], [h2]), sq, rstd, S.psum[0])
            for sti in range(n // 128):
                tok = slice(sti * 128, (sti + 1) * 128)
                gi = G.next
                G.next += 1
                G.tiles.append((b, t0 + sti * 128))
                pl = S.psum[6]
                for kt in range(8):
                    P.mm(pl[:, 0:36], h2[:, kt, tok], X.wr[:, kt, :], kt == 0, kt == 7, R=[h2, X.wr], W=[pl])
                lg = rt["lg"]
                _tt(P, lg[:], pl[:, 0:36], X.rb[:], ALU.add, [pl, X.rb], [lg])
                mx, ohg, el, ee, t8, oh1, oh2 = rt["mx"], rt["ohg"], rt["el"], rt["ee"], rt["t8"], rt["oh1"], rt["oh2"]
                s1, gs = rt["s1"], rt["gs"]
                P.V("tensor_reduce", R=[lg], W=[s1], out=s1[:, 0:1], in_=lg[:, 0:4], axis=AX.X, op=ALU.max)
                _ts(P, ohg[:], lg[:, 0:4], s1[:, 0:1], ALU.is_equal, [lg, s1], [ohg])
                _ts(P, gs[:], lg[:, 0:4], s1[:, 0:1], ALU.subtract, [lg, s1], [gs])
                P.A("activation", R=[gs], W=[gs], out=gs[:], in_=gs[:], func=AF.Exp)
                P.V("tensor_reduce", R=[gs], W=[s1], out=s1[:, 1:2], in_=gs[:], axis=AX.X, op=ALU.add)
                _ts(P, el[:], lg[:, 4:12], ohg[:, 0:1], ALU.mult, [lg, ohg], [el])
                for j in range(1, 4):
                    P.V("scalar_tensor_tensor", R=[lg, ohg, el], W=[el], out=el[:], in0=lg[:, 4 + 8 * j:12 + 8 * j],
                        scalar=ohg[:, j:j + 1], in1=el[:], op0=ALU.mult, op1=ALU.add)
                P.V("max", R=[el], W=[mx], out=mx[:], in_=el[:])
                _ts(P, oh1[:], el[:], mx[:, 0:1], ALU.is_equal, [el, mx], [oh1])
                _ts(P, oh2[:], el[:], mx[:, 1:2], ALU.is_equal, [el, mx], [oh2])
                _tt(P, s1[:, 2:3], mx[:, 1:2], mx[:, 0:1], ALU.subtract, [mx], [s1])
                P.A("activation", R=[s1], W=[s1], out=s1[:, 2:3], in_=s1[:, 2:3], func=AF.Exp)
                _ts(P, s1[:, 3:4], s1[:, 2:3], 1.0, ALU.add, [s1], [s1])
                _tt(P, s1[:, 3:4], s1[:, 3:4], s1[:, 1:2], ALU.mult, [s1], [s1])
                P.V("reciprocal", R=[s1], W=[s1], out=s1[:, 3:4], in_=s1[:, 3:4])
                P.V("tensor_copy", R=[s1], W=[G.gate], out=G.gate[:, gi, 0:1], in_=s1[:, 3:4])
                _tt(P, G.gate[:, gi, 1:2], s1[:, 3:4], s1[:, 2:3], ALU.mult, [s1], [G.gate])
                for Mk, oh in ((rt["M1"], oh1), (rt["M2"], oh2)):
                    P.V("tensor_tensor", R=[ohg, oh], W=[Mk], out=Mk[:], in0=ohg[:].unsqueeze(2).to_broadcast([128, 4, 8]),
                        in1=oh[:].unsqueeze(1).to_broadcast([128, 4, 8]), op=ALU.mult)
                M = rt["M"]
                _tt(P, M[:], rt["M1"][:].rearrange("p a b -> p (a b)"), rt["M2"][:].rearrange("p a b -> p (a b)"), ALU.add,
                    [rt["M1"], rt["M2"]], [M])
                pp = S.psum[7]
                P.mm(pp[:, 0:NEXP], S.tri[:], M[:], True, True, R=[S.tri, M], W=[pp])
                pos = rt["pos"]
                _tt(P, pos[:], pp[:, 0:NEXP], X.carry[:], ALU.add, [pp, X.carry], [pos])
                _tt(P, pos[:], pos[:], X.eoff[:], ALU.add, [pos, X.eoff], [pos])
                P.mm(pp[:, 0:NEXP], S.ones[:], M[:], True, True, R=[S.ones, M], W=[pp])
                _tt(P, X.carry[:], X.carry[:], pp[:, 0:NEXP], ALU.add, [pp, X.carry], [X.carry])
                for k, Mk in enumerate((rt["M1"], rt["M2"])):
                    _tt(P, M[:], Mk[:].rearrange("p a b -> p (a b)"), pos[:], ALU.mult, [Mk, pos], [M])
                    P.V("tensor_reduce", R=[M], W=[s1], out=s1[:, 0:1], in_=M[:], axis=AX.X, op=ALU.add)
                    P.V("tensor_copy", R=[s1], W=[G.slot], out=G.slot[:, gi, k:k + 1], in_=s1[:, 0:1])
                for half in range(2):
                    ph = S.psum[2 + half]
                    for q in range(4):
                        kt = half * 4 + q
                        P.PE("transpose", R=[h2, S.ident], W=[ph], out=ph[:, q * 128:(q + 1) * 128], in_=h2[:, kt, tok], identity=S.ident[:])
                    P.A("activation", R=[ph], W=[htm], out=htm[:, half * 512:(half + 1) * 512], in_=ph[:], func=AF.Identity)
                for k in range(2):
                    P.dma("pool", R=[htm, G.slot], W=[D.Xe], meth="indirect_dma_start", out=D.Xe[:, :],
                          out_offset=bass.IndirectOffsetOnAxis(ap=G.slot[:, gi, k:k + 1], axis=0), in_=htm[:], in_offset=None)
    P.barrier()


def phase_experts(P, l, D, S):
    with contextlib.ExitStack() as st:
        wg = [P.sb(f"e_wg{i}", [128, 8, DEXP], BF16, st) for i in range(2)]
        wu = [P.sb(f"e_wu{i}", [128, 8, DEXP], BF16, st) for i in range(2)]
        wd = [P.sb(f"e_wd{i}", [128, 4, DM], BF16, st) for i in range(2)]
        xtm = P.sb("e_xtm", [128, 4, DM], F32, st)
        xbT = P.sb("e_xbT", [128, 8, 512], BF16, st)
        sg = P.sb("e_sg", [128, 512], F32, st)
        act = P.sb("e_act", [128, 4, 512], BF16, st)
        yb = P.sb("e_yb", [128, 4, DM], F32, st)
        ci = 0
        for e in range(NEXP):
            k = e % 2
            P.dma("pool", R=[D.exp_w_gate], W=[wg[k]], out=wg[k][:], in_=D.exp_w_gate[l, e].rearrange("(kt p) n -> p kt n", p=128))
            P.dma("pool", R=[D.exp_w_up], W=[wu[k]], out=wu[k][:], in_=D.exp_w_up[l, e].rearrange("(kt p) n -> p kt n", p=128))
            P.dma("pool", R=[D.exp_w_down], W=[wd[k]], out=wd[k][:], in_=D.exp_w_down[l, e].rearrange("(kt p) n -> p kt n", p=128))
            for blk in range(CAP // 512):
                r0 = e * CAP + blk * 512
                P.dma("sp", R=[D.Xe], W=[xtm], out=xtm[:], in_=D.Xe[r0:r0 + 512, :].rearrange("(s p) f -> p s f", p=128))
                for kt in range(8):
                    ph = S.psum[ci % 2]
                    ci += 1
                    for s_ in range(4):
                        P.PE("transpose", R=[xtm, S.ident], W=[ph], out=ph[:, s_ * 128:(s_ + 1) * 128],
                             in_=xtm[:, s_, kt * 128:(kt + 1) * 128], identity=S.ident[:])
                    if kt % 2 == 0:
                        P.A("activation", R=[ph], W=[xbT], out=xbT[:, kt, :], in_=ph[:], func=AF.Identity)
                    else:
                        P.V("tensor_copy", R=[ph], W=[xbT], out=xbT[:, kt, :], in_=ph[:])
                for hm in range(4):
                    pg, pu = S.psum[2 + 2 * (hm % 2)], S.psum[3 + 2 * (hm % 2)]
                    for kt in range(8):
                        P.mm(pg[:], wg[k][:, kt, hm * 128:(hm + 1) * 128], xbT[:, kt, :], kt == 0, kt == 7, R=[wg[k], xbT], W=[pg])
                    for kt in range(8):
                        P.mm(pu[:], wu[k][:, kt, hm * 128:(hm + 1) * 128], xbT[:, kt, :], kt == 0, kt == 7, R=[wu[k], xbT], W=[pu])
                    P.A("activation", R=[pg], W=[sg], out=sg[:], in_=pg[:], func=AF.Silu)
                    _tt(P, act[:, hm, :], pu[:], sg[:], ALU.mult, [pu, sg], [act])
                for s_ in range(4):
                    for half in range(2):
                        pd = S.psum[6 + half]
                        for hm in range(4):
                            P.mm(pd[:], act[:, hm, s_ * 128:(s_ + 1) * 128], wd[k][:, hm, half * 512:(half + 1) * 512], hm == 0, hm == 3,
                                 R=[act, wd[k]], W=[pd])
                        if half == 0:
                            P.A("activation", R=[pd], W=[yb], out=yb[:, s_, 0:512], in_=pd[:], func=AF.Identity)
                        else:
                            P.V("tensor_copy", R=[pd], W=[yb], out=yb[:, s_, 512:1024], in_=pd[:])
                P.dma("sp", R=[yb], W=[D.Ye], out=D.Ye[r0:r0 + 512, :].rearrange("(s p) f -> p s f", p=128), in_=yb[:])
    P.barrier()


def phase_combine(P, l, D, S, G, dst, last):
    with contextlib.ExitStack() as st:
        y1 = [P.sb(f"c_y1{i}", [128, DM], F32, st) for i in range(2)]
        y2 = [P.sb(f"c_y2{i}", [128, DM], F32, st) for i in range(2)]
        xt = [P.sb(f"c_x{i}", [128, 8, 128], F32, st) for i in range(2)]
        for gi, (b, t0) in enumerate(G.tiles):
            k = gi % 2
            col = 4 if t0 < CTX else b
            for yy, kk in ((y1[k], 0), (y2[k], 1)):
                P.dma("pool", R=[D.Ye, G.slot], W=[yy], meth="indirect_dma_start", out=yy[:], out_offset=None, in_=D.Ye[:, :],
                      in_offset=bass.IndirectOffsetOnAxis(ap=G.slot[:, gi, kk:kk + 1], axis=0))
            P.dma("sp", R=[D.x1T.s(b)], W=[xt[k]], out=xt[k][:], in_=D.x1T[b, :, t0:t0 + 128].rearrange("(kt p) t -> p kt t", p=128))
            _ts(P, y1[k][:], y1[k][:], G.gate[:, gi, 0:1], ALU.mult, [y1[k], G.gate], [y1[k]])
            P.V("scalar_tensor_tensor", R=[y1[k], y2[k], G.gate], W=[y1[k]], out=y1[k][:], in0=y2[k][:], scalar=G.gate[:, gi, 1:2],
                in1=y1[k][:], op0=ALU.mult, op1=ALU.add)
            for half in range(2):
                ph = S.psum[2 * k + half]
                for q in range(4):
                    m = half * 4 + q
                    P.PE("transpose", R=[y1[k], S.ident], W=[ph], out=ph[:, q * 128:(q + 1) * 128], in_=y1[k][:, m * 128:(m + 1) * 128],
                         identity=S.ident[:])
                for q in range(4):
                    m = half * 4 + q
                    P.V("scalar_tensor_tensor", R=[ph, xt[k], S.modT], W=[xt[k]], out=xt[k][:, m, :], in0=ph[:, q * 128:(q + 1) * 128],
                        scalar=S.modT[:, 5 * 8 + m, col:col + 1], in1=xt[k][:, m, :], op0=ALU.mult, op1=ALU.add)
            if last:
                P.dma("sp", R=[xt[k]], W=[D.out], out=D.out[b, :, t0 - CTX:t0 - CTX + 128].rearrange("(kt p) t -> p kt t", p=128), in_=xt[k][:])
            else:
                P.dma("sp", R=[xt[k]], W=[dst.s(b)], out=dst[b, :, t0:t0 + 128].rearrange("(kt p) t -> p kt t", p=128), in_=xt[k][:])
    P.barrier()


RC = 64
NCH = T // RC
GN_EPS = 64e-5
W_SCALE = -math.exp(-0.5)


def rwkv_setup(P, l, D, S, st):
    X = NS()
    ld = lambda nm, src, shp: _load(P, st, nm, src, shp)
    X.cw = ld("rw_cw", D.rw_conv[:, l, :], [128, 24])
    X.w0 = ld("rw_w0", D.rw_w0[:, l, :], [128, 4])
    X.a0 = ld("rw_a0", D.rw_a0[:, l, :], [128, 4])
    X.pv = ld("rw_pv", D.rw_pv[:, l, :], [128, 10])
    X.w2a2 = [ld(f"rw_w2a2{d}", D.rw_w2a2[l, d], [128, 256]) for d in range(2)]
    X.g2 = ld("rw_g2", D.rw_g2[l], [128, 256])
    X.msk = {k: ld("rw_" + k, D.consts[k][:, :], list(D.consts[k].t.shape)) for k in ("MA0", "MA1", "MB0", "MB1", "MC0", "MC1", "YXI")}
    X.rmask = ld("rw_rmask", D.consts["rmask"][:, :], [128, T])
    X.gneps = P.sb("rw_gneps", [128, 1], F32, st)
    P.V("memset", W=[X.gneps], ap=X.gneps[:], constant=GN_EPS)
    X.kkeps = P.sb("rw_kkeps", [128, 1], F32, st)
    P.V("memset", W=[X.kkeps], ap=X.kkeps[:], constant=1e-12)
    return X


def phase_rwkv(P, l, D, S, b, X, need_ctx):
    zbase = OFF_RW
    SEG = ((0, CTX), (CTX, T))
    for ih in range(2):
        with contextlib.ExitStack() as st:
            A = lambda nm, shp=(128, T): P.sb("rw_" + nm, list(shp), F32, st)
            zt = A("zt")
            r, k, v, kk, kts, g, ysum = A("r"), A("k"), A("v"), A("kk"), A("kts"), A("g"), A("ysum")
            z6 = A("z6")
            t1, t2, t3 = A("t1"), A("t2"), A("t3")
            t2x = g
            KRr = P.sb("rw_KR", [128, NCH, 2, RC], F32R, st)
            BKr = P.sb("rw_BK", [128, NCH, 2, RC], F32R, st)
            KR = _View(KRr, F32)
            BK = _View(BKr, F32)
            wtot = A("wtot", (128, NCH))

            def conv(dst, tile):
                P.dma("sp", R=[D.uT.s(b)], W=[zt], out=zt[:], in_=D.uT[b, zbase + tile * 128:zbase + (tile + 1) * 128, :])
                _ts(P, dst[:], zt[:], X.cw[:, 8 + tile:9 + tile], ALU.mult, [zt, X.cw], [dst])
                for (s0, s1) in SEG:
                    P.V("scalar_tensor_tensor", R=[zt, X.cw, dst], W=[dst], out=dst[:, s0 + 1:s1], in0=zt[:, s0:s1 - 1],
                        scalar=X.cw[:, tile:tile + 1], in1=dst[:, s0 + 1:s1], op0=ALU.mult, op1=ALU.add)
                    P.V("scalar_tensor_tensor", R=[zt, X.cw, dst], W=[dst], out=dst[:, s0:s1 - 1], in0=zt[:, s0 + 1:s1],
                        scalar=X.cw[:, 16 + tile:17 + tile], in1=dst[:, s0:s1 - 1], op0=ALU.mult, op1=ALU.add)

            conv(r, ih)
            conv(k, 2 + ih)
            conv(v, 4 + ih)
            conv(z6, 6)
            P.A("activation", R=[z6], W=[z6], out=z6[0:64, :], in_=z6[0:64, :], func=AF.Tanh)
            P.V("memset", W=[ysum], ap=ysum[:], constant=0.0)
            P.V("memset", W=[kts], ap=kts[:], constant=0.0)
            _ts(P, kk[:], k[:], X.pv[:, 0 + ih:1 + ih], ALU.mult, [k, X.pv], [kk])
            for (t0, n) in TILES:
                ps = S.psum[0]
                P.A("activation", R=[kk], W=[t1], out=t1[:, t0:t0 + n], in_=kk[:, t0:t0 + n], func=AF.Square)
                P.mm(ps[:, 0:n], S.bs64[:], t1[:, t0:t0 + n], True, True, R=[S.bs64, t1], W=[ps])
                P.A("activation", R=[ps, X.kkeps], W=[t1], out=t1[:, t0:t0 + n], in_=ps[:, 0:n], func=AF.Sqrt, bias=X.kkeps[:, 0:1], scale=1.0)
                P.V("reciprocal", R=[t1], W=[t1], out=t1[:, t0:t0 + n], in_=t1[:, t0:t0 + n])
                _tt(P, kk[:, t0:t0 + n], kk[:, t0:t0 + n], t1[:, t0:t0 + n], ALU.mult, [kk, t1], [kk])
            for d in range(2):
                MA, MB, MC = X.msk[f"MA{d}"], X.msk[f"MB{d}"], X.msk[f"MC{d}"]
                for (t0, n) in TILES:
                    pw, pa = S.psum[0], S.psum[1]
                    P.mm(pw[:, 0:n], X.w2a2[d][0:64, ih * 128:(ih + 1) * 128], z6[0:64, t0:t0 + n], True, True, R=[X.w2a2[d], z6], W=[pw])
                    P.mm(pa[:, 0:n], X.w2a2[d][64:128, ih * 128:(ih + 1) * 128], z6[64:128, t0:t0 + n], True, True, R=[X.w2a2[d], z6], W=[pa])
                    P.A("activation", R=[pw, X.w0], W=[t1], out=t1[:, t0:t0 + n], in_=pw[:, 0:n], func=AF.Sigmoid,
                        bias=X.w0[:, 2 * d + ih:2 * d + ih + 1], scale=1.0)
                    P.A("activation", R=[pa, X.a0], W=[t2], out=t2[:, t0:t0 + n], in_=pa[:, 0:n], func=AF.Sigmoid,
                        bias=X.a0[:, 2 * d + ih:2 * d + ih + 1], scale=1.0)
                _ts(P, t1[:], t1[:], W_SCALE, ALU.mult, [t1], [t1])
                _ts(P, t3[:], t2[:], -1.0, ALU.add, [t2], [t3], X.pv[:, 2 + ih:3 + ih], ALU.mult)
                P.V("scalar_tensor_tensor", R=[t3, k], W=[t3], out=t3[:], in0=t3[:], scalar=1.0, in1=k[:], op0=ALU.add, op1=ALU.mult)
                _tt(P, kts[:], kts[:], t3[:], ALU.add, [kts, t3], [kts])
                _tt(P, t2[:], t2[:], kk[:], ALU.mult, [t2, kk], [t2])
                P.V("tensor_tensor_scan", R=[X.rmask, t1], W=[zt], out=zt[:], data0=X.rmask[:], data1=t1[:], initial=0.0,
                    op0=ALU.mult, op1=ALU.add)
                zt3 = zt[:].rearrange("p (c j) -> p c j", j=RC)
                if d == 1:
                    P.V("tensor_tensor", R=[zt], W=[t2x], out=t2x[:].rearrange("p (c j) -> p c j", j=RC),
                        in0=zt3[:, :, RC - 1:RC].to_broadcast([128, NCH, RC]), in1=zt3, op=ALU.subtract)
                    _tt(P, zt[:], t2x[:], t1[:], ALU.add, [t2x, t1], [zt])
                last = RC - 1 if d == 0 else 0
                P.A("activation", R=[zt], W=[wtot], out=wtot[:], in_=zt3[:, :, last], func=AF.Exp)
                c3 = lambda tt_: tt_[:].rearrange("p (c j) -> p c j", j=RC)
                P.A("activation", R=[zt], W=[t2x], out=t2x[:], in_=zt[:], func=AF.Exp)
                _tt(P, KRr[:, :, 1, :], c3(t2x), c3(r), ALU.mult, [t2x, r], [KRr])
                P.A("activation", R=[zt], W=[t2x], out=t2x[:], in_=zt[:], func=AF.Exp, scale=-1.0)
                _tt(P, BKr[:, :, 1, :], c3(t2x), c3(t3), ALU.mult, [t2x, t3], [BKr])
                _tt(P, BKr[:, :, 0, :], c3(t2x), c3(t2), ALU.mult, [t2x, t2], [BKr])
                _tt(P, zt[:], zt[:], t1[:], ALU.subtract, [zt, t1], [zt])
                P.A("activation", R=[zt], W=[t2x], out=t2x[:], in_=zt[:], func=AF.Exp)
                _tt(P, KRr[:, :, 0, :], c3(t2x), c3(kk), ALU.mult, [t2x, kk], [KRr])
                rwkv_scan(P, S, X, d, st, KRr, BKr, wtot, v, ysum, MA, MB, MC)
            z7 = t3
            conv(z7, 7)
            P.A("activation", R=[z7], W=[z7], out=z7[:], in_=z7[:], func=AF.Sigmoid)
            for (t0, n) in TILES:
                ps2 = S.psum[1]
                P.mm(ps2[:, 0:n], X.g2[:, ih * 128:(ih + 1) * 128], z7[:, t0:t0 + n], True, True, R=[X.g2, z7], W=[ps2])
                P.A("activation", R=[ps2], W=[g], out=g[:, t0:t0 + n], in_=ps2[:, 0:n], func=AF.Identity)
            for (t0, n) in TILES:
                if t0 < CTX and not need_ctx:
                    continue
                sl = slice(t0, t0 + n)
                pm, pv_, pb = S.psum[0], S.psum[1], S.psum[2]
                P.mm(pm[:, 0:n], S.bd64[:], ysum[:, sl], True, True, R=[S.bd64, ysum], W=[pm])
                _tt(P, t1[:, sl], ysum[:, sl], pm[:, 0:n], ALU.subtract, [ysum, pm], [t1])
                P.A("activation", R=[t1], W=[t2], out=t2[:, sl], in_=t1[:, sl], func=AF.Square)
                P.mm(pv_[:, 0:n], S.bd64[:], t2[:, sl], True, True, R=[S.bd64, t2], W=[pv_])
                P.A("activation", R=[pv_, X.gneps], W=[t2], out=t2[:, sl], in_=pv_[:, 0:n], func=AF.Sqrt, bias=X.gneps[:, 0:1], scale=1.0)
                P.V("reciprocal", R=[t2], W=[t2], out=t2[:, sl], in_=t2[:, sl])
                _tt(P, t1[:, sl], t1[:, sl], t2[:, sl], ALU.mult, [t1, t2], [t1])
                _ts(P, t1[:, sl], t1[:, sl], X.pv[:, 6 + ih:7 + ih], ALU.mult, [t1, X.pv], [t1], X.pv[:, 8 + ih:9 + ih], ALU.add)
                P.V("scalar_tensor_tensor", R=[r, kts, X.pv], W=[t2], out=t2[:, sl], in0=r[:, sl], scalar=X.pv[:, 4 + ih:5 + ih],
                    in1=kts[:, sl], op0=ALU.mult, op1=ALU.mult)
                P.mm(pb[:, 0:n], S.bs64[:], t2[:, sl], True, True, R=[S.bs64, t2], W=[pb])
                _tt(P, t2[:, sl], pb[:, 0:n], v[:, sl], ALU.mult, [pb, v], [t2])
                _tt(P, t1[:, sl], t1[:, sl], t2[:, sl], ALU.add, [t1, t2], [t1])
                _tt(P, t1[:, sl], t1[:, sl], g[:, sl], ALU.mult, [t1, g], [t1])
            c0 = 0 if need_ctx else CTX
            P.dma("sp", R=[t1], W=[D.orw.s(b)], out=D.orw[b, ih * 128:(ih + 1) * 128, c0:T], in_=t1[:, c0:T])
        P.barrier()


def rwkv_scan(P, S, X, d, st0, KR, BK, wtot, v, ysum, MA, MB, MC):
    G = 2
    with contextlib.ExitStack() as st:
        B2 = lambda nm, shp: [P.sb(f"rs_{nm}{i}", list(shp), F32R, st) for i in range(2)]
        ST = P.sb("rs_ST", [128, 128], F32R, st)
        P.V("tensor_scalar", R=[S.ident], W=[ST], out=ST[:], in0=S.ident[:], scalar1=0.0, scalar2=None, op0=ALU.mult)
        identR = P.sb("rs_identR", [128, 128], F32R, st)
        P.V("tensor_copy", R=[S.ident], W=[identR], out=identR[:], in_=S.ident[:])
        bf = lambda ap: ap.bitcast(F32)
        YA, AB = B2("YA", (64, G, 2, 128)), B2("AB", (64, G, 2, 128))
        YX = B2("YX", (64, G, 2, 2, RC))
        YT = B2("YT", (64, G, 2, RC))
        X6 = B2("X6", (64, G, 2, RC))
        BKt, Vt = B2("BKt", (64, G, 2, 128)), B2("Vt", (64, G, 128))
        RHS, Ps = B2("RHS", (64, 128)), B2("Ps", (64, 128))
        if d == 0:
            order = list(range(NCH))
        else:
            nc_ctx = CTX // RC
            order = list(range(nc_ctx - 1, -1, -1)) + list(range(NCH - 1, nc_ctx - 1, -1))
        pairs = [order[i:i + G] for i in range(0, NCH, G)]

        def pre_stages(pi):
            cs = pairs[pi]
            q = pi % 2
            ya, ab, bkt, vt, x6 = YA[q], AB[q], BKt[q], Vt[q], X6[q]
            stages = []

            def s_init():
                pA, pB, pC = S.psum[0], S.psum[1], S.psum[2]
                for gi, c in enumerate(cs):
                    for hp in range(2):
                        lo = hp * 64
                        o = (gi * 2 + hp)
                        krc = KR[lo:lo + 64, c, :, :].rearrange("p a b -> p (a b)")
                        P.mm(pA[0:64, o * 128:(o + 1) * 128], BK[lo:lo + 64, c, 0, :], krc, True, True, R=[BK, KR], W=[pA])
                        P.mm(pB[0:64, o * 128:(o + 1) * 128], BK[lo:lo + 64, c, 1, :], krc, True, True, R=[BK, KR], W=[pB])
                        P.mm(pC[0:64, o * 64:(o + 1) * 64], KR[lo:lo + 64, c, 0, :], BK[lo:lo + 64, c, 0, :], True, True, R=[BK, KR], W=[pC])
                _tt(P, ya[:].rearrange("p g a b -> p (g a b)"), pA[0:64, 0:512], MA[:], ALU.mult, [pA, MA], [ya])
                _tt(P, ab[:].rearrange("p g a b -> p (g a b)"), pB[0:64, 0:512], MB[:], ALU.mult, [pB, MB], [ab])
                yx, yt = YX[0], YT[0]
                _tt(P, yt[:].rearrange("p g a b -> p (g a b)"), pC[0:64, 0:256], MC[:], ALU.mult, [pC, MC], [yt])
                P.A("activation", R=[X.msk["YXI"]], W=[yx], out=yx[:].rearrange("p g a b c -> p (g a b c)"), in_=X.msk["YXI"][:],
                    func=AF.Identity)
                P.V("tensor_copy", R=[ya], W=[yx], out=yx[:, :, :, 0, :], in_=bf(ya[:, :, :, 0:RC]))
            stages.append(s_init)

            def mk_step(kstep):
                def s_step():
                    yx, yt = YX[kstep % 2], YT[kstep % 2]
                    yxn, ytn = YX[(kstep + 1) % 2], YT[(kstep + 1) % 2]
                    pa_, pc_ = S.psum[3], S.psum[4]
                    for gi in range(G):
                        for hp in range(2):
                            o = gi * 2 + hp
                            P.mm(pa_[0:64, o * 128:(o + 1) * 128], yt[:, gi, hp, :], yx[:, gi, hp, :, :].rearrange("p a b -> p (a b)"),
                                 True, True, R=[yt, yx], W=[pa_])
                            if kstep < 5:
                                P.mm(pc_[0:64, o * 64:(o + 1) * 64], yx[:, gi, hp, 0, :], yt[:, gi, hp, :], True, True, R=[yt, yx], W=[pc_])
                    pa4 = pa_[0:64, 0:512].rearrange("p (g a b c) -> p g a b c", g=G, a=2, b=2)
                    if kstep < 5:
                        P.A("activation", R=[pa_], W=[yxn], out=yxn[:, :, :, 0, :], in_=pa4[:, :, :, 0, :], func=AF.Identity)
                        _tt(P, yxn[:, :, :, 1, :], bf(yx[:, :, :, 1, :]), pa4[:, :, :, 1, :], ALU.add, [yx, pa_], [yxn])
                        P.A("activation", R=[pc_], W=[ytn], out=ytn[:].rearrange("p g a b -> p (g a b)"), in_=pc_[0:64, 0:256],
                            func=AF.Identity)
                    else:
                        _tt(P, x6[:], bf(yx[:, :, :, 1, :]), pa4[:, :, :, 1, :], ALU.add, [yx, pa_], [x6])
                return s_step
            for kstep in range(6):
                stages.append(mk_step(kstep))

            def s_tr():
                pT, pV = S.psum[5], S.psum[2]
                for gi, c in enumerate(cs):
                    P.PE("transpose", R=[BK, S.ident], W=[pT], out=pT[0:64, gi * 256:gi * 256 + 128], in_=BK[:, c, 0, :].bitcast(F32), identity=S.ident[:])
                    P.PE("transpose", R=[BK, S.ident], W=[pT], out=pT[0:64, gi * 256 + 128:gi * 256 + 256], in_=BK[:, c, 1, :].bitcast(F32),
                         identity=S.ident[:])
                    P.PE("transpose", R=[v, S.ident], W=[pV], out=pV[0:64, 256 + gi * 128:256 + (gi + 1) * 128], in_=v[:, c * RC:(c + 1) * RC],
                         identity=S.ident[:])
                pT4 = pT[0:64, 0:512].rearrange("p (g a b) -> p g a b", g=G, a=2)
                _ts(P, bkt[:, :, 0, :], pT4[:, :, 0, :], -1.0, ALU.mult, [pT], [bkt])
                P.A("activation", R=[pT], W=[bkt], out=bkt[:, :, 1, :], in_=pT4[:, :, 1, :], func=AF.Identity)
                P.A("activation", R=[pV], W=[vt], out=vt[:].rearrange("p g b -> p (g b)"), in_=pV[0:64, 256:512], func=AF.Identity)
            stages.append(s_tr)
            return stages

        def seq_stages(pi):
            cs = pairs[pi]
            q = pi % 2
            ya, ab, bkt, vt, x6 = YA[q], AB[q], BKt[q], Vt[q], X6[q]
            stages = []
            for gi, c in enumerate(cs):
                rhs, ps_ = RHS[gi], Ps[gi]
                pR, pP, pY, pS = S.psum[6], S.psum[7], S.psum[6], S.psum[7]

                def s1(gi=gi, c=c, rhs=rhs):
                    for hp in range(2):
                        lo = hp * 64
                        P.mm(pR[0:64, lo:lo + 64], KR[lo:lo + 64, c, 0, :], ST[lo:lo + 64, lo:lo + 64], True, False, R=[KR, ST], W=[pR])
                        P.mm(pR[0:64, lo:lo + 64], ab[:, gi, hp, 0:RC], vt[:, gi, lo:lo + 64], False, True, R=[ab, vt], W=[pR])
                    P.V("tensor_copy", R=[pR], W=[rhs], out=rhs[:], in_=pR[0:64, 0:128])

                def s2(gi=gi, c=c, rhs=rhs, ps_=ps_):
                    for hp in range(2):
                        lo = hp * 64
                        P.mm(pP[0:64, lo:lo + 64], x6[:, gi, hp, :], rhs[:, lo:lo + 64], True, True, R=[x6, rhs], W=[pP])
                    P.V("tensor_copy", R=[pP], W=[ps_], out=ps_[:], in_=pP[0:64, 0:128])

                def s3(gi=gi, c=c, ps_=ps_):
                    P.mm(pS[:, 256:384], bkt[:, gi, 0, :], ps_[:], True, False, R=[bkt, ps_], W=[pS])
                    P.mm(pS[:, 256:384], bkt[:, gi, 1, :], vt[:, gi, :], False, False, R=[bkt, vt], W=[pS])
                    P.mm(pS[:, 256:384], identR[:], ST[:], False, True, R=[identR, ST], W=[pS])
                    for hp2 in range(2):
                        P.mm(pY[:, 256 + hp2 * 64:256 + (hp2 + 1) * 64], ST[:], KR[:, c, 1, :], hp2 == 0, False, R=[ST, KR], W=[pY])
                    P.mm(pY[:, 256:384], ps_[:], ya[:, gi, :, RC:2 * RC], False, False, R=[ps_, ya], W=[pY])
                    P.mm(pY[:, 256:384], vt[:, gi, :], ab[:, gi, :, RC:2 * RC], False, True, R=[vt, ab], W=[pY])
                    P.V("scalar_tensor_tensor", R=[pS, wtot, S.bs64], W=[ST], out=ST[:], in0=pS[:, 256:384], scalar=wtot[:, c:c + 1],
                        in1=S.bs64[:], op0=ALU.mult, op1=ALU.mult)
                    for hp in range(2):
                        lo = hp * 64
                        _tt(P, ysum[lo:lo + 64, c * RC:(c + 1) * RC], ysum[lo:lo + 64, c * RC:(c + 1) * RC],
                            pY[lo:lo + 64, 256 + lo:256 + lo + 64], ALU.add, [ysum, pY], [ysum])
                stages += [s1, s2, s3]
            return stages

        for f in pre_stages(0):
            f()
        for pi in range(len(pairs)):
            sq = seq_stages(pi)
            pr = pre_stages(pi + 1) if pi + 1 < len(pairs) else []
            n = max(len(sq), len(pr))
            for i in range(n):
                if i < len(pr):
                    pr[i]()
                if i < len(sq):
                    sq[i]()
    P.barrier()


def build(ncores_debug=False, nbc=NBC, phases=("mod", "inproj", "attn", "s5", "rwkv", "moe"), depth=DEPTH):
    nc = bass.Bass("TRN2", target_bir_lowering=False)
    P = Prog(nc, debug=ncores_debug)
    D = NS()
    S = NS()

    def ext(name, shape, dtype=F32):
        return P.dram(name, shape, dtype, kind="ExternalInput")

    D.xinT = ext("xinT", [nbc, DM, T])
    D.cT = ext("cT", [128, 8, 8])
    D.w_mod = ext("w_mod", [DEPTH, DM, 6 * DM])
    D.w_in = ext("w_in", [DEPTH, DM, INW])
    D.bmodT = ext("bmodT", [128, DEPTH, 48])
    D.norm1T = ext("norm1T", [128, DEPTH, 8])
    D.norm2T = ext("norm2T", [128, DEPTH, 8])
    D.qg = ext("qg", [128, DEPTH])
    D.kg = ext("kg", [128, DEPTH])
    for nm, shp in (("s5_A_rep_re", [128, DEPTH, 256]), ("s5_A_rep_im", [128, DEPTH, 256]), ("s5_ldt_rep", [128, DEPTH, 256]),
                    ("s5_Bt_re", [128, DEPTH, 256]), ("s5_Bt_im", [128, DEPTH, 256]), ("s5_A_st_re", [128, DEPTH, 16]),
                    ("s5_A_st_im", [128, DEPTH, 16]), ("s5_ldt_st", [128, DEPTH, 16]), ("s5_C_st_re", [128, DEPTH, 1024]),
                    ("s5_C_st_im", [128, DEPTH, 1024]), ("s5_dT", [128, DEPTH, 2]), ("s5_w_glu", [DEPTH, 256, 512])):
        setattr(D, nm, ext(nm, shp))
    D.os5 = P.dram("os5", [nbc, 256, T])
    for nm, shp in (("rw_conv", [128, DEPTH, 24]), ("rw_w0", [128, DEPTH, 4]), ("rw_a0", [128, DEPTH, 4]), ("rw_pv", [128, DEPTH, 10]),
                    ("rw_w2a2", [DEPTH, 2, 128, 256]), ("rw_g2", [DEPTH, 128, 256])):
        setattr(D, nm, ext(nm, shp))
    D.orw = P.dram("orw", [nbc, 256, T])
    D.x1T = P.dram("x1T", [nbc, DM, T])
    D.x2T = P.dram("x2T", [nbc, DM, T])
    D.Xe = P.dram("Xe", [NEXP * CAP, DM])
    D.Ye = P.dram("Ye", [NEXP * CAP, DM])
    for nm, shp in (("proj_att", [DEPTH, 512, DM]), ("proj_s5", [DEPTH, 256, DM]), ("proj_rwkv", [DEPTH, 256, DM]),
                    ("w_out", [DEPTH, DM, DM]), ("router_w", [DEPTH, DM, 36]), ("router_b", [128, DEPTH, 36]),
                    ("exp_w_gate", [DEPTH, NEXP, DM, DEXP]), ("exp_w_up", [DEPTH, NEXP, DM, DEXP]), ("exp_w_down", [DEPTH, NEXP, DEXP, DM])):
        setattr(D, nm, ext(nm, shp))
    consts = make_consts()
    D.consts = {k: ext("c_" + k, list(v.shape)) for k, v in consts.items()}
    D.out = P.dram("out", [nbc, DM, LAT], kind="ExternalOutput")
    D.uT = P.dram("uT", [nbc, INW, T])
    D.oatt = P.dram("oatt", [nbc, 512, T])

    S.psum = [P.ps(f"ps{i}", [128, 512]) for i in range(8)]
    for k in ("ident", "ones", "bd64", "bs64", "rot", "sh0", "sh1", "tri"):
        t = P.sb("k_" + k, [128, 128])
        P.dma("sp", R=[D.consts[k]], W=[t], out=t[:], in_=D.consts[k][:, :])
        setattr(S, k, t)
    S.maskG = P.sb("k_maskG", [128, 8])
    P.dma("sp", R=[D.consts["maskG"]], W=[S.maskG], out=S.maskG[:], in_=D.consts["maskG"][:, :])
    S.modT = P.sb("modT", [128, 48, 8])
    S.epsb = P.sb("epsb", [128, 1])
    P.V("memset", W=[S.epsb], ap=S.epsb[:], constant=EPS)
    for k, shp in (("bmodT", [128, DEPTH, 48]), ("norm1T", [128, DEPTH, 8]), ("norm2T", [128, DEPTH, 8])):
        t = P.sb(k, shp)
        P.dma("sp", R=[getattr(D, k)], W=[t], out=t[:], in_=getattr(D, k)[:, :, :])
        setattr(S, k, t)
    S.qg = []
    S.kg = []
    qgt = P.sb("qgt", [128, DEPTH])
    kgt = P.sb("kgt", [128, DEPTH])
    P.dma("sp", R=[D.qg], W=[qgt], out=qgt[:], in_=D.qg[:, :])
    P.dma("sp", R=[D.kg], W=[kgt], out=kgt[:], in_=D.kg[:, :])
    for l in range(DEPTH):
        a = NS.__new__(NS)
        S.qg.append(_ColView(qgt, l))
        S.kg.append(_ColView(kgt, l))

    xsrc = D.xinT
    for l in range(depth):
        need_ctx = l < DEPTH - 1
        if "mod" in phases:
            phase_mod(P, l, D, S)
        if "inproj" in phases:
            for b in range(nbc):
                phase_inproj(P, l, D, S, b, xsrc)
        if "attn" in phases:
            with contextlib.ExitStack() as lst:
                for k in ("cos", "sin"):
                    t = P.sb("k_" + k, [128, LAT], F32, lst)
                    P.dma("sp", R=[D.consts[k]], W=[t], out=t[:], in_=D.consts[k][:, :])
                    setattr(S, k, t)
                P.barrier()
                for b in range(nbc):
                    phase_attn(P, l, D, S, b, need_ctx)
        if "s5" in phases:
            with contextlib.ExitStack() as lst:
                X5 = s5_setup(P, l, D, S, lst)
                P.barrier()
                for b in range(nbc):
                    phase_s5(P, l, D, S, b, X5, need_ctx)
        if "rwkv" in phases:
            with contextlib.ExitStack() as lst:
                XR = rwkv_setup(P, l, D, S, lst)
                P.barrier()
                for b in range(nbc):
                    phase_rwkv(P, l, D, S, b, XR, need_ctx)
        if "rwkv0" in phases:
            with contextlib.ExitStack() as lst:
                z = P.sb("zrw", [128, T], F32, lst)
                P.V("memset", W=[z], ap=z[:], constant=0.0)
                for b in range(nbc):
                    for m in range(2):
                        P.dma("sp", R=[z], W=[D.orw.s(b)], out=D.orw[b, m * 128:(m + 1) * 128, :], in_=z[:])
            P.barrier()
        if "moe" in phases:
            with contextlib.ExitStack() as lst:
                G = NS()
                G.next = 0
                G.tiles = []
                G.slot = P.sb("g_slot", [128, 72, 2], U32, lst)
                G.gate = P.sb("g_gate", [128, 72, 2], F32, lst)
                with contextlib.ExitStack() as lst2:
                    XM = merge_setup(P, l, D, S, lst2)
                    P.barrier()
                    for b in range(nbc):
                        phase_merge(P, l, D, S, b, XM, need_ctx, xsrc, G)
                P.barrier()
                phase_experts(P, l, D, S)
                phase_combine(P, l, D, S, G, D.x2T, l == DEPTH - 1)
            xsrc = D.x2T
    P.barrier()
    P.finish(getattr(P, "dbg_out", []))
    P.finish([D.out])
    P.finish([D.uT.s(b) for b in range(nbc)] + [D.oatt.s(b) for b in range(nbc)] + [D.os5.s(b) for b in range(nbc)] + [D.orw.s(b) for b in range(nbc)])
    info = (P.ninstr, P.nwait)
    P.close()
    return nc, consts, info


class _View:
    def __init__(self, tt, dt):
        self.tt = tt
        self.dt = dt
        self.tok = tt.tok

    def __getitem__(self, idx):
        return self.tt[idx].bitcast(self.dt)

    def s(self, k):
        return self.tt.s(k)


class _ColView:
    def __init__(self, tt, l):
        self.tt = tt
        self.l = l
        self.tok = tt.tok

    def __getitem__(self, idx):
        return self.tt[:, self.l:self.l + 1]


def _tok(x):
    return x.tok if hasattr(x, "tok") else x


def host_inputs(inp, core, nbc=NBC):
    b0 = core * nbc
    x = np.asarray(inp["x"][b0:b0 + nbc], np.float32)
    ctx = np.asarray(inp["ctx"][b0:b0 + nbc], np.float32)
    m = {}
    m["xinT"] = np.ascontiguousarray(np.concatenate([ctx, x], axis=1).transpose(0, 2, 1))
    call = np.zeros((8, DM), np.float32)
    call[:nbc] = inp["c"][b0:b0 + nbc]
    call[4] = inp["c_ctx"]
    m["cT"] = np.ascontiguousarray(call.reshape(8, 8, 128).transpose(2, 1, 0))
    m["w_mod"] = np.asarray(inp["w_mod"], np.float32)
    m["w_in"] = np.asarray(inp["w_in"], np.float32)
    m["bmodT"] = fm(inp["b_mod"])
    m["norm1T"] = fm(inp["norm1"])
    m["norm2T"] = fm(inp["norm2"])
    L = DEPTH
    f32 = lambda k: np.asarray(inp[k], np.float32)
    for nm, key in (("re", "s5_a_re"), ("im", "s5_a_im")):
        a = f32(key)
        m["s5_A_rep_" + nm] = np.ascontiguousarray(np.repeat(a.reshape(L, 2, 2, 8, 64).transpose(3, 0, 1, 2, 4), 16, axis=0).reshape(128, L, 256))
        m["s5_A_st_" + nm] = np.ascontiguousarray(a.reshape(L, 2, 8, 2, 64).transpose(3, 4, 0, 1, 2).reshape(128, L, 16))
    ldt = f32("s5_log_dt")
    r = np.repeat(ldt.reshape(L, 2, 2, 8).transpose(3, 0, 1, 2), 16, axis=0)
    m["s5_ldt_rep"] = np.ascontiguousarray(np.broadcast_to(r[..., None], (128, L, 2, 2, 64)).reshape(128, L, 256))
    r = ldt.reshape(L, 2, 8, 2).transpose(3, 0, 1, 2)
    m["s5_ldt_st"] = np.ascontiguousarray(np.repeat(r[:, None], 64, axis=1).reshape(128, L, 16))
    for nm, key in (("re", "s5_b_re"), ("im", "s5_b_im")):
        bb = f32(key)
        m["s5_Bt_" + nm] = np.ascontiguousarray(bb.reshape(L, 2, 2, 8, 64, 16).transpose(3, 5, 0, 1, 2, 4).reshape(128, L, 256))
    for nm, key in (("re", "s5_c_re"), ("im", "s5_c_im")):
        cc = f32(key).reshape(L, 2, 8, 2, 16, 64)
        o = np.zeros((2, 64, L, 2, 8, 2, 2, 16), np.float32)
        for e in range(2):
            for pr in range(8):
                o[e, :, :, :, pr, pr % 2, e, :] = cc[:, :, pr, e].transpose(3, 0, 1, 2)
        m["s5_C_st_" + nm] = np.ascontiguousarray(o.reshape(128, L, 1024))
    m["s5_dT"] = fm(inp["s5_d"])
    m["s5_w_glu"] = f32("s5_w_glu")
    m["rw_conv"] = np.ascontiguousarray(fm(inp["rwkv_conv"]).reshape(128, L, 24))
    m["rw_w0"] = np.ascontiguousarray(fm(inp["rwkv_w0"]).reshape(128, L, 4))
    m["rw_a0"] = np.ascontiguousarray(fm(inp["rwkv_a0"]).reshape(128, L, 4))
    pv = np.stack([fm(inp[k_]) for k_ in ("rwkv_k_k", "rwkv_k_a", "rwkv_r_k", "rwkv_ln_w", "rwkv_ln_b")], axis=2)
    m["rw_pv"] = np.ascontiguousarray(pv.reshape(128, L, 10))
    m["rw_w2a2"] = np.ascontiguousarray(np.concatenate([f32("rwkv_w2"), f32("rwkv_a2")], axis=2))
    m["rw_g2"] = f32("rwkv_g2")
    for k_ in ("proj_att", "proj_s5", "proj_rwkv", "w_out", "exp_w_gate", "exp_w_up", "exp_w_down"):
        m[k_] = f32(k_)
    m["router_w"] = np.ascontiguousarray(np.concatenate([f32("router_g_w"), f32("router_e_w")], axis=-1))
    rb = np.concatenate([f32("router_g_b"), f32("router_e_b")], axis=-1)
    m["router_b"] = np.ascontiguousarray(np.broadcast_to(rb[None], (128, L, 36)))
    m["qg"] = np.ascontiguousarray(np.tile(np.asarray(inp["q_gain"], np.float32), (1, 2)).T)
    m["kg"] = np.ascontiguousarray(np.tile(np.asarray(inp["k_gain"], np.float32), (1, 2)).T)
    return m


def kernel(**inp):
    nc, consts, info = build()
    in_maps = []
    for core in range(NCORES):
        m = host_inputs(inp, core)
        for k, v in consts.items():
            m["c_" + k] = v
        in_maps.append(m)
    res = run_bass_kernel_spmd(nc, in_maps, core_ids=list(range(NCORES)))
    outs = [r["out"] for r in res.results]
    o = np.concatenate(outs, axis=0)
    return np.ascontiguousarray(o.transpose(0, 2, 1)).astype(np.float32)
```

```python
import contextlib
import math
import numpy as np
import concourse.bass as bass
import concourse.mybir as mybir
from concourse.bass_utils import run_bass_kernel_spmd

F32 = mybir.dt.float32
BF16 = mybir.dt.bfloat16
F32R = mybir.dt.float32r
I32 = mybir.dt.int32
U32 = mybir.dt.uint32
ALU = mybir.AluOpType
AF = mybir.ActivationFunctionType
AX = mybir.AxisListType

NCORES = 8
NBC = 4
DM = 1024
LAT = 2048
CTX = 256
T = LAT + CTX
DEPTH = 2
INW = 5376
OFF_S5 = 1024
OFF_RW = 1280
OFF_GATE = 2304
EPS = 1e-6
NEXP = 32
DEXP = 512
CAP = 1536


class Tok:
    __slots__ = ("lw", "rd", "name")

    def __init__(self, name=""):
        self.lw = None
        self.rd = {}
        self.name = name


class TT:
    def __init__(self, t, name):
        self.t = t
        self.name = name
        self.tok = Tok(name)
        self.slots = {}

    def __getitem__(self, idx):
        return self.t[idx]

    def s(self, k):
        if k not in self.slots:
            self.slots[k] = Tok(f"{self.name}.{k}")
        return self.slots[k]


def _tok(x):
    return x.tok if isinstance(x, TT) else x


class Prog:
    NDMA = 6

    def __init__(self, nc, debug=False):
        self.nc = nc
        self.debug = debug
        self.es = contextlib.ExitStack()
        self.engs = {"pe": nc.tensor, "act": nc.scalar, "dve": nc.vector, "pool": nc.gpsimd, "sp": nc.sync}
        self.sems = []
        self.vals = []
        self.esem = {}
        for n in self.engs:
            self.esem[n] = self._newsem("e_" + n)
        self.dsem = {}
        self.dnext = {}
        for q in ("sp", "act", "pool"):
            self.dsem[q] = [self._newsem(f"d_{q}{i}") for i in range(self.NDMA)]
            self.dnext[q] = 0
        self.seen = {n: {} for n in self.engs}
        self.ninstr = 0
        self.nwait = 0
        self.uid = 0

    def _newsem(self, name):
        s = self.es.enter_context(self.nc.semaphore(name))
        self.sems.append(s)
        self.vals.append(0)
        return len(self.sems) - 1

    def sb(self, name, shape, dtype=F32, stack=None):
        self.uid += 1
        t = (stack if stack is not None else self.es).enter_context(
            self.nc.sbuf_tensor(f"{name}_{self.uid}", list(shape), dtype))
        return TT(t, name)

    def ps(self, name, shape, dtype=F32, stack=None):
        self.uid += 1
        t = (stack if stack is not None else self.es).enter_context(
            self.nc.psum_tensor(f"{name}_{self.uid}", list(shape), dtype))
        return TT(t, name)

    def dram(self, name, shape, dtype=F32, kind="Internal"):
        if kind == "Internal" and self.debug:
            kind = "ExternalOutput"
        t = self.nc.dram_tensor(name, list(shape), dtype, kind=kind)
        return TT(t.ap(), name)

    def _need(self, eng, ev):
        if ev is None:
            return
        s, v = ev
        if self.seen[eng].get(s, 0) >= v:
            return
        self.engs[eng].wait_ge(self.sems[s], v)
        self.seen[eng][s] = v
        self.nwait += 1

    def _deps(self, eng, R, W):
        for x in R:
            self._need(eng, _tok(x).lw)
        for x in W:
            tk = _tok(x)
            self._need(eng, tk.lw)
            for s, v in tk.rd.items():
                self._need(eng, (s, v))

    def _mark(self, ev, R, W):
        for x in R:
            _tok(x).rd[ev[0]] = ev[1]
        for x in W:
            tk = _tok(x)
            tk.lw = ev
            tk.rd = {}

    def op(self, eng, meth, R=(), W=(), **kw):
        self._deps(eng, R, W)
        ins = getattr(self.engs[eng], meth)(**kw)
        s = self.esem[eng]
        self.vals[s] += 1
        ins.then_inc(self.sems[s], 1)
        self._mark((s, self.vals[s]), R, W)
        self.ninstr += 1
        return ins

    def V(self, meth, **kw):
        return self.op("dve", meth, **kw)

    def A(self, meth, **kw):
        return self.op("act", meth, **kw)

    def G(self, meth, **kw):
        return self.op("pool", meth, **kw)

    def PE(self, meth, **kw):
        return self.op("pe", meth, **kw)

    def mm(self, out, lhsT, rhs, start, stop, R, W):
        return self.op("pe", "matmul", R=R, W=W, out=out, lhsT=lhsT, rhs=rhs, start=start, stop=stop)

    def dma(self, q, R=(), W=(), meth="dma_start", **kw):
        self._deps(q, R, W)
        k = self.dnext[q]
        self.dnext[q] = (k + 1) % self.NDMA
        s = self.dsem[q][k]
        if self.vals[s] > 0:
            self._need(q, (s, self.vals[s]))
        ins = getattr(self.engs[q], meth)(**kw)
        self.vals[s] += 16
        ins.then_inc(self.sems[s], 16)
        self._mark((s, self.vals[s]), R, W)
        self.ninstr += 1
        return ins

    def barrier(self):
        for e in self.engs:
            for s in range(len(self.sems)):
                if self.vals[s] > 0:
                    self._need(e, (s, self.vals[s]))

    def finish(self, toks):
        for x in toks:
            self._need("sp", _tok(x).lw)

    def close(self):
        self.es.close()


class NS:
    pass


def fm(v):
    v = np.asarray(v, np.float32)
    lead = v.shape[:-1]
    n = v.shape[-1] // 128
    r = v.reshape(lead + (n, 128))
    return np.ascontiguousarray(np.moveaxis(r, -1, 0))


def make_consts():
    c = {}
    c["ident"] = np.eye(128, dtype=np.float32)
    c["ones"] = np.ones((128, 128), np.float32)
    bd = np.zeros((128, 128), np.float32)
    bd[:64, :64] = 1.0 / 64
    bd[64:, 64:] = 1.0 / 64
    c["bd64"] = bd
    bd1 = np.zeros((128, 128), np.float32)
    bd1[:64, :64] = 1.0
    bd1[64:, 64:] = 1.0
    c["bs64"] = bd1
    rot = np.zeros((128, 128), np.float32)
    for hh in range(2):
        for blk in range(2):
            base = hh * 64 + blk * 32
            for i in range(16):
                rot[base + 16 + i, base + i] = -1.0
                rot[base + i, base + 16 + i] = 1.0
    c["rot"] = rot
    pos = np.arange(LAT)
    row = pos // 64
    col = pos % 64
    inv = 10000.0 ** (-np.arange(16, dtype=np.float32) / 16.0)
    cos = np.zeros((64, LAT), np.float32)
    sin = np.zeros((64, LAT), np.float32)
    for blk, pp in enumerate((row, col)):
        ang = pp[None, :].astype(np.float32) * inv[:, None]
        cc = np.cos(ang).astype(np.float32)
        ss = np.sin(ang).astype(np.float32)
        cos[blk * 32:blk * 32 + 16] = cc
        cos[blk * 32 + 16:blk * 32 + 32] = cc
        sin[blk * 32:blk * 32 + 16] = ss
        sin[blk * 32 + 16:blk * 32 + 32] = ss
    c["cos"] = np.concatenate([cos, cos], 0)
    c["sin"] = np.concatenate([sin, sin], 0)
    sh0 = np.zeros((128, 128), np.float32)
    sh1 = np.zeros((128, 128), np.float32)
    for j in range(64):
        sh0[64 + j, j] = 1.0
        sh1[j, 64 + j] = 1.0
    c["sh0"] = sh0
    c["sh1"] = sh1
    ss = np.arange(64)[:, None]
    tt = np.arange(64)[None, :]
    for d in range(2):
        before = (ss < tt) if d == 0 else (ss > tt)
        beq = (ss <= tt) if d == 0 else (ss >= tt)
        st_ = before.astype(np.float32)
        inc = beq.astype(np.float32)
        c[f"MA{d}"] = np.tile(np.concatenate([-st_, -inc], 1), (1, 4))
        c[f"MB{d}"] = np.tile(np.concatenate([st_, inc], 1), (1, 4))
        c[f"MC{d}"] = np.tile(-(st_.T), (1, 4))
    c["YXI"] = np.tile(np.concatenate([np.zeros((64, 64), np.float32), np.eye(64, dtype=np.float32)], 1), (1, 4))
    rm = np.ones((128, T), np.float32)
    rm[:, ::64] = 0.0
    c["rmask"] = rm
    c["eoff"] = np.tile((np.arange(NEXP, dtype=np.float32) * CAP)[None, :], (128, 1))
    mg = np.zeros((128, 8), np.float32)
    for p in range(128):
        mg[p, p // 16] = 1.0
    c["maskG"] = mg
    tri = np.zeros((128, 128), np.float32)
    for k in range(128):
        tri[k, k + 1:] = 1.0
    c["tri"] = tri
    return c


def phase_mod(P, l, D, S):
    modT = S.modT
    with contextlib.ExitStack() as st:
        wm = [P.sb(f"wm{i}", [128, 8, 1024], F32, st) for i in range(2)]
        ct = P.sb("ct", [128, 8, 8], F32, st)
        sct = P.sb("sct", [128, 8, 8], F32, st)
        psA = S.psum[0]
        P.dma("sp", R=[D.cT], W=[ct], out=ct[:], in_=D.cT[:, :, :])
        P.A("activation", R=[ct], W=[sct], out=sct[:], in_=ct[:], func=AF.Silu)
        for j in range(6):
            w = wm[j % 2]
            P.dma("sp", R=[D.w_mod], W=[w], out=w[:],
                  in_=D.w_mod[l, :, j * 1024:(j + 1) * 1024].rearrange("(kt p) n -> p kt n", p=128))
            for m in range(8):
                o = (j * 8 + m) * 8
                for kt in range(8):
                    P.mm(psA[:, o:o + 8], w[:, kt, m * 128:(m + 1) * 128], sct[:, kt, :], kt == 0, kt == 7,
                         R=[w, sct], W=[psA])
        P.V("tensor_tensor", R=[psA, S.bmodT], W=[modT],
            out=modT[:], in0=psA[:, 0:384].rearrange("p (a b) -> p a b", b=8),
            in1=S.bmodT[:, l, :].unsqueeze(2).to_broadcast([128, 48, 8]), op=ALU.add)
        for j, nt in ((1, S.norm1T), (4, S.norm2T)):
            P.V("tensor_scalar", R=[modT], W=[modT], out=modT[:, j * 8:(j + 1) * 8, :], in0=modT[:, j * 8:(j + 1) * 8, :],
                scalar1=1.0, scalar2=None, op0=ALU.add)
            P.V("tensor_tensor", R=[modT, nt], W=[modT], out=modT[:, j * 8:(j + 1) * 8, :],
                in0=modT[:, j * 8:(j + 1) * 8, :],
                in1=nt[:, l, :].unsqueeze(2).to_broadcast([128, 8, 8]), op=ALU.mult)
    P.barrier()


TILES = [(0, 256)] + [(256 + i * 512, 512) for i in range(4)]


def rms_mod(P, S, x_t, n, col, jsc, jsh, out_fn, sq, rstd, ps):
    P.A("activation", R=[x_t], W=[sq], out=sq[:, :, 0:n], in_=x_t[:, :, 0:n], func=AF.Square)
    for kt in range(8):
        P.mm(ps[:, 0:n], S.ones[:], sq[:, kt, 0:n], kt == 0, kt == 7, R=[S.ones, sq], W=[ps])
    P.A("activation", R=[ps], W=[rstd], out=rstd[:, 0:n], in_=ps[:, 0:n], func=AF.Sqrt, scale=1.0 / DM,
        bias=S.epsb[:, 0:1])
    P.V("reciprocal", R=[rstd], W=[rstd], out=rstd[:, 0:n], in_=rstd[:, 0:n])
    for kt in range(8):
        P.V("tensor_tensor", R=[x_t, rstd], W=[sq], out=sq[:, kt, 0:n], in0=x_t[:, kt, 0:n], in1=rstd[:, 0:n],
            op=ALU.mult)
        o, wt = out_fn(kt)
        P.V("tensor_scalar", R=[sq, S.modT], W=wt, out=o, in0=sq[:, kt, 0:n],
            scalar1=S.modT[:, jsc * 8 + kt, col:col + 1], scalar2=S.modT[:, jsh * 8 + kt, col:col + 1],
            op0=ALU.mult, op1=ALU.add)


def phase_inproj(P, l, D, S, b, xsrc):
    with contextlib.ExitStack() as st:
        hT = P.sb("hT", [128, 8, T], BF16, st)
        xt = [P.sb(f"xt{i}", [128, 8, 512], F32, st) for i in range(2)]
        sq = P.sb("sq", [128, 8, 512], F32, st)
        rstd = P.sb("rstd", [128, 512], F32, st)
        wb = [P.sb(f"wb{i}", [128, 8, 768], BF16, st) for i in range(2)]
        stg = [P.sb(f"stg{i}", [128, T], F32, st) for i in range(2)]
        for ti, (t0, n) in enumerate(TILES):
            col = 4 if t0 < CTX else b
            x_t = xt[ti % 2]
            P.dma("sp", R=[xsrc], W=[x_t], out=x_t[:, :, 0:n],
                  in_=xsrc[b, :, t0:t0 + n].rearrange("(kt p) t -> p kt t", p=128))
            rms_mod(P, S, x_t, n, col, 1, 0, lambda kt: (hT[:, kt, t0:t0 + n], [hT.s(ti)]), sq, rstd, S.psum[0])
        hall = [hT.s(ti) for ti in range(len(TILES))]
        ci = 0
        for cb in range(7):
            w = wb[cb % 2]
            P.dma("pool", R=[D.w_in], W=[w], out=w[:],
                  in_=D.w_in[l, :, cb * 768:(cb + 1) * 768].rearrange("(kt p) n -> p kt n", p=128))
            for m in range(6):
                chunk = cb * 6 + m
                sg = stg[chunk % 2]
                for ti, (t0, n) in enumerate(TILES):
                    ps = S.psum[1 + (ci % 4)]
                    ci += 1
                    for kt in range(8):
                        P.mm(ps[:, 0:n], w[:, kt, m * 128:(m + 1) * 128], hT[:, kt, t0:t0 + n], kt == 0, kt == 7,
                             R=[w, hT.s(ti)], W=[ps])
                    if chunk * 128 >= OFF_GATE:
                        P.A("activation", R=[ps], W=[sg], out=sg[:, t0:t0 + n], in_=ps[:, 0:n], func=AF.Sigmoid)
                    elif ci % 2 == 0:
                        P.A("activation", R=[ps], W=[sg], out=sg[:, t0:t0 + n], in_=ps[:, 0:n], func=AF.Identity)
                    else:
                        P.V("tensor_copy", R=[ps], W=[sg], out=sg[:, t0:t0 + n], in_=ps[:, 0:n])
                P.dma("sp", R=[sg], W=[D.uT.s(b)], out=D.uT[b, chunk * 128:(chunk + 1) * 128, :], in_=sg[:])
    P.barrier()


def qk_norm_rope(P, S, load_fn, dst, nt, gain, stg, tmps):
    tmp, tmp2 = tmps
    for i in range(nt):
        sg = stg.s(i % 2)
        load_fn(i, i % 2)
        for ti, (t0, n) in enumerate(TILES):
            ps = S.psum[0]
            x = stg[:, i % 2, t0:t0 + n]
            d_ = dst[:, i, t0:t0 + n]
            P.A("activation", R=[sg], W=[tmp], out=tmp[:, 0:n], in_=x, func=AF.Square)
            P.mm(ps[:, 0:n], S.bd64[:], tmp[:, 0:n], True, True, R=[S.bd64, tmp], W=[ps])
            P.A("activation", R=[ps], W=[tmp], out=tmp[:, 0:n], in_=ps[:, 0:n], func=AF.Sqrt, scale=1.0,
                bias=S.epsb[:, 0:1])
            P.V("reciprocal", R=[tmp], W=[tmp], out=tmp[:, 0:n], in_=tmp[:, 0:n])
            if t0 < CTX:
                P.V("scalar_tensor_tensor", R=[sg, tmp, gain], W=[dst.s(i)], out=d_, in0=x, scalar=gain[:, 0:1], in1=tmp[:, 0:n],
                    op0=ALU.mult, op1=ALU.mult)
            else:
                P.V("scalar_tensor_tensor", R=[sg, tmp, gain], W=[sg], out=x, in0=x, scalar=gain[:, 0:1], in1=tmp[:, 0:n],
                    op0=ALU.mult, op1=ALU.mult)
                p0 = t0 - CTX
                ps2 = S.psum[1]
                P.mm(ps2[:, 0:n], S.rot[:], x, True, True, R=[S.rot, sg], W=[ps2])
                P.V("tensor_tensor", R=[ps2, S.sin], W=[tmp2], out=tmp2[:, 0:n], in0=ps2[:, 0:n],
                    in1=S.sin[:, p0:p0 + n], op=ALU.mult)
                P.V("tensor_tensor", R=[sg, S.cos], W=[tmp], out=tmp[:, 0:n], in0=x,
                    in1=S.cos[:, p0:p0 + n], op=ALU.mult)
                P.V("tensor_tensor", R=[tmp, tmp2], W=[dst.s(i)], out=d_, in0=tmp[:, 0:n],
                    in1=tmp2[:, 0:n], op=ALU.add)


def phase_attn(P, l, D, S, b, need_ctx):
    with contextlib.ExitStack() as st:
        qT = P.sb("qT", [128, 4, T], F32R, st)
        kTa = P.sb("kTa", [128, 2, T], F32R, st)
        kTb = P.sb("kTb", [128, 2, T], F32R, st)
        vaug = P.sb("vaug", [128, 18, 4, 192], BF16, st)
        oT = P.sb("oT", [128, 4, T], F32, st)
        pT = [P.sb(f"pT{i}", [128, 512], BF16, st) for i in range(3)]
        oacc = P.sb("oacc", [128, 512], F32, st)
        rden = P.sb("rden", [128, 512], F32, st)
        with contextlib.ExitStack() as st2:
            vT = P.sb("vT", [128, 2, T], F32, st2)
            stg = P.sb("qkstg", [128, 2, T], F32, st2)
            for i in range(2):
                P.dma("sp", R=[D.uT.s(b)], W=[vT.s(i)], out=vT[:, i, :], in_=D.uT[b, 768 + i * 128:768 + (i + 1) * 128, :])
            P.G("memset", W=[vaug], ap=vaug[:], constant=1.0)

            def ld_q(i, k):
                P.dma("sp", R=[D.uT.s(b)], W=[stg.s(k)], out=stg[:, k, :], in_=D.uT[b, i * 128:(i + 1) * 128, :])

            def ld_ka(i, k):
                P.dma("sp", R=[D.uT.s(b)], W=[stg.s(k)], out=stg[:, k, :], in_=D.uT[b, 512 + i * 128:512 + (i + 1) * 128, :])

            def ld_kb(i, k):
                P.dma("sp", R=[D.uT.s(b)], W=[stg.s(k)], out=stg[0:64, k, :], in_=D.uT[b, 512 + i * 128 + 64:512 + (i + 1) * 128, :])
                P.dma("sp", R=[D.uT.s(b)], W=[stg.s(k)], out=stg[64:128, k, :], in_=D.uT[b, 512 + i * 128:512 + i * 128 + 64, :])

            tmps = (P.sb("qk_tmp", [128, 512], F32, st2), P.sb("qk_tmp2", [128, 512], F32, st2))
            qk_norm_rope(P, S, ld_q, qT, 4, S.qg[l], stg, tmps)
            qk_norm_rope(P, S, ld_ka, kTa, 2, S.kg[l], stg, tmps)
            qk_norm_rope(P, S, ld_kb, kTb, 2, S.kg[l], stg, tmps)
            cnt = 0
            for i in range(2):
                for kt in range(18):
                    ps = S.psum[2 + cnt % 2]
                    cnt += 1
                    P.PE("transpose", R=[vT.s(i), S.ident], W=[ps], out=ps[:, 0:128], in_=vT[:, i, kt * 128:(kt + 1) * 128],
                         identity=S.ident[:])
                    for e in range(2):
                        P.V("tensor_copy", R=[ps], W=[vaug], out=vaug[:, kt, 2 * i + e, 64:128],
                            in_=ps[:, e * 64:(e + 1) * 64])
        P.barrier()
        qsegs = [(CTX + qc * 512, 512, list(range(18))) for qc in range(4)]
        if need_ctx:
            qsegs.append((0, CTX, [0, 1]))
        gi_ = 0
        for h in range(4):
            i, e = h // 2, h % 2
            for g in range(2):
                ksrc = kTa if e == g else kTb
                shm = S.sh0 if g == 0 else S.sh1
                lo = g * 64
                for (q0, qn, kts) in qsegs:
                    acc = S.psum[4 + (gi_ % 2)]
                    gi_ += 1
                    va_of = (lambda kt: vaug[:, kt, h, 64:192]) if g == 0 else (lambda kt: vaug[:, kt, h, 0:128])

                    def score(ki):
                        kt = kts[ki]
                        pss = S.psum[6 + (ki % 2)]
                        P.mm(pss[:, 0:qn], ksrc[lo:lo + 64, i, kt * 128:(kt + 1) * 128], qT[lo:lo + 64, h, q0:q0 + qn],
                             True, True, R=[ksrc.s(i), qT.s(h)], W=[pss])

                    score(0)
                    for ki, kt in enumerate(kts):
                        pss = S.psum[6 + (ki % 2)]
                        pt = pT[ki % 3]
                        P.A("activation", R=[pss], W=[pt], out=pt[:, 0:qn], in_=pss[:, 0:qn], func=AF.Exp, scale=0.125)
                        if ki + 1 < len(kts):
                            score(ki + 1)
                        P.mm(acc[:, 0:qn], va_of(kt), pt[:, 0:qn], ki == 0, ki == len(kts) - 1, R=[vaug, pt], W=[acc])
                    P.A("activation", R=[acc], W=[oacc], out=oacc[:, 0:qn], in_=acc[:, 0:qn], func=AF.Identity)
                    psd = S.psum[0]
                    P.mm(psd[:, 0:qn], shm[:], oacc[:, 0:qn], True, True, R=[shm, oacc], W=[psd])
                    P.V("reciprocal", R=[psd], W=[rden], out=rden[lo:lo + 64, 0:qn], in_=psd[lo:lo + 64, 0:qn])
                    P.V("tensor_tensor", R=[oacc, rden], W=[oT.s(h)], out=oT[lo:lo + 64, h, q0:q0 + qn],
                        in0=oacc[lo:lo + 64, 0:qn], in1=rden[lo:lo + 64, 0:qn], op=ALU.mult)
        c0 = 0 if need_ctx else CTX
        for h in range(4):
            P.dma("sp", R=[oT.s(h)], W=[D.oatt.s(b)], out=D.oatt[b, h * 128:(h + 1) * 128, c0:T], in_=oT[:, h, c0:T])
    P.barrier()


TWO_PI = 2.0 * math.pi


def _tt(P, out, a, b, op, R, W):
    P.V("tensor_tensor", R=R, W=W, out=out, in0=a, in1=b, op=op)


def _ts(P, out, a, s1, op0, R, W, s2=None, op1=None):
    if op1 is None:
        P.V("tensor_scalar", R=R, W=W, out=out, in0=a, scalar1=s1, scalar2=None, op0=op0)
    else:
        P.V("tensor_scalar", R=R, W=W, out=out, in0=a, scalar1=s1, scalar2=s2, op0=op0, op1=op1)


def sin_turns(P, st, r, out, F, name):
    ki = P.sb(name + "_ki", [128, F], I32, st)
    kf = P.sb(name + "_kf", [128, F], F32, st)
    m = P.sb(name + "_m", [128, F], F32, st)
    P.V("tensor_copy", R=[r], W=[ki], out=ki[:], in_=r[:])
    P.V("tensor_copy", R=[ki], W=[kf], out=kf[:], in_=ki[:])
    _tt(P, kf[:], r[:], kf[:], ALU.subtract, [r, kf], [kf])
    _ts(P, m[:], kf[:], 0.5, ALU.is_gt, [kf], [m])
    _tt(P, kf[:], kf[:], m[:], ALU.subtract, [kf, m], [kf])
    _ts(P, m[:], kf[:], -0.5, ALU.is_lt, [kf], [m])
    _tt(P, kf[:], kf[:], m[:], ALU.add, [kf, m], [kf])
    P.A("activation", R=[kf], W=[out], out=out[:], in_=kf[:], func=AF.Sin, scale=TWO_PI)


def s5_derive(P, st, are, aim, ldt, F, name, need_z):
    mk = lambda n: P.sb(f"{name}_{n}", [128, F], F32, st)
    dt, rho, th, r, r2, cth, sth = mk("dt"), mk("rho"), mk("th"), mk("r"), mk("r2"), mk("cth"), mk("sth")
    P.A("activation", R=[ldt], W=[dt], out=dt[:], in_=ldt[:], func=AF.Exp)
    _tt(P, rho[:], are[:], dt[:], ALU.mult, [are, dt], [rho])
    P.A("activation", R=[rho], W=[rho], out=rho[:], in_=rho[:], func=AF.Exp)
    _tt(P, th[:], aim[:], dt[:], ALU.mult, [aim, dt], [th])
    _ts(P, r[:], th[:], 1.0 / TWO_PI, ALU.mult, [th], [r])
    _ts(P, r2[:], r[:], 0.25, ALU.add, [r], [r2])
    sin_turns(P, st, r, sth, F, name + "_s")
    sin_turns(P, st, r2, cth, F, name + "_c")
    res = dict(rho=rho, cth=cth, sth=sth)
    if need_z:
        abr, abi, den, nr, zre, zim, t1 = mk("abr"), mk("abi"), mk("den"), mk("nr"), mk("zre"), mk("zim"), mk("t1")
        _tt(P, abr[:], rho[:], cth[:], ALU.mult, [rho, cth], [abr])
        _tt(P, abi[:], rho[:], sth[:], ALU.mult, [rho, sth], [abi])
        _tt(P, den[:], are[:], are[:], ALU.mult, [are], [den])
        _tt(P, t1[:], aim[:], aim[:], ALU.mult, [aim], [t1])
        _tt(P, den[:], den[:], t1[:], ALU.add, [den, t1], [den])
        P.V("reciprocal", R=[den], W=[den], out=den[:], in_=den[:])
        _ts(P, nr[:], abr[:], -1.0, ALU.add, [abr], [nr])
        _tt(P, zre[:], nr[:], are[:], ALU.mult, [nr, are], [zre])
        _tt(P, t1[:], abi[:], aim[:], ALU.mult, [abi, aim], [t1])
        _tt(P, zre[:], zre[:], t1[:], ALU.add, [zre, t1], [zre])
        _tt(P, zre[:], zre[:], den[:], ALU.mult, [zre, den], [zre])
        _tt(P, zim[:], abi[:], are[:], ALU.mult, [abi, are], [zim])
        _tt(P, t1[:], nr[:], aim[:], ALU.mult, [nr, aim], [t1])
        _tt(P, zim[:], zim[:], t1[:], ALU.subtract, [zim, t1], [zim])
        _tt(P, zim[:], zim[:], den[:], ALU.mult, [zim, den], [zim])
        res.update(zre=zre, zim=zim)
    return res


def s5_setup(P, l, D, S, st):
    X = NS()
    ld = lambda nm, src, shp, dt=F32: _load(P, st, nm, src, shp, dt)
    are = ld("s5are", D.s5_A_rep_re[:, l, :], [128, 256])
    aim = ld("s5aim", D.s5_A_rep_im[:, l, :], [128, 256])
    ldt = ld("s5ldt", D.s5_ldt_rep[:, l, :], [128, 256])
    bre = ld("s5bre", D.s5_Bt_re[:, l, :], [128, 256])
    bim = ld("s5bim", D.s5_Bt_im[:, l, :], [128, 256])
    X.BTr = P.sb("BTr", [128, 4, 8, 64], BF16, st)
    X.BTi = P.sb("BTi", [128, 4, 8, 64], BF16, st)
    with contextlib.ExitStack() as st2:
        r = s5_derive(P, st2, are, aim, ldt, 256, "dr", True)
        bbr = P.sb("bbr", [128, 256], F32, st2)
        bbi = P.sb("bbi", [128, 256], F32, st2)
        t1 = P.sb("bt1", [128, 256], F32, st2)
        _tt(P, bbr[:], r["zre"][:], bre[:], ALU.mult, [r["zre"], bre], [bbr])
        _tt(P, t1[:], r["zim"][:], bim[:], ALU.mult, [r["zim"], bim], [t1])
        _tt(P, bbr[:], bbr[:], t1[:], ALU.subtract, [bbr, t1], [bbr])
        _tt(P, bbi[:], r["zre"][:], bim[:], ALU.mult, [r["zre"], bim], [bbi])
        _tt(P, t1[:], r["zim"][:], bre[:], ALU.mult, [r["zim"], bre], [t1])
        _tt(P, bbi[:], bbi[:], t1[:], ALU.add, [bbi, t1], [bbi])
        for dst, src in ((X.BTr, bbr), (X.BTi, bbi)):
            for q in range(4):
                P.V("tensor_tensor", R=[src, S.maskG], W=[dst], out=dst[:, q, :, :],
                    in0=src[:, q * 64:(q + 1) * 64].unsqueeze(1).to_broadcast([128, 8, 64]),
                    in1=S.maskG[:, :].unsqueeze(2).to_broadcast([128, 8, 64]), op=ALU.mult)
    P.barrier()
    sare = ld("s5sare", D.s5_A_st_re[:, l, :], [128, 16])
    saim = ld("s5saim", D.s5_A_st_im[:, l, :], [128, 16])
    sldt = ld("s5sldt", D.s5_ldt_st[:, l, :], [128, 16])
    dbg(P, "sare", sare, sare[:], [128, 16])
    dbg(P, "sldt", sldt, sldt[:], [128, 16])
    r2 = s5_derive(P, st, sare, saim, sldt, 16, "ds", False)
    X.rho = r2["rho"]
    dbg(P, "rho", X.rho, X.rho[:], [128, 16])
    dbg(P, "cth", r2["cth"], r2["cth"][:], [128, 16])
    dbg(P, "sth", r2["sth"], r2["sth"][:], [128, 16])
    X.cosT = P.sb("s5cos", [128, 16, 256], F32, st)
    X.sinT = P.sb("s5sin", [128, 16, 256], F32, st)
    nsin = P.sb("s5nsin", [128, 16, 256], F32, st)
    X.nsinT = nsin
    P.V("memset", W=[X.cosT], ap=X.cosT[:, :, 0:1], constant=1.0)
    P.V("memset", W=[X.sinT], ap=X.sinT[:, :, 0:1], constant=0.0)
    P.V("tensor_copy", R=[r2["cth"]], W=[X.cosT], out=X.cosT[:, :, 1], in_=r2["cth"][:])
    P.V("tensor_copy", R=[r2["sth"]], W=[X.sinT], out=X.sinT[:, :, 1], in_=r2["sth"][:])
    tmpa = P.sb("s5tmpa", [128, 16, 128], F32, st)
    m = 2
    while m < 256:
        cm, sm = X.cosT[:, :, m], X.sinT[:, :, m]
        c1, s1 = X.cosT[:, :, 1], X.sinT[:, :, 1]
        cp, sp_ = X.cosT[:, :, m - 1], X.sinT[:, :, m - 1]
        ta = tmpa[:, :, 0]
        tb = tmpa[:, :, 1]
        _tt(P, ta, cp, c1, ALU.mult, [X.cosT], [tmpa])
        _tt(P, tb, sp_, s1, ALU.mult, [X.sinT], [tmpa])
        _tt(P, cm, ta, tb, ALU.subtract, [tmpa], [X.cosT])
        _tt(P, ta, cp, s1, ALU.mult, [X.cosT, X.sinT], [tmpa])
        _tt(P, tb, sp_, c1, ALU.mult, [X.cosT, X.sinT], [tmpa])
        _tt(P, sm, ta, tb, ALU.add, [tmpa], [X.sinT])
        n = m - 1
        cmb = X.cosT[:, :, m:m + 1].to_broadcast([128, 16, n])
        smb = X.sinT[:, :, m:m + 1].to_broadcast([128, 16, n])
        cj, sj = X.cosT[:, :, 1:m], X.sinT[:, :, 1:m]
        co, so = X.cosT[:, :, m + 1:2 * m], X.sinT[:, :, m + 1:2 * m]
        ta = tmpa[:, :, 0:n]
        _tt(P, ta, sj, smb, ALU.mult, [X.sinT], [tmpa])
        _tt(P, co, cj, cmb, ALU.mult, [X.cosT], [X.cosT])
        _tt(P, co, co, ta, ALU.subtract, [X.cosT, tmpa], [X.cosT])
        _tt(P, ta, cj, smb, ALU.mult, [X.cosT, X.sinT], [tmpa])
        _tt(P, so, sj, cmb, ALU.mult, [X.sinT, X.cosT], [X.sinT])
        _tt(P, so, so, ta, ALU.add, [X.sinT, tmpa], [X.sinT])
        m *= 2
    _ts(P, nsin[:], X.sinT[:], -1.0, ALU.mult, [X.sinT], [nsin])
    dbg(P, "cosT", X.cosT, X.cosT[:], [128, 16, 256])
    dbg(P, "sinT", X.sinT, X.sinT[:], [128, 16, 256])
    dbg(P, "BTr", X.BTr, X.BTr[:], [128, 4, 8, 64], BF16)
    X.rhoT = P.sb("s5rhoT", [128, 16, 256], F32, st)
    P.V("tensor_copy", R=[X.rho], W=[X.rhoT], out=X.rhoT[:], in_=X.rho[:].unsqueeze(2).to_broadcast([128, 16, 256]))
    cre = ld("s5cre", D.s5_C_st_re[:, l, :], [128, 16 * 64])
    cim = ld("s5cim", D.s5_C_st_im[:, l, :], [128, 16 * 64])
    X.Cr = P.sb("s5Cr", [128, 16, 64], BF16, st)
    X.Ci = P.sb("s5Ci", [128, 16, 64], BF16, st)
    P.V("tensor_copy", R=[cre], W=[X.Cr], out=X.Cr[:], in_=cre[:].rearrange("p (a b) -> p a b", b=64))
    _ts(P, X.Ci[:], cim[:].rearrange("p (a b) -> p a b", b=64), -1.0, ALU.mult, [cim], [X.Ci])
    X.dT = ld("s5dT", D.s5_dT[:, l, :], [128, 2])
    X.wglu = P.sb("s5wglu", [128, 2, 512], BF16, st)
    P.dma("pool", R=[D.s5_w_glu], W=[X.wglu], out=X.wglu[:], in_=D.s5_w_glu[l].rearrange("(kt p) n -> p kt n", p=128))
    return X


def dbg(P, name, tt, ap, shape, dt=F32):
    if not P.debug:
        return
    d = P.dram("dbg_" + name, shape, dt, kind="ExternalOutput")
    P.dma("sp", R=[tt], W=[d], out=d[tuple(slice(None) for _ in shape)], in_=ap)
    P.dbg_out = getattr(P, "dbg_out", []) + [d]


def _load(P, st, nm, src_ap, shp, dt=F32):
    t = P.sb(nm, shp, dt, st)
    P.dma("sp", R=[], W=[t], out=t[:], in_=src_ap)
    return t


S5CH = 256


def phase_s5(P, l, D, S, b, X, need_ctx):
    nch = T // S5CH
    with contextlib.ExitStack() as st:
        sT = P.sb("s5sT", [128, 2, T], F32, st)
        sT16 = P.sb("s5sT16", [128, 2, T], BF16, st)
        yacc = P.sb("s5yacc", [128, 2, T], F32, st)
        for ft in range(2):
            P.dma("sp", R=[D.uT.s(b)], W=[sT], out=sT[:, ft, :], in_=D.uT[b, OFF_S5 + ft * 128:OFF_S5 + (ft + 1) * 128, :])
        P.A("activation", R=[sT], W=[sT16], out=sT16[:], in_=sT[:], func=AF.Identity)
        for ft in range(2):
            _ts(P, yacc[:, ft, :], sT[:, ft, :], X.dT[:, ft:ft + 1], ALU.mult, [sT, X.dT], [yacc])
        mk = lambda n, dt=F32: [P.sb(f"s5{n}{i}", [128, S5CH], dt, st) for i in range(2)]
        xr_re, xr_im, g_re, g_im = mk("xrr"), mk("xri"), mk("gr"), mk("gi")
        h_re, h_im = mk("hr", BF16), mk("hi", BF16)
        hp = P.sb("s5hp", [128, 4], F32, st)
        tA = P.sb("s5tA", [128, S5CH], F32, st)
        tB = P.sb("s5tB", [128, S5CH], F32, st)
        it = 0
        for d in range(2):
            for pr in range(8):
                ft, pp = pr // 4, pr % 4
                dp = d * 8 + pr
                cT_, sT_, nsT_ = X.cosT[:, dp, :], X.sinT[:, dp, :], X.nsinT[:, dp, :]
                order = list(range(nch)) if d == 0 else [0] + list(range(nch - 1, 0, -1))
                for oi, ch in enumerate(order):
                    c0 = ch * S5CH
                    k = it % 2
                    it += 1
                    ps_r, ps_i = S.psum[2 * k], S.psum[2 * k + 1]
                    lr = X.BTr[:, d * 2 + ft, 2 * pp:2 * pp + 2, :].rearrange("p a b -> p (a b)")
                    li = X.BTi[:, d * 2 + ft, 2 * pp:2 * pp + 2, :].rearrange("p a b -> p (a b)")
                    P.mm(ps_r[:, 0:S5CH], lr, sT16[:, ft, c0:c0 + S5CH], True, True, R=[X.BTr, sT16], W=[ps_r])
                    P.mm(ps_i[:, 0:S5CH], li, sT16[:, ft, c0:c0 + S5CH], True, True, R=[X.BTi, sT16], W=[ps_i])
                    rv = (lambda ap: ap) if d == 0 else (lambda ap: ap[:, ::-1])
                    xrr, xri, gr, gi, hr, hi = xr_re[k], xr_im[k], g_re[k], g_im[k], h_re[k], h_im[k]
                    _tt(P, tA[:], rv(ps_r[:, 0:S5CH]), cT_, ALU.mult, [ps_r, X.cosT], [tA]) if d == 0 else \
                        _tt(P, rv(tA[:]), ps_r[:, 0:S5CH], rv(cT_), ALU.mult, [ps_r, X.cosT], [tA])
                    if d == 0:
                        _tt(P, tB[:], ps_i[:, 0:S5CH], sT_, ALU.mult, [ps_i, X.sinT], [tB])
                        _tt(P, xrr[:], tA[:], tB[:], ALU.add, [tA, tB], [xrr])
                        _tt(P, tA[:], ps_i[:, 0:S5CH], cT_, ALU.mult, [ps_i, X.cosT], [tA])
                        _tt(P, tB[:], ps_r[:, 0:S5CH], nsT_, ALU.mult, [ps_r, X.nsinT], [tB])
                        _tt(P, xri[:], tA[:], tB[:], ALU.add, [tA, tB], [xri])
                    else:
                        _tt(P, rv(tB[:]), ps_i[:, 0:S5CH], rv(sT_), ALU.mult, [ps_i, X.sinT], [tB])
                        _tt(P, xrr[:], tA[:], tB[:], ALU.add, [tA, tB], [xrr])
                        _tt(P, rv(tA[:]), ps_i[:, 0:S5CH], rv(cT_), ALU.mult, [ps_i, X.cosT], [tA])
                        _tt(P, rv(tB[:]), ps_r[:, 0:S5CH], rv(nsT_), ALU.mult, [ps_r, X.nsinT], [tB])
                        _tt(P, xri[:], tA[:], tB[:], ALU.add, [tA, tB], [xri])
                    if oi == 0:
                        ini_r, ini_i = 0.0, 0.0
                        Rini = []
                    else:
                        c1, s1 = X.cosT[:, dp, 1:2], X.sinT[:, dp, 1:2]
                        ns1 = X.nsinT[:, dp, 1:2]
                        _tt(P, hp[:, 2:3], hp[:, 0:1], c1, ALU.mult, [hp, X.cosT], [hp])
                        P.V("scalar_tensor_tensor", R=[hp, X.nsinT], W=[hp], out=hp[:, 2:3], in0=hp[:, 1:2], scalar=ns1,
                            in1=hp[:, 2:3], op0=ALU.mult, op1=ALU.add)
                        _tt(P, hp[:, 3:4], hp[:, 0:1], s1, ALU.mult, [hp, X.sinT], [hp])
                        P.V("scalar_tensor_tensor", R=[hp, X.cosT], W=[hp], out=hp[:, 3:4], in0=hp[:, 1:2], scalar=c1,
                            in1=hp[:, 3:4], op0=ALU.mult, op1=ALU.add)
                        ini_r, ini_i = hp[:, 2:3], hp[:, 3:4]
                        Rini = [hp]
                    P.V("tensor_tensor_scan", R=[X.rhoT, xrr] + Rini, W=[gr], out=gr[:], data0=X.rhoT[:, dp, :], data1=xrr[:],
                        initial=ini_r, op0=ALU.mult, op1=ALU.add)
                    P.V("tensor_tensor_scan", R=[X.rhoT, xri] + Rini, W=[gi], out=gi[:], data0=X.rhoT[:, dp, :], data1=xri[:],
                        initial=ini_i, op0=ALU.mult, op1=ALU.add)
                    _tt(P, tA[:], gr[:], cT_, ALU.mult, [gr, X.cosT], [tA])
                    _tt(P, tB[:], gi[:], nsT_, ALU.mult, [gi, X.nsinT], [tB])
                    _tt(P, tA[:], tA[:], tB[:], ALU.add, [tA, tB], [tA])
                    P.V("tensor_copy", R=[tA], W=[hr], out=rv(hr[:]), in_=tA[:])
                    P.V("tensor_copy", R=[tA], W=[hp], out=hp[:, 0:1], in_=tA[:, S5CH - 1:S5CH])
                    _tt(P, tA[:], gi[:], cT_, ALU.mult, [gi, X.cosT], [tA])
                    _tt(P, tB[:], gr[:], sT_, ALU.mult, [gr, X.sinT], [tB])
                    _tt(P, tA[:], tA[:], tB[:], ALU.add, [tA, tB], [tA])
                    P.V("tensor_copy", R=[tA], W=[hi], out=rv(hi[:]), in_=tA[:])
                    P.V("tensor_copy", R=[tA], W=[hp], out=hp[:, 1:2], in_=tA[:, S5CH - 1:S5CH])
                    ps_y = S.psum[4 + k]
                    pb = (pp // 2) * 64
                    P.mm(ps_y[pb:pb + 64, 0:S5CH], X.Cr[:, dp, :], hr[:], True, False, R=[X.Cr, hr], W=[ps_y])
                    P.mm(ps_y[pb:pb + 64, 0:S5CH], X.Ci[:, dp, :], hi[:], False, True, R=[X.Ci, hi], W=[ps_y])
                    _tt(P, yacc[pb:pb + 64, ft, c0:c0 + S5CH], yacc[pb:pb + 64, ft, c0:c0 + S5CH],
                        ps_y[pb:pb + 64, 0:S5CH], ALU.add, [yacc, ps_y], [yacc])
        dbg(P, f"yacc{b}", yacc, yacc[:], [128, 2, T])
        ge = P.sb("s5ge", [128, 2, T], BF16, st)
        gt = P.sb("s5gt", [128, 512], F32, st)
        for ft in range(2):
            for (t0, n) in TILES:
                y = yacc[:, ft, t0:t0 + n]
                P.A("activation", R=[yacc], W=[gt], out=gt[:, 0:n], in_=y, func=AF.Square)
                _ts(P, gt[:, 0:n], gt[:, 0:n], 0.044715, ALU.mult, [gt], [gt], 1.0, ALU.add)
                _tt(P, gt[:, 0:n], gt[:, 0:n], y, ALU.mult, [gt, yacc], [gt])
                P.A("activation", R=[gt], W=[gt], out=gt[:, 0:n], in_=gt[:, 0:n], func=AF.Tanh,
                    scale=math.sqrt(2.0 / math.pi))
                _ts(P, gt[:, 0:n], gt[:, 0:n], 1.0, ALU.add, [gt], [gt], 0.5, ALU.mult)
                _tt(P, ge[:, ft, t0:t0 + n], gt[:, 0:n], y, ALU.mult, [gt, yacc], [ge])
        osb = sT
        for m in range(2):
            for (t0, n) in TILES:
                p1, p2 = S.psum[0], S.psum[1]
                for kt in range(2):
                    P.mm(p1[:, 0:n], X.wglu[:, kt, m * 128:(m + 1) * 128], ge[:, kt, t0:t0 + n], kt == 0, kt == 1, R=[X.wglu, ge], W=[p1])
                for kt in range(2):
                    P.mm(p2[:, 0:n], X.wglu[:, kt, 256 + m * 128:256 + (m + 1) * 128], ge[:, kt, t0:t0 + n], kt == 0, kt == 1,
                         R=[X.wglu, ge], W=[p2])
                P.A("activation", R=[p2], W=[gt], out=gt[:, 0:n], in_=p2[:, 0:n], func=AF.Sigmoid)
                _tt(P, osb[:, m, t0:t0 + n], p1[:, 0:n], gt[:, 0:n], ALU.mult, [p1, gt], [osb])
        c0 = 0 if need_ctx else CTX
        for m in range(2):
            P.dma("sp", R=[osb], W=[D.os5.s(b)], out=D.os5[b, m * 128:(m + 1) * 128, c0:T], in_=osb[:, m, c0:T])
    P.barrier()


def merge_setup(P, l, D, S, st):
    X = NS()
    X.pa = P.sb("m_pa", [128, 4, DM], BF16, st)
    X.p5 = P.sb("m_p5", [128, 2, DM], BF16, st)
    X.pr = P.sb("m_pr", [128, 2, DM], BF16, st)
    X.wo = P.sb("m_wo", [128, 8, DM], BF16, st)
    for t, src in ((X.pa, D.proj_att), (X.p5, D.proj_s5), (X.pr, D.proj_rwkv), (X.wo, D.w_out)):
        P.dma("pool", R=[src], W=[t], out=t[:], in_=src[l].rearrange("(kt p) n -> p kt n", p=128))
    X.wr = P.sb("m_wr", [128, 8, 36], F32, st)
    P.dma("sp", R=[D.router_w], W=[X.wr], out=X.wr[:], in_=D.router_w[l].rearrange("(kt p) n -> p kt n", p=128))
    X.rb = P.sb("m_rb", [128, 36], F32, st)
    P.dma("sp", R=[D.router_b], W=[X.rb], out=X.rb[:], in_=D.router_b[:, l, :])
    X.carry = P.sb("m_carry", [128, NEXP], F32, st)
    P.V("memset", W=[X.carry], ap=X.carry[:], constant=0.0)
    X.eoff = P.sb("m_eoff", [128, NEXP], F32, st)
    P.dma("sp", R=[D.consts["eoff"]], W=[X.eoff], out=X.eoff[:], in_=D.consts["eoff"][:, :])
    return X


def phase_merge(P, l, D, S, b, X, need_ctx, xsrc, G):
    tiles = TILES if need_ctx else TILES[1:]
    with contextlib.ExitStack() as st:
        oa = P.sb("mg_oa", [128, 4, 512], BF16, st)
        o5 = P.sb("mg_o5", [128, 2, 512], BF16, st)
        orw = P.sb("mg_or", [128, 2, 512], BF16, st)
        gt = [P.sb(f"mg_g{i}", [128, 3, 512], F32, st) for i in range(2)]
        xt = P.sb("mg_x", [128, 8, 512], F32, st)
        mg = P.sb("mg_m", [128, 8, 512], BF16, st)
        t1 = P.sb("mg_t1", [128, 512], F32, st)
        t2 = P.sb("mg_t2", [128, 512], F32, st)
        sq = P.sb("mg_sq", [128, 8, 512], F32, st)
        rstd = P.sb("mg_rstd", [128, 512], F32, st)
        h2 = P.sb("mg_h2", [128, 8, 512], F32, st)
        htm = P.sb("mg_htm", [128, DM], F32, st)
        rt = {k: P.sb("mg_r" + k, shp, dt, st) for k, shp, dt in (
            ("lg", [128, 36], F32), ("mx", [128, 8], F32), ("ohg", [128, 4], F32), ("el", [128, 8], F32),
            ("ee", [128, 8], F32), ("t8", [128, 8], F32), ("oh1", [128, 8], F32), ("oh2", [128, 8], F32),
            ("M1", [128, 4, 8], F32), ("M2", [128, 4, 8], F32), ("M", [128, NEXP], F32), ("pos", [128, NEXP], F32),
            ("s1", [128, 4], F32), ("gs", [128, 4], F32), ("si", [128, 2], I32))}
        for (t0, n) in tiles:
            col = 4 if t0 < CTX else b
            P.dma("pool", R=[D.oatt.s(b)], W=[oa], out=oa[:, :, 0:n], in_=D.oatt[b, :, t0:t0 + n].rearrange("(k p) t -> p k t", p=128))
            P.dma("pool", R=[D.os5.s(b)], W=[o5], out=o5[:, :, 0:n], in_=D.os5[b, :, t0:t0 + n].rearrange("(k p) t -> p k t", p=128))
            P.dma("pool", R=[D.orw.s(b)], W=[orw], out=orw[:, :, 0:n], in_=D.orw[b, :, t0:t0 + n].rearrange("(k p) t -> p k t", p=128))
            P.dma("sp", R=[xsrc], W=[xt], out=xt[:, :, 0:n], in_=xsrc[b, :, t0:t0 + n].rearrange("(kt p) t -> p kt t", p=128))
            for m in range(8):
                g = gt[m % 2]
                for j in range(3):
                    r0 = OFF_GATE + j * DM + m * 128
                    P.dma("sp", R=[D.uT.s(b)], W=[g], out=g[:, j, 0:n], in_=D.uT[b, r0:r0 + 128, t0:t0 + n])
                pa_, p5_, pr_ = S.psum[1], S.psum[2], S.psum[3]
                for k in range(4):
                    P.mm(pa_[:, 0:n], X.pa[:, k, m * 128:(m + 1) * 128], oa[:, k, 0:n], k == 0, k == 3, R=[X.pa, oa], W=[pa_])
                for k in range(2):
                    P.mm(p5_[:, 0:n], X.p5[:, k, m * 128:(m + 1) * 128], o5[:, k, 0:n], k == 0, k == 1, R=[X.p5, o5], W=[p5_])
                for k in range(2):
                    P.mm(pr_[:, 0:n], X.pr[:, k, m * 128:(m + 1) * 128], orw[:, k, 0:n], k == 0, k == 1, R=[X.pr, orw], W=[pr_])
                _tt(P, t1[:, 0:n], pa_[:, 0:n], g[:, 0, 0:n], ALU.mult, [pa_, g], [t1])
                _tt(P, t2[:, 0:n], p5_[:, 0:n], g[:, 1, 0:n], ALU.mult, [p5_, g], [t2])
                _tt(P, t1[:, 0:n], t1[:, 0:n], t2[:, 0:n], ALU.add, [t1, t2], [t1])
                _tt(P, t2[:, 0:n], pr_[:, 0:n], g[:, 2, 0:n], ALU.mult, [pr_, g], [t2])
                _tt(P, mg[:, m, 0:n], t1[:, 0:n], t2[:, 0:n], ALU.add, [t1, t2], [mg])
            for m in range(8):
                po = S.psum[4 + m % 2]
                for k in range(8):
                    P.mm(po[:, 0:n], X.wo[:, k, m * 128:(m + 1) * 128], mg[:, k, 0:n], k == 0, k == 7, R=[X.wo, mg], W=[po])
                P.V("scalar_tensor_tensor", R=[po, xt, S.modT], W=[xt], out=xt[:, m, 0:n], in0=po[:, 0:n],
                    scalar=S.modT[:, 2 * 8 + m, col:col + 1], in1=xt[:, m, 0:n], op0=ALU.mult, op1=ALU.add)
            P.dma("sp", R=[xt], W=[D.x1T.s(b)], out=D.x1T[b, :, t0:t0 + n].rearrange("(kt p) t -> p kt t", p=128), in_=xt[:, :, 0:n])
            rms_mod(P, S, xt, n, col, 4, 3, lambda kt: (h2[:, kt, 0:n], [h2]), sq, rstd, S.psum[0])
            for sti in range(n // 128):
                tok = slice(sti * 128, (sti + 1) * 128)
                gi = G.next
                G.next += 1
                G.tiles.append((b, t0 + sti * 128))
                pl = S.psum[6]
                for kt in range(8):
                    P.mm(pl[:, 0:36], h2[:, kt, tok], X.wr[:, kt, :], kt == 0, kt == 7, R=[h2, X.wr], W=[pl])
                lg = rt["lg"]
                _tt(P, lg[:], pl[:, 0:36], X.rb[:], ALU.add, [pl, X.rb], [lg])
                mx, ohg, el, ee, t8, oh1, oh2 = rt["mx"], rt["ohg"], rt["el"], rt["ee"], rt["t8"], rt["oh1"], rt["oh2"]
                s1, gs = rt["s1"], rt["gs"]
                P.V("tensor_reduce", R=[lg], W=[s1], out=s1[:, 0:1], in_=lg[:, 0:4], axis=AX.X, op=ALU.max)
                _ts(P, ohg[:], lg[:, 0:4], s1[:, 0:1], ALU.is_equal, [lg, s1], [ohg])
                _ts(P, gs[:], lg[:, 0:4], s1[:, 0:1], ALU.subtract, [lg, s1], [gs])
                P.A("activation", R=[gs], W=[gs], out=gs[:], in_=gs[:], func=AF.Exp)
                P.V("tensor_reduce", R=[gs], W=[s1], out=s1[:, 1:2], in_=gs[:], axis=AX.X, op=ALU.add)
                _ts(P, el[:], lg[:, 4:12], ohg[:, 0:1], ALU.mult, [lg, ohg], [el])
                for j in range(1, 4):
                    P.V("scalar_tensor_tensor", R=[lg, ohg, el], W=[el], out=el[:], in0=lg[:, 4 + 8 * j:12 + 8 * j],
                        scalar=ohg[:, j:j + 1], in1=el[:], op0=ALU.mult, op1=ALU.add)
                P.V("max", R=[el], W=[mx], out=mx[:], in_=el[:])
                _ts(P, oh1[:], el[:], mx[:, 0:1], ALU.is_equal, [el, mx], [oh1])
                _ts(P, oh2[:], el[:], mx[:, 1:2], ALU.is_equal, [el, mx], [oh2])
                _tt(P, s1[:, 2:3], mx[:, 1:2], mx[:, 0:1], ALU.subtract, [mx], [s1])
                P.A("activation", R=[s1], W=[s1], out=s1[:, 2:3], in_=s1[:, 2:3], func=AF.Exp)
                _ts(P, s1[:, 3:4], s1[:, 2:3], 1.0, ALU.add, [s1], [s1])
                _tt(P, s1[:, 3:4], s1[:, 3:4], s1[:, 1:2], ALU.mult, [s1], [s1])
                P.V("reciprocal", R=[s1], W=[s1], out=s1[:, 3:4], in_=s1[:, 3:4])
                P.V("tensor_copy", R=[s1], W=[G.gate], out=G.gate[:, gi, 0:1], in_=s1[:, 3:4])
                _tt(P, G.gate[:, gi, 1:2], s1[:, 3:4], s1[:, 2:3], ALU.mult, [s1], [G.gate])
                for Mk, oh in ((rt["M1"], oh1), (rt["M2"], oh2)):
                    P.V("tensor_tensor", R=[ohg, oh], W=[Mk], out=Mk[:], in0=ohg[:].unsqueeze(2).to_broadcast([128, 4, 8]),
                        in1=oh[:].unsqueeze(1).to_broadcast([128, 4, 8]), op=ALU.mult)
                M = rt["M"]
                _tt(P, M[:], rt["M1"][:].rearrange("p a b -> p (a b)"), rt["M2"][:].rearrange("p a b -> p (a b)"), ALU.add,
                    [rt["M1"], rt["M2"]], [M])
                pp = S.psum[7]
                P.mm(pp[:, 0:NEXP], S.tri[:], M[:], True, True, R=[S.tri, M], W=[pp])
                pos = rt["pos"]
                _tt(P, pos[:], pp[:, 0:NEXP], X.carry[:], ALU.add, [pp, X.carry], [pos])
                _tt(P, pos[:], pos[:], X.eoff[:], ALU.add, [pos, X.eoff], [pos])
                P.mm(pp[:, 0:NEXP], S.ones[:], M[:], True, True, R=[S.ones, M], W=[pp])
                _tt(P, X.carry[:], X.carry[:], pp[:, 0:NEXP], ALU.add, [pp, X.carry], [X.carry])
                for k, Mk in enumerate((rt["M1"], rt["M2"])):
                    _tt(P, M[:], Mk[:].rearrange("p a b -> p (a b)"), pos[:], ALU.mult, [Mk, pos], [M])
                    P.V("tensor_reduce", R=[M], W=[s1], out=s1[:, 0:1], in_=M[:], axis=AX.X, op=ALU.add)
                    P.V("tensor_copy", R=[s1], W=[G.slot], out=G.slot[:, gi, k:k + 1], in_=s1[:, 0:1])
                for half in range(2):
                    ph = S.psum[2 + half]
                    for q in range(4):
                        kt = half * 4 + q
                        P.PE("transpose", R=[h2, S.ident], W=[ph], out=ph[:, q * 128:(q + 1) * 128], in_=h2[:, kt, tok], identity=S.ident[:])
                    P.A("activation", R=[ph], W=[htm], out=htm[:, half * 512:(half + 1) * 512], in_=ph[:], func=AF.Identity)
                for k in range(2):
                    P.dma("pool", R=[htm, G.slot], W=[D.Xe], meth="indirect_dma_start", out=D.Xe[:, :],
                          out_offset=bass.IndirectOffsetOnAxis(ap=G.slot[:, gi, k:k + 1], axis=0), in_=htm[:], in_offset=None)
    P.barrier()


def phase_experts(P, l, D, S):
    with contextlib.ExitStack() as st:
        wg = [P.sb(f"e_wg{i}", [128, 8, DEXP], BF16, st) for i in range(2)]
        wu = [P.sb(f"e_wu{i}", [128, 8, DEXP], BF16, st) for i in range(2)]
        wd = [P.sb(f"e_wd{i}", [128, 4, DM], BF16, st) for i in range(2)]
        xtm2 = [P.sb(f"e_xtm{i}", [128, 4, DM], F32, st) for i in range(2)]
        xbT = P.sb("e_xbT", [128, 8, 512], BF16, st)
        sg = P.sb("e_sg", [128, 512], F32, st)
        act = P.sb("e_act", [128, 4, 512], BF16, st)
        yb2 = [P.sb(f"e_yb{i}", [128, 4, DM], F32, st) for i in range(2)]
        ci = 0
        bi = 0
        for e in range(NEXP):
            k = e % 2
            P.dma("pool", R=[D.exp_w_gate], W=[wg[k]], out=wg[k][:], in_=D.exp_w_gate[l, e].rearrange("(kt p) n -> p kt n", p=128))
            P.dma("pool", R=[D.exp_w_up], W=[wu[k]], out=wu[k][:], in_=D.exp_w_up[l, e].rearrange("(kt p) n -> p kt n", p=128))
            P.dma("pool", R=[D.exp_w_down], W=[wd[k]], out=wd[k][:], in_=D.exp_w_down[l, e].rearrange("(kt p) n -> p kt n", p=128))
            for blk in range(CAP // 512):
                r0 = e * CAP + blk * 512
                xtm, yb = xtm2[bi % 2], yb2[bi % 2]
                bi += 1
                P.dma("sp", R=[D.Xe], W=[xtm], out=xtm[:], in_=D.Xe[r0:r0 + 512, :].rearrange("(s p) f -> p s f", p=128))
                for kt in range(8):
                    ph = S.psum[ci % 2]
                    ci += 1
                    for s_ in range(4):
                        P.PE("transpose", R=[xtm, S.ident], W=[ph], out=ph[:, s_ * 128:(s_ + 1) * 128],
                             in_=xtm[:, s_, kt * 128:(kt + 1) * 128], identity=S.ident[:])
                    if kt % 2 == 0:
                        P.A("activation", R=[ph], W=[xbT], out=xbT[:, kt, :], in_=ph[:], func=AF.Identity)
                    else:
                        P.V("tensor_copy", R=[ph], W=[xbT], out=xbT[:, kt, :], in_=ph[:])
                for hm in range(4):
                    pg, pu = S.psum[2 + 2 * (hm % 2)], S.psum[3 + 2 * (hm % 2)]
                    for kt in range(8):
                        P.mm(pg[:], wg[k][:, kt, hm * 128:(hm + 1) * 128], xbT[:, kt, :], kt == 0, kt == 7, R=[wg[k], xbT], W=[pg])
                    for kt in range(8):
                        P.mm(pu[:], wu[k][:, kt, hm * 128:(hm + 1) * 128], xbT[:, kt, :], kt == 0, kt == 7, R=[wu[k], xbT], W=[pu])
                    P.A("activation", R=[pg], W=[sg], out=sg[:], in_=pg[:], func=AF.Silu)
                    _tt(P, act[:, hm, :], pu[:], sg[:], ALU.mult, [pu, sg], [act])
                for s_ in range(4):
                    for half in range(2):
                        pd = S.psum[6 + half]
                        for hm in range(4):
                            P.mm(pd[:], act[:, hm, s_ * 128:(s_ + 1) * 128], wd[k][:, hm, half * 512:(half + 1) * 512], hm == 0, hm == 3,
                                 R=[act, wd[k]], W=[pd])
                        if half == 0:
                            P.A("activation", R=[pd], W=[yb], out=yb[:, s_, 0:512], in_=pd[:], func=AF.Identity)
                        else:
                            P.V("tensor_copy", R=[pd], W=[yb], out=yb[:, s_, 512:1024], in_=pd[:])
                P.dma("sp", R=[yb], W=[D.Ye], out=D.Ye[r0:r0 + 512, :].rearrange("(s p) f -> p s f", p=128), in_=yb[:])
    P.barrier()


def phase_combine(P, l, D, S, G, dst, last):
    with contextlib.ExitStack() as st:
        y1 = [P.sb(f"c_y1{i}", [128, DM], F32, st) for i in range(2)]
        y2 = [P.sb(f"c_y2{i}", [128, DM], F32, st) for i in range(2)]
        xt = [P.sb(f"c_x{i}", [128, 8, 128], F32, st) for i in range(2)]
        for gi, (b, t0) in enumerate(G.tiles):
            k = gi % 2
            col = 4 if t0 < CTX else b
            for yy, kk in ((y1[k], 0), (y2[k], 1)):
                P.dma("pool", R=[D.Ye, G.slot], W=[yy], meth="indirect_dma_start", out=yy[:], out_offset=None, in_=D.Ye[:, :],
                      in_offset=bass.IndirectOffsetOnAxis(ap=G.slot[:, gi, kk:kk + 1], axis=0))
            P.dma("sp", R=[D.x1T.s(b)], W=[xt[k]], out=xt[k][:], in_=D.x1T[b, :, t0:t0 + 128].rearrange("(kt p) t -> p kt t", p=128))
            _ts(P, y1[k][:], y1[k][:], G.gate[:, gi, 0:1], ALU.mult, [y1[k], G.gate], [y1[k]])
            P.V("scalar_tensor_tensor", R=[y1[k], y2[k], G.gate], W=[y1[k]], out=y1[k][:], in0=y2[k][:], scalar=G.gate[:, gi, 1:2],
                in1=y1[k][:], op0=ALU.mult, op1=ALU.add)
            for half in range(2):
                ph = S.psum[2 * k + half]
                for q in range(4):
                    m = half * 4 + q
                    P.PE("transpose", R=[y1[k], S.ident], W=[ph], out=ph[:, q * 128:(q + 1) * 128], in_=y1[k][:, m * 128:(m + 1) * 128],
                         identity=S.ident[:])
                for q in range(4):
                    m = half * 4 + q
                    P.V("scalar_tensor_tensor", R=[ph, xt[k], S.modT], W=[xt[k]], out=xt[k][:, m, :], in0=ph[:, q * 128:(q + 1) * 128],
                        scalar=S.modT[:, 5 * 8 + m, col:col + 1], in1=xt[k][:, m, :], op0=ALU.mult, op1=ALU.add)
            if last:
                P.dma("sp", R=[xt[k]], W=[D.out], out=D.out[b, :, t0 - CTX:t0 - CTX + 128].rearrange("(kt p) t -> p kt t", p=128), in_=xt[k][:])
            else:
                P.dma("sp", R=[xt[k]], W=[dst.s(b)], out=dst[b, :, t0:t0 + 128].rearrange("(kt p) t -> p kt t", p=128), in_=xt[k][:])
    P.barrier()


RC = 64
NCH = T // RC
GN_EPS = 64e-5
W_SCALE = -math.exp(-0.5)


def rwkv_setup(P, l, D, S, st):
    X = NS()
    ld = lambda nm, src, shp: _load(P, st, nm, src, shp)
    X.cw = ld("rw_cw", D.rw_conv[:, l, :], [128, 24])
    X.w0 = ld("rw_w0", D.rw_w0[:, l, :], [128, 4])
    X.a0 = ld("rw_a0", D.rw_a0[:, l, :], [128, 4])
    X.pv = ld("rw_pv", D.rw_pv[:, l, :], [128, 10])
    X.w2a2 = [ld(f"rw_w2a2{d}", D.rw_w2a2[l, d], [128, 256]) for d in range(2)]
    X.g2 = ld("rw_g2", D.rw_g2[l], [128, 256])
    X.msk = {k: ld("rw_" + k, D.consts[k][:, :], list(D.consts[k].t.shape)) for k in ("MA0", "MA1", "MB0", "MB1", "MC0", "MC1", "YXI")}
    X.rmask = ld("rw_rmask", D.consts["rmask"][:, :], [128, T])
    X.gneps = P.sb("rw_gneps", [128, 1], F32, st)
    P.V("memset", W=[X.gneps], ap=X.gneps[:], constant=GN_EPS)
    X.kkeps = P.sb("rw_kkeps", [128, 1], F32, st)
    P.V("memset", W=[X.kkeps], ap=X.kkeps[:], constant=1e-12)
    return X


def phase_rwkv(P, l, D, S, b, X, need_ctx):
    zbase = OFF_RW
    SEG = ((0, CTX), (CTX, T))
    for ih in range(2):
        with contextlib.ExitStack() as st:
            A = lambda nm, shp=(128, T): P.sb("rw_" + nm, list(shp), F32, st)
            zt = A("zt")
            r, k, v, kk, kts, g, ysum = A("r"), A("k"), A("v"), A("kk"), A("kts"), A("g"), A("ysum")
            z6 = A("z6")
            t1, t2, t3 = A("t1"), A("t2"), A("t3")
            t2x = g
            KRr = P.sb("rw_KR", [128, NCH, 2, RC], F32R, st)
            BKr = P.sb("rw_BK", [128, NCH, 2, RC], F32R, st)
            KR = _View(KRr, F32)
            BK = _View(BKr, F32)
            wtot = A("wtot", (128, NCH))

            def conv(dst, tile):
                P.dma("sp", R=[D.uT.s(b)], W=[zt], out=zt[:], in_=D.uT[b, zbase + tile * 128:zbase + (tile + 1) * 128, :])
                _ts(P, dst[:], zt[:], X.cw[:, 8 + tile:9 + tile], ALU.mult, [zt, X.cw], [dst])
                for (s0, s1) in SEG:
                    P.V("scalar_tensor_tensor", R=[zt, X.cw, dst], W=[dst], out=dst[:, s0 + 1:s1], in0=zt[:, s0:s1 - 1],
                        scalar=X.cw[:, tile:tile + 1], in1=dst[:, s0 + 1:s1], op0=ALU.mult, op1=ALU.add)
                    P.V("scalar_tensor_tensor", R=[zt, X.cw, dst], W=[dst], out=dst[:, s0:s1 - 1], in0=zt[:, s0 + 1:s1],
                        scalar=X.cw[:, 16 + tile:17 + tile], in1=dst[:, s0:s1 - 1], op0=ALU.mult, op1=ALU.add)

            conv(r, ih)
            conv(k, 2 + ih)
            conv(v, 4 + ih)
            conv(z6, 6)
            P.A("activation", R=[z6], W=[z6], out=z6[0:64, :], in_=z6[0:64, :], func=AF.Tanh)
            P.V("memset", W=[ysum], ap=ysum[:], constant=0.0)
            P.V("memset", W=[kts], ap=kts[:], constant=0.0)
            _ts(P, kk[:], k[:], X.pv[:, 0 + ih:1 + ih], ALU.mult, [k, X.pv], [kk])
            for (t0, n) in TILES:
                ps = S.psum[0]
                P.A("activation", R=[kk], W=[t1], out=t1[:, t0:t0 + n], in_=kk[:, t0:t0 + n], func=AF.Square)
                P.mm(ps[:, 0:n], S.bs64[:], t1[:, t0:t0 + n], True, True, R=[S.bs64, t1], W=[ps])
                P.A("activation", R=[ps, X.kkeps], W=[t1], out=t1[:, t0:t0 + n], in_=ps[:, 0:n], func=AF.Sqrt, bias=X.kkeps[:, 0:1], scale=1.0)
                P.V("reciprocal", R=[t1], W=[t1], out=t1[:, t0:t0 + n], in_=t1[:, t0:t0 + n])
                _tt(P, kk[:, t0:t0 + n], kk[:, t0:t0 + n], t1[:, t0:t0 + n], ALU.mult, [kk, t1], [kk])
            for d in range(2):
                MA, MB, MC = X.msk[f"MA{d}"], X.msk[f"MB{d}"], X.msk[f"MC{d}"]
                for (t0, n) in TILES:
                    pw, pa = S.psum[0], S.psum[1]
                    P.mm(pw[:, 0:n], X.w2a2[d][0:64, ih * 128:(ih + 1) * 128], z6[0:64, t0:t0 + n], True, True, R=[X.w2a2[d], z6], W=[pw])
                    P.mm(pa[:, 0:n], X.w2a2[d][64:128, ih * 128:(ih + 1) * 128], z6[64:128, t0:t0 + n], True, True, R=[X.w2a2[d], z6], W=[pa])
                    P.A("activation", R=[pw, X.w0], W=[t1], out=t1[:, t0:t0 + n], in_=pw[:, 0:n], func=AF.Sigmoid,
                        bias=X.w0[:, 2 * d + ih:2 * d + ih + 1], scale=1.0)
                    P.A("activation", R=[pa, X.a0], W=[t2], out=t2[:, t0:t0 + n], in_=pa[:, 0:n], func=AF.Sigmoid,
                        bias=X.a0[:, 2 * d + ih:2 * d + ih + 1], scale=1.0)
                _ts(P, t1[:], t1[:], W_SCALE, ALU.mult, [t1], [t1])
                _ts(P, t3[:], t2[:], -1.0, ALU.add, [t2], [t3], X.pv[:, 2 + ih:3 + ih], ALU.mult)
                P.V("scalar_tensor_tensor", R=[t3, k], W=[t3], out=t3[:], in0=t3[:], scalar=1.0, in1=k[:], op0=ALU.add, op1=ALU.mult)
                _tt(P, kts[:], kts[:], t3[:], ALU.add, [kts, t3], [kts])
                _tt(P, t2[:], t2[:], kk[:], ALU.mult, [t2, kk], [t2])
                P.V("tensor_tensor_scan", R=[X.rmask, t1], W=[zt], out=zt[:], data0=X.rmask[:], data1=t1[:], initial=0.0,
                    op0=ALU.mult, op1=ALU.add)
                zt3 = zt[:].rearrange("p (c j) -> p c j", j=RC)
                if d == 1:
                    P.V("tensor_tensor", R=[zt], W=[t2x], out=t2x[:].rearrange("p (c j) -> p c j", j=RC),
                        in0=zt3[:, :, RC - 1:RC].to_broadcast([128, NCH, RC]), in1=zt3, op=ALU.subtract)
                    _tt(P, zt[:], t2x[:], t1[:], ALU.add, [t2x, t1], [zt])
                last = RC - 1 if d == 0 else 0
                P.A("activation", R=[zt], W=[wtot], out=wtot[:], in_=zt3[:, :, last], func=AF.Exp)
                c3 = lambda tt_: tt_[:].rearrange("p (c j) -> p c j", j=RC)
                P.A("activation", R=[zt], W=[t2x], out=t2x[:], in_=zt[:], func=AF.Exp)
                _tt(P, KRr[:, :, 1, :], c3(t2x), c3(r), ALU.mult, [t2x, r], [KRr])
                P.A("activation", R=[zt], W=[t2x], out=t2x[:], in_=zt[:], func=AF.Exp, scale=-1.0)
                _tt(P, BKr[:, :, 1, :], c3(t2x), c3(t3), ALU.mult, [t2x, t3], [BKr])
                _tt(P, BKr[:, :, 0, :], c3(t2x), c3(t2), ALU.mult, [t2x, t2], [BKr])
                _tt(P, zt[:], zt[:], t1[:], ALU.subtract, [zt, t1], [zt])
                P.A("activation", R=[zt], W=[t2x], out=t2x[:], in_=zt[:], func=AF.Exp)
                _tt(P, KRr[:, :, 0, :], c3(t2x), c3(kk), ALU.mult, [t2x, kk], [KRr])
                rwkv_scan(P, S, X, d, st, KRr, BKr, wtot, v, ysum, MA, MB, MC)
            z7 = t3
            conv(z7, 7)
            P.A("activation", R=[z7], W=[z7], out=z7[:], in_=z7[:], func=AF.Sigmoid)
            for (t0, n) in TILES:
                ps2 = S.psum[1]
                P.mm(ps2[:, 0:n], X.g2[:, ih * 128:(ih + 1) * 128], z7[:, t0:t0 + n], True, True, R=[X.g2, z7], W=[ps2])
                P.A("activation", R=[ps2], W=[g], out=g[:, t0:t0 + n], in_=ps2[:, 0:n], func=AF.Identity)
            for (t0, n) in TILES:
                if t0 < CTX and not need_ctx:
                    continue
                sl = slice(t0, t0 + n)
                pm, pv_, pb = S.psum[0], S.psum[1], S.psum[2]
                P.mm(pm[:, 0:n], S.bd64[:], ysum[:, sl], True, True, R=[S.bd64, ysum], W=[pm])
                _tt(P, t1[:, sl], ysum[:, sl], pm[:, 0:n], ALU.subtract, [ysum, pm], [t1])
                P.A("activation", R=[t1], W=[t2], out=t2[:, sl], in_=t1[:, sl], func=AF.Square)
                P.mm(pv_[:, 0:n], S.bd64[:], t2[:, sl], True, True, R=[S.bd64, t2], W=[pv_])
                P.A("activation", R=[pv_, X.gneps], W=[t2], out=t2[:, sl], in_=pv_[:, 0:n], func=AF.Sqrt, bias=X.gneps[:, 0:1], scale=1.0)
                P.V("reciprocal", R=[t2], W=[t2], out=t2[:, sl], in_=t2[:, sl])
                _tt(P, t1[:, sl], t1[:, sl], t2[:, sl], ALU.mult, [t1, t2], [t1])
                _ts(P, t1[:, sl], t1[:, sl], X.pv[:, 6 + ih:7 + ih], ALU.mult, [t1, X.pv], [t1], X.pv[:, 8 + ih:9 + ih], ALU.add)
                P.V("scalar_tensor_tensor", R=[r, kts, X.pv], W=[t2], out=t2[:, sl], in0=r[:, sl], scalar=X.pv[:, 4 + ih:5 + ih],
                    in1=kts[:, sl], op0=ALU.mult, op1=ALU.mult)
                P.mm(pb[:, 0:n], S.bs64[:], t2[:, sl], True, True, R=[S.bs64, t2], W=[pb])
                _tt(P, t2[:, sl], pb[:, 0:n], v[:, sl], ALU.mult, [pb, v], [t2])
                _tt(P, t1[:, sl], t1[:, sl], t2[:, sl], ALU.add, [t1, t2], [t1])
                _tt(P, t1[:, sl], t1[:, sl], g[:, sl], ALU.mult, [t1, g], [t1])
            c0 = 0 if need_ctx else CTX
            P.dma("sp", R=[t1], W=[D.orw.s(b)], out=D.orw[b, ih * 128:(ih + 1) * 128, c0:T], in_=t1[:, c0:T])
        P.barrier()


def rwkv_scan(P, S, X, d, st0, KR, BK, wtot, v, ysum, MA, MB, MC):
    G = 2
    with contextlib.ExitStack() as st:
        B2 = lambda nm, shp: [P.sb(f"rs_{nm}{i}", list(shp), F32R, st) for i in range(2)]
        ST = P.sb("rs_ST", [128, 128], F32R, st)
        P.V("tensor_scalar", R=[S.ident], W=[ST], out=ST[:], in0=S.ident[:], scalar1=0.0, scalar2=None, op0=ALU.mult)
        identR = P.sb("rs_identR", [128, 128], F32R, st)
        P.V("tensor_copy", R=[S.ident], W=[identR], out=identR[:], in_=S.ident[:])
        bf = lambda ap: ap.bitcast(F32)
        YA, AB = B2("YA", (64, G, 2, 128)), B2("AB", (64, G, 2, 128))
        YX = B2("YX", (64, G, 2, 2, RC))
        YT = B2("YT", (64, G, 2, RC))
        X6 = B2("X6", (64, G, 2, RC))
        BKt, Vt = B2("BKt", (64, G, 2, 128)), B2("Vt", (64, G, 128))
        RHS, Ps = B2("RHS", (64, 128)), B2("Ps", (64, 128))
        if d == 0:
            order = list(range(NCH))
        else:
            nc_ctx = CTX // RC
            order = list(range(nc_ctx - 1, -1, -1)) + list(range(NCH - 1, nc_ctx - 1, -1))
        pairs = [order[i:i + G] for i in range(0, NCH, G)]

        def pre_stages(pi):
            cs = pairs[pi]
            q = pi % 2
            ya, ab, bkt, vt, x6 = YA[q], AB[q], BKt[q], Vt[q], X6[q]
            stages = []

            def s_init():
                pA, pB, pC = S.psum[0], S.psum[1], S.psum[2]
                for gi, c in enumerate(cs):
                    for hp in range(2):
                        lo = hp * 64
                        o = (gi * 2 + hp)
                        krc = KR[lo:lo + 64, c, :, :].rearrange("p a b -> p (a b)")
                        P.mm(pA[0:64, o * 128:(o + 1) * 128], BK[lo:lo + 64, c, 0, :], krc, True, True, R=[BK, KR], W=[pA])
                        P.mm(pB[0:64, o * 128:(o + 1) * 128], BK[lo:lo + 64, c, 1, :], krc, True, True, R=[BK, KR], W=[pB])
                        P.mm(pC[0:64, o * 64:(o + 1) * 64], KR[lo:lo + 64, c, 0, :], BK[lo:lo + 64, c, 0, :], True, True, R=[BK, KR], W=[pC])
                _tt(P, ya[:].rearrange("p g a b -> p (g a b)"), pA[0:64, 0:512], MA[:], ALU.mult, [pA, MA], [ya])
                _tt(P, ab[:].rearrange("p g a b -> p (g a b)"), pB[0:64, 0:512], MB[:], ALU.mult, [pB, MB], [ab])
                yx, yt = YX[0], YT[0]
                _tt(P, yt[:].rearrange("p g a b -> p (g a b)"), pC[0:64, 0:256], MC[:], ALU.mult, [pC, MC], [yt])
                P.A("activation", R=[X.msk["YXI"]], W=[yx], out=yx[:].rearrange("p g a b c -> p (g a b c)"), in_=X.msk["YXI"][:],
                    func=AF.Identity)
                P.V("tensor_copy", R=[ya], W=[yx], out=yx[:, :, :, 0, :], in_=bf(ya[:, :, :, 0:RC]))
            stages.append(s_init)

            def mk_step(kstep):
                def s_step():
                    yx, yt = YX[kstep % 2], YT[kstep % 2]
                    yxn, ytn = YX[(kstep + 1) % 2], YT[(kstep + 1) % 2]
                    pa_, pc_ = S.psum[3], S.psum[4]
                    for gi in range(G):
                        for hp in range(2):
                            o = gi * 2 + hp
                            P.mm(pa_[0:64, o * 128:(o + 1) * 128], yt[:, gi, hp, :], yx[:, gi, hp, :, :].rearrange("p a b -> p (a b)"),
                                 True, True, R=[yt, yx], W=[pa_])
                            if kstep < 5:
                                P.mm(pc_[0:64, o * 64:(o + 1) * 64], yx[:, gi, hp, 0, :], yt[:, gi, hp, :], True, True, R=[yt, yx], W=[pc_])
                    pa4 = pa_[0:64, 0:512].rearrange("p (g a b c) -> p g a b c", g=G, a=2, b=2)
                    if kstep < 5:
                        P.A("activation", R=[pa_], W=[yxn], out=yxn[:, :, :, 0, :], in_=pa4[:, :, :, 0, :], func=AF.Identity)
                        _tt(P, yxn[:, :, :, 1, :], bf(yx[:, :, :, 1, :]), pa4[:, :, :, 1, :], ALU.add, [yx, pa_], [yxn])
                        P.A("activation", R=[pc_], W=[ytn], out=ytn[:].rearrange("p g a b -> p (g a b)"), in_=pc_[0:64, 0:256],
                            func=AF.Identity)
                    else:
                        _tt(P, x6[:], bf(yx[:, :, :, 1, :]), pa4[:, :, :, 1, :], ALU.add, [yx, pa_], [x6])
                return s_step
            for kstep in range(6):
                stages.append(mk_step(kstep))

            def s_tr():
                pT, pV = S.psum[5], S.psum[2]
                for gi, c in enumerate(cs):
                    P.PE("transpose", R=[BK, S.ident], W=[pT], out=pT[0:64, gi * 256:gi * 256 + 128], in_=BK[:, c, 0, :].bitcast(F32), identity=S.ident[:])
                    P.PE("transpose", R=[BK, S.ident], W=[pT], out=pT[0:64, gi * 256 + 128:gi * 256 + 256], in_=BK[:, c, 1, :].bitcast(F32),
                         identity=S.ident[:])
                    P.PE("transpose", R=[v, S.ident], W=[pV], out=pV[0:64, 256 + gi * 128:256 + (gi + 1) * 128], in_=v[:, c * RC:(c + 1) * RC],
                         identity=S.ident[:])
                pT4 = pT[0:64, 0:512].rearrange("p (g a b) -> p g a b", g=G, a=2)
                _ts(P, bkt[:, :, 0, :], pT4[:, :, 0, :], -1.0, ALU.mult, [pT], [bkt])
                P.A("activation", R=[pT], W=[bkt], out=bkt[:, :, 1, :], in_=pT4[:, :, 1, :], func=AF.Identity)
                P.A("activation", R=[pV], W=[vt], out=vt[:].rearrange("p g b -> p (g b)"), in_=pV[0:64, 256:512], func=AF.Identity)
            stages.append(s_tr)
            return stages

        def seq_stages(pi):
            cs = pairs[pi]
            q = pi % 2
            ya, ab, bkt, vt, x6 = YA[q], AB[q], BKt[q], Vt[q], X6[q]
            stages = []
            for gi, c in enumerate(cs):
                rhs, ps_ = RHS[gi], Ps[gi]
                pR, pP, pY, pS = S.psum[6], S.psum[7], S.psum[6], S.psum[7]

                def s1(gi=gi, c=c, rhs=rhs):
                    for hp in range(2):
                        lo = hp * 64
                        P.mm(pR[0:64, lo:lo + 64], KR[lo:lo + 64, c, 0, :], ST[lo:lo + 64, lo:lo + 64], True, False, R=[KR, ST], W=[pR])
                        P.mm(pR[0:64, lo:lo + 64], ab[:, gi, hp, 0:RC], vt[:, gi, lo:lo + 64], False, True, R=[ab, vt], W=[pR])
                    P.V("tensor_copy", R=[pR], W=[rhs], out=rhs[:], in_=pR[0:64, 0:128])

                def s2(gi=gi, c=c, rhs=rhs, ps_=ps_):
                    for hp in range(2):
                        lo = hp * 64
                        P.mm(pP[0:64, lo:lo + 64], x6[:, gi, hp, :], rhs[:, lo:lo + 64], True, True, R=[x6, rhs], W=[pP])
                    P.V("tensor_copy", R=[pP], W=[ps_], out=ps_[:], in_=pP[0:64, 0:128])

                def s3(gi=gi, c=c, ps_=ps_):
                    P.mm(pS[:, 256:384], bkt[:, gi, 0, :], ps_[:], True, False, R=[bkt, ps_], W=[pS])
                    P.mm(pS[:, 256:384], bkt[:, gi, 1, :], vt[:, gi, :], False, False, R=[bkt, vt], W=[pS])
                    P.mm(pS[:, 256:384], identR[:], ST[:], False, True, R=[identR, ST], W=[pS])
                    for hp2 in range(2):
                        P.mm(pY[:, 256 + hp2 * 64:256 + (hp2 + 1) * 64], ST[:], KR[:, c, 1, :], hp2 == 0, False, R=[ST, KR], W=[pY])
                    P.mm(pY[:, 256:384], ps_[:], ya[:, gi, :, RC:2 * RC], False, False, R=[ps_, ya], W=[pY])
                    P.mm(pY[:, 256:384], vt[:, gi, :], ab[:, gi, :, RC:2 * RC], False, True, R=[vt, ab], W=[pY])
                    P.V("scalar_tensor_tensor", R=[pS, wtot, S.bs64], W=[ST], out=ST[:], in0=pS[:, 256:384], scalar=wtot[:, c:c + 1],
                        in1=S.bs64[:], op0=ALU.mult, op1=ALU.mult)
                    for hp in range(2):
                        lo = hp * 64
                        _tt(P, ysum[lo:lo + 64, c * RC:(c + 1) * RC], ysum[lo:lo + 64, c * RC:(c + 1) * RC],
                            pY[lo:lo + 64, 256 + lo:256 + lo + 64], ALU.add, [ysum, pY], [ysum])
                stages += [s1, s2, s3]
            return stages

        for f in pre_stages(0):
            f()
        for pi in range(len(pairs)):
            sq = seq_stages(pi)
            pr = pre_stages(pi + 1) if pi + 1 < len(pairs) else []
            n = max(len(sq), len(pr))
            for i in range(n):
                if i < len(pr):
                    pr[i]()
                if i < len(sq):
                    sq[i]()
    P.barrier()


def build(ncores_debug=False, nbc=NBC, phases=("mod", "inproj", "attn", "s5", "rwkv", "moe"), depth=DEPTH):
    nc = bass.Bass("TRN2", target_bir_lowering=False)
    P = Prog(nc, debug=ncores_debug)
    D = NS()
    S = NS()

    def ext(name, shape, dtype=F32):
        return P.dram(name, shape, dtype, kind="ExternalInput")

    D.xinT = ext("xinT", [nbc, DM, T])
    D.cT = ext("cT", [128, 8, 8])
    D.w_mod = ext("w_mod", [DEPTH, DM, 6 * DM])
    D.w_in = ext("w_in", [DEPTH, DM, INW])
    D.bmodT = ext("bmodT", [128, DEPTH, 48])
    D.norm1T = ext("norm1T", [128, DEPTH, 8])
    D.norm2T = ext("norm2T", [128, DEPTH, 8])
    D.qg = ext("qg", [128, DEPTH])
    D.kg = ext("kg", [128, DEPTH])
    for nm, shp in (("s5_A_rep_re", [128, DEPTH, 256]), ("s5_A_rep_im", [128, DEPTH, 256]), ("s5_ldt_rep", [128, DEPTH, 256]),
                    ("s5_Bt_re", [128, DEPTH, 256]), ("s5_Bt_im", [128, DEPTH, 256]), ("s5_A_st_re", [128, DEPTH, 16]),
                    ("s5_A_st_im", [128, DEPTH, 16]), ("s5_ldt_st", [128, DEPTH, 16]), ("s5_C_st_re", [128, DEPTH, 1024]),
                    ("s5_C_st_im", [128, DEPTH, 1024]), ("s5_dT", [128, DEPTH, 2]), ("s5_w_glu", [DEPTH, 256, 512])):
        setattr(D, nm, ext(nm, shp))
    D.os5 = P.dram("os5", [nbc, 256, T])
    for nm, shp in (("rw_conv", [128, DEPTH, 24]), ("rw_w0", [128, DEPTH, 4]), ("rw_a0", [128, DEPTH, 4]), ("rw_pv", [128, DEPTH, 10]),
                    ("rw_w2a2", [DEPTH, 2, 128, 256]), ("rw_g2", [DEPTH, 128, 256])):
        setattr(D, nm, ext(nm, shp))
    D.orw = P.dram("orw", [nbc, 256, T])
    D.x1T = P.dram("x1T", [nbc, DM, T])
    D.x2T = P.dram("x2T", [nbc, DM, T])
    D.Xe = P.dram("Xe", [NEXP * CAP, DM])
    D.Ye = P.dram("Ye", [NEXP * CAP, DM])
    for nm, shp in (("proj_att", [DEPTH, 512, DM]), ("proj_s5", [DEPTH, 256, DM]), ("proj_rwkv", [DEPTH, 256, DM]),
                    ("w_out", [DEPTH, DM, DM]), ("router_w", [DEPTH, DM, 36]), ("router_b", [128, DEPTH, 36]),
                    ("exp_w_gate", [DEPTH, NEXP, DM, DEXP]), ("exp_w_up", [DEPTH, NEXP, DM, DEXP]), ("exp_w_down", [DEPTH, NEXP, DEXP, DM])):
        setattr(D, nm, ext(nm, shp))
    consts = make_consts()
    D.consts = {k: ext("c_" + k, list(v.shape)) for k, v in consts.items()}
    D.out = P.dram("out", [nbc, DM, LAT], kind="ExternalOutput")
    D.uT = P.dram("uT", [nbc, INW, T])
    D.oatt = P.dram("oatt", [nbc, 512, T])

    S.psum = [P.ps(f"ps{i}", [128, 512]) for i in range(8)]
    for k in ("ident", "ones", "bd64", "bs64", "rot", "sh0", "sh1", "tri"):
        t = P.sb("k_" + k, [128, 128])
        P.dma("sp", R=[D.consts[k]], W=[t], out=t[:], in_=D.consts[k][:, :])
        setattr(S, k, t)
    S.maskG = P.sb("k_maskG", [128, 8])
    P.dma("sp", R=[D.consts["maskG"]], W=[S.maskG], out=S.maskG[:], in_=D.consts["maskG"][:, :])
    S.modT = P.sb("modT", [128, 48, 8])
    S.epsb = P.sb("epsb", [128, 1])
    P.V("memset", W=[S.epsb], ap=S.epsb[:], constant=EPS)
    for k, shp in (("bmodT", [128, DEPTH, 48]), ("norm1T", [128, DEPTH, 8]), ("norm2T", [128, DEPTH, 8])):
        t = P.sb(k, shp)
        P.dma("sp", R=[getattr(D, k)], W=[t], out=t[:], in_=getattr(D, k)[:, :, :])
        setattr(S, k, t)
    S.qg = []
    S.kg = []
    qgt = P.sb("qgt", [128, DEPTH])
    kgt = P.sb("kgt", [128, DEPTH])
    P.dma("sp", R=[D.qg], W=[qgt], out=qgt[:], in_=D.qg[:, :])
    P.dma("sp", R=[D.kg], W=[kgt], out=kgt[:], in_=D.kg[:, :])
    for l in range(DEPTH):
        a = NS.__new__(NS)
        S.qg.append(_ColView(qgt, l))
        S.kg.append(_ColView(kgt, l))

    xsrc = D.xinT
    for l in range(depth):
        need_ctx = l < DEPTH - 1
        if "mod" in phases:
            phase_mod(P, l, D, S)
        if "inproj" in phases:
            for b in range(nbc):
                phase_inproj(P, l, D, S, b, xsrc)
        if "attn" in phases:
            with contextlib.ExitStack() as lst:
                for k in ("cos", "sin"):
                    t = P.sb("k_" + k, [128, LAT], F32, lst)
                    P.dma("sp", R=[D.consts[k]], W=[t], out=t[:], in_=D.consts[k][:, :])
                    setattr(S, k, t)
                P.barrier()
                for b in range(nbc):
                    phase_attn(P, l, D, S, b, need_ctx)
        if "s5" in phases:
            with contextlib.ExitStack() as lst:
                X5 = s5_setup(P, l, D, S, lst)
                P.barrier()
                for b in range(nbc):
                    phase_s5(P, l, D, S, b, X5, need_ctx)
        if "rwkv" in phases:
            with contextlib.ExitStack() as lst:
                XR = rwkv_setup(P, l, D, S, lst)
                P.barrier()
                for b in range(nbc):
                    phase_rwkv(P, l, D, S, b, XR, need_ctx)
        if "rwkv0" in phases:
            with contextlib.ExitStack() as lst:
                z = P.sb("zrw", [128, T], F32, lst)
                P.V("memset", W=[z], ap=z[:], constant=0.0)
                for b in range(nbc):
                    for m in range(2):
                        P.dma("sp", R=[z], W=[D.orw.s(b)], out=D.orw[b, m * 128:(m + 1) * 128, :], in_=z[:])
            P.barrier()
        if "moe" in phases:
            with contextlib.ExitStack() as lst:
                G = NS()
                G.next = 0
                G.tiles = []
                G.slot = P.sb("g_slot", [128, 72, 2], U32, lst)
                G.gate = P.sb("g_gate", [128, 72, 2], F32, lst)
                with contextlib.ExitStack() as lst2:
                    XM = merge_setup(P, l, D, S, lst2)
                    P.barrier()
                    for b in range(nbc):
                        phase_merge(P, l, D, S, b, XM, need_ctx, xsrc, G)
                P.barrier()
                phase_experts(P, l, D, S)
                phase_combine(P, l, D, S, G, D.x2T, l == DEPTH - 1)
            xsrc = D.x2T
    P.barrier()
    P.finish(getattr(P, "dbg_out", []))
    P.finish([D.out])
    P.finish([D.uT.s(b) for b in range(nbc)] + [D.oatt.s(b) for b in range(nbc)] + [D.os5.s(b) for b in range(nbc)] + [D.orw.s(b) for b in range(nbc)])
    info = (P.ninstr, P.nwait)
    P.close()
    return nc, consts, info


class _View:
    def __init__(self, tt, dt):
        self.tt = tt
        self.dt = dt
        self.tok = tt.tok

    def __getitem__(self, idx):
        return self.tt[idx].bitcast(self.dt)

    def s(self, k):
        return self.tt.s(k)


class _ColView:
    def __init__(self, tt, l):
        self.tt = tt
        self.l = l
        self.tok = tt.tok

    def __getitem__(self, idx):
        return self.tt[:, self.l:self.l + 1]


def _tok(x):
    return x.tok if hasattr(x, "tok") else x


def host_inputs(inp, core, nbc=NBC):
    b0 = core * nbc
    x = np.asarray(inp["x"][b0:b0 + nbc], np.float32)
    ctx = np.asarray(inp["ctx"][b0:b0 + nbc], np.float32)
    m = {}
    m["xinT"] = np.ascontiguousarray(np.concatenate([ctx, x], axis=1).transpose(0, 2, 1))
    call = np.zeros((8, DM), np.float32)
    call[:nbc] = inp["c"][b0:b0 + nbc]
    call[4] = inp["c_ctx"]
    m["cT"] = np.ascontiguousarray(call.reshape(8, 8, 128).transpose(2, 1, 0))
    m["w_mod"] = np.asarray(inp["w_mod"], np.float32)
    m["w_in"] = np.asarray(inp["w_in"], np.float32)
    m["bmodT"] = fm(inp["b_mod"])
    m["norm1T"] = fm(inp["norm1"])
    m["norm2T"] = fm(inp["norm2"])
    L = DEPTH
    f32 = lambda k: np.asarray(inp[k], np.float32)
    for nm, key in (("re", "s5_a_re"), ("im", "s5_a_im")):
        a = f32(key)
        m["s5_A_rep_" + nm] = np.ascontiguousarray(np.repeat(a.reshape(L, 2, 2, 8, 64).transpose(3, 0, 1, 2, 4), 16, axis=0).reshape(128, L, 256))
        m["s5_A_st_" + nm] = np.ascontiguousarray(a.reshape(L, 2, 8, 2, 64).transpose(3, 4, 0, 1, 2).reshape(128, L, 16))
    ldt = f32("s5_log_dt")
    r = np.repeat(ldt.reshape(L, 2, 2, 8).transpose(3, 0, 1, 2), 16, axis=0)
    m["s5_ldt_rep"] = np.ascontiguousarray(np.broadcast_to(r[..., None], (128, L, 2, 2, 64)).reshape(128, L, 256))
    r = ldt.reshape(L, 2, 8, 2).transpose(3, 0, 1, 2)
    m["s5_ldt_st"] = np.ascontiguousarray(np.repeat(r[:, None], 64, axis=1).reshape(128, L, 16))
    for nm, key in (("re", "s5_b_re"), ("im", "s5_b_im")):
        bb = f32(key)
        m["s5_Bt_" + nm] = np.ascontiguousarray(bb.reshape(L, 2, 2, 8, 64, 16).transpose(3, 5, 0, 1, 2, 4).reshape(128, L, 256))
    for nm, key in (("re", "s5_c_re"), ("im", "s5_c_im")):
        cc = f32(key).reshape(L, 2, 8, 2, 16, 64)
        o = np.zeros((2, 64, L, 2, 8, 2, 2, 16), np.float32)
        for e in range(2):
            for pr in range(8):
                o[e, :, :, :, pr, pr % 2, e, :] = cc[:, :, pr, e].transpose(3, 0, 1, 2)
        m["s5_C_st_" + nm] = np.ascontiguousarray(o.reshape(128, L, 1024))
    m["s5_dT"] = fm(inp["s5_d"])
    m["s5_w_glu"] = f32("s5_w_glu")
    m["rw_conv"] = np.ascontiguousarray(fm(inp["rwkv_conv"]).reshape(128, L, 24))
    m["rw_w0"] = np.ascontiguousarray(fm(inp["rwkv_w0"]).reshape(128, L, 4))
    m["rw_a0"] = np.ascontiguousarray(fm(inp["rwkv_a0"]).reshape(128, L, 4))
    pv = np.stack([fm(inp[k_]) for k_ in ("rwkv_k_k", "rwkv_k_a", "rwkv_r_k", "rwkv_ln_w", "rwkv_ln_b")], axis=2)
    m["rw_pv"] = np.ascontiguousarray(pv.reshape(128, L, 10))
    m["rw_w2a2"] = np.ascontiguousarray(np.concatenate([f32("rwkv_w2"), f32("rwkv_a2")], axis=2))
    m["rw_g2"] = f32("rwkv_g2")
    for k_ in ("proj_att", "proj_s5", "proj_rwkv", "w_out", "exp_w_gate", "exp_w_up", "exp_w_down"):
        m[k_] = f32(k_)
    m["router_w"] = np.ascontiguousarray(np.concatenate([f32("router_g_w"), f32("router_e_w")], axis=-1))
    rb = np.concatenate([f32("router_g_b"), f32("router_e_b")], axis=-1)
    m["router_b"] = np.ascontiguousarray(np.broadcast_to(rb[None], (128, L, 36)))
    m["qg"] = np.ascontiguousarray(np.tile(np.asarray(inp["q_gain"], np.float32), (1, 2)).T)
    m["kg"] = np.ascontiguousarray(np.tile(np.asarray(inp["k_gain"], np.float32), (1, 2)).T)
    return m


def kernel(**inp):
    nc, consts, info = build()
    in_maps = []
    for core in range(NCORES):
        m = host_inputs(inp, core)
        for k, v in consts.items():
            m["c_" + k] = v
        in_maps.append(m)
    res = run_bass_kernel_spmd(nc, in_maps, core_ids=list(range(NCORES)))
    outs = [r["out"] for r in res.results]
    o = np.concatenate(outs, axis=0)
    return np.ascontiguousarray(o.transpose(0, 2, 1)).astype(np.float32)
```

```python
import contextlib
import math
import numpy as np
import concourse.bass as bass
import concourse.mybir as mybir
from concourse.bass_utils import run_bass_kernel_spmd

F32 = mybir.dt.float32
BF16 = mybir.dt.bfloat16
F32R = mybir.dt.float32r
I32 = mybir.dt.int32
U32 = mybir.dt.uint32
ALU = mybir.AluOpType
AF = mybir.ActivationFunctionType
AX = mybir.AxisListType

NCORES = 8
NBC = 4
DM = 1024
LAT = 2048
CTX = 256
T = LAT + CTX
DEPTH = 2
INW = 5376
OFF_S5 = 1024
OFF_RW = 1280
OFF_GATE = 2304
EPS = 1e-6
NEXP = 32
DEXP = 512
CAP = 1536


class Tok:
    __slots__ = ("lw", "rd", "name")

    def __init__(self, name=""):
        self.lw = None
        self.rd = {}
        self.name = name


class TT:
    def __init__(self, t, name):
        self.t = t
        self.name = name
        self.tok = Tok(name)
        self.slots = {}

    def __getitem__(self, idx):
        return self.t[idx]

    def s(self, k):
        if k not in self.slots:
            self.slots[k] = Tok(f"{self.name}.{k}")
        return self.slots[k]


def _tok(x):
    return x.tok if isinstance(x, TT) else x


class Prog:
    NDMA = 6

    def __init__(self, nc, debug=False):
        self.nc = nc
        self.debug = debug
        self.es = contextlib.ExitStack()
        self.engs = {"pe": nc.tensor, "act": nc.scalar, "dve": nc.vector, "pool": nc.gpsimd, "sp": nc.sync}
        self.sems = []
        self.vals = []
        self.esem = {}
        for n in self.engs:
            self.esem[n] = self._newsem("e_" + n)
        self.dsem = {}
        self.dnext = {}
        for q in ("sp", "act", "pool"):
            self.dsem[q] = [self._newsem(f"d_{q}{i}") for i in range(self.NDMA)]
            self.dnext[q] = 0
        self.seen = {n: {} for n in self.engs}
        self.ninstr = 0
        self.nwait = 0
        self.uid = 0

    def _newsem(self, name):
        s = self.es.enter_context(self.nc.semaphore(name))
        self.sems.append(s)
        self.vals.append(0)
        return len(self.sems) - 1

    def sb(self, name, shape, dtype=F32, stack=None):
        self.uid += 1
        t = (stack if stack is not None else self.es).enter_context(
            self.nc.sbuf_tensor(f"{name}_{self.uid}", list(shape), dtype))
        return TT(t, name)

    def ps(self, name, shape, dtype=F32, stack=None):
        self.uid += 1
        t = (stack if stack is not None else self.es).enter_context(
            self.nc.psum_tensor(f"{name}_{self.uid}", list(shape), dtype))
        return TT(t, name)

    def dram(self, name, shape, dtype=F32, kind="Internal"):
        if kind == "Internal" and self.debug:
            kind = "ExternalOutput"
        t = self.nc.dram_tensor(name, list(shape), dtype, kind=kind)
        return TT(t.ap(), name)

    def _need(self, eng, ev):
        if ev is None:
            return
        s, v = ev
        if self.seen[eng].get(s, 0) >= v:
            return
        self.engs[eng].wait_ge(self.sems[s], v)
        self.seen[eng][s] = v
        self.nwait += 1

    def _deps(self, eng, R, W):
        for x in R:
            self._need(eng, _tok(x).lw)
        for x in W:
            tk = _tok(x)
            self._need(eng, tk.lw)
            for s, v in tk.rd.items():
                self._need(eng, (s, v))

    def _mark(self, ev, R, W):
        for x in R:
            _tok(x).rd[ev[0]] = ev[1]
        for x in W:
            tk = _tok(x)
            tk.lw = ev
            tk.rd = {}

    def op(self, eng, meth, R=(), W=(), **kw):
        self._deps(eng, R, W)
        ins = getattr(self.engs[eng], meth)(**kw)
        s = self.esem[eng]
        self.vals[s] += 1
        ins.then_inc(self.sems[s], 1)
        self._mark((s, self.vals[s]), R, W)
        self.ninstr += 1
        return ins

    def V(self, meth, **kw):
        return self.op("dve", meth, **kw)

    def A(self, meth, **kw):
        return self.op("act", meth, **kw)

    def G(self, meth, **kw):
        return self.op("pool", meth, **kw)

    def PE(self, meth, **kw):
        return self.op("pe", meth, **kw)

    def mm(self, out, lhsT, rhs, start, stop, R, W):
        return self.op("pe", "matmul", R=R, W=W, out=out, lhsT=lhsT, rhs=rhs, start=start, stop=stop)

    def dma(self, q, R=(), W=(), meth="dma_start", **kw):
        self._deps(q, R, W)
        k = self.dnext[q]
        self.dnext[q] = (k + 1) % self.NDMA
        s = self.dsem[q][k]
        if self.vals[s] > 0:
            self._need(q, (s, self.vals[s]))
        ins = getattr(self.engs[q], meth)(**kw)
        self.vals[s] += 16
        ins.then_inc(self.sems[s], 16)
        self._mark((s, self.vals[s]), R, W)
        self.ninstr += 1
        return ins

    def barrier(self):
        for e in self.engs:
            for s in range(len(self.sems)):
                if self.vals[s] > 0:
                    self._need(e, (s, self.vals[s]))

    def finish(self, toks):
        for x in toks:
            self._need("sp", _tok(x).lw)

    def close(self):
        self.es.close()


class NS:
    pass


def fm(v):
    v = np.asarray(v, np.float32)
    lead = v.shape[:-1]
    n = v.shape[-1] // 128
    r = v.reshape(lead + (n, 128))
    return np.ascontiguousarray(np.moveaxis(r, -1, 0))


def make_consts():
    c = {}
    c["ident"] = np.eye(128, dtype=np.float32)
    c["ones"] = np.ones((128, 128), np.float32)
    bd = np.zeros((128, 128), np.float32)
    bd[:64, :64] = 1.0 / 64
    bd[64:, 64:] = 1.0 / 64
    c["bd64"] = bd
    bd1 = np.zeros((128, 128), np.float32)
    bd1[:64, :64] = 1.0
    bd1[64:, 64:] = 1.0
    c["bs64"] = bd1
    rot = np.zeros((128, 128), np.float32)
    for hh in range(2):
        for blk in range(2):
            base = hh * 64 + blk * 32
            for i in range(16):
                rot[base + 16 + i, base + i] = -1.0
                rot[base + i, base + 16 + i] = 1.0
    c["rot"] = rot
    pos = np.arange(LAT)
    row = pos // 64
    col = pos % 64
    inv = 10000.0 ** (-np.arange(16, dtype=np.float32) / 16.0)
    cos = np.zeros((64, LAT), np.float32)
    sin = np.zeros((64, LAT), np.float32)
    for blk, pp in enumerate((row, col)):
        ang = pp[None, :].astype(np.float32) * inv[:, None]
        cc = np.cos(ang).astype(np.float32)
        ss = np.sin(ang).astype(np.float32)
        cos[blk * 32:blk * 32 + 16] = cc
        cos[blk * 32 + 16:blk * 32 + 32] = cc
        sin[blk * 32:blk * 32 + 16] = ss
        sin[blk * 32 + 16:blk * 32 + 32] = ss
    c["cos"] = np.concatenate([cos, cos], 0)
    c["sin"] = np.concatenate([sin, sin], 0)
    sh0 = np.zeros((128, 128), np.float32)
    sh1 = np.zeros((128, 128), np.float32)
    for j in range(64):
        sh0[64 + j, j] = 1.0
        sh1[j, 64 + j] = 1.0
    c["sh0"] = sh0
    c["sh1"] = sh1
    ss = np.arange(64)[:, None]
    tt = np.arange(64)[None, :]
    for d in range(2):
        before = (ss < tt) if d == 0 else (ss > tt)
        beq = (ss <= tt) if d == 0 else (ss >= tt)
        st_ = before.astype(np.float32)
        inc = beq.astype(np.float32)
        c[f"MA{d}"] = np.tile(np.concatenate([-st_, -inc], 1), (1, 4))
        c[f"MB{d}"] = np.tile(np.concatenate([st_, inc], 1), (1, 4))
        c[f"MC{d}"] = np.tile(-(st_.T), (1, 4))
    c["YXI"] = np.tile(np.concatenate([np.zeros((64, 64), np.float32), np.eye(64, dtype=np.float32)], 1), (1, 4))
    rm = np.ones((128, T), np.float32)
    rm[:, ::64] = 0.0
    c["rmask"] = rm
    c["eoff"] = np.tile((np.arange(NEXP, dtype=np.float32) * CAP)[None, :], (128, 1))
    mg = np.zeros((128, 8), np.float32)
    for p in range(128):
        mg[p, p // 16] = 1.0
    c["maskG"] = mg
    tri = np.zeros((128, 128), np.float32)
    for k in range(128):
        tri[k, k + 1:] = 1.0
    c["tri"] = tri
    return c


def phase_mod(P, l, D, S):
    modT = S.modT
    with contextlib.ExitStack() as st:
        wm = [P.sb(f"wm{i}", [128, 8, 1024], F32, st) for i in range(2)]
        ct = P.sb("ct", [128, 8, 8], F32, st)
        sct = P.sb("sct", [128, 8, 8], F32, st)
        psA = S.psum[0]
        P.dma("sp", R=[D.cT], W=[ct], out=ct[:], in_=D.cT[:, :, :])
        P.A("activation", R=[ct], W=[sct], out=sct[:], in_=ct[:], func=AF.Silu)
        for j in range(6):
            w = wm[j % 2]
            P.dma("sp", R=[D.w_mod], W=[w], out=w[:],
                  in_=D.w_mod[l, :, j * 1024:(j + 1) * 1024].rearrange("(kt p) n -> p kt n", p=128))
            for m in range(8):
                o = (j * 8 + m) * 8
                for kt in range(8):
                    P.mm(psA[:, o:o + 8], w[:, kt, m * 128:(m + 1) * 128], sct[:, kt, :], kt == 0, kt == 7,
                         R=[w, sct], W=[psA])
        P.V("tensor_tensor", R=[psA, S.bmodT], W=[modT],
            out=modT[:], in0=psA[:, 0:384].rearrange("p (a b) -> p a b", b=8),
            in1=S.bmodT[:, l, :].unsqueeze(2).to_broadcast([128, 48, 8]), op=ALU.add)
        for j, nt in ((1, S.norm1T), (4, S.norm2T)):
            P.V("tensor_scalar", R=[modT], W=[modT], out=modT[:, j * 8:(j + 1) * 8, :], in0=modT[:, j * 8:(j + 1) * 8, :],
                scalar1=1.0, scalar2=None, op0=ALU.add)
            P.V("tensor_tensor", R=[modT, nt], W=[modT], out=modT[:, j * 8:(j + 1) * 8, :],
                in0=modT[:, j * 8:(j + 1) * 8, :],
                in1=nt[:, l, :].unsqueeze(2).to_broadcast([128, 8, 8]), op=ALU.mult)
    P.barrier()


TILES = [(0, 256)] + [(256 + i * 512, 512) for i in range(4)]


def rms_mod(P, S, x_t, n, col, jsc, jsh, out_fn, sq, rstd, ps):
    P.A("activation", R=[x_t], W=[sq], out=sq[:, :, 0:n], in_=x_t[:, :, 0:n], func=AF.Square)
    for kt in range(8):
        P.mm(ps[:, 0:n], S.ones[:], sq[:, kt, 0:n], kt == 0, kt == 7, R=[S.ones, sq], W=[ps])
    P.A("activation", R=[ps], W=[rstd], out=rstd[:, 0:n], in_=ps[:, 0:n], func=AF.Sqrt, scale=1.0 / DM,
        bias=S.epsb[:, 0:1])
    P.V("reciprocal", R=[rstd], W=[rstd], out=rstd[:, 0:n], in_=rstd[:, 0:n])
    for kt in range(8):
        P.V("tensor_tensor", R=[x_t, rstd], W=[sq], out=sq[:, kt, 0:n], in0=x_t[:, kt, 0:n], in1=rstd[:, 0:n],
            op=ALU.mult)
        o, wt = out_fn(kt)
        P.V("tensor_scalar", R=[sq, S.modT], W=wt, out=o, in0=sq[:, kt, 0:n],
            scalar1=S.modT[:, jsc * 8 + kt, col:col + 1], scalar2=S.modT[:, jsh * 8 + kt, col:col + 1],
            op0=ALU.mult, op1=ALU.add)


def phase_inproj(P, l, D, S, b, xsrc):
    with contextlib.ExitStack() as st:
        hT = P.sb("hT", [128, 8, T], BF16, st)
        xt = [P.sb(f"xt{i}", [128, 8, 512], F32, st) for i in range(2)]
        sq = P.sb("sq", [128, 8, 512], F32, st)
        rstd = P.sb("rstd", [128, 512], F32, st)
        wb = [P.sb(f"wb{i}", [128, 8, 768], BF16, st) for i in range(2)]
        stg = [P.sb(f"stg{i}", [128, T], F32, st) for i in range(2)]
        for ti, (t0, n) in enumerate(TILES):
            col = 4 if t0 < CTX else b
            x_t = xt[ti % 2]
            P.dma("sp", R=[xsrc], W=[x_t], out=x_t[:, :, 0:n],
                  in_=xsrc[b, :, t0:t0 + n].rearrange("(kt p) t -> p kt t", p=128))
            rms_mod(P, S, x_t, n, col, 1, 0, lambda kt: (hT[:, kt, t0:t0 + n], [hT.s(ti)]), sq, rstd, S.psum[0])
        hall = [hT.s(ti) for ti in range(len(TILES))]
        ci = 0
        for cb in range(7):
            w = wb[cb % 2]
            P.dma("pool", R=[D.w_in], W=[w], out=w[:],
                  in_=D.w_in[l, :, cb * 768:(cb + 1) * 768].rearrange("(kt p) n -> p kt n", p=128))
            for m in range(6):
                chunk = cb * 6 + m
                sg = stg[chunk % 2]
                for ti, (t0, n) in enumerate(TILES):
                    ps = S.psum[1 + (ci % 4)]
                    ci += 1
                    for kt in range(8):
                        P.mm(ps[:, 0:n], w[:, kt, m * 128:(m + 1) * 128], hT[:, kt, t0:t0 + n], kt == 0, kt == 7,
                             R=[w, hT.s(ti)], W=[ps])
                    if chunk * 128 >= OFF_GATE:
                        P.A("activation", R=[ps], W=[sg], out=sg[:, t0:t0 + n], in_=ps[:, 0:n], func=AF.Sigmoid)
                    elif ci % 2 == 0:
                        P.A("activation", R=[ps], W=[sg], out=sg[:, t0:t0 + n], in_=ps[:, 0:n], func=AF.Identity)
                    else:
                        P.V("tensor_copy", R=[ps], W=[sg], out=sg[:, t0:t0 + n], in_=ps[:, 0:n])
                P.dma("sp", R=[sg], W=[D.uT.s(b)], out=D.uT[b, chunk * 128:(chunk + 1) * 128, :], in_=sg[:])
    P.barrier()


def qk_norm_rope(P, S, load_fn, dst, nt, gain, stg, tmps):
    tmp, tmp2 = tmps
    for i in range(nt):
        sg = stg.s(i % 2)
        load_fn(i, i % 2)
        for ti, (t0, n) in enumerate(TILES):
            ps = S.psum[0]
            x = stg[:, i % 2, t0:t0 + n]
            d_ = dst[:, i, t0:t0 + n]
            P.A("activation", R=[sg], W=[tmp], out=tmp[:, 0:n], in_=x, func=AF.Square)
            P.mm(ps[:, 0:n], S.bd64[:], tmp[:, 0:n], True, True, R=[S.bd64, tmp], W=[ps])
            P.A("activation", R=[ps], W=[tmp], out=tmp[:, 0:n], in_=ps[:, 0:n], func=AF.Sqrt, scale=1.0,
                bias=S.epsb[:, 0:1])
            P.V("reciprocal", R=[tmp], W=[tmp], out=tmp[:, 0:n], in_=tmp[:, 0:n])
            if t0 < CTX:
                P.V("scalar_tensor_tensor", R=[sg, tmp, gain], W=[dst.s(i)], out=d_, in0=x, scalar=gain[:, 0:1], in1=tmp[:, 0:n],
                    op0=ALU.mult, op1=ALU.mult)
            else:
                P.V("scalar_tensor_tensor", R=[sg, tmp, gain], W=[sg], out=x, in0=x, scalar=gain[:, 0:1], in1=tmp[:, 0:n],
                    op0=ALU.mult, op1=ALU.mult)
                p0 = t0 - CTX
                ps2 = S.psum[1]
                P.mm(ps2[:, 0:n], S.rot[:], x, True, True, R=[S.rot, sg], W=[ps2])
                P.V("tensor_tensor", R=[ps2, S.sin], W=[tmp2], out=tmp2[:, 0:n], in0=ps2[:, 0:n],
                    in1=S.sin[:, p0:p0 + n], op=ALU.mult)
                P.V("tensor_tensor", R=[sg, S.cos], W=[tmp], out=tmp[:, 0:n], in0=x,
                    in1=S.cos[:, p0:p0 + n], op=ALU.mult)
                P.V("tensor_tensor", R=[tmp, tmp2], W=[dst.s(i)], out=d_, in0=tmp[:, 0:n],
                    in1=tmp2[:, 0:n], op=ALU.add)


def phase_attn(P, l, D, S, b, need_ctx):
    with contextlib.ExitStack() as st:
        qT = P.sb("qT", [128, 4, T], F32R, st)
        kTa = P.sb("kTa", [128, 2, T], F32R, st)
        kTb = P.sb("kTb", [128, 2, T], F32R, st)
        vaug = P.sb("vaug", [128, 18, 4, 192], BF16, st)
        oT = P.sb("oT", [128, 4, T], F32, st)
        pT = [P.sb(f"pT{i}", [128, 512], BF16, st) for i in range(3)]
        oacc = P.sb("oacc", [128, 512], F32, st)
        rden = P.sb("rden", [128, 512], F32, st)
        with contextlib.ExitStack() as st2:
            vT = P.sb("vT", [128, 2, T], F32, st2)
            stg = P.sb("qkstg", [128, 2, T], F32, st2)
            for i in range(2):
                P.dma("sp", R=[D.uT.s(b)], W=[vT.s(i)], out=vT[:, i, :], in_=D.uT[b, 768 + i * 128:768 + (i + 1) * 128, :])
            P.G("memset", W=[vaug], ap=vaug[:], constant=1.0)

            def ld_q(i, k):
                P.dma("sp", R=[D.uT.s(b)], W=[stg.s(k)], out=stg[:, k, :], in_=D.uT[b, i * 128:(i + 1) * 128, :])

            def ld_ka(i, k):
                P.dma("sp", R=[D.uT.s(b)], W=[stg.s(k)], out=stg[:, k, :], in_=D.uT[b, 512 + i * 128:512 + (i + 1) * 128, :])

            def ld_kb(i, k):
                P.dma("sp", R=[D.uT.s(b)], W=[stg.s(k)], out=stg[0:64, k, :], in_=D.uT[b, 512 + i * 128 + 64:512 + (i + 1) * 128, :])
                P.dma("sp", R=[D.uT.s(b)], W=[stg.s(k)], out=stg[64:128, k, :], in_=D.uT[b, 512 + i * 128:512 + i * 128 + 64, :])

            tmps = (P.sb("qk_tmp", [128, 512], F32, st2), P.sb("qk_tmp2", [128, 512], F32, st2))
            qk_norm_rope(P, S, ld_q, qT, 4, S.qg[l], stg, tmps)
            qk_norm_rope(P, S, ld_ka, kTa, 2, S.kg[l], stg, tmps)
            qk_norm_rope(P, S, ld_kb, kTb, 2, S.kg[l], stg, tmps)
            cnt = 0
            for i in range(2):
                for kt in range(18):
                    ps = S.psum[2 + cnt % 2]
                    cnt += 1
                    P.PE("transpose", R=[vT.s(i), S.ident], W=[ps], out=ps[:, 0:128], in_=vT[:, i, kt * 128:(kt + 1) * 128],
                         identity=S.ident[:])
                    for e in range(2):
                        P.V("tensor_copy", R=[ps], W=[vaug], out=vaug[:, kt, 2 * i + e, 64:128],
                            in_=ps[:, e * 64:(e + 1) * 64])
        P.barrier()
        qsegs = [(CTX + qc * 512, 512, list(range(18))) for qc in range(4)]
        if need_ctx:
            qsegs.append((0, CTX, [0, 1]))
        gi_ = 0
        for h in range(4):
            i, e = h // 2, h % 2
            for g in range(2):
                ksrc = kTa if e == g else kTb
                shm = S.sh0 if g == 0 else S.sh1
                lo = g * 64
                for (q0, qn, kts) in qsegs:
                    acc = S.psum[4 + (gi_ % 2)]
                    gi_ += 1
                    va_of = (lambda kt: vaug[:, kt, h, 64:192]) if g == 0 else (lambda kt: vaug[:, kt, h, 0:128])

                    def score(ki):
                        kt = kts[ki]
                        pss = S.psum[6 + (ki % 2)]
                        P.mm(pss[:, 0:qn], ksrc[lo:lo + 64, i, kt * 128:(kt + 1) * 128], qT[lo:lo + 64, h, q0:q0 + qn],
                             True, True, R=[ksrc.s(i), qT.s(h)], W=[pss])

                    score(0)
                    for ki, kt in enumerate(kts):
                        pss = S.psum[6 + (ki % 2)]
                        pt = pT[ki % 3]
                        P.A("activation", R=[pss], W=[pt], out=pt[:, 0:qn], in_=pss[:, 0:qn], func=AF.Exp, scale=0.125)
                        if ki + 1 < len(kts):
                            score(ki + 1)
                        P.mm(acc[:, 0:qn], va_of(kt), pt[:, 0:qn], ki == 0, ki == len(kts) - 1, R=[vaug, pt], W=[acc])
                    P.A("activation", R=[acc], W=[oacc], out=oacc[:, 0:qn], in_=acc[:, 0:qn], func=AF.Identity)
                    psd = S.psum[0]
                    P.mm(psd[:, 0:qn], shm[:], oacc[:, 0:qn], True, True, R=[shm, oacc], W=[psd])
                    P.V("reciprocal", R=[psd], W=[rden], out=rden[lo:lo + 64, 0:qn], in_=psd[lo:lo + 64, 0:qn])
                    P.V("tensor_tensor", R=[oacc, rden], W=[oT.s(h)], out=oT[lo:lo + 64, h, q0:q0 + qn],
                        in0=oacc[lo:lo + 64, 0:qn], in1=rden[lo:lo + 64, 0:qn], op=ALU.mult)
        c0 = 0 if need_ctx else CTX
        for h in range(4):
            P.dma("sp", R=[oT.s(h)], W=[D.oatt.s(b)], out=D.oatt[b, h * 128:(h + 1) * 128, c0:T], in_=oT[:, h, c0:T])
    P.barrier()


TWO_PI = 2.0 * math.pi


def _tt(P, out, a, b, op, R, W):
    P.V("tensor_tensor", R=R, W=W, out=out, in0=a, in1=b, op=op)


def _ts(P, out, a, s1, op0, R, W, s2=None, op1=None):
    if op1 is None:
        P.V("tensor_scalar", R=R, W=W, out=out, in0=a, scalar1=s1, scalar2=None, op0=op0)
    else:
        P.V("tensor_scalar", R=R, W=W, out=out, in0=a, scalar1=s1, scalar2=s2, op0=op0, op1=op1)


def sin_turns(P, st, r, out, F, name):
    ki = P.sb(name + "_ki", [128, F], I32, st)
    kf = P.sb(name + "_kf", [128, F], F32, st)
    m = P.sb(name + "_m", [128, F], F32, st)
    P.V("tensor_copy", R=[r], W=[ki], out=ki[:], in_=r[:])
    P.V("tensor_copy", R=[ki], W=[kf], out=kf[:], in_=ki[:])
    _tt(P, kf[:], r[:], kf[:], ALU.subtract, [r, kf], [kf])
    _ts(P, m[:], kf[:], 0.5, ALU.is_gt, [kf], [m])
    _tt(P, kf[:], kf[:], m[:], ALU.subtract, [kf, m], [kf])
    _ts(P, m[:], kf[:], -0.5, ALU.is_lt, [kf], [m])
    _tt(P, kf[:], kf[:], m[:], ALU.add, [kf, m], [kf])
    P.A("activation", R=[kf], W=[out], out=out[:], in_=kf[:], func=AF.Sin, scale=TWO_PI)


def s5_derive(P, st, are, aim, ldt, F, name, need_z):
    mk = lambda n: P.sb(f"{name}_{n}", [128, F], F32, st)
    dt, rho, th, r, r2, cth, sth = mk("dt"), mk("rho"), mk("th"), mk("r"), mk("r2"), mk("cth"), mk("sth")
    P.A("activation", R=[ldt], W=[dt], out=dt[:], in_=ldt[:], func=AF.Exp)
    _tt(P, rho[:], are[:], dt[:], ALU.mult, [are, dt], [rho])
    P.A("activation", R=[rho], W=[rho], out=rho[:], in_=rho[:], func=AF.Exp)
    _tt(P, th[:], aim[:], dt[:], ALU.mult, [aim, dt], [th])
    _ts(P, r[:], th[:], 1.0 / TWO_PI, ALU.mult, [th], [r])
    _ts(P, r2[:], r[:], 0.25, ALU.add, [r], [r2])
    sin_turns(P, st, r, sth, F, name + "_s")
    sin_turns(P, st, r2, cth, F, name + "_c")
    res = dict(rho=rho, cth=cth, sth=sth)
    if need_z:
        abr, abi, den, nr, zre, zim, t1 = mk("abr"), mk("abi"), mk("den"), mk("nr"), mk("zre"), mk("zim"), mk("t1")
        _tt(P, abr[:], rho[:], cth[:], ALU.mult, [rho, cth], [abr])
        _tt(P, abi[:], rho[:], sth[:], ALU.mult, [rho, sth], [abi])
        _tt(P, den[:], are[:], are[:], ALU.mult, [are], [den])
        _tt(P, t1[:], aim[:], aim[:], ALU.mult, [aim], [t1])
        _tt(P, den[:], den[:], t1[:], ALU.add, [den, t1], [den])
        P.V("reciprocal", R=[den], W=[den], out=den[:], in_=den[:])
        _ts(P, nr[:], abr[:], -1.0, ALU.add, [abr], [nr])
        _tt(P, zre[:], nr[:], are[:], ALU.mult, [nr, are], [zre])
        _tt(P, t1[:], abi[:], aim[:], ALU.mult, [abi, aim], [t1])
        _tt(P, zre[:], zre[:], t1[:], ALU.add, [zre, t1], [zre])
        _tt(P, zre[:], zre[:], den[:], ALU.mult, [zre, den], [zre])
        _tt(P, zim[:], abi[:], are[:], ALU.mult, [abi, are], [zim])
        _tt(P, t1[:], nr[:], aim[:], ALU.mult, [nr, aim], [t1])
        _tt(P, zim[:], zim[:], t1[:], ALU.subtract, [zim, t1], [zim])
        _tt(P, zim[:], zim[:], den[:], ALU.mult, [zim, den], [zim])
        res.update(zre=zre, zim=zim)
    return res


def s5_setup(P, l, D, S, st):
    X = NS()
    ld = lambda nm, src, shp, dt=F32: _load(P, st, nm, src, shp, dt)
    are = ld("s5are", D.s5_A_rep_re[:, l, :], [128, 256])
    aim = ld("s5aim", D.s5_A_rep_im[:, l, :], [128, 256])
    ldt = ld("s5ldt", D.s5_ldt_rep[:, l, :], [128, 256])
    bre = ld("s5bre", D.s5_Bt_re[:, l, :], [128, 256])
    bim = ld("s5bim", D.s5_Bt_im[:, l, :], [128, 256])
    X.BTr = P.sb("BTr", [128, 4, 8, 64], BF16, st)
    X.BTi = P.sb("BTi", [128, 4, 8, 64], BF16, st)
    with contextlib.ExitStack() as st2:
        r = s5_derive(P, st2, are, aim, ldt, 256, "dr", True)
        bbr = P.sb("bbr", [128, 256], F32, st2)
        bbi = P.sb("bbi", [128, 256], F32, st2)
        t1 = P.sb("bt1", [128, 256], F32, st2)
        _tt(P, bbr[:], r["zre"][:], bre[:], ALU.mult, [r["zre"], bre], [bbr])
        _tt(P, t1[:], r["zim"][:], bim[:], ALU.mult, [r["zim"], bim], [t1])
        _tt(P, bbr[:], bbr[:], t1[:], ALU.subtract, [bbr, t1], [bbr])
        _tt(P, bbi[:], r["zre"][:], bim[:], ALU.mult, [r["zre"], bim], [bbi])
        _tt(P, t1[:], r["zim"][:], bre[:], ALU.mult, [r["zim"], bre], [t1])
        _tt(P, bbi[:], bbi[:], t1[:], ALU.add, [bbi, t1], [bbi])
        for dst, src in ((X.BTr, bbr), (X.BTi, bbi)):
            for q in range(4):
                P.V("tensor_tensor", R=[src, S.maskG], W=[dst], out=dst[:, q, :, :],
                    in0=src[:, q * 64:(q + 1) * 64].unsqueeze(1).to_broadcast([128, 8, 64]),
                    in1=S.maskG[:, :].unsqueeze(2).to_broadcast([128, 8, 64]), op=ALU.mult)
    P.barrier()
    sare = ld("s5sare", D.s5_A_st_re[:, l, :], [128, 16])
    saim = ld("s5saim", D.s5_A_st_im[:, l, :], [128, 16])
    sldt = ld("s5sldt", D.s5_ldt_st[:, l, :], [128, 16])
    dbg(P, "sare", sare, sare[:], [128, 16])
    dbg(P, "sldt", sldt, sldt[:], [128, 16])
    r2 = s5_derive(P, st, sare, saim, sldt, 16, "ds", False)
    X.rho = r2["rho"]
    dbg(P, "rho", X.rho, X.rho[:], [128, 16])
    dbg(P, "cth", r2["cth"], r2["cth"][:], [128, 16])
    dbg(P, "sth", r2["sth"], r2["sth"][:], [128, 16])
    X.cosT = P.sb("s5cos", [128, 16, 256], F32, st)
    X.sinT = P.sb("s5sin", [128, 16, 256], F32, st)
    nsin = P.sb("s5nsin", [128, 16, 256], F32, st)
    X.nsinT = nsin
    P.V("memset", W=[X.cosT], ap=X.cosT[:, :, 0:1], constant=1.0)
    P.V("memset", W=[X.sinT], ap=X.sinT[:, :, 0:1], constant=0.0)
    P.V("tensor_copy", R=[r2["cth"]], W=[X.cosT], out=X.cosT[:, :, 1], in_=r2["cth"][:])
    P.V("tensor_copy", R=[r2["sth"]], W=[X.sinT], out=X.sinT[:, :, 1], in_=r2["sth"][:])
    tmpa = P.sb("s5tmpa", [128, 16, 128], F32, st)
    m = 2
    while m < 256:
        cm, sm = X.cosT[:, :, m], X.sinT[:, :, m]
        c1, s1 = X.cosT[:, :, 1], X.sinT[:, :, 1]
        cp, sp_ = X.cosT[:, :, m - 1], X.sinT[:, :, m - 1]
        ta = tmpa[:, :, 0]
        tb = tmpa[:, :, 1]
        _tt(P, ta, cp, c1, ALU.mult, [X.cosT], [tmpa])
        _tt(P, tb, sp_, s1, ALU.mult, [X.sinT], [tmpa])
        _tt(P, cm, ta, tb, ALU.subtract, [tmpa], [X.cosT])
        _tt(P, ta, cp, s1, ALU.mult, [X.cosT, X.sinT], [tmpa])
        _tt(P, tb, sp_, c1, ALU.mult, [X.cosT, X.sinT], [tmpa])
        _tt(P, sm, ta, tb, ALU.add, [tmpa], [X.sinT])
        n = m - 1
        cmb = X.cosT[:, :, m:m + 1].to_broadcast([128, 16, n])
        smb = X.sinT[:, :, m:m + 1].to_broadcast([128, 16, n])
        cj, sj = X.cosT[:, :, 1:m], X.sinT[:, :, 1:m]
        co, so = X.cosT[:, :, m + 1:2 * m], X.sinT[:, :, m + 1:2 * m]
        ta = tmpa[:, :, 0:n]
        _tt(P, ta, sj, smb, ALU.mult, [X.sinT], [tmpa])
        _tt(P, co, cj, cmb, ALU.mult, [X.cosT], [X.cosT])
        _tt(P, co, co, ta, ALU.subtract, [X.cosT, tmpa], [X.cosT])
        _tt(P, ta, cj, smb, ALU.mult, [X.cosT, X.sinT], [tmpa])
        _tt(P, so, sj, cmb, ALU.mult, [X.sinT, X.cosT], [X.sinT])
        _tt(P, so, so, ta, ALU.add, [X.sinT, tmpa], [X.sinT])
        m *= 2
    _ts(P, nsin[:], X.sinT[:], -1.0, ALU.mult, [X.sinT], [nsin])
    dbg(P, "cosT", X.cosT, X.cosT[:], [128, 16, 256])
    dbg(P, "sinT", X.sinT, X.sinT[:], [128, 16, 256])
    dbg(P, "BTr", X.BTr, X.BTr[:], [128, 4, 8, 64], BF16)
    X.rhoT = P.sb("s5rhoT", [128, 16, 256], F32, st)
    P.V("tensor_copy", R=[X.rho], W=[X.rhoT], out=X.rhoT[:], in_=X.rho[:].unsqueeze(2).to_broadcast([128, 16, 256]))
    cre = ld("s5cre", D.s5_C_st_re[:, l, :], [128, 16 * 64])
    cim = ld("s5cim", D.s5_C_st_im[:, l, :], [128, 16 * 64])
    X.Cr = P.sb("s5Cr", [128, 16, 64], BF16, st)
    X.Ci = P.sb("s5Ci", [128, 16, 64], BF16, st)
    P.V("tensor_copy", R=[cre], W=[X.Cr], out=X.Cr[:], in_=cre[:].rearrange("p (a b) -> p a b", b=64))
    _ts(P, X.Ci[:], cim[:].rearrange("p (a b) -> p a b", b=64), -1.0, ALU.mult, [cim], [X.Ci])
    X.dT = ld("s5dT", D.s5_dT[:, l, :], [128, 2])
    X.wglu = P.sb("s5wglu", [128, 2, 512], BF16, st)
    P.dma("pool", R=[D.s5_w_glu], W=[X.wglu], out=X.wglu[:], in_=D.s5_w_glu[l].rearrange("(kt p) n -> p kt n", p=128))
    return X


def dbg(P, name, tt, ap, shape, dt=F32):
    if not P.debug:
        return
    d = P.dram("dbg_" + name, shape, dt, kind="ExternalOutput")
    P.dma("sp", R=[tt], W=[d], out=d[tuple(slice(None) for _ in shape)], in_=ap)
    P.dbg_out = getattr(P, "dbg_out", []) + [d]


def _load(P, st, nm, src_ap, shp, dt=F32):
    t = P.sb(nm, shp, dt, st)
    P.dma("sp", R=[], W=[t], out=t[:], in_=src_ap)
    return t


S5CH = 256


def phase_s5(P, l, D, S, b, X, need_ctx):
    nch = T // S5CH
    with contextlib.ExitStack() as st:
        sT = P.sb("s5sT", [128, 2, T], F32, st)
        sT16 = P.sb("s5sT16", [128, 2, T], BF16, st)
        yacc = P.sb("s5yacc", [128, 2, T], F32, st)
        for ft in range(2):
            P.dma("sp", R=[D.uT.s(b)], W=[sT], out=sT[:, ft, :], in_=D.uT[b, OFF_S5 + ft * 128:OFF_S5 + (ft + 1) * 128, :])
        P.A("activation", R=[sT], W=[sT16], out=sT16[:], in_=sT[:], func=AF.Identity)
        for ft in range(2):
            _ts(P, yacc[:, ft, :], sT[:, ft, :], X.dT[:, ft:ft + 1], ALU.mult, [sT, X.dT], [yacc.s((ft, 0)), yacc.s((ft, 1))])
        NCHAIN = 2
        mk = lambda n, dt=F32: [P.sb(f"s5{n}{i}", [128, S5CH], dt, st) for i in range(2 * NCHAIN)]
        xr_re, xr_im, g_re, g_im = mk("xrr"), mk("xri"), mk("gr"), mk("gi")
        h_re, h_im = mk("hr", BF16), mk("hi", BF16)
        hps = [P.sb(f"s5hp{i}", [128, 4], F32, st) for i in range(NCHAIN)]
        tAs = [P.sb(f"s5tA{i}", [128, S5CH], F32, st) for i in range(NCHAIN)]
        tBs = [P.sb(f"s5tB{i}", [128, S5CH], F32, st) for i in range(NCHAIN)]
        tCs = [P.sb(f"s5tC{i}", [128, S5CH], F32, st) for i in range(NCHAIN)]

        def chain(ci, d, pr):
            ft, pp = pr // 4, pr % 4
            dp = d * 8 + pr
            hp, tA, tB, tC = hps[ci], tAs[ci], tBs[ci], tCs[ci]
            cT_, sT_, nsT_ = X.cosT[:, dp, :], X.sinT[:, dp, :], X.nsinT[:, dp, :]
            order = list(range(nch)) if d == 0 else [0] + list(range(nch - 1, 0, -1))
            rv = (lambda ap: ap) if d == 0 else (lambda ap: ap[:, ::-1])
            ytok = yacc.s((ft, pp // 2))
            for oi, ch in enumerate(order):
                c0 = ch * S5CH
                k = ci * 2 + (oi % 2)
                ps_r, ps_i, ps_y = S.psum[3 * ci], S.psum[3 * ci + 1], S.psum[3 * ci + 2]
                lr = X.BTr[:, d * 2 + ft, 2 * pp:2 * pp + 2, :].rearrange("p a b -> p (a b)")
                li = X.BTi[:, d * 2 + ft, 2 * pp:2 * pp + 2, :].rearrange("p a b -> p (a b)")
                P.mm(ps_r[:, 0:S5CH], lr, sT16[:, ft, c0:c0 + S5CH], True, True, R=[X.BTr, sT16], W=[ps_r])
                P.mm(ps_i[:, 0:S5CH], li, sT16[:, ft, c0:c0 + S5CH], True, True, R=[X.BTi, sT16], W=[ps_i])
                yield
                xrr, xri, gr, gi, hr, hi = xr_re[k], xr_im[k], g_re[k], g_im[k], h_re[k], h_im[k]
                pr_, pi_ = ps_r[:, 0:S5CH], ps_i[:, 0:S5CH]
                _tt(P, rv(tA[:]), pr_, rv(cT_), ALU.mult, [ps_r, X.cosT], [tA])
                yield
                _tt(P, rv(tB[:]), pi_, rv(sT_), ALU.mult, [ps_i, X.sinT], [tB])
                yield
                _tt(P, xrr[:], tA[:], tB[:], ALU.add, [tA, tB], [xrr])
                yield
                _tt(P, rv(tA[:]), pi_, rv(cT_), ALU.mult, [ps_i, X.cosT], [tA])
                yield
                _tt(P, rv(tB[:]), pr_, rv(nsT_), ALU.mult, [ps_r, X.nsinT], [tB])
                yield
                _tt(P, xri[:], tA[:], tB[:], ALU.add, [tA, tB], [xri])
                yield
                if oi == 0:
                    ini_r, ini_i = 0.0, 0.0
                    Rini = []
                else:
                    c1, s1 = X.cosT[:, dp, 1:2], X.sinT[:, dp, 1:2]
                    ns1 = X.nsinT[:, dp, 1:2]
                    _tt(P, hp[:, 2:3], hp[:, 0:1], c1, ALU.mult, [hp, X.cosT], [hp])
                    yield
                    P.V("scalar_tensor_tensor", R=[hp, X.nsinT], W=[hp], out=hp[:, 2:3], in0=hp[:, 1:2], scalar=ns1,
                        in1=hp[:, 2:3], op0=ALU.mult, op1=ALU.add)
                    yield
                    _tt(P, hp[:, 3:4], hp[:, 0:1], s1, ALU.mult, [hp, X.sinT], [hp])
                    yield
                    P.V("scalar_tensor_tensor", R=[hp, X.cosT], W=[hp], out=hp[:, 3:4], in0=hp[:, 1:2], scalar=c1,
                        in1=hp[:, 3:4], op0=ALU.mult, op1=ALU.add)
                    yield
                    ini_r, ini_i = hp[:, 2:3], hp[:, 3:4]
                    Rini = [hp]
                P.V("tensor_tensor_scan", R=[X.rhoT, xrr] + Rini, W=[gr], out=gr[:], data0=X.rhoT[:, dp, :], data1=xrr[:],
                    initial=ini_r, op0=ALU.mult, op1=ALU.add)
                yield
                P.V("tensor_tensor_scan", R=[X.rhoT, xri] + Rini, W=[gi], out=gi[:], data0=X.rhoT[:, dp, :], data1=xri[:],
                    initial=ini_i, op0=ALU.mult, op1=ALU.add)
                yield
                _tt(P, tA[:], gr[:], cT_, ALU.mult, [gr, X.cosT], [tA])
                yield
                _tt(P, tB[:], gi[:], nsT_, ALU.mult, [gi, X.nsinT], [tB])
                yield
                _tt(P, tA[:], tA[:], tB[:], ALU.add, [tA, tB], [tA])
                yield
                P.A("activation", R=[tA], W=[hr], out=rv(hr[:]), in_=tA[:], func=AF.Identity)
                P.A("activation", R=[tA], W=[hp], out=hp[:, 0:1], in_=tA[:, S5CH - 1:S5CH], func=AF.Identity)
                _tt(P, tC[:], gi[:], cT_, ALU.mult, [gi, X.cosT], [tC])
                yield
                _tt(P, tB[:], gr[:], sT_, ALU.mult, [gr, X.sinT], [tB])
                yield
                _tt(P, tC[:], tC[:], tB[:], ALU.add, [tC, tB], [tC])
                yield
                P.A("activation", R=[tC], W=[hi], out=rv(hi[:]), in_=tC[:], func=AF.Identity)
                P.A("activation", R=[tC], W=[hp], out=hp[:, 1:2], in_=tC[:, S5CH - 1:S5CH], func=AF.Identity)
                pb = (pp // 2) * 64
                P.mm(ps_y[pb:pb + 64, 0:S5CH], X.Cr[:, dp, :], hr[:], True, False, R=[X.Cr, hr], W=[ps_y])
                P.mm(ps_y[pb:pb + 64, 0:S5CH], X.Ci[:, dp, :], hi[:], False, True, R=[X.Ci, hi], W=[ps_y])
                yield
                _tt(P, yacc[pb:pb + 64, ft, c0:c0 + S5CH], yacc[pb:pb + 64, ft, c0:c0 + S5CH],
                    ps_y[pb:pb + 64, 0:S5CH], ALU.add, [ytok, ps_y], [ytok])
                yield

        for pr in range(8):
            gens = [chain(0, 0, pr), chain(1, 1, pr)]
            alive = [True, True]
            while any(alive):
                for gi_, gq in enumerate(gens):
                    if alive[gi_]:
                        try:
                            next(gq)
                        except StopIteration:
                            alive[gi_] = False
        P.barrier()
        dbg(P, f"yacc{b}", yacc, yacc[:], [128, 2, T])
        ge = P.sb("s5ge", [128, 2, T], BF16, st)
        gt = P.sb("s5gt", [128, 512], F32, st)
        for ft in range(2):
            for (t0, n) in TILES:
                y = yacc[:, ft, t0:t0 + n]
                P.A("activation", R=[yacc], W=[gt], out=gt[:, 0:n], in_=y, func=AF.Square)
                _ts(P, gt[:, 0:n], gt[:, 0:n], 0.044715, ALU.mult, [gt], [gt], 1.0, ALU.add)
                _tt(P, gt[:, 0:n], gt[:, 0:n], y, ALU.mult, [gt, yacc], [gt])
                P.A("activation", R=[gt], W=[gt], out=gt[:, 0:n], in_=gt[:, 0:n], func=AF.Tanh,
                    scale=math.sqrt(2.0 / math.pi))
                _ts(P, gt[:, 0:n], gt[:, 0:n], 1.0, ALU.add, [gt], [gt], 0.5, ALU.mult)
                _tt(P, ge[:, ft, t0:t0 + n], gt[:, 0:n], y, ALU.mult, [gt, yacc], [ge])
        osb = sT
        for m in range(2):
            for (t0, n) in TILES:
                p1, p2 = S.psum[0], S.psum[1]
                for kt in range(2):
                    P.mm(p1[:, 0:n], X.wglu[:, kt, m * 128:(m + 1) * 128], ge[:, kt, t0:t0 + n], kt == 0, kt == 1, R=[X.wglu, ge], W=[p1])
                for kt in range(2):
                    P.mm(p2[:, 0:n], X.wglu[:, kt, 256 + m * 128:256 + (m + 1) * 128], ge[:, kt, t0:t0 + n], kt == 0, kt == 1,
                         R=[X.wglu, ge], W=[p2])
                P.A("activation", R=[p2], W=[gt], out=gt[:, 0:n], in_=p2[:, 0:n], func=AF.Sigmoid)
                _tt(P, osb[:, m, t0:t0 + n], p1[:, 0:n], gt[:, 0:n], ALU.mult, [p1, gt], [osb])
        c0 = 0 if need_ctx else CTX
        for m in range(2):
            P.dma("sp", R=[osb], W=[D.os5.s(b)], out=D.os5[b, m * 128:(m + 1) * 128, c0:T], in_=osb[:, m, c0:T])
    P.barrier()


def merge_setup(P, l, D, S, st):
    X = NS()
    X.pa = P.sb("m_pa", [128, 4, DM], BF16, st)
    X.p5 = P.sb("m_p5", [128, 2, DM], BF16, st)
    X.pr = P.sb("m_pr", [128, 2, DM], BF16, st)
    X.wo = P.sb("m_wo", [128, 8, DM], BF16, st)
    for t, src in ((X.pa, D.proj_att), (X.p5, D.proj_s5), (X.pr, D.proj_rwkv), (X.wo, D.w_out)):
        P.dma("pool", R=[src], W=[t], out=t[:], in_=src[l].rearrange("(kt p) n -> p kt n", p=128))
    X.wr = P.sb("m_wr", [128, 8, 36], F32, st)
    P.dma("sp", R=[D.router_w], W=[X.wr], out=X.wr[:], in_=D.router_w[l].rearrange("(kt p) n -> p kt n", p=128))
    X.rb = P.sb("m_rb", [128, 36], F32, st)
    P.dma("sp", R=[D.router_b], W=[X.rb], out=X.rb[:], in_=D.router_b[:, l, :])
    X.carry = P.sb("m_carry", [128, NEXP], F32, st)
    P.V("memset", W=[X.carry], ap=X.carry[:], constant=0.0)
    X.eoff = P.sb("m_eoff", [128, NEXP], F32, st)
    P.dma("sp", R=[D.consts["eoff"]], W=[X.eoff], out=X.eoff[:], in_=D.consts["eoff"][:, :])
    return X


def phase_merge(P, l, D, S, b, X, need_ctx, xsrc, G):
    tiles = TILES if need_ctx else TILES[1:]
    with contextlib.ExitStack() as st:
        oa = P.sb("mg_oa", [128, 4, 512], BF16, st)
        o5 = P.sb("mg_o5", [128, 2, 512], BF16, st)
        orw = P.sb("mg_or", [128, 2, 512], BF16, st)
        gt = [P.sb(f"mg_g{i}", [128, 3, 512], F32, st) for i in range(2)]
        xt = P.sb("mg_x", [128, 8, 512], F32, st)
        mg = P.sb("mg_m", [128, 8, 512], BF16, st)
        t1 = P.sb("mg_t1", [128, 512], F32, st)
        t2 = P.sb("mg_t2", [128, 512], F32, st)
        sq = P.sb("mg_sq", [128, 8, 512], F32, st)
        rstd = P.sb("mg_rstd", [128, 512], F32, st)
        h2 = P.sb("mg_h2", [128, 8, 512], F32, st)
        htm = P.sb("mg_htm", [128, DM], F32, st)
        rt = {k: P.sb("mg_r" + k, shp, dt, st) for k, shp, dt in (
            ("lg", [128, 36], F32), ("mx", [128, 8], F32), ("ohg", [128, 4], F32), ("el", [128, 8], F32),
            ("ee", [128, 8], F32), ("t8", [128, 8], F32), ("oh1", [128, 8], F32), ("oh2", [128, 8], F32),
            ("M1", [128, 4, 8], F32), ("M2", [128, 4, 8], F32), ("M", [128, NEXP], F32), ("pos", [128, NEXP], F32),
            ("s1", [128, 4], F32), ("gs", [128, 4], F32), ("si", [128, 2], I32))}
        for (t0, n) in tiles:
            col = 4 if t0 < CTX else b
            P.dma("pool", R=[D.oatt.s(b)], W=[oa], out=oa[:, :, 0:n], in_=D.oatt[b, :, t0:t0 + n].rearrange("(k p) t -> p k t", p=128))
            P.dma("pool", R=[D.os5.s(b)], W=[o5], out=o5[:, :, 0:n], in_=D.os5[b, :, t0:t0 + n].rearrange("(k p) t -> p k t", p=128))
            P.dma("pool", R=[D.orw.s(b)], W=[orw], out=orw[:, :, 0:n], in_=D.orw[b, :, t0:t0 + n].rearrange("(k p) t -> p k t", p=128))
            P.dma("sp", R=[xsrc], W=[xt], out=xt[:, :, 0:n], in_=xsrc[b, :, t0:t0 + n].rearrange("(kt p) t -> p kt t", p=128))
            for m in range(8):
                g = gt[m % 2]
                for j in range(3):
                    r0 = OFF_GATE + j * DM + m * 128
                    P.dma("sp", R=[D.uT.s(b)], W=[g], out=g[:, j, 0:n], in_=D.uT[b, r0:r0 + 128, t0:t0 + n])
                pa_, p5_, pr_ = S.psum[1], S.psum[2], S.psum[3]
                for k in range(4):
                    P.mm(pa_[:, 0:n], X.pa[:, k, m * 128:(m + 1) * 128], oa[:, k, 0:n], k == 0, k == 3, R=[X.pa, oa], W=[pa_])
                for k in range(2):
                    P.mm(p5_[:, 0:n], X.p5[:, k, m * 128:(m + 1) * 128], o5[:, k, 0:n], k == 0, k == 1, R=[X.p5, o5], W=[p5_])
                for k in range(2):
                    P.mm(pr_[:, 0:n], X.pr[:, k, m * 128:(m + 1) * 128], orw[:, k, 0:n], k == 0, k == 1, R=[X.pr, orw], W=[pr_])
                _tt(P, t1[:, 0:n], pa_[:, 0:n], g[:, 0, 0:n], ALU.mult, [pa_, g], [t1])
                _tt(P, t2[:, 0:n], p5_[:, 0:n], g[:, 1, 0:n], ALU.mult, [p5_, g], [t2])
                _tt(P, t1[:, 0:n], t1[:, 0:n], t2[:, 0:n], ALU.add, [t1, t2], [t1])
                _tt(P, t2[:, 0:n], pr_[:, 0:n], g[:, 2, 0:n], ALU.mult, [pr_, g], [t2])
                _tt(P, mg[:, m, 0:n], t1[:, 0:n], t2[:, 0:n], ALU.add, [t1, t2], [mg])
            for m in range(8):
                po = S.psum[4 + m % 2]
                for k in range(8):
                    P.mm(po[:, 0:n], X.wo[:, k, m * 128:(m + 1) * 128], mg[:, k, 0:n], k == 0, k == 7, R=[X.wo, mg], W=[po])
                P.V("scalar_tensor_tensor", R=[po, xt, S.modT], W=[xt], out=xt[:, m, 0:n], in0=po[:, 0:n],
                    scalar=S.modT[:, 2 * 8 + m, col:col + 1], in1=xt[:, m, 0:n], op0=ALU.mult, op1=ALU.add)
            P.dma("sp", R=[xt], W=[D.x1T.s(b)], out=D.x1T[b, :, t0:t0 + n].rearrange("(kt p) t -> p kt t", p=128), in_=xt[:, :, 0:n])
            rms_mod(P, S, xt, n, col, 4, 3, lambda kt: (h2[:, kt, 0:n], [h2]), sq, rstd, S.psum[0])
            for sti in range(n // 128):
                tok = slice(sti * 128, (sti + 1) * 128)
                gi = G.next
                G.next += 1
                G.tiles.append((b, t0 + sti * 128))
                pl = S.psum[6]
                for kt in range(8):
                    P.mm(pl[:, 0:36], h2[:, kt, tok], X.wr[:, kt, :], kt == 0, kt == 7, R=[h2, X.wr], W=[pl])
                lg = rt["lg"]
                _tt(P, lg[:], pl[:, 0:36], X.rb[:], ALU.add, [pl, X.rb], [lg])
                mx, ohg, el, ee, t8, oh1, oh2 = rt["mx"], rt["ohg"], rt["el"], rt["ee"], rt["t8"], rt["oh1"], rt["oh2"]
                s1, gs = rt["s1"], rt["gs"]
                P.V("tensor_reduce", R=[lg], W=[s1], out=s1[:, 0:1], in_=lg[:, 0:4], axis=AX.X, op=ALU.max)
                _ts(P, ohg[:], lg[:, 0:4], s1[:, 0:1], ALU.is_equal, [lg, s1], [ohg])
                _ts(P, gs[:], lg[:, 0:4], s1[:, 0:1], ALU.subtract, [lg, s1], [gs])
                P.A("activation", R=[gs], W=[gs], out=gs[:], in_=gs[:], func=AF.Exp)
                P.V("tensor_reduce", R=[gs], W=[s1], out=s1[:, 1:2], in_=gs[:], axis=AX.X, op=ALU.add)
                _ts(P, el[:], lg[:, 4:12], ohg[:, 0:1], ALU.mult, [lg, ohg], [el])
                for j in range(1, 4):
                    P.V("scalar_tensor_tensor", R=[lg, ohg, el], W=[el], out=el[:], in0=lg[:, 4 + 8 * j:12 + 8 * j],
                        scalar=ohg[:, j:j + 1], in1=el[:], op0=ALU.mult, op1=ALU.add)
                P.V("max", R=[el], W=[mx], out=mx[:], in_=el[:])
                _ts(P, oh1[:], el[:], mx[:, 0:1], ALU.is_equal, [el, mx], [oh1])
                _ts(P, oh2[:], el[:], mx[:, 1:2], ALU.is_equal, [el, mx], [oh2])
                _tt(P, s1[:, 2:3], mx[:, 1:2], mx[:, 0:1], ALU.subtract, [mx], [s1])
                P.A("activation", R=[s1], W=[s1], out=s1[:, 2:3], in_=s1[:, 2:3], func=AF.Exp)
                _ts(P, s1[:, 3:4], s1[:, 2:3], 1.0, ALU.add, [s1], [s1])
                _tt(P, s1[:, 3:4], s1[:, 3:4], s1[:, 1:2], ALU.mult, [s1], [s1])
                P.V("reciprocal", R=[s1], W=[s1], out=s1[:, 3:4], in_=s1[:, 3:4])
                P.V("tensor_copy", R=[s1], W=[G.gate], out=G.gate[:, gi, 0:1], in_=s1[:, 3:4])
                _tt(P, G.gate[:, gi, 1:2], s1[:, 3:4], s1[:, 2:3], ALU.mult, [s1], [G.gate])
                for Mk, oh in ((rt["M1"], oh1), (rt["M2"], oh2)):
                    P.V("tensor_tensor", R=[ohg, oh], W=[Mk], out=Mk[:], in0=ohg[:].unsqueeze(2).to_broadcast([128, 4, 8]),
                        in1=oh[:].unsqueeze(1).to_broadcast([128, 4, 8]), op=ALU.mult)
                M = rt["M"]
                _tt(P, M[:], rt["M1"][:].rearrange("p a b -> p (a b)"), rt["M2"][:].rearrange("p a b -> p (a b)"), ALU.add,
                    [rt["M1"], rt["M2"]], [M])
                pp = S.psum[7]
                P.mm(pp[:, 0:NEXP], S.tri[:], M[:], True, True, R=[S.tri, M], W=[pp])
                pos = rt["pos"]
                _tt(P, pos[:], pp[:, 0:NEXP], X.carry[:], ALU.add, [pp, X.carry], [pos])
                _ts(P, pos[:], pos[:], float(CAP - 1), ALU.min, [pos], [pos])
                _tt(P, pos[:], pos[:], X.eoff[:], ALU.add, [pos, X.eoff], [pos])
                P.mm(pp[:, 0:NEXP], S.ones[:], M[:], True, True, R=[S.ones, M], W=[pp])
                _tt(P, X.carry[:], X.carry[:], pp[:, 0:NEXP], ALU.add, [pp, X.carry], [X.carry])
                for k, Mk in enumerate((rt["M1"], rt["M2"])):
                    _tt(P, M[:], Mk[:].rearrange("p a b -> p (a b)"), pos[:], ALU.mult, [Mk, pos], [M])
                    P.V("tensor_reduce", R=[M], W=[s1], out=s1[:, 0:1], in_=M[:], axis=AX.X, op=ALU.add)
                    P.V("tensor_copy", R=[s1], W=[G.slot], out=G.slot[:, gi, k:k + 1], in_=s1[:, 0:1])
                for half in range(2):
                    ph = S.psum[2 + half]
                    for q in range(4):
                        kt = half * 4 + q
                        P.PE("transpose", R=[h2, S.ident], W=[ph], out=ph[:, q * 128:(q + 1) * 128], in_=h2[:, kt, tok], identity=S.ident[:])
                    P.A("activation", R=[ph], W=[htm], out=htm[:, half * 512:(half + 1) * 512], in_=ph[:], func=AF.Identity)
                for k in range(2):
                    P.dma("pool", R=[htm, G.slot], W=[D.Xe], meth="indirect_dma_start", out=D.Xe[:, :],
                          out_offset=bass.IndirectOffsetOnAxis(ap=G.slot[:, gi, k:k + 1], axis=0), in_=htm[:], in_offset=None)
    P.barrier()


def phase_experts(P, l, D, S):
    with contextlib.ExitStack() as st:
        wg = [P.sb(f"e_wg{i}", [128, 8, DEXP], BF16, st) for i in range(2)]
        wu = [P.sb(f"e_wu{i}", [128, 8, DEXP], BF16, st) for i in range(2)]
        wd = [P.sb(f"e_wd{i}", [128, 4, DM], BF16, st) for i in range(2)]
        xtm2 = [P.sb(f"e_xtm{i}", [128, 4, DM], F32, st) for i in range(2)]
        xbT2 = [P.sb(f"e_xbT{i}", [128, 8, 512], BF16, st) for i in range(2)]
        sg = [P.sb(f"e_sg{i}", [128, 512], F32, st) for i in range(2)]
        act = P.sb("e_act", [128, 4, 512], BF16, st)
        yb2 = [P.sb(f"e_yb{i}", [128, 4, DM], F32, st) for i in range(2)]
        blocks = [(e, blk) for e in range(NEXP) for blk in range(CAP // 512)]
        cnt = {"ci": 0}

        def load_w(e):
            k = e % 2
            P.dma("pool", R=[D.exp_w_gate], W=[wg[k]], out=wg[k][:], in_=D.exp_w_gate[l, e].rearrange("(kt p) n -> p kt n", p=128))
            P.dma("pool", R=[D.exp_w_up], W=[wu[k]], out=wu[k][:], in_=D.exp_w_up[l, e].rearrange("(kt p) n -> p kt n", p=128))
            P.dma("pool", R=[D.exp_w_down], W=[wd[k]], out=wd[k][:], in_=D.exp_w_down[l, e].rearrange("(kt p) n -> p kt n", p=128))

        def stage_a(bi):
            e, blk = blocks[bi]
            r0 = e * CAP + blk * 512
            xtm, xbT = xtm2[bi % 2], xbT2[bi % 2]
            P.dma("sp", R=[D.Xe], W=[xtm], out=xtm[:], in_=D.Xe[r0:r0 + 512, :].rearrange("(s p) f -> p s f", p=128))
            for kt in range(8):
                ph = S.psum[cnt["ci"] % 2]
                cnt["ci"] += 1
                for s_ in range(4):
                    P.PE("transpose", R=[xtm, S.ident], W=[ph], out=ph[:, s_ * 128:(s_ + 1) * 128],
                         in_=xtm[:, s_, kt * 128:(kt + 1) * 128], identity=S.ident[:])
                if kt % 2 == 0:
                    P.A("activation", R=[ph], W=[xbT], out=xbT[:, kt, :], in_=ph[:], func=AF.Identity)
                else:
                    P.V("tensor_copy", R=[ph], W=[xbT], out=xbT[:, kt, :], in_=ph[:])

        def stage_bc(bi):
            e, blk = blocks[bi]
            k = e % 2
            r0 = e * CAP + blk * 512
            xbT, yb = xbT2[bi % 2], yb2[bi % 2]
            for hm in range(4):
                pg, pu = S.psum[2 + 2 * (hm % 2)], S.psum[3 + 2 * (hm % 2)]
                for kt in range(8):
                    P.mm(pg[:], wg[k][:, kt, hm * 128:(hm + 1) * 128], xbT[:, kt, :], kt == 0, kt == 7, R=[wg[k], xbT], W=[pg])
                for kt in range(8):
                    P.mm(pu[:], wu[k][:, kt, hm * 128:(hm + 1) * 128], xbT[:, kt, :], kt == 0, kt == 7, R=[wu[k], xbT], W=[pu])
                P.A("activation", R=[pg], W=[sg[hm % 2]], out=sg[hm % 2][:], in_=pg[:], func=AF.Silu)
                _tt(P, act[:, hm, :], pu[:], sg[hm % 2][:], ALU.mult, [pu, sg[hm % 2]], [act])
            for s_ in range(4):
                for half in range(2):
                    pd = S.psum[6 + half]
                    for hm in range(4):
                        P.mm(pd[:], act[:, hm, s_ * 128:(s_ + 1) * 128], wd[k][:, hm, half * 512:(half + 1) * 512], hm == 0, hm == 3,
                             R=[act, wd[k]], W=[pd])
                    if half == 0:
                        P.A("activation", R=[pd], W=[yb], out=yb[:, s_, 0:512], in_=pd[:], func=AF.Identity)
                    else:
                        P.V("tensor_copy", R=[pd], W=[yb], out=yb[:, s_, 512:1024], in_=pd[:])
            P.dma("sp", R=[yb], W=[D.Ye], out=D.Ye[r0:r0 + 512, :].rearrange("(s p) f -> p s f", p=128), in_=yb[:])

        load_w(0)
        stage_a(0)
        for bi in range(len(blocks)):
            e, blk = blocks[bi]
            if blk == 0 and e + 1 < NEXP:
                load_w(e + 1)
            if bi + 1 < len(blocks):
                stage_a(bi + 1)
            stage_bc(bi)
    P.barrier()


def phase_combine(P, l, D, S, G, dst, last):
    with contextlib.ExitStack() as st:
        y1 = [P.sb(f"c_y1{i}", [128, DM], F32, st) for i in range(2)]
        y2 = [P.sb(f"c_y2{i}", [128, DM], F32, st) for i in range(2)]
        xt = [P.sb(f"c_x{i}", [128, 8, 128], F32, st) for i in range(2)]
        for gi, (b, t0) in enumerate(G.tiles):
            k = gi % 2
            col = 4 if t0 < CTX else b
            for yy, kk in ((y1[k], 0), (y2[k], 1)):
                P.dma("pool", R=[D.Ye, G.slot], W=[yy], meth="indirect_dma_start", out=yy[:], out_offset=None, in_=D.Ye[:, :],
                      in_offset=bass.IndirectOffsetOnAxis(ap=G.slot[:, gi, kk:kk + 1], axis=0))
            P.dma("sp", R=[D.x1T.s(b)], W=[xt[k]], out=xt[k][:], in_=D.x1T[b, :, t0:t0 + 128].rearrange("(kt p) t -> p kt t", p=128))
            _ts(P, y1[k][:], y1[k][:], G.gate[:, gi, 0:1], ALU.mult, [y1[k], G.gate], [y1[k]])
            P.V("scalar_tensor_tensor", R=[y1[k], y2[k], G.gate], W=[y1[k]], out=y1[k][:], in0=y2[k][:], scalar=G.gate[:, gi, 1:2],
                in1=y1[k][:], op0=ALU.mult, op1=ALU.add)
            for half in range(2):
                ph = S.psum[2 * k + half]
                for q in range(4):
                    m = half * 4 + q
                    P.PE("transpose", R=[y1[k], S.ident], W=[ph], out=ph[:, q * 128:(q + 1) * 128], in_=y1[k][:, m * 128:(m + 1) * 128],
                         identity=S.ident[:])
                for q in range(4):
                    m = half * 4 + q
                    P.V("scalar_tensor_tensor", R=[ph, xt[k], S.modT], W=[xt[k]], out=xt[k][:, m, :], in0=ph[:, q * 128:(q + 1) * 128],
                        scalar=S.modT[:, 5 * 8 + m, col:col + 1], in1=xt[k][:, m, :], op0=ALU.mult, op1=ALU.add)
            if last:
                P.dma("sp", R=[xt[k]], W=[D.out], out=D.out[b, :, t0 - CTX:t0 - CTX + 128].rearrange("(kt p) t -> p kt t", p=128), in_=xt[k][:])
            else:
                P.dma("sp", R=[xt[k]], W=[dst.s(b)], out=dst[b, :, t0:t0 + 128].rearrange("(kt p) t -> p kt t", p=128), in_=xt[k][:])
    P.barrier()


RC = 64
NCH = T // RC
GN_EPS = 64e-5
W_SCALE = -math.exp(-0.5)


def rwkv_setup(P, l, D, S, st):
    X = NS()
    ld = lambda nm, src, shp: _load(P, st, nm, src, shp)
    X.cw = ld("rw_cw", D.rw_conv[:, l, :], [128, 24])
    X.w0 = ld("rw_w0", D.rw_w0[:, l, :], [128, 4])
    X.a0 = ld("rw_a0", D.rw_a0[:, l, :], [128, 4])
    X.pv = ld("rw_pv", D.rw_pv[:, l, :], [128, 10])
    X.w2a2 = [ld(f"rw_w2a2{d}", D.rw_w2a2[l, d], [128, 256]) for d in range(2)]
    X.g2 = ld("rw_g2", D.rw_g2[l], [128, 256])
    X.msk = {k: ld("rw_" + k, D.consts[k][:, :], list(D.consts[k].t.shape)) for k in ("MA0", "MA1", "MB0", "MB1", "MC0", "MC1", "YXI")}
    X.rmask = ld("rw_rmask", D.consts["rmask"][:, :], [128, T])
    X.gneps = P.sb("rw_gneps", [128, 1], F32, st)
    P.V("memset", W=[X.gneps], ap=X.gneps[:], constant=GN_EPS)
    X.kkeps = P.sb("rw_kkeps", [128, 1], F32, st)
    P.V("memset", W=[X.kkeps], ap=X.kkeps[:], constant=1e-12)
    return X


def phase_rwkv(P, l, D, S, b, X, need_ctx):
    zbase = OFF_RW
    SEG = ((0, CTX), (CTX, T))
    for ih in range(2):
        with contextlib.ExitStack() as st:
            A = lambda nm, shp=(128, T): P.sb("rw_" + nm, list(shp), F32, st)
            zt = A("zt")
            r, k, v, kk, kts, g, ysum = A("r"), A("k"), A("v"), A("kk"), A("kts"), A("g"), A("ysum")
            z6 = A("z6")
            t1, t2, t3 = A("t1"), A("t2"), A("t3")
            t2x = g
            KRr = P.sb("rw_KR", [128, NCH, 2, RC], F32R, st)
            BKr = P.sb("rw_BK", [128, NCH, 2, RC], F32R, st)
            KR = _View(KRr, F32)
            BK = _View(BKr, F32)
            wtot = A("wtot", (128, NCH))

            def conv(dst, tile):
                P.dma("sp", R=[D.uT.s(b)], W=[zt], out=zt[:], in_=D.uT[b, zbase + tile * 128:zbase + (tile + 1) * 128, :])
                _ts(P, dst[:], zt[:], X.cw[:, 8 + tile:9 + tile], ALU.mult, [zt, X.cw], [dst])
                for (s0, s1) in SEG:
                    P.V("scalar_tensor_tensor", R=[zt, X.cw, dst], W=[dst], out=dst[:, s0 + 1:s1], in0=zt[:, s0:s1 - 1],
                        scalar=X.cw[:, tile:tile + 1], in1=dst[:, s0 + 1:s1], op0=ALU.mult, op1=ALU.add)
                    P.V("scalar_tensor_tensor", R=[zt, X.cw, dst], W=[dst], out=dst[:, s0:s1 - 1], in0=zt[:, s0 + 1:s1],
                        scalar=X.cw[:, 16 + tile:17 + tile], in1=dst[:, s0:s1 - 1], op0=ALU.mult, op1=ALU.add)

            conv(r, ih)
            conv(k, 2 + ih)
            conv(v, 4 + ih)
            conv(z6, 6)
            P.A("activation", R=[z6], W=[z6], out=z6[0:64, :], in_=z6[0:64, :], func=AF.Tanh)
            P.V("memset", W=[ysum], ap=ysum[:], constant=0.0)
            P.V("memset", W=[kts], ap=kts[:], constant=0.0)
            _ts(P, kk[:], k[:], X.pv[:, 0 + ih:1 + ih], ALU.mult, [k, X.pv], [kk])
            for (t0, n) in TILES:
                ps = S.psum[0]
                P.A("activation", R=[kk], W=[t1], out=t1[:, t0:t0 + n], in_=kk[:, t0:t0 + n], func=AF.Square)
                P.mm(ps[:, 0:n], S.bs64[:], t1[:, t0:t0 + n], True, True, R=[S.bs64, t1], W=[ps])
                P.A("activation", R=[ps, X.kkeps], W=[t1], out=t1[:, t0:t0 + n], in_=ps[:, 0:n], func=AF.Sqrt, bias=X.kkeps[:, 0:1], scale=1.0)
                P.V("reciprocal", R=[t1], W=[t1], out=t1[:, t0:t0 + n], in_=t1[:, t0:t0 + n])
                _tt(P, kk[:, t0:t0 + n], kk[:, t0:t0 + n], t1[:, t0:t0 + n], ALU.mult, [kk, t1], [kk])
            for d in range(2):
                MA, MB, MC = X.msk[f"MA{d}"], X.msk[f"MB{d}"], X.msk[f"MC{d}"]
                for (t0, n) in TILES:
                    pw, pa = S.psum[0], S.psum[1]
                    P.mm(pw[:, 0:n], X.w2a2[d][0:64, ih * 128:(ih + 1) * 128], z6[0:64, t0:t0 + n], True, True, R=[X.w2a2[d], z6], W=[pw])
                    P.mm(pa[:, 0:n], X.w2a2[d][64:128, ih * 128:(ih + 1) * 128], z6[64:128, t0:t0 + n], True, True, R=[X.w2a2[d], z6], W=[pa])
                    P.A("activation", R=[pw, X.w0], W=[t1], out=t1[:, t0:t0 + n], in_=pw[:, 0:n], func=AF.Sigmoid,
                        bias=X.w0[:, 2 * d + ih:2 * d + ih + 1], scale=1.0)
                    P.A("activation", R=[pa, X.a0], W=[t2], out=t2[:, t0:t0 + n], in_=pa[:, 0:n], func=AF.Sigmoid,
                        bias=X.a0[:, 2 * d + ih:2 * d + ih + 1], scale=1.0)
                _ts(P, t1[:], t1[:], W_SCALE, ALU.mult, [t1], [t1])
                _ts(P, t3[:], t2[:], -1.0, ALU.add, [t2], [t3], X.pv[:, 2 + ih:3 + ih], ALU.mult)
                P.V("scalar_tensor_tensor", R=[t3, k], W=[t3], out=t3[:], in0=t3[:], scalar=1.0, in1=k[:], op0=ALU.add, op1=ALU.mult)
                _tt(P, kts[:], kts[:], t3[:], ALU.add, [kts, t3], [kts])
                _tt(P, t2[:], t2[:], kk[:], ALU.mult, [t2, kk], [t2])
                P.V("tensor_tensor_scan", R=[X.rmask, t1], W=[zt], out=zt[:], data0=X.rmask[:], data1=t1[:], initial=0.0,
                    op0=ALU.mult, op1=ALU.add)
                zt3 = zt[:].rearrange("p (c j) -> p c j", j=RC)
                if d == 1:
                    P.V("tensor_tensor", R=[zt], W=[t2x], out=t2x[:].rearrange("p (c j) -> p c j", j=RC),
                        in0=zt3[:, :, RC - 1:RC].to_broadcast([128, NCH, RC]), in1=zt3, op=ALU.subtract)
                    _tt(P, zt[:], t2x[:], t1[:], ALU.add, [t2x, t1], [zt])
                last = RC - 1 if d == 0 else 0
                P.A("activation", R=[zt], W=[wtot], out=wtot[:], in_=zt3[:, :, last], func=AF.Exp)
                c3 = lambda tt_: tt_[:].rearrange("p (c j) -> p c j", j=RC)
                P.A("activation", R=[zt], W=[t2x], out=t2x[:], in_=zt[:], func=AF.Exp)
                _tt(P, KRr[:, :, 1, :], c3(t2x), c3(r), ALU.mult, [t2x, r], [KRr])
                P.A("activation", R=[zt], W=[t2x], out=t2x[:], in_=zt[:], func=AF.Exp, scale=-1.0)
                _tt(P, BKr[:, :, 1, :], c3(t2x), c3(t3), ALU.mult, [t2x, t3], [BKr])
                _tt(P, BKr[:, :, 0, :], c3(t2x), c3(t2), ALU.mult, [t2x, t2], [BKr])
                _tt(P, zt[:], zt[:], t1[:], ALU.subtract, [zt, t1], [zt])
                P.A("activation", R=[zt], W=[t2x], out=t2x[:], in_=zt[:], func=AF.Exp)
                _tt(P, KRr[:, :, 0, :], c3(t2x), c3(kk), ALU.mult, [t2x, kk], [KRr])
                rwkv_scan(P, S, X, d, st, KRr, BKr, wtot, v, ysum, MA, MB, MC)
            z7 = t3
            conv(z7, 7)
            P.A("activation", R=[z7], W=[z7], out=z7[:], in_=z7[:], func=AF.Sigmoid)
            for (t0, n) in TILES:
                ps2 = S.psum[1]
                P.mm(ps2[:, 0:n], X.g2[:, ih * 128:(ih + 1) * 128], z7[:, t0:t0 + n], True, True, R=[X.g2, z7], W=[ps2])
                P.A("activation", R=[ps2], W=[g], out=g[:, t0:t0 + n], in_=ps2[:, 0:n], func=AF.Identity)
            for (t0, n) in TILES:
                if t0 < CTX and not need_ctx:
                    continue
                sl = slice(t0, t0 + n)
                pm, pv_, pb = S.psum[0], S.psum[1], S.psum[2]
                P.mm(pm[:, 0:n], S.bd64[:], ysum[:, sl], True, True, R=[S.bd64, ysum], W=[pm])
                _tt(P, t1[:, sl], ysum[:, sl], pm[:, 0:n], ALU.subtract, [ysum, pm], [t1])
                P.A("activation", R=[t1], W=[t2], out=t2[:, sl], in_=t1[:, sl], func=AF.Square)
                P.mm(pv_[:, 0:n], S.bd64[:], t2[:, sl], True, True, R=[S.bd64, t2], W=[pv_])
                P.A("activation", R=[pv_, X.gneps], W=[t2], out=t2[:, sl], in_=pv_[:, 0:n], func=AF.Sqrt, bias=X.gneps[:, 0:1], scale=1.0)
                P.V("reciprocal", R=[t2], W=[t2], out=t2[:, sl], in_=t2[:, sl])
                _tt(P, t1[:, sl], t1[:, sl], t2[:, sl], ALU.mult, [t1, t2], [t1])
                _ts(P, t1[:, sl], t1[:, sl], X.pv[:, 6 + ih:7 + ih], ALU.mult, [t1, X.pv], [t1], X.pv[:, 8 + ih:9 + ih], ALU.add)
                P.V("scalar_tensor_tensor", R=[r, kts, X.pv], W=[t2], out=t2[:, sl], in0=r[:, sl], scalar=X.pv[:, 4 + ih:5 + ih],
                    in1=kts[:, sl], op0=ALU.mult, op1=ALU.mult)
                P.mm(pb[:, 0:n], S.bs64[:], t2[:, sl], True, True, R=[S.bs64, t2], W=[pb])
                _tt(P, t2[:, sl], pb[:, 0:n], v[:, sl], ALU.mult, [pb, v], [t2])
                _tt(P, t1[:, sl], t1[:, sl], t2[:, sl], ALU.add, [t1, t2], [t1])
                _tt(P, t1[:, sl], t1[:, sl], g[:, sl], ALU.mult, [t1, g], [t1])
            c0 = 0 if need_ctx else CTX
            P.dma("sp", R=[t1], W=[D.orw.s(b)], out=D.orw[b, ih * 128:(ih + 1) * 128, c0:T], in_=t1[:, c0:T])
        P.barrier()


def rwkv_scan(P, S, X, d, st0, KR, BK, wtot, v, ysum, MA, MB, MC):
    G = 2
    with contextlib.ExitStack() as st:
        B2 = lambda nm, shp: [P.sb(f"rs_{nm}{i}", list(shp), F32R, st) for i in range(2)]
        ST = P.sb("rs_ST", [128, 128], F32R, st)
        P.V("tensor_scalar", R=[S.ident], W=[ST], out=ST[:], in0=S.ident[:], scalar1=0.0, scalar2=None, op0=ALU.mult)
        identR = P.sb("rs_identR", [128, 128], F32R, st)
        P.V("tensor_copy", R=[S.ident], W=[identR], out=identR[:], in_=S.ident[:])
        bf = lambda ap: ap.bitcast(F32)
        YA, AB = B2("YA", (64, G, 2, 128)), B2("AB", (64, G, 2, 128))
        YX = B2("YX", (64, G, 2, 2, RC))
        YT = B2("YT", (64, G, 2, RC))
        X6 = B2("X6", (64, G, 2, RC))
        BKt, Vt = B2("BKt", (64, G, 2, 128)), B2("Vt", (64, G, 128))
        RHS, Ps = B2("RHS", (64, 128)), B2("Ps", (64, 128))
        if d == 0:
            order = list(range(NCH))
        else:
            nc_ctx = CTX // RC
            order = list(range(nc_ctx - 1, -1, -1)) + list(range(NCH - 1, nc_ctx - 1, -1))
        pairs = [order[i:i + G] for i in range(0, NCH, G)]

        def pre_stages(pi):
            cs = pairs[pi]
            q = pi % 2
            ya, ab, bkt, vt, x6 = YA[q], AB[q], BKt[q], Vt[q], X6[q]
            stages = []

            def s_init():
                pA, pB, pC = S.psum[0], S.psum[1], S.psum[2]
                for gi, c in enumerate(cs):
                    for hp in range(2):
                        lo = hp * 64
                        o = (gi * 2 + hp)
                        krc = KR[lo:lo + 64, c, :, :].rearrange("p a b -> p (a b)")
                        P.mm(pA[0:64, o * 128:(o + 1) * 128], BK[lo:lo + 64, c, 0, :], krc, True, True, R=[BK, KR], W=[pA])
                        P.mm(pB[0:64, o * 128:(o + 1) * 128], BK[lo:lo + 64, c, 1, :], krc, True, True, R=[BK, KR], W=[pB])
                        P.mm(pC[0:64, o * 64:(o + 1) * 64], KR[lo:lo + 64, c, 0, :], BK[lo:lo + 64, c, 0, :], True, True, R=[BK, KR], W=[pC])
                _tt(P, ya[:].rearrange("p g a b -> p (g a b)"), pA[0:64, 0:512], MA[:], ALU.mult, [pA, MA], [ya])
                _tt(P, ab[:].rearrange("p g a b -> p (g a b)"), pB[0:64, 0:512], MB[:], ALU.mult, [pB, MB], [ab])
                yx, yt = YX[0], YT[0]
                _tt(P, yt[:].rearrange("p g a b -> p (g a b)"), pC[0:64, 0:256], MC[:], ALU.mult, [pC, MC], [yt])
                P.A("activation", R=[X.msk["YXI"]], W=[yx], out=yx[:].rearrange("p g a b c -> p (g a b c)"), in_=X.msk["YXI"][:],
                    func=AF.Identity)
                P.V("tensor_copy", R=[ya], W=[yx], out=yx[:, :, :, 0, :], in_=bf(ya[:, :, :, 0:RC]))
            stages.append(s_init)

            def mk_step(kstep):
                def s_step():
                    yx, yt = YX[kstep % 2], YT[kstep % 2]
                    yxn, ytn = YX[(kstep + 1) % 2], YT[(kstep + 1) % 2]
                    pa_, pc_ = S.psum[3], S.psum[4]
                    for gi in range(G):
                        for hp in range(2):
                            o = gi * 2 + hp
                            P.mm(pa_[0:64, o * 128:(o + 1) * 128], yt[:, gi, hp, :], yx[:, gi, hp, :, :].rearrange("p a b -> p (a b)"),
                                 True, True, R=[yt, yx], W=[pa_])
                            if kstep < 5:
                                P.mm(pc_[0:64, o * 64:(o + 1) * 64], yx[:, gi, hp, 0, :], yt[:, gi, hp, :], True, True, R=[yt, yx], W=[pc_])
                    pa4 = pa_[0:64, 0:512].rearrange("p (g a b c) -> p g a b c", g=G, a=2, b=2)
                    if kstep < 5:
                        P.A("activation", R=[pa_], W=[yxn], out=yxn[:, :, :, 0, :], in_=pa4[:, :, :, 0, :], func=AF.Identity)
                        _tt(P, yxn[:, :, :, 1, :], bf(yx[:, :, :, 1, :]), pa4[:, :, :, 1, :], ALU.add, [yx, pa_], [yxn])
                        P.A("activation", R=[pc_], W=[ytn], out=ytn[:].rearrange("p g a b -> p (g a b)"), in_=pc_[0:64, 0:256],
                            func=AF.Identity)
                    else:
                        _tt(P, x6[:], bf(yx[:, :, :, 1, :]), pa4[:, :, :, 1, :], ALU.add, [yx, pa_], [x6])
                return s_step
            for kstep in range(6):
                stages.append(mk_step(kstep))

            def s_tr():
                pT, pV = S.psum[5], S.psum[2]
                for gi, c in enumerate(cs):
                    P.PE("transpose", R=[BK, S.ident], W=[pT], out=pT[0:64, gi * 256:gi * 256 + 128], in_=BK[:, c, 0, :].bitcast(F32), identity=S.ident[:])
                    P.PE("transpose", R=[BK, S.ident], W=[pT], out=pT[0:64, gi * 256 + 128:gi * 256 + 256], in_=BK[:, c, 1, :].bitcast(F32),
                         identity=S.ident[:])
                    P.PE("transpose", R=[v, S.ident], W=[pV], out=pV[0:64, 256 + gi * 128:256 + (gi + 1) * 128], in_=v[:, c * RC:(c + 1) * RC],
                         identity=S.ident[:])
                pT4 = pT[0:64, 0:512].rearrange("p (g a b) -> p g a b", g=G, a=2)
                _ts(P, bkt[:, :, 0, :], pT4[:, :, 0, :], -1.0, ALU.mult, [pT], [bkt])
                P.A("activation", R=[pT], W=[bkt], out=bkt[:, :, 1, :], in_=pT4[:, :, 1, :], func=AF.Identity)
                P.A("activation", R=[pV], W=[vt], out=vt[:].rearrange("p g b -> p (g b)"), in_=pV[0:64, 256:512], func=AF.Identity)
            stages.append(s_tr)
            return stages

        def seq_stages(pi):
            cs = pairs[pi]
            q = pi % 2
            ya, ab, bkt, vt, x6 = YA[q], AB[q], BKt[q], Vt[q], X6[q]
            stages = []
            for gi, c in enumerate(cs):
                rhs, ps_ = RHS[gi], Ps[gi]
                pR, pP, pY, pS = S.psum[6], S.psum[7], S.psum[6], S.psum[7]

                def s1(gi=gi, c=c, rhs=rhs):
                    for hp in range(2):
                        lo = hp * 64
                        P.mm(pR[0:64, lo:lo + 64], KR[lo:lo + 64, c, 0, :], ST[lo:lo + 64, lo:lo + 64], True, False, R=[KR, ST], W=[pR])
                        P.mm(pR[0:64, lo:lo + 64], ab[:, gi, hp, 0:RC], vt[:, gi, lo:lo + 64], False, True, R=[ab, vt], W=[pR])
                    P.V("tensor_copy", R=[pR], W=[rhs], out=rhs[:], in_=pR[0:64, 0:128])

                def s2(gi=gi, c=c, rhs=rhs, ps_=ps_):
                    for hp in range(2):
                        lo = hp * 64
                        P.mm(pP[0:64, lo:lo + 64], x6[:, gi, hp, :], rhs[:, lo:lo + 64], True, True, R=[x6, rhs], W=[pP])
                    P.V("tensor_copy", R=[pP], W=[ps_], out=ps_[:], in_=pP[0:64, 0:128])

                def s3(gi=gi, c=c, ps_=ps_):
                    P.mm(pS[:, 256:384], bkt[:, gi, 0, :], ps_[:], True, False, R=[bkt, ps_], W=[pS])
                    P.mm(pS[:, 256:384], bkt[:, gi, 1, :], vt[:, gi, :], False, False, R=[bkt, vt], W=[pS])
                    P.mm(pS[:, 256:384], identR[:], ST[:], False, True, R=[identR, ST], W=[pS])
                    for hp2 in range(2):
                        P.mm(pY[:, 256 + hp2 * 64:256 + (hp2 + 1) * 64], ST[:], KR[:, c, 1, :], hp2 == 0, False, R=[ST, KR], W=[pY])
                    P.mm(pY[:, 256:384], ps_[:], ya[:, gi, :, RC:2 * RC], False, False, R=[ps_, ya], W=[pY])
                    P.mm(pY[:, 256:384], vt[:, gi, :], ab[:, gi, :, RC:2 * RC], False, True, R=[vt, ab], W=[pY])
                    P.V("scalar_tensor_tensor", R=[pS, wtot, S.bs64], W=[ST], out=ST[:], in0=pS[:, 256:384], scalar=wtot[:, c:c + 1],
                        in1=S.bs64[:], op0=ALU.mult, op1=ALU.mult)
                    for hp in range(2):
                        lo = hp * 64
                        _tt(P, ysum[lo:lo + 64, c * RC:(c + 1) * RC], ysum[lo:lo + 64, c * RC:(c + 1) * RC],
                            pY[lo:lo + 64, 256 + lo:256 + lo + 64], ALU.add, [ysum, pY], [ysum])
                stages += [s1, s2, s3]
            return stages

        for f in pre_stages(0):
            f()
        for pi in range(len(pairs)):
            sq = seq_stages(pi)
            pr = pre_stages(pi + 1) if pi + 1 < len(pairs) else []
            n = max(len(sq), len(pr))
            for i in range(n):
                if i < len(pr):
                    pr[i]()
                if i < len(sq):
                    sq[i]()
    P.barrier()


def build(ncores_debug=False, nbc=NBC, phases=("mod", "inproj", "attn", "s5", "rwkv", "moe"), depth=DEPTH):
    nc = bass.Bass("TRN2", target_bir_lowering=False)
    P = Prog(nc, debug=ncores_debug)
    D = NS()
    S = NS()

    def ext(name, shape, dtype=F32):
        return P.dram(name, shape, dtype, kind="ExternalInput")

    D.xinT = ext("xinT", [nbc, DM, T])
    D.cT = ext("cT", [128, 8, 8])
    D.w_mod = ext("w_mod", [DEPTH, DM, 6 * DM])
    D.w_in = ext("w_in", [DEPTH, DM, INW])
    D.bmodT = ext("bmodT", [128, DEPTH, 48])
    D.norm1T = ext("norm1T", [128, DEPTH, 8])
    D.norm2T = ext("norm2T", [128, DEPTH, 8])
    D.qg = ext("qg", [128, DEPTH])
    D.kg = ext("kg", [128, DEPTH])
    for nm, shp in (("s5_A_rep_re", [128, DEPTH, 256]), ("s5_A_rep_im", [128, DEPTH, 256]), ("s5_ldt_rep", [128, DEPTH, 256]),
                    ("s5_Bt_re", [128, DEPTH, 256]), ("s5_Bt_im", [128, DEPTH, 256]), ("s5_A_st_re", [128, DEPTH, 16]),
                    ("s5_A_st_im", [128, DEPTH, 16]), ("s5_ldt_st", [128, DEPTH, 16]), ("s5_C_st_re", [128, DEPTH, 1024]),
                    ("s5_C_st_im", [128, DEPTH, 1024]), ("s5_dT", [128, DEPTH, 2]), ("s5_w_glu", [DEPTH, 256, 512])):
        setattr(D, nm, ext(nm, shp))
    D.os5 = P.dram("os5", [nbc, 256, T])
    for nm, shp in (("rw_conv", [128, DEPTH, 24]), ("rw_w0", [128, DEPTH, 4]), ("rw_a0", [128, DEPTH, 4]), ("rw_pv", [128, DEPTH, 10]),
                    ("rw_w2a2", [DEPTH, 2, 128, 256]), ("rw_g2", [DEPTH, 128, 256])):
        setattr(D, nm, ext(nm, shp))
    D.orw = P.dram("orw", [nbc, 256, T])
    D.x1T = P.dram("x1T", [nbc, DM, T])
    D.x2T = P.dram("x2T", [nbc, DM, T])
    D.Xe = P.dram("Xe", [NEXP * CAP, DM])
    D.Ye = P.dram("Ye", [NEXP * CAP, DM])
    for nm, shp in (("proj_att", [DEPTH, 512, DM]), ("proj_s5", [DEPTH, 256, DM]), ("proj_rwkv", [DEPTH, 256, DM]),
                    ("w_out", [DEPTH, DM, DM]), ("router_w", [DEPTH, DM, 36]), ("router_b", [128, DEPTH, 36]),
                    ("exp_w_gate", [DEPTH, NEXP, DM, DEXP]), ("exp_w_up", [DEPTH, NEXP, DM, DEXP]), ("exp_w_down", [DEPTH, NEXP, DEXP, DM])):
        setattr(D, nm, ext(nm, shp))
    consts = make_consts()
    D.consts = {k: ext("c_" + k, list(v.shape)) for k, v in consts.items()}
    D.out = P.dram("out", [nbc, DM, LAT], kind="ExternalOutput")
    D.uT = P.dram("uT", [nbc, INW, T])
    D.oatt = P.dram("oatt", [nbc, 512, T])

    S.psum = [P.ps(f"ps{i}", [128, 512]) for i in range(8)]
    for k in ("ident", "ones", "bd64", "bs64", "rot", "sh0", "sh1", "tri"):
        t = P.sb("k_" + k, [128, 128])
        P.dma("sp", R=[D.consts[k]], W=[t], out=t[:], in_=D.consts[k][:, :])
        setattr(S, k, t)
    S.maskG = P.sb("k_maskG", [128, 8])
    P.dma("sp", R=[D.consts["maskG"]], W=[S.maskG], out=S.maskG[:], in_=D.consts["maskG"][:, :])
    S.modT = P.sb("modT", [128, 48, 8])
    S.epsb = P.sb("epsb", [128, 1])
    P.V("memset", W=[S.epsb], ap=S.epsb[:], constant=EPS)
    for k, shp in (("bmodT", [128, DEPTH, 48]), ("norm1T", [128, DEPTH, 8]), ("norm2T", [128, DEPTH, 8])):
        t = P.sb(k, shp)
        P.dma("sp", R=[getattr(D, k)], W=[t], out=t[:], in_=getattr(D, k)[:, :, :])
        setattr(S, k, t)
    S.qg = []
    S.kg = []
    qgt = P.sb("qgt", [128, DEPTH])
    kgt = P.sb("kgt", [128, DEPTH])
    P.dma("sp", R=[D.qg], W=[qgt], out=qgt[:], in_=D.qg[:, :])
    P.dma("sp", R=[D.kg], W=[kgt], out=kgt[:], in_=D.kg[:, :])
    for l in range(DEPTH):
        a = NS.__new__(NS)
        S.qg.append(_ColView(qgt, l))
        S.kg.append(_ColView(kgt, l))

    xsrc = D.xinT
    for l in range(depth):
        need_ctx = l < DEPTH - 1
        if "mod" in phases:
            phase_mod(P, l, D, S)
        if "inproj" in phases:
            for b in range(nbc):
                phase_inproj(P, l, D, S, b, xsrc)
        if "attn" in phases:
            with contextlib.ExitStack() as lst:
                for k in ("cos", "sin"):
                    t = P.sb("k_" + k, [128, LAT], F32, lst)
                    P.dma("sp", R=[D.consts[k]], W=[t], out=t[:], in_=D.consts[k][:, :])
                    setattr(S, k, t)
                P.barrier()
                for b in range(nbc):
                    phase_attn(P, l, D, S, b, need_ctx)
        if "s5" in phases:
            with contextlib.ExitStack() as lst:
                X5 = s5_setup(P, l, D, S, lst)
                P.barrier()
                for b in range(nbc):
                    phase_s5(P, l, D, S, b, X5, need_ctx)
        if "rwkv" in phases:
            with contextlib.ExitStack() as lst:
                XR = rwkv_setup(P, l, D, S, lst)
                P.barrier()
                for b in range(nbc):
                    phase_rwkv(P, l, D, S, b, XR, need_ctx)
        if "rwkv0" in phases:
            with contextlib.ExitStack() as lst:
                z = P.sb("zrw", [128, T], F32, lst)
                P.V("memset", W=[z], ap=z[:], constant=0.0)
                for b in range(nbc):
                    for m in range(2):
                        P.dma("sp", R=[z], W=[D.orw.s(b)], out=D.orw[b, m * 128:(m + 1) * 128, :], in_=z[:])
            P.barrier()
        if "moe" in phases:
            with contextlib.ExitStack() as lst:
                G = NS()
                G.next = 0
                G.tiles = []
                G.slot = P.sb("g_slot", [128, 72, 2], U32, lst)
                G.gate = P.sb("g_gate", [128, 72, 2], F32, lst)
                with contextlib.ExitStack() as lst2:
                    XM = merge_setup(P, l, D, S, lst2)
                    P.barrier()
                    for b in range(nbc):
                        phase_merge(P, l, D, S, b, XM, need_ctx, xsrc, G)
                P.barrier()
                if "noexp" not in phases:
                    phase_experts(P, l, D, S)
                if "nocomb" not in phases:
                    phase_combine(P, l, D, S, G, D.x2T, l == DEPTH - 1)
            xsrc = D.x2T
    P.barrier()
    P.finish(getattr(P, "dbg_out", []))
    P.finish([D.out])
    P.finish([D.uT.s(b) for b in range(nbc)] + [D.oatt.s(b) for b in range(nbc)] + [D.os5.s(b) for b in range(nbc)] + [D.orw.s(b) for b in range(nbc)])
    info = (P.ninstr, P.nwait)
    P.close()
    return nc, consts, info


class _View:
    def __init__(self, tt, dt):
        self.tt = tt
        self.dt = dt
        self.tok = tt.tok

    def __getitem__(self, idx):
        return self.tt[idx].bitcast(self.dt)

    def s(self, k):
        return self.tt.s(k)


class _ColView:
    def __init__(self, tt, l):
        self.tt = tt
        self.l = l
        self.tok = tt.tok

    def __getitem__(self, idx):
        return self.tt[:, self.l:self.l + 1]


def _tok(x):
    return x.tok if hasattr(x, "tok") else x


def host_inputs(inp, core, nbc=NBC):
    b0 = core * nbc
    x = np.asarray(inp["x"][b0:b0 + nbc], np.float32)
    ctx = np.asarray(inp["ctx"][b0:b0 + nbc], np.float32)
    m = {}
    m["xinT"] = np.ascontiguousarray(np.concatenate([ctx, x], axis=1).transpose(0, 2, 1))
    call = np.zeros((8, DM), np.float32)
    call[:nbc] = inp["c"][b0:b0 + nbc]
    call[4] = inp["c_ctx"]
    m["cT"] = np.ascontiguousarray(call.reshape(8, 8, 128).transpose(2, 1, 0))
    m["w_mod"] = np.asarray(inp["w_mod"], np.float32)
    m["w_in"] = np.asarray(inp["w_in"], np.float32)
    m["bmodT"] = fm(inp["b_mod"])
    m["norm1T"] = fm(inp["norm1"])
    m["norm2T"] = fm(inp["norm2"])
    L = DEPTH
    f32 = lambda k: np.asarray(inp[k], np.float32)
    for nm, key in (("re", "s5_a_re"), ("im", "s5_a_im")):
        a = f32(key)
        m["s5_A_rep_" + nm] = np.ascontiguousarray(np.repeat(a.reshape(L, 2, 2, 8, 64).transpose(3, 0, 1, 2, 4), 16, axis=0).reshape(128, L, 256))
        m["s5_A_st_" + nm] = np.ascontiguousarray(a.reshape(L, 2, 8, 2, 64).transpose(3, 4, 0, 1, 2).reshape(128, L, 16))
    ldt = f32("s5_log_dt")
    r = np.repeat(ldt.reshape(L, 2, 2, 8).transpose(3, 0, 1, 2), 16, axis=0)
    m["s5_ldt_rep"] = np.ascontiguousarray(np.broadcast_to(r[..., None], (128, L, 2, 2, 64)).reshape(128, L, 256))
    r = ldt.reshape(L, 2, 8, 2).transpose(3, 0, 1, 2)
    m["s5_ldt_st"] = np.ascontiguousarray(np.repeat(r[:, None], 64, axis=1).reshape(128, L, 16))
    for nm, key in (("re", "s5_b_re"), ("im", "s5_b_im")):
        bb = f32(key)
        m["s5_Bt_" + nm] = np.ascontiguousarray(bb.reshape(L, 2, 2, 8, 64, 16).transpose(3, 5, 0, 1, 2, 4).reshape(128, L, 256))
    for nm, key in (("re", "s5_c_re"), ("im", "s5_c_im")):
        cc = f32(key).reshape(L, 2, 8, 2, 16, 64)
        o = np.zeros((2, 64, L, 2, 8, 2, 2, 16), np.float32)
        for e in range(2):
            for pr in range(8):
                o[e, :, :, :, pr, pr % 2, e, :] = cc[:, :, pr, e].transpose(3, 0, 1, 2)
        m["s5_C_st_" + nm] = np.ascontiguousarray(o.reshape(128, L, 1024))
    m["s5_dT"] = fm(inp["s5_d"])
    m["s5_w_glu"] = f32("s5_w_glu")
    m["rw_conv"] = np.ascontiguousarray(fm(inp["rwkv_conv"]).reshape(128, L, 24))
    m["rw_w0"] = np.ascontiguousarray(fm(inp["rwkv_w0"]).reshape(128, L, 4))
    m["rw_a0"] = np.ascontiguousarray(fm(inp["rwkv_a0"]).reshape(128, L, 4))
    pv = np.stack([fm(inp[k_]) for k_ in ("rwkv_k_k", "rwkv_k_a", "rwkv_r_k", "rwkv_ln_w", "rwkv_ln_b")], axis=2)
    m["rw_pv"] = np.ascontiguousarray(pv.reshape(128, L, 10))
    m["rw_w2a2"] = np.ascontiguousarray(np.concatenate([f32("rwkv_w2"), f32("rwkv_a2")], axis=2))
    m["rw_g2"] = f32("rwkv_g2")
    for k_ in ("proj_att", "proj_s5", "proj_rwkv", "w_out", "exp_w_gate", "exp_w_up", "exp_w_down"):
        m[k_] = f32(k_)
    m["router_w"] = np.ascontiguousarray(np.concatenate([f32("router_g_w"), f32("router_e_w")], axis=-1))
    rb = np.concatenate([f32("router_g_b"), f32("router_e_b")], axis=-1)
    m["router_b"] = np.ascontiguousarray(np.broadcast_to(rb[None], (128, L, 36)))
    m["qg"] = np.ascontiguousarray(np.tile(np.asarray(inp["q_gain"], np.float32), (1, 2)).T)
    m["kg"] = np.ascontiguousarray(np.tile(np.asarray(inp["k_gain"], np.float32), (1, 2)).T)
    return m


def kernel(**inp):
    nc, consts, info = build()
    in_maps = []
    for core in range(NCORES):
        m = host_inputs(inp, core)
        for k, v in consts.items():
            m["c_" + k] = v
        in_maps.append(m)
    res = run_bass_kernel_spmd(nc, in_maps, core_ids=list(range(NCORES)))
    outs = [r["out"] for r in res.results]
    o = np.concatenate(outs, axis=0)
    return np.ascontiguousarray(o.transpose(0, 2, 1)).astype(np.float32)
```

```python
import contextlib
import math
import numpy as np
import concourse.bass as bass
import concourse.mybir as mybir
from concourse.bass_utils import run_bass_kernel_spmd

F32 = mybir.dt.float32
BF16 = mybir.dt.bfloat16
F32R = mybir.dt.float32r
I32 = mybir.dt.int32
U32 = mybir.dt.uint32
ALU = mybir.AluOpType
AF = mybir.ActivationFunctionType
AX = mybir.AxisListType

NCORES = 8
NBC = 4
DM = 1024
LAT = 2048
CTX = 256
T = LAT + CTX
DEPTH = 2
INW = 5376
OFF_S5 = 1024
OFF_RW = 1280
OFF_GATE = 2304
EPS = 1e-6
NEXP = 32
DEXP = 512
CAP = 1536


class Tok:
    __slots__ = ("lw", "rd", "name")

    def __init__(self, name=""):
        self.lw = None
        self.rd = {}
        self.name = name


class TT:
    def __init__(self, t, name):
        self.t = t
        self.name = name
        self.tok = Tok(name)
        self.slots = {}

    def __getitem__(self, idx):
        return self.t[idx]

    def s(self, k):
        if k not in self.slots:
            self.slots[k] = Tok(f"{self.name}.{k}")
        return self.slots[k]


def _tok(x):
    return x.tok if isinstance(x, TT) else x


class Prog:
    NDMA = 6

    def __init__(self, nc, debug=False):
        self.nc = nc
        self.debug = debug
        self.es = contextlib.ExitStack()
        self.engs = {"pe": nc.tensor, "act": nc.scalar, "dve": nc.vector, "pool": nc.gpsimd, "sp": nc.sync}
        self.sems = []
        self.vals = []
        self.esem = {}
        for n in self.engs:
            self.esem[n] = self._newsem("e_" + n)
        self.dsem = {}
        self.dnext = {}
        for q in ("sp", "act", "pool"):
            self.dsem[q] = [self._newsem(f"d_{q}{i}") for i in range(self.NDMA)]
            self.dnext[q] = 0
        self.seen = {n: {} for n in self.engs}
        self.ninstr = 0
        self.nwait = 0
        self.uid = 0

    def _newsem(self, name):
        s = self.es.enter_context(self.nc.semaphore(name))
        self.sems.append(s)
        self.vals.append(0)
        return len(self.sems) - 1

    def sb(self, name, shape, dtype=F32, stack=None):
        self.uid += 1
        t = (stack if stack is not None else self.es).enter_context(
            self.nc.sbuf_tensor(f"{name}_{self.uid}", list(shape), dtype))
        return TT(t, name)

    def ps(self, name, shape, dtype=F32, stack=None):
        self.uid += 1
        t = (stack if stack is not None else self.es).enter_context(
            self.nc.psum_tensor(f"{name}_{self.uid}", list(shape), dtype))
        return TT(t, name)

    def dram(self, name, shape, dtype=F32, kind="Internal"):
        if kind == "Internal" and self.debug:
            kind = "ExternalOutput"
        t = self.nc.dram_tensor(name, list(shape), dtype, kind=kind)
        return TT(t.ap(), name)

    def _need(self, eng, ev):
        if ev is None:
            return
        s, v = ev
        if self.seen[eng].get(s, 0) >= v:
            return
        self.engs[eng].wait_ge(self.sems[s], v)
        self.seen[eng][s] = v
        self.nwait += 1

    def _deps(self, eng, R, W):
        for x in R:
            self._need(eng, _tok(x).lw)
        for x in W:
            tk = _tok(x)
            self._need(eng, tk.lw)
            for s, v in tk.rd.items():
                self._need(eng, (s, v))

    def _mark(self, ev, R, W):
        for x in R:
            _tok(x).rd[ev[0]] = ev[1]
        for x in W:
            tk = _tok(x)
            tk.lw = ev
            tk.rd = {}

    def op(self, eng, meth, R=(), W=(), **kw):
        self._deps(eng, R, W)
        ins = getattr(self.engs[eng], meth)(**kw)
        s = self.esem[eng]
        self.vals[s] += 1
        ins.then_inc(self.sems[s], 1)
        self._mark((s, self.vals[s]), R, W)
        self.ninstr += 1
        return ins

    def V(self, meth, **kw):
        return self.op("dve", meth, **kw)

    def A(self, meth, **kw):
        return self.op("act", meth, **kw)

    def G(self, meth, **kw):
        return self.op("pool", meth, **kw)

    def PE(self, meth, **kw):
        return self.op("pe", meth, **kw)

    def mm(self, out, lhsT, rhs, start, stop, R, W):
        return self.op("pe", "matmul", R=R, W=W, out=out, lhsT=lhsT, rhs=rhs, start=start, stop=stop)

    def dma(self, q, R=(), W=(), meth="dma_start", **kw):
        self._deps(q, R, W)
        k = self.dnext[q]
        self.dnext[q] = (k + 1) % self.NDMA
        s = self.dsem[q][k]
        if self.vals[s] > 0:
            self._need(q, (s, self.vals[s]))
        ins = getattr(self.engs[q], meth)(**kw)
        self.vals[s] += 16
        ins.then_inc(self.sems[s], 16)
        self._mark((s, self.vals[s]), R, W)
        self.ninstr += 1
        return ins

    def barrier(self):
        for e in self.engs:
            for s in range(len(self.sems)):
                if self.vals[s] > 0:
                    self._need(e, (s, self.vals[s]))

    def finish(self, toks):
        for x in toks:
            self._need("sp", _tok(x).lw)

    def close(self):
        self.es.close()


class NS:
    pass


def fm(v):
    v = np.asarray(v, np.float32)
    lead = v.shape[:-1]
    n = v.shape[-1] // 128
    r = v.reshape(lead + (n, 128))
    return np.ascontiguousarray(np.moveaxis(r, -1, 0))


def make_consts():
    c = {}
    c["ident"] = np.eye(128, dtype=np.float32)
    c["ones"] = np.ones((128, 128), np.float32)
    bd = np.zeros((128, 128), np.float32)
    bd[:64, :64] = 1.0 / 64
    bd[64:, 64:] = 1.0 / 64
    c["bd64"] = bd
    bd1 = np.zeros((128, 128), np.float32)
    bd1[:64, :64] = 1.0
    bd1[64:, 64:] = 1.0
    c["bs64"] = bd1
    rot = np.zeros((128, 128), np.float32)
    for hh in range(2):
        for blk in range(2):
            base = hh * 64 + blk * 32
            for i in range(16):
                rot[base + 16 + i, base + i] = -1.0
                rot[base + i, base + 16 + i] = 1.0
    c["rot"] = rot
    pos = np.arange(LAT)
    row = pos // 64
    col = pos % 64
    inv = 10000.0 ** (-np.arange(16, dtype=np.float32) / 16.0)
    cos = np.zeros((64, LAT), np.float32)
    sin = np.zeros((64, LAT), np.float32)
    for blk, pp in enumerate((row, col)):
        ang = pp[None, :].astype(np.float32) * inv[:, None]
        cc = np.cos(ang).astype(np.float32)
        ss = np.sin(ang).astype(np.float32)
        cos[blk * 32:blk * 32 + 16] = cc
        cos[blk * 32 + 16:blk * 32 + 32] = cc
        sin[blk * 32:blk * 32 + 16] = ss
        sin[blk * 32 + 16:blk * 32 + 32] = ss
    c["cos"] = np.concatenate([cos, cos], 0)
    c["sin"] = np.concatenate([sin, sin], 0)
    sh0 = np.zeros((128, 128), np.float32)
    sh1 = np.zeros((128, 128), np.float32)
    for j in range(64):
        sh0[64 + j, j] = 1.0
        sh1[j, 64 + j] = 1.0
    c["sh0"] = sh0
    c["sh1"] = sh1
    ss = np.arange(64)[:, None]
    tt = np.arange(64)[None, :]
    for d in range(2):
        before = (ss < tt) if d == 0 else (ss > tt)
        beq = (ss <= tt) if d == 0 else (ss >= tt)
        st_ = before.astype(np.float32)
        inc = beq.astype(np.float32)
        c[f"MA{d}"] = np.tile(np.concatenate([-st_, -inc], 1), (1, 4))
        c[f"MB{d}"] = np.tile(np.concatenate([st_, inc], 1), (1, 4))
        c[f"MC{d}"] = np.tile(-(st_.T), (1, 4))
    c["YXI"] = np.tile(np.concatenate([np.zeros((64, 64), np.float32), np.eye(64, dtype=np.float32)], 1), (1, 4))
    rm = np.ones((128, T), np.float32)
    rm[:, ::64] = 0.0
    c["rmask"] = rm
    c["eoff"] = np.tile((np.arange(NEXP, dtype=np.float32) * CAP)[None, :], (128, 1))
    mg = np.zeros((128, 8), np.float32)
    for p in range(128):
        mg[p, p // 16] = 1.0
    c["maskG"] = mg
    tri = np.zeros((128, 128), np.float32)
    for k in range(128):
        tri[k, k + 1:] = 1.0
    c["tri"] = tri
    return c


def phase_mod(P, l, D, S):
    modT = S.modT
    with contextlib.ExitStack() as st:
        wm = [P.sb(f"wm{i}", [128, 8, 1024], F32, st) for i in range(2)]
        ct = P.sb("ct", [128, 8, 8], F32, st)
        sct = P.sb("sct", [128, 8, 8], F32, st)
        psA = S.psum[0]
        P.dma("sp", R=[D.cT], W=[ct], out=ct[:], in_=D.cT[:, :, :])
        P.A("activation", R=[ct], W=[sct], out=sct[:], in_=ct[:], func=AF.Silu)
        for j in range(6):
            w = wm[j % 2]
            P.dma("sp", R=[D.w_mod], W=[w], out=w[:],
                  in_=D.w_mod[l, :, j * 1024:(j + 1) * 1024].rearrange("(kt p) n -> p kt n", p=128))
            for m in range(8):
                o = (j * 8 + m) * 8
                for kt in range(8):
                    P.mm(psA[:, o:o + 8], w[:, kt, m * 128:(m + 1) * 128], sct[:, kt, :], kt == 0, kt == 7,
                         R=[w, sct], W=[psA])
        P.V("tensor_tensor", R=[psA, S.bmodT], W=[modT],
            out=modT[:], in0=psA[:, 0:384].rearrange("p (a b) -> p a b", b=8),
            in1=S.bmodT[:, l, :].unsqueeze(2).to_broadcast([128, 48, 8]), op=ALU.add)
        for j, nt in ((1, S.norm1T), (4, S.norm2T)):
            P.V("tensor_scalar", R=[modT], W=[modT], out=modT[:, j * 8:(j + 1) * 8, :], in0=modT[:, j * 8:(j + 1) * 8, :],
                scalar1=1.0, scalar2=None, op0=ALU.add)
            P.V("tensor_tensor", R=[modT, nt], W=[modT], out=modT[:, j * 8:(j + 1) * 8, :],
                in0=modT[:, j * 8:(j + 1) * 8, :],
                in1=nt[:, l, :].unsqueeze(2).to_broadcast([128, 8, 8]), op=ALU.mult)
    P.barrier()


TILES = [(0, 256)] + [(256 + i * 512, 512) for i in range(4)]


def rms_mod(P, S, x_t, n, col, jsc, jsh, out_fn, sq, rstd, ps):
    P.A("activation", R=[x_t], W=[sq], out=sq[:, :, 0:n], in_=x_t[:, :, 0:n], func=AF.Square)
    for kt in range(8):
        P.mm(ps[:, 0:n], S.ones[:], sq[:, kt, 0:n], kt == 0, kt == 7, R=[S.ones, sq], W=[ps])
    P.A("activation", R=[ps], W=[rstd], out=rstd[:, 0:n], in_=ps[:, 0:n], func=AF.Sqrt, scale=1.0 / DM,
        bias=S.epsb[:, 0:1])
    P.V("reciprocal", R=[rstd], W=[rstd], out=rstd[:, 0:n], in_=rstd[:, 0:n])
    for kt in range(8):
        P.V("tensor_tensor", R=[x_t, rstd], W=[sq], out=sq[:, kt, 0:n], in0=x_t[:, kt, 0:n], in1=rstd[:, 0:n],
            op=ALU.mult)
        o, wt = out_fn(kt)
        P.V("tensor_scalar", R=[sq, S.modT], W=wt, out=o, in0=sq[:, kt, 0:n],
            scalar1=S.modT[:, jsc * 8 + kt, col:col + 1], scalar2=S.modT[:, jsh * 8 + kt, col:col + 1],
            op0=ALU.mult, op1=ALU.add)


def phase_inproj(P, l, D, S, b, xsrc):
    with contextlib.ExitStack() as st:
        hT = P.sb("hT", [128, 8, T], BF16, st)
        xt = [P.sb(f"xt{i}", [128, 8, 512], F32, st) for i in range(2)]
        sq = P.sb("sq", [128, 8, 512], F32, st)
        rstd = P.sb("rstd", [128, 512], F32, st)
        wb = [P.sb(f"wb{i}", [128, 8, 768], BF16, st) for i in range(2)]
        stg = [P.sb(f"stg{i}", [128, T], F32, st) for i in range(2)]
        for ti, (t0, n) in enumerate(TILES):
            col = 4 if t0 < CTX else b
            x_t = xt[ti % 2]
            P.dma("sp", R=[xsrc], W=[x_t], out=x_t[:, :, 0:n],
                  in_=xsrc[b, :, t0:t0 + n].rearrange("(kt p) t -> p kt t", p=128))
            rms_mod(P, S, x_t, n, col, 1, 0, lambda kt: (hT[:, kt, t0:t0 + n], [hT.s(ti)]), sq, rstd, S.psum[0])
        hall = [hT.s(ti) for ti in range(len(TILES))]
        ci = 0
        for cb in range(7):
            w = wb[cb % 2]
            P.dma("pool", R=[D.w_in], W=[w], out=w[:],
                  in_=D.w_in[l, :, cb * 768:(cb + 1) * 768].rearrange("(kt p) n -> p kt n", p=128))
            for m in range(6):
                chunk = cb * 6 + m
                sg = stg[chunk % 2]
                for ti, (t0, n) in enumerate(TILES):
                    ps = S.psum[1 + (ci % 4)]
                    ci += 1
                    for kt in range(8):
                        P.mm(ps[:, 0:n], w[:, kt, m * 128:(m + 1) * 128], hT[:, kt, t0:t0 + n], kt == 0, kt == 7,
                             R=[w, hT.s(ti)], W=[ps])
                    if chunk * 128 >= OFF_GATE:
                        P.A("activation", R=[ps], W=[sg], out=sg[:, t0:t0 + n], in_=ps[:, 0:n], func=AF.Sigmoid)
                    elif ci % 2 == 0:
                        P.A("activation", R=[ps], W=[sg], out=sg[:, t0:t0 + n], in_=ps[:, 0:n], func=AF.Identity)
                    else:
                        P.V("tensor_copy", R=[ps], W=[sg], out=sg[:, t0:t0 + n], in_=ps[:, 0:n])
                P.dma("sp", R=[sg], W=[D.uT.s(b)], out=D.uT[b, chunk * 128:(chunk + 1) * 128, :], in_=sg[:])
    P.barrier()


def qk_norm_rope(P, S, load_fn, dst, nt, gain, stg, tmps):
    tmp, tmp2 = tmps
    for i in range(nt):
        sg = stg.s(i % 2)
        load_fn(i, i % 2)
        for ti, (t0, n) in enumerate(TILES):
            ps = S.psum[0]
            x = stg[:, i % 2, t0:t0 + n]
            d_ = dst[:, i, t0:t0 + n]
            P.A("activation", R=[sg], W=[tmp], out=tmp[:, 0:n], in_=x, func=AF.Square)
            P.mm(ps[:, 0:n], S.bd64[:], tmp[:, 0:n], True, True, R=[S.bd64, tmp], W=[ps])
            P.A("activation", R=[ps], W=[tmp], out=tmp[:, 0:n], in_=ps[:, 0:n], func=AF.Sqrt, scale=1.0,
                bias=S.epsb[:, 0:1])
            P.V("reciprocal", R=[tmp], W=[tmp], out=tmp[:, 0:n], in_=tmp[:, 0:n])
            if t0 < CTX:
                P.V("scalar_tensor_tensor", R=[sg, tmp, gain], W=[dst.s(i)], out=d_, in0=x, scalar=gain[:, 0:1], in1=tmp[:, 0:n],
                    op0=ALU.mult, op1=ALU.mult)
            else:
                P.V("scalar_tensor_tensor", R=[sg, tmp, gain], W=[sg], out=x, in0=x, scalar=gain[:, 0:1], in1=tmp[:, 0:n],
                    op0=ALU.mult, op1=ALU.mult)
                p0 = t0 - CTX
                ps2 = S.psum[1]
                P.mm(ps2[:, 0:n], S.rot[:], x, True, True, R=[S.rot, sg], W=[ps2])
                P.V("tensor_tensor", R=[ps2, S.sin], W=[tmp2], out=tmp2[:, 0:n], in0=ps2[:, 0:n],
                    in1=S.sin[:, p0:p0 + n], op=ALU.mult)
                P.V("tensor_tensor", R=[sg, S.cos], W=[tmp], out=tmp[:, 0:n], in0=x,
                    in1=S.cos[:, p0:p0 + n], op=ALU.mult)
                P.V("tensor_tensor", R=[tmp, tmp2], W=[dst.s(i)], out=d_, in0=tmp[:, 0:n],
                    in1=tmp2[:, 0:n], op=ALU.add)


def phase_attn(P, l, D, S, b, need_ctx):
    with contextlib.ExitStack() as st:
        qT = P.sb("qT", [128, 4, T], F32R, st)
        kTa = P.sb("kTa", [128, 2, T], F32R, st)
        kTb = P.sb("kTb", [128, 2, T], F32R, st)
        vaug = P.sb("vaug", [128, 18, 4, 192], BF16, st)
        oT = P.sb("oT", [128, 4, T], F32, st)
        pT = [P.sb(f"pT{i}", [128, 512], BF16, st) for i in range(3)]
        oacc = P.sb("oacc", [128, 512], F32, st)
        rden = P.sb("rden", [128, 512], F32, st)
        with contextlib.ExitStack() as st2:
            vT = P.sb("vT", [128, 2, T], F32, st2)
            stg = P.sb("qkstg", [128, 2, T], F32, st2)
            for i in range(2):
                P.dma("sp", R=[D.uT.s(b)], W=[vT.s(i)], out=vT[:, i, :], in_=D.uT[b, 768 + i * 128:768 + (i + 1) * 128, :])
            P.G("memset", W=[vaug], ap=vaug[:], constant=1.0)

            def ld_q(i, k):
                P.dma("sp", R=[D.uT.s(b)], W=[stg.s(k)], out=stg[:, k, :], in_=D.uT[b, i * 128:(i + 1) * 128, :])

            def ld_ka(i, k):
                P.dma("sp", R=[D.uT.s(b)], W=[stg.s(k)], out=stg[:, k, :], in_=D.uT[b, 512 + i * 128:512 + (i + 1) * 128, :])

            def ld_kb(i, k):
                P.dma("sp", R=[D.uT.s(b)], W=[stg.s(k)], out=stg[0:64, k, :], in_=D.uT[b, 512 + i * 128 + 64:512 + (i + 1) * 128, :])
                P.dma("sp", R=[D.uT.s(b)], W=[stg.s(k)], out=stg[64:128, k, :], in_=D.uT[b, 512 + i * 128:512 + i * 128 + 64, :])

            tmps = (P.sb("qk_tmp", [128, 512], F32, st2), P.sb("qk_tmp2", [128, 512], F32, st2))
            qk_norm_rope(P, S, ld_q, qT, 4, S.qg[l], stg, tmps)
            qk_norm_rope(P, S, ld_ka, kTa, 2, S.kg[l], stg, tmps)
            qk_norm_rope(P, S, ld_kb, kTb, 2, S.kg[l], stg, tmps)
            cnt = 0
            for i in range(2):
                for kt in range(18):
                    ps = S.psum[2 + cnt % 2]
                    cnt += 1
                    P.PE("transpose", R=[vT.s(i), S.ident], W=[ps], out=ps[:, 0:128], in_=vT[:, i, kt * 128:(kt + 1) * 128],
                         identity=S.ident[:])
                    for e in range(2):
                        P.V("tensor_copy", R=[ps], W=[vaug], out=vaug[:, kt, 2 * i + e, 64:128],
                            in_=ps[:, e * 64:(e + 1) * 64])
        P.barrier()
        qsegs = [(CTX + qc * 512, 512, list(range(18))) for qc in range(4)]
        if need_ctx:
            qsegs.append((0, CTX, [0, 1]))
        gi_ = 0
        for h in range(4):
            i, e = h // 2, h % 2
            for g in range(2):
                ksrc = kTa if e == g else kTb
                shm = S.sh0 if g == 0 else S.sh1
                lo = g * 64
                for (q0, qn, kts) in qsegs:
                    acc = S.psum[4 + (gi_ % 2)]
                    gi_ += 1
                    va_of = (lambda kt: vaug[:, kt, h, 64:192]) if g == 0 else (lambda kt: vaug[:, kt, h, 0:128])

                    def score(ki):
                        kt = kts[ki]
                        pss = S.psum[6 + (ki % 2)]
                        P.mm(pss[:, 0:qn], ksrc[lo:lo + 64, i, kt * 128:(kt + 1) * 128], qT[lo:lo + 64, h, q0:q0 + qn],
                             True, True, R=[ksrc.s(i), qT.s(h)], W=[pss])

                    score(0)
                    for ki, kt in enumerate(kts):
                        pss = S.psum[6 + (ki % 2)]
                        pt = pT[ki % 3]
                        P.A("activation", R=[pss], W=[pt], out=pt[:, 0:qn], in_=pss[:, 0:qn], func=AF.Exp, scale=0.125)
                        if ki + 1 < len(kts):
                            score(ki + 1)
                        P.mm(acc[:, 0:qn], va_of(kt), pt[:, 0:qn], ki == 0, ki == len(kts) - 1, R=[vaug, pt], W=[acc])
                    P.A("activation", R=[acc], W=[oacc], out=oacc[:, 0:qn], in_=acc[:, 0:qn], func=AF.Identity)
                    psd = S.psum[0]
                    P.mm(psd[:, 0:qn], shm[:], oacc[:, 0:qn], True, True, R=[shm, oacc], W=[psd])
                    P.V("reciprocal", R=[psd], W=[rden], out=rden[lo:lo + 64, 0:qn], in_=psd[lo:lo + 64, 0:qn])
                    P.V("tensor_tensor", R=[oacc, rden], W=[oT.s(h)], out=oT[lo:lo + 64, h, q0:q0 + qn],
                        in0=oacc[lo:lo + 64, 0:qn], in1=rden[lo:lo + 64, 0:qn], op=ALU.mult)
        c0 = 0 if need_ctx else CTX
        for h in range(4):
            P.dma("sp", R=[oT.s(h)], W=[D.oatt.s(b)], out=D.oatt[b, h * 128:(h + 1) * 128, c0:T], in_=oT[:, h, c0:T])
    P.barrier()


TWO_PI = 2.0 * math.pi


def _tt(P, out, a, b, op, R, W):
    P.V("tensor_tensor", R=R, W=W, out=out, in0=a, in1=b, op=op)


def _ts(P, out, a, s1, op0, R, W, s2=None, op1=None):
    if op1 is None:
        P.V("tensor_scalar", R=R, W=W, out=out, in0=a, scalar1=s1, scalar2=None, op0=op0)
    else:
        P.V("tensor_scalar", R=R, W=W, out=out, in0=a, scalar1=s1, scalar2=s2, op0=op0, op1=op1)


def sin_turns(P, st, r, out, F, name):
    ki = P.sb(name + "_ki", [128, F], I32, st)
    kf = P.sb(name + "_kf", [128, F], F32, st)
    m = P.sb(name + "_m", [128, F], F32, st)
    P.V("tensor_copy", R=[r], W=[ki], out=ki[:], in_=r[:])
    P.V("tensor_copy", R=[ki], W=[kf], out=kf[:], in_=ki[:])
    _tt(P, kf[:], r[:], kf[:], ALU.subtract, [r, kf], [kf])
    _ts(P, m[:], kf[:], 0.5, ALU.is_gt, [kf], [m])
    _tt(P, kf[:], kf[:], m[:], ALU.subtract, [kf, m], [kf])
    _ts(P, m[:], kf[:], -0.5, ALU.is_lt, [kf], [m])
    _tt(P, kf[:], kf[:], m[:], ALU.add, [kf, m], [kf])
    P.A("activation", R=[kf], W=[out], out=out[:], in_=kf[:], func=AF.Sin, scale=TWO_PI)


def s5_derive(P, st, are, aim, ldt, F, name, need_z):
    mk = lambda n: P.sb(f"{name}_{n}", [128, F], F32, st)
    dt, rho, th, r, r2, cth, sth = mk("dt"), mk("rho"), mk("th"), mk("r"), mk("r2"), mk("cth"), mk("sth")
    P.A("activation", R=[ldt], W=[dt], out=dt[:], in_=ldt[:], func=AF.Exp)
    _tt(P, rho[:], are[:], dt[:], ALU.mult, [are, dt], [rho])
    P.A("activation", R=[rho], W=[rho], out=rho[:], in_=rho[:], func=AF.Exp)
    _tt(P, th[:], aim[:], dt[:], ALU.mult, [aim, dt], [th])
    _ts(P, r[:], th[:], 1.0 / TWO_PI, ALU.mult, [th], [r])
    _ts(P, r2[:], r[:], 0.25, ALU.add, [r], [r2])
    sin_turns(P, st, r, sth, F, name + "_s")
    sin_turns(P, st, r2, cth, F, name + "_c")
    res = dict(rho=rho, cth=cth, sth=sth)
    if need_z:
        abr, abi, den, nr, zre, zim, t1 = mk("abr"), mk("abi"), mk("den"), mk("nr"), mk("zre"), mk("zim"), mk("t1")
        _tt(P, abr[:], rho[:], cth[:], ALU.mult, [rho, cth], [abr])
        _tt(P, abi[:], rho[:], sth[:], ALU.mult, [rho, sth], [abi])
        _tt(P, den[:], are[:], are[:], ALU.mult, [are], [den])
        _tt(P, t1[:], aim[:], aim[:], ALU.mult, [aim], [t1])
        _tt(P, den[:], den[:], t1[:], ALU.add, [den, t1], [den])
        P.V("reciprocal", R=[den], W=[den], out=den[:], in_=den[:])
        _ts(P, nr[:], abr[:], -1.0, ALU.add, [abr], [nr])
        _tt(P, zre[:], nr[:], are[:], ALU.mult, [nr, are], [zre])
        _tt(P, t1[:], abi[:], aim[:], ALU.mult, [abi, aim], [t1])
        _tt(P, zre[:], zre[:], t1[:], ALU.add, [zre, t1], [zre])
        _tt(P, zre[:], zre[:], den[:], ALU.mult, [zre, den], [zre])
        _tt(P, zim[:], abi[:], are[:], ALU.mult, [abi, are], [zim])
        _tt(P, t1[:], nr[:], aim[:], ALU.mult, [nr, aim], [t1])
        _tt(P, zim[:], zim[:], t1[:], ALU.subtract, [zim, t1], [zim])
        _tt(P, zim[:], zim[:], den[:], ALU.mult, [zim, den], [zim])
        res.update(zre=zre, zim=zim)
    return res


def s5_setup(P, l, D, S, st):
    X = NS()
    ld = lambda nm, src, shp, dt=F32: _load(P, st, nm, src, shp, dt)
    are = ld("s5are", D.s5_A_rep_re[:, l, :], [128, 256])
    aim = ld("s5aim", D.s5_A_rep_im[:, l, :], [128, 256])
    ldt = ld("s5ldt", D.s5_ldt_rep[:, l, :], [128, 256])
    bre = ld("s5bre", D.s5_Bt_re[:, l, :], [128, 256])
    bim = ld("s5bim", D.s5_Bt_im[:, l, :], [128, 256])
    X.BTr = P.sb("BTr", [128, 4, 8, 64], BF16, st)
    X.BTi = P.sb("BTi", [128, 4, 8, 64], BF16, st)
    with contextlib.ExitStack() as st2:
        r = s5_derive(P, st2, are, aim, ldt, 256, "dr", True)
        bbr = P.sb("bbr", [128, 256], F32, st2)
        bbi = P.sb("bbi", [128, 256], F32, st2)
        t1 = P.sb("bt1", [128, 256], F32, st2)
        _tt(P, bbr[:], r["zre"][:], bre[:], ALU.mult, [r["zre"], bre], [bbr])
        _tt(P, t1[:], r["zim"][:], bim[:], ALU.mult, [r["zim"], bim], [t1])
        _tt(P, bbr[:], bbr[:], t1[:], ALU.subtract, [bbr, t1], [bbr])
        _tt(P, bbi[:], r["zre"][:], bim[:], ALU.mult, [r["zre"], bim], [bbi])
        _tt(P, t1[:], r["zim"][:], bre[:], ALU.mult, [r["zim"], bre], [t1])
        _tt(P, bbi[:], bbi[:], t1[:], ALU.add, [bbi, t1], [bbi])
        for dst, src in ((X.BTr, bbr), (X.BTi, bbi)):
            for q in range(4):
                P.V("tensor_tensor", R=[src, S.maskG], W=[dst], out=dst[:, q, :, :],
                    in0=src[:, q * 64:(q + 1) * 64].unsqueeze(1).to_broadcast([128, 8, 64]),
                    in1=S.maskG[:, :].unsqueeze(2).to_broadcast([128, 8, 64]), op=ALU.mult)
    P.barrier()
    sare = ld("s5sare", D.s5_A_st_re[:, l, :], [128, 16])
    saim = ld("s5saim", D.s5_A_st_im[:, l, :], [128, 16])
    sldt = ld("s5sldt", D.s5_ldt_st[:, l, :], [128, 16])
    dbg(P, "sare", sare, sare[:], [128, 16])
    dbg(P, "sldt", sldt, sldt[:], [128, 16])
    r2 = s5_derive(P, st, sare, saim, sldt, 16, "ds", False)
    X.rho = r2["rho"]
    dbg(P, "rho", X.rho, X.rho[:], [128, 16])
    dbg(P, "cth", r2["cth"], r2["cth"][:], [128, 16])
    dbg(P, "sth", r2["sth"], r2["sth"][:], [128, 16])
    X.cosT = P.sb("s5cos", [128, 16, 256], F32, st)
    X.sinT = P.sb("s5sin", [128, 16, 256], F32, st)
    nsin = P.sb("s5nsin", [128, 16, 256], F32, st)
    X.nsinT = nsin
    P.V("memset", W=[X.cosT], ap=X.cosT[:, :, 0:1], constant=1.0)
    P.V("memset", W=[X.sinT], ap=X.sinT[:, :, 0:1], constant=0.0)
    P.V("tensor_copy", R=[r2["cth"]], W=[X.cosT], out=X.cosT[:, :, 1], in_=r2["cth"][:])
    P.V("tensor_copy", R=[r2["sth"]], W=[X.sinT], out=X.sinT[:, :, 1], in_=r2["sth"][:])
    tmpa = P.sb("s5tmpa", [128, 16, 128], F32, st)
    m = 2
    while m < 256:
        cm, sm = X.cosT[:, :, m], X.sinT[:, :, m]
        c1, s1 = X.cosT[:, :, 1], X.sinT[:, :, 1]
        cp, sp_ = X.cosT[:, :, m - 1], X.sinT[:, :, m - 1]
        ta = tmpa[:, :, 0]
        tb = tmpa[:, :, 1]
        _tt(P, ta, cp, c1, ALU.mult, [X.cosT], [tmpa])
        _tt(P, tb, sp_, s1, ALU.mult, [X.sinT], [tmpa])
        _tt(P, cm, ta, tb, ALU.subtract, [tmpa], [X.cosT])
        _tt(P, ta, cp, s1, ALU.mult, [X.cosT, X.sinT], [tmpa])
        _tt(P, tb, sp_, c1, ALU.mult, [X.cosT, X.sinT], [tmpa])
        _tt(P, sm, ta, tb, ALU.add, [tmpa], [X.sinT])
        n = m - 1
        cmb = X.cosT[:, :, m:m + 1].to_broadcast([128, 16, n])
        smb = X.sinT[:, :, m:m + 1].to_broadcast([128, 16, n])
        cj, sj = X.cosT[:, :, 1:m], X.sinT[:, :, 1:m]
        co, so = X.cosT[:, :, m + 1:2 * m], X.sinT[:, :, m + 1:2 * m]
        ta = tmpa[:, :, 0:n]
        _tt(P, ta, sj, smb, ALU.mult, [X.sinT], [tmpa])
        _tt(P, co, cj, cmb, ALU.mult, [X.cosT], [X.cosT])
        _tt(P, co, co, ta, ALU.subtract, [X.cosT, tmpa], [X.cosT])
        _tt(P, ta, cj, smb, ALU.mult, [X.cosT, X.sinT], [tmpa])
        _tt(P, so, sj, cmb, ALU.mult, [X.sinT, X.cosT], [X.sinT])
        _tt(P, so, so, ta, ALU.add, [X.sinT, tmpa], [X.sinT])
        m *= 2
    _ts(P, nsin[:], X.sinT[:], -1.0, ALU.mult, [X.sinT], [nsin])
    dbg(P, "cosT", X.cosT, X.cosT[:], [128, 16, 256])
    dbg(P, "sinT", X.sinT, X.sinT[:], [128, 16, 256])
    dbg(P, "BTr", X.BTr, X.BTr[:], [128, 4, 8, 64], BF16)
    X.rhoT = P.sb("s5rhoT", [128, 16, 256], F32, st)
    P.V("tensor_copy", R=[X.rho], W=[X.rhoT], out=X.rhoT[:], in_=X.rho[:].unsqueeze(2).to_broadcast([128, 16, 256]))
    cre = ld("s5cre", D.s5_C_st_re[:, l, :], [128, 16 * 64])
    cim = ld("s5cim", D.s5_C_st_im[:, l, :], [128, 16 * 64])
    X.Cr = P.sb("s5Cr", [128, 16, 64], BF16, st)
    X.Ci = P.sb("s5Ci", [128, 16, 64], BF16, st)
    P.V("tensor_copy", R=[cre], W=[X.Cr], out=X.Cr[:], in_=cre[:].rearrange("p (a b) -> p a b", b=64))
    _ts(P, X.Ci[:], cim[:].rearrange("p (a b) -> p a b", b=64), -1.0, ALU.mult, [cim], [X.Ci])
    X.dT = ld("s5dT", D.s5_dT[:, l, :], [128, 2])
    X.wglu = P.sb("s5wglu", [128, 2, 512], BF16, st)
    P.dma("pool", R=[D.s5_w_glu], W=[X.wglu], out=X.wglu[:], in_=D.s5_w_glu[l].rearrange("(kt p) n -> p kt n", p=128))
    return X


def dbg(P, name, tt, ap, shape, dt=F32):
    if not P.debug:
        return
    d = P.dram("dbg_" + name, shape, dt, kind="ExternalOutput")
    P.dma("sp", R=[tt], W=[d], out=d[tuple(slice(None) for _ in shape)], in_=ap)
    P.dbg_out = getattr(P, "dbg_out", []) + [d]


def _load(P, st, nm, src_ap, shp, dt=F32):
    t = P.sb(nm, shp, dt, st)
    P.dma("sp", R=[], W=[t], out=t[:], in_=src_ap)
    return t


S5CH = 256


def phase_s5(P, l, D, S, b, X, need_ctx):
    nch = T // S5CH
    with contextlib.ExitStack() as st:
        sT = P.sb("s5sT", [128, 2, T], F32, st)
        sT16 = P.sb("s5sT16", [128, 2, T], BF16, st)
        yacc = P.sb("s5yacc", [128, 2, T], F32, st)
        for ft in range(2):
            P.dma("sp", R=[D.uT.s(b)], W=[sT], out=sT[:, ft, :], in_=D.uT[b, OFF_S5 + ft * 128:OFF_S5 + (ft + 1) * 128, :])
        P.A("activation", R=[sT], W=[sT16], out=sT16[:], in_=sT[:], func=AF.Identity)
        for ft in range(2):
            _ts(P, yacc[:, ft, :], sT[:, ft, :], X.dT[:, ft:ft + 1], ALU.mult, [sT, X.dT], [yacc.s((ft, 0)), yacc.s((ft, 1))])
        NCHAIN = 4
        mk = lambda n, dt=F32: [P.sb(f"s5{n}{i}", [128, S5CH], dt, st) for i in range(2 * NCHAIN)]
        xr_re, xr_im, g_re, g_im = mk("xrr"), mk("xri"), mk("gr"), mk("gi")
        h_re, h_im = mk("hr", BF16), mk("hi", BF16)
        hps = [P.sb(f"s5hp{i}", [128, 4], F32, st) for i in range(NCHAIN)]
        tAs = [P.sb(f"s5tA{i}", [128, S5CH], F32, st) for i in range(NCHAIN)]
        tBs = [P.sb(f"s5tB{i}", [128, S5CH], F32, st) for i in range(NCHAIN)]
        tCs = [P.sb(f"s5tC{i}", [128, S5CH], F32, st) for i in range(NCHAIN)]

        def chain(ci, d, pr):
            ft, pp = pr // 4, pr % 4
            dp = d * 8 + pr
            hp, tA, tB, tC = hps[ci], tAs[ci], tBs[ci], tCs[ci]
            cT_, sT_, nsT_ = X.cosT[:, dp, :], X.sinT[:, dp, :], X.nsinT[:, dp, :]
            order = list(range(nch)) if d == 0 else [0] + list(range(nch - 1, 0, -1))
            rv = (lambda ap: ap) if d == 0 else (lambda ap: ap[:, ::-1])
            ytok = yacc.s((ft, pp // 2))
            for oi, ch in enumerate(order):
                c0 = ch * S5CH
                k = ci * 2 + (oi % 2)
                ps_r, ps_i, ps_yb = S.psum[2 * ci], S.psum[2 * ci + 1], S.psum[2 * ci]
                lr = X.BTr[:, d * 2 + ft, 2 * pp:2 * pp + 2, :].rearrange("p a b -> p (a b)")
                li = X.BTi[:, d * 2 + ft, 2 * pp:2 * pp + 2, :].rearrange("p a b -> p (a b)")
                P.mm(ps_r[:, 0:S5CH], lr, sT16[:, ft, c0:c0 + S5CH], True, True, R=[X.BTr, sT16], W=[ps_r])
                P.mm(ps_i[:, 0:S5CH], li, sT16[:, ft, c0:c0 + S5CH], True, True, R=[X.BTi, sT16], W=[ps_i])
                yield
                xrr, xri, gr, gi, hr, hi = xr_re[k], xr_im[k], g_re[k], g_im[k], h_re[k], h_im[k]
                pr_, pi_ = ps_r[:, 0:S5CH], ps_i[:, 0:S5CH]
                _tt(P, rv(tA[:]), pr_, rv(cT_), ALU.mult, [ps_r, X.cosT], [tA])
                yield
                _tt(P, rv(tB[:]), pi_, rv(sT_), ALU.mult, [ps_i, X.sinT], [tB])
                yield
                _tt(P, xrr[:], tA[:], tB[:], ALU.add, [tA, tB], [xrr])
                yield
                _tt(P, rv(tA[:]), pi_, rv(cT_), ALU.mult, [ps_i, X.cosT], [tA])
                yield
                _tt(P, rv(tB[:]), pr_, rv(nsT_), ALU.mult, [ps_r, X.nsinT], [tB])
                yield
                _tt(P, xri[:], tA[:], tB[:], ALU.add, [tA, tB], [xri])
                yield
                if oi == 0:
                    ini_r, ini_i = 0.0, 0.0
                    Rini = []
                else:
                    c1, s1 = X.cosT[:, dp, 1:2], X.sinT[:, dp, 1:2]
                    ns1 = X.nsinT[:, dp, 1:2]
                    _tt(P, hp[:, 2:3], hp[:, 0:1], c1, ALU.mult, [hp, X.cosT], [hp])
                    yield
                    P.V("scalar_tensor_tensor", R=[hp, X.nsinT], W=[hp], out=hp[:, 2:3], in0=hp[:, 1:2], scalar=ns1,
                        in1=hp[:, 2:3], op0=ALU.mult, op1=ALU.add)
                    yield
                    _tt(P, hp[:, 3:4], hp[:, 0:1], s1, ALU.mult, [hp, X.sinT], [hp])
                    yield
                    P.V("scalar_tensor_tensor", R=[hp, X.cosT], W=[hp], out=hp[:, 3:4], in0=hp[:, 1:2], scalar=c1,
                        in1=hp[:, 3:4], op0=ALU.mult, op1=ALU.add)
                    yield
                    ini_r, ini_i = hp[:, 2:3], hp[:, 3:4]
                    Rini = [hp]
                P.V("tensor_tensor_scan", R=[X.rhoT, xrr] + Rini, W=[gr], out=gr[:], data0=X.rhoT[:, dp, :], data1=xrr[:],
                    initial=ini_r, op0=ALU.mult, op1=ALU.add)
                yield
                P.V("tensor_tensor_scan", R=[X.rhoT, xri] + Rini, W=[gi], out=gi[:], data0=X.rhoT[:, dp, :], data1=xri[:],
                    initial=ini_i, op0=ALU.mult, op1=ALU.add)
                yield
                _tt(P, tA[:], gr[:], cT_, ALU.mult, [gr, X.cosT], [tA])
                yield
                _tt(P, tB[:], gi[:], nsT_, ALU.mult, [gi, X.nsinT], [tB])
                yield
                _tt(P, tA[:], tA[:], tB[:], ALU.add, [tA, tB], [tA])
                yield
                P.A("activation", R=[tA], W=[hr], out=rv(hr[:]), in_=tA[:], func=AF.Identity)
                P.A("activation", R=[tA], W=[hp], out=hp[:, 0:1], in_=tA[:, S5CH - 1:S5CH], func=AF.Identity)
                _tt(P, tC[:], gi[:], cT_, ALU.mult, [gi, X.cosT], [tC])
                yield
                _tt(P, tB[:], gr[:], sT_, ALU.mult, [gr, X.sinT], [tB])
                yield
                _tt(P, tC[:], tC[:], tB[:], ALU.add, [tC, tB], [tC])
                yield
                P.A("activation", R=[tC], W=[hi], out=rv(hi[:]), in_=tC[:], func=AF.Identity)
                P.A("activation", R=[tC], W=[hp], out=hp[:, 1:2], in_=tC[:, S5CH - 1:S5CH], func=AF.Identity)
                pb = (pp // 2) * 64
                P.mm(ps_yb[pb:pb + 64, 256:256 + S5CH], X.Cr[:, dp, :], hr[:], True, False, R=[X.Cr, hr], W=[ps_yb])
                P.mm(ps_yb[pb:pb + 64, 256:256 + S5CH], X.Ci[:, dp, :], hi[:], False, True, R=[X.Ci, hi], W=[ps_yb])
                yield
                _tt(P, yacc[pb:pb + 64, ft, c0:c0 + S5CH], yacc[pb:pb + 64, ft, c0:c0 + S5CH],
                    ps_yb[pb:pb + 64, 256:256 + S5CH], ALU.add, [ytok, ps_yb], [ytok])
                yield

        for pr in range(0, 8, 2):
            gens = [chain(0, 0, pr), chain(1, 1, pr), chain(2, 0, pr + 1), chain(3, 1, pr + 1)]
            alive = [True] * 4
            while any(alive):
                for gi_, gq in enumerate(gens):
                    if alive[gi_]:
                        try:
                            next(gq)
                        except StopIteration:
                            alive[gi_] = False
        P.barrier()
        dbg(P, f"yacc{b}", yacc, yacc[:], [128, 2, T])
        ge = sT16
        gt = P.sb("s5gt", [128, 512], F32, st)
        for ft in range(2):
            for (t0, n) in TILES:
                y = yacc[:, ft, t0:t0 + n]
                P.A("activation", R=[yacc], W=[gt], out=gt[:, 0:n], in_=y, func=AF.Square)
                _ts(P, gt[:, 0:n], gt[:, 0:n], 0.044715, ALU.mult, [gt], [gt], 1.0, ALU.add)
                _tt(P, gt[:, 0:n], gt[:, 0:n], y, ALU.mult, [gt, yacc], [gt])
                P.A("activation", R=[gt], W=[gt], out=gt[:, 0:n], in_=gt[:, 0:n], func=AF.Tanh,
                    scale=math.sqrt(2.0 / math.pi))
                _ts(P, gt[:, 0:n], gt[:, 0:n], 1.0, ALU.add, [gt], [gt], 0.5, ALU.mult)
                _tt(P, ge[:, ft, t0:t0 + n], gt[:, 0:n], y, ALU.mult, [gt, yacc], [ge])
        osb = sT
        for m in range(2):
            for (t0, n) in TILES:
                p1, p2 = S.psum[0], S.psum[1]
                for kt in range(2):
                    P.mm(p1[:, 0:n], X.wglu[:, kt, m * 128:(m + 1) * 128], ge[:, kt, t0:t0 + n], kt == 0, kt == 1, R=[X.wglu, ge], W=[p1])
                for kt in range(2):
                    P.mm(p2[:, 0:n], X.wglu[:, kt, 256 + m * 128:256 + (m + 1) * 128], ge[:, kt, t0:t0 + n], kt == 0, kt == 1,
                         R=[X.wglu, ge], W=[p2])
                P.A("activation", R=[p2], W=[gt], out=gt[:, 0:n], in_=p2[:, 0:n], func=AF.Sigmoid)
                _tt(P, osb[:, m, t0:t0 + n], p1[:, 0:n], gt[:, 0:n], ALU.mult, [p1, gt], [osb])
        c0 = 0 if need_ctx else CTX
        for m in range(2):
            P.dma("sp", R=[osb], W=[D.os5.s(b)], out=D.os5[b, m * 128:(m + 1) * 128, c0:T], in_=osb[:, m, c0:T])
    P.barrier()


def merge_setup(P, l, D, S, st):
    X = NS()
    X.pa = P.sb("m_pa", [128, 4, DM], BF16, st)
    X.p5 = P.sb("m_p5", [128, 2, DM], BF16, st)
    X.pr = P.sb("m_pr", [128, 2, DM], BF16, st)
    X.wo = P.sb("m_wo", [128, 8, DM], BF16, st)
    for t, src in ((X.pa, D.proj_att), (X.p5, D.proj_s5), (X.pr, D.proj_rwkv), (X.wo, D.w_out)):
        P.dma("pool", R=[src], W=[t], out=t[:], in_=src[l].rearrange("(kt p) n -> p kt n", p=128))
    X.wr = P.sb("m_wr", [128, 8, 36], F32, st)
    P.dma("sp", R=[D.router_w], W=[X.wr], out=X.wr[:], in_=D.router_w[l].rearrange("(kt p) n -> p kt n", p=128))
    X.rb = P.sb("m_rb", [128, 36], F32, st)
    P.dma("sp", R=[D.router_b], W=[X.rb], out=X.rb[:], in_=D.router_b[:, l, :])
    X.carry = P.sb("m_carry", [128, NEXP], F32, st)
    P.V("memset", W=[X.carry], ap=X.carry[:], constant=0.0)
    X.eoff = P.sb("m_eoff", [128, NEXP], F32, st)
    P.dma("sp", R=[D.consts["eoff"]], W=[X.eoff], out=X.eoff[:], in_=D.consts["eoff"][:, :])
    return X


def phase_merge(P, l, D, S, b, X, need_ctx, xsrc, G):
    tiles = TILES if need_ctx else TILES[1:]
    with contextlib.ExitStack() as st:
        oa = P.sb("mg_oa", [128, 4, 512], BF16, st)
        o5 = P.sb("mg_o5", [128, 2, 512], BF16, st)
        orw = P.sb("mg_or", [128, 2, 512], BF16, st)
        gt = [P.sb(f"mg_g{i}", [128, 3, 512], F32, st) for i in range(2)]
        xt = P.sb("mg_x", [128, 8, 512], F32, st)
        mg = P.sb("mg_m", [128, 8, 512], BF16, st)
        t1 = P.sb("mg_t1", [128, 512], F32, st)
        t2 = P.sb("mg_t2", [128, 512], F32, st)
        sq = P.sb("mg_sq", [128, 8, 512], F32, st)
        rstd = P.sb("mg_rstd", [128, 512], F32, st)
        h2 = P.sb("mg_h2", [128, 8, 512], F32, st)
        htm = P.sb("mg_htm", [128, DM], F32, st)
        rt = {k: P.sb("mg_r" + k, shp, dt, st) for k, shp, dt in (
            ("lg", [128, 36], F32), ("mx", [128, 8], F32), ("ohg", [128, 4], F32), ("el", [128, 8], F32),
            ("ee", [128, 8], F32), ("t8", [128, 8], F32), ("oh1", [128, 8], F32), ("oh2", [128, 8], F32),
            ("M1", [128, 4, 8], F32), ("M2", [128, 4, 8], F32), ("M", [128, NEXP], F32), ("pos", [128, NEXP], F32),
            ("s1", [128, 4], F32), ("gs", [128, 4], F32), ("si", [128, 2], I32))}
        for (t0, n) in tiles:
            col = 4 if t0 < CTX else b
            P.dma("pool", R=[D.oatt.s(b)], W=[oa], out=oa[:, :, 0:n], in_=D.oatt[b, :, t0:t0 + n].rearrange("(k p) t -> p k t", p=128))
            P.dma("pool", R=[D.os5.s(b)], W=[o5], out=o5[:, :, 0:n], in_=D.os5[b, :, t0:t0 + n].rearrange("(k p) t -> p k t", p=128))
            P.dma("pool", R=[D.orw.s(b)], W=[orw], out=orw[:, :, 0:n], in_=D.orw[b, :, t0:t0 + n].rearrange("(k p) t -> p k t", p=128))
            P.dma("sp", R=[xsrc], W=[xt], out=xt[:, :, 0:n], in_=xsrc[b, :, t0:t0 + n].rearrange("(kt p) t -> p kt t", p=128))
            for m in range(8):
                g = gt[m % 2]
                for j in range(3):
                    r0 = OFF_GATE + j * DM + m * 128
                    P.dma("sp", R=[D.uT.s(b)], W=[g], out=g[:, j, 0:n], in_=D.uT[b, r0:r0 + 128, t0:t0 + n])
                pa_, p5_, pr_ = S.psum[1], S.psum[2], S.psum[3]
                for k in range(4):
                    P.mm(pa_[:, 0:n], X.pa[:, k, m * 128:(m + 1) * 128], oa[:, k, 0:n], k == 0, k == 3, R=[X.pa, oa], W=[pa_])
                for k in range(2):
                    P.mm(p5_[:, 0:n], X.p5[:, k, m * 128:(m + 1) * 128], o5[:, k, 0:n], k == 0, k == 1, R=[X.p5, o5], W=[p5_])
                for k in range(2):
                    P.mm(pr_[:, 0:n], X.pr[:, k, m * 128:(m + 1) * 128], orw[:, k, 0:n], k == 0, k == 1, R=[X.pr, orw], W=[pr_])
                _tt(P, t1[:, 0:n], pa_[:, 0:n], g[:, 0, 0:n], ALU.mult, [pa_, g], [t1])
                _tt(P, t2[:, 0:n], p5_[:, 0:n], g[:, 1, 0:n], ALU.mult, [p5_, g], [t2])
                _tt(P, t1[:, 0:n], t1[:, 0:n], t2[:, 0:n], ALU.add, [t1, t2], [t1])
                _tt(P, t2[:, 0:n], pr_[:, 0:n], g[:, 2, 0:n], ALU.mult, [pr_, g], [t2])
                _tt(P, mg[:, m, 0:n], t1[:, 0:n], t2[:, 0:n], ALU.add, [t1, t2], [mg])
            for m in range(8):
                po = S.psum[4 + m % 2]
                for k in range(8):
                    P.mm(po[:, 0:n], X.wo[:, k, m * 128:(m + 1) * 128], mg[:, k, 0:n], k == 0, k == 7, R=[X.wo, mg], W=[po])
                P.V("scalar_tensor_tensor", R=[po, xt, S.modT], W=[xt], out=xt[:, m, 0:n], in0=po[:, 0:n],
                    scalar=S.modT[:, 2 * 8 + m, col:col + 1], in1=xt[:, m, 0:n], op0=ALU.mult, op1=ALU.add)
            P.dma("sp", R=[xt], W=[D.x1T.s(b)], out=D.x1T[b, :, t0:t0 + n].rearrange("(kt p) t -> p kt t", p=128), in_=xt[:, :, 0:n])
            rms_mod(P, S, xt, n, col, 4, 3, lambda kt: (h2[:, kt, 0:n], [h2]), sq, rstd, S.psum[0])
            for sti in range(n // 128):
                tok = slice(sti * 128, (sti + 1) * 128)
                gi = G.next
                G.next += 1
                G.tiles.append((b, t0 + sti * 128))
                pl = S.psum[6]
                for kt in range(8):
                    P.mm(pl[:, 0:36], h2[:, kt, tok], X.wr[:, kt, :], kt == 0, kt == 7, R=[h2, X.wr], W=[pl])
                lg = rt["lg"]
                _tt(P, lg[:], pl[:, 0:36], X.rb[:], ALU.add, [pl, X.rb], [lg])
                mx, ohg, el, ee, t8, oh1, oh2 = rt["mx"], rt["ohg"], rt["el"], rt["ee"], rt["t8"], rt["oh1"], rt["oh2"]
                s1, gs = rt["s1"], rt["gs"]
                P.V("tensor_reduce", R=[lg], W=[s1], out=s1[:, 0:1], in_=lg[:, 0:4], axis=AX.X, op=ALU.max)
                _ts(P, ohg[:], lg[:, 0:4], s1[:, 0:1], ALU.is_equal, [lg, s1], [ohg])
                _ts(P, gs[:], lg[:, 0:4], s1[:, 0:1], ALU.subtract, [lg, s1], [gs])
                P.A("activation", R=[gs], W=[gs], out=gs[:], in_=gs[:], func=AF.Exp)
                P.V("tensor_reduce", R=[gs], W=[s1], out=s1[:, 1:2], in_=gs[:], axis=AX.X, op=ALU.add)
                _ts(P, el[:], lg[:, 4:12], ohg[:, 0:1], ALU.mult, [lg, ohg], [el])
                for j in range(1, 4):
                    P.V("scalar_tensor_tensor", R=[lg, ohg, el], W=[el], out=el[:], in0=lg[:, 4 + 8 * j:12 + 8 * j],
                        scalar=ohg[:, j:j + 1], in1=el[:], op0=ALU.mult, op1=ALU.add)
                P.V("max", R=[el], W=[mx], out=mx[:], in_=el[:])
                _ts(P, oh1[:], el[:], mx[:, 0:1], ALU.is_equal, [el, mx], [oh1])
                _ts(P, oh2[:], el[:], mx[:, 1:2], ALU.is_equal, [el, mx], [oh2])
                _tt(P, s1[:, 2:3], mx[:, 1:2], mx[:, 0:1], ALU.subtract, [mx], [s1])
                P.A("activation", R=[s1], W=[s1], out=s1[:, 2:3], in_=s1[:, 2:3], func=AF.Exp)
                _ts(P, s1[:, 3:4], s1[:, 2:3], 1.0, ALU.add, [s1], [s1])
                _tt(P, s1[:, 3:4], s1[:, 3:4], s1[:, 1:2], ALU.mult, [s1], [s1])
                P.V("reciprocal", R=[s1], W=[s1], out=s1[:, 3:4], in_=s1[:, 3:4])
                P.V("tensor_copy", R=[s1], W=[G.gate], out=G.gate[:, gi, 0:1], in_=s1[:, 3:4])
                _tt(P, G.gate[:, gi, 1:2], s1[:, 3:4], s1[:, 2:3], ALU.mult, [s1], [G.gate])
                for Mk, oh in ((rt["M1"], oh1), (rt["M2"], oh2)):
                    P.V("tensor_tensor", R=[ohg, oh], W=[Mk], out=Mk[:], in0=ohg[:].unsqueeze(2).to_broadcast([128, 4, 8]),
                        in1=oh[:].unsqueeze(1).to_broadcast([128, 4, 8]), op=ALU.mult)
                M = rt["M"]
                _tt(P, M[:], rt["M1"][:].rearrange("p a b -> p (a b)"), rt["M2"][:].rearrange("p a b -> p (a b)"), ALU.add,
                    [rt["M1"], rt["M2"]], [M])
                pp = S.psum[7]
                P.mm(pp[:, 0:NEXP], S.tri[:], M[:], True, True, R=[S.tri, M], W=[pp])
                pos = rt["pos"]
                _tt(P, pos[:], pp[:, 0:NEXP], X.carry[:], ALU.add, [pp, X.carry], [pos])
                _ts(P, pos[:], pos[:], float(CAP - 1), ALU.min, [pos], [pos])
                _tt(P, pos[:], pos[:], X.eoff[:], ALU.add, [pos, X.eoff], [pos])
                P.mm(pp[:, 0:NEXP], S.ones[:], M[:], True, True, R=[S.ones, M], W=[pp])
                _tt(P, X.carry[:], X.carry[:], pp[:, 0:NEXP], ALU.add, [pp, X.carry], [X.carry])
                for k, Mk in enumerate((rt["M1"], rt["M2"])):
                    _tt(P, M[:], Mk[:].rearrange("p a b -> p (a b)"), pos[:], ALU.mult, [Mk, pos], [M])
                    P.V("tensor_reduce", R=[M], W=[s1], out=s1[:, 0:1], in_=M[:], axis=AX.X, op=ALU.add)
                    P.V("tensor_copy", R=[s1], W=[G.slot], out=G.slot[:, gi, k:k + 1], in_=s1[:, 0:1])
                for half in range(2):
                    ph = S.psum[2 + half]
                    for q in range(4):
                        kt = half * 4 + q
                        P.PE("transpose", R=[h2, S.ident], W=[ph], out=ph[:, q * 128:(q + 1) * 128], in_=h2[:, kt, tok], identity=S.ident[:])
                    P.A("activation", R=[ph], W=[htm], out=htm[:, half * 512:(half + 1) * 512], in_=ph[:], func=AF.Identity)
                for k in range(2):
                    P.dma("pool", R=[htm, G.slot], W=[D.Xe], meth="indirect_dma_start", out=D.Xe[:, :],
                          out_offset=bass.IndirectOffsetOnAxis(ap=G.slot[:, gi, k:k + 1], axis=0), in_=htm[:], in_offset=None)
    P.barrier()


def phase_experts(P, l, D, S):
    with contextlib.ExitStack() as st:
        wg = [P.sb(f"e_wg{i}", [128, 8, DEXP], BF16, st) for i in range(2)]
        wu = [P.sb(f"e_wu{i}", [128, 8, DEXP], BF16, st) for i in range(2)]
        wd = [P.sb(f"e_wd{i}", [128, 4, DM], BF16, st) for i in range(2)]
        xtm2 = [P.sb(f"e_xtm{i}", [128, 4, DM], F32, st) for i in range(2)]
        xbT2 = [P.sb(f"e_xbT{i}", [128, 8, 512], BF16, st) for i in range(2)]
        sg = [P.sb(f"e_sg{i}", [128, 512], F32, st) for i in range(2)]
        act = P.sb("e_act", [128, 4, 512], BF16, st)
        yb2 = [P.sb(f"e_yb{i}", [128, 4, DM], F32, st) for i in range(2)]
        blocks = [(e, blk) for e in range(NEXP) for blk in range(CAP // 512)]
        cnt = {"ci": 0}

        def load_w(e):
            k = e % 2
            P.dma("pool", R=[D.exp_w_gate], W=[wg[k]], out=wg[k][:], in_=D.exp_w_gate[l, e].rearrange("(kt p) n -> p kt n", p=128))
            P.dma("pool", R=[D.exp_w_up], W=[wu[k]], out=wu[k][:], in_=D.exp_w_up[l, e].rearrange("(kt p) n -> p kt n", p=128))
            P.dma("pool", R=[D.exp_w_down], W=[wd[k]], out=wd[k][:], in_=D.exp_w_down[l, e].rearrange("(kt p) n -> p kt n", p=128))

        def stage_a(bi):
            e, blk = blocks[bi]
            r0 = e * CAP + blk * 512
            xtm, xbT = xtm2[bi % 2], xbT2[bi % 2]
            P.dma("sp", R=[D.Xe], W=[xtm], out=xtm[:], in_=D.Xe[r0:r0 + 512, :].rearrange("(s p) f -> p s f", p=128))
            for kt in range(8):
                ph = S.psum[cnt["ci"] % 2]
                cnt["ci"] += 1
                for s_ in range(4):
                    P.PE("transpose", R=[xtm, S.ident], W=[ph], out=ph[:, s_ * 128:(s_ + 1) * 128],
                         in_=xtm[:, s_, kt * 128:(kt + 1) * 128], identity=S.ident[:])
                if kt % 2 == 0:
                    P.A("activation", R=[ph], W=[xbT], out=xbT[:, kt, :], in_=ph[:], func=AF.Identity)
                else:
                    P.V("tensor_copy", R=[ph], W=[xbT], out=xbT[:, kt, :], in_=ph[:])

        def stage_bc(bi):
            e, blk = blocks[bi]
            k = e % 2
            r0 = e * CAP + blk * 512
            xbT, yb = xbT2[bi % 2], yb2[bi % 2]
            for hm in range(4):
                pg, pu = S.psum[2 + 2 * (hm % 2)], S.psum[3 + 2 * (hm % 2)]
                for kt in range(8):
                    P.mm(pg[:], wg[k][:, kt, hm * 128:(hm + 1) * 128], xbT[:, kt, :], kt == 0, kt == 7, R=[wg[k], xbT], W=[pg])
                for kt in range(8):
                    P.mm(pu[:], wu[k][:, kt, hm * 128:(hm + 1) * 128], xbT[:, kt, :], kt == 0, kt == 7, R=[wu[k], xbT], W=[pu])
                P.A("activation", R=[pg], W=[sg[hm % 2]], out=sg[hm % 2][:], in_=pg[:], func=AF.Silu)
                _tt(P, act[:, hm, :], pu[:], sg[hm % 2][:], ALU.mult, [pu, sg[hm % 2]], [act])
            for s_ in range(4):
                for half in range(2):
                    pd = S.psum[6 + half]
                    for hm in range(4):
                        P.mm(pd[:], act[:, hm, s_ * 128:(s_ + 1) * 128], wd[k][:, hm, half * 512:(half + 1) * 512], hm == 0, hm == 3,
                             R=[act, wd[k]], W=[pd])
                    if half == 0:
                        P.A("activation", R=[pd], W=[yb], out=yb[:, s_, 0:512], in_=pd[:], func=AF.Identity)
                    else:
                        P.V("tensor_copy", R=[pd], W=[yb], out=yb[:, s_, 512:1024], in_=pd[:])
            P.dma("sp", R=[yb], W=[D.Ye], out=D.Ye[r0:r0 + 512, :].rearrange("(s p) f -> p s f", p=128), in_=yb[:])

        load_w(0)
        stage_a(0)
        for bi in range(len(blocks)):
            e, blk = blocks[bi]
            if blk == 0 and e + 1 < NEXP:
                load_w(e + 1)
            if bi + 1 < len(blocks):
                stage_a(bi + 1)
            stage_bc(bi)
    P.barrier()


def phase_combine(P, l, D, S, G, dst, last):
    with contextlib.ExitStack() as st:
        y1 = [P.sb(f"c_y1{i}", [128, DM], F32, st) for i in range(2)]
        y2 = [P.sb(f"c_y2{i}", [128, DM], F32, st) for i in range(2)]
        xt = [P.sb(f"c_x{i}", [128, 8, 128], F32, st) for i in range(2)]
        for gi, (b, t0) in enumerate(G.tiles):
            k = gi % 2
            col = 4 if t0 < CTX else b
            for yy, kk in ((y1[k], 0), (y2[k], 1)):
                P.dma("pool", R=[D.Ye, G.slot], W=[yy], meth="indirect_dma_start", out=yy[:], out_offset=None, in_=D.Ye[:, :],
                      in_offset=bass.IndirectOffsetOnAxis(ap=G.slot[:, gi, kk:kk + 1], axis=0))
            P.dma("sp", R=[D.x1T.s(b)], W=[xt[k]], out=xt[k][:], in_=D.x1T[b, :, t0:t0 + 128].rearrange("(kt p) t -> p kt t", p=128))
            _ts(P, y1[k][:], y1[k][:], G.gate[:, gi, 0:1], ALU.mult, [y1[k], G.gate], [y1[k]])
            P.V("scalar_tensor_tensor", R=[y1[k], y2[k], G.gate], W=[y1[k]], out=y1[k][:], in0=y2[k][:], scalar=G.gate[:, gi, 1:2],
                in1=y1[k][:], op0=ALU.mult, op1=ALU.add)
            for half in range(2):
                ph = S.psum[2 * k + half]
                for q in range(4):
                    m = half * 4 + q
                    P.PE("transpose", R=[y1[k], S.ident], W=[ph], out=ph[:, q * 128:(q + 1) * 128], in_=y1[k][:, m * 128:(m + 1) * 128],
                         identity=S.ident[:])
                for q in range(4):
                    m = half * 4 + q
                    P.V("scalar_tensor_tensor", R=[ph, xt[k], S.modT], W=[xt[k]], out=xt[k][:, m, :], in0=ph[:, q * 128:(q + 1) * 128],
                        scalar=S.modT[:, 5 * 8 + m, col:col + 1], in1=xt[k][:, m, :], op0=ALU.mult, op1=ALU.add)
            if last:
                P.dma("sp", R=[xt[k]], W=[D.out], out=D.out[b, :, t0 - CTX:t0 - CTX + 128].rearrange("(kt p) t -> p kt t", p=128), in_=xt[k][:])
            else:
                P.dma("sp", R=[xt[k]], W=[dst.s(b)], out=dst[b, :, t0:t0 + 128].rearrange("(kt p) t -> p kt t", p=128), in_=xt[k][:])
    P.barrier()


RC = 64
NCH = T // RC
GN_EPS = 64e-5
W_SCALE = -math.exp(-0.5)


def rwkv_setup(P, l, D, S, st):
    X = NS()
    ld = lambda nm, src, shp: _load(P, st, nm, src, shp)
    X.cw = ld("rw_cw", D.rw_conv[:, l, :], [128, 24])
    X.w0 = ld("rw_w0", D.rw_w0[:, l, :], [128, 4])
    X.a0 = ld("rw_a0", D.rw_a0[:, l, :], [128, 4])
    X.pv = ld("rw_pv", D.rw_pv[:, l, :], [128, 10])
    X.w2a2 = [ld(f"rw_w2a2{d}", D.rw_w2a2[l, d], [128, 256]) for d in range(2)]
    X.g2 = ld("rw_g2", D.rw_g2[l], [128, 256])
    X.msk = {k: ld("rw_" + k, D.consts[k][:, :], list(D.consts[k].t.shape)) for k in ("MA0", "MA1", "MB0", "MB1", "MC0", "MC1", "YXI")}
    X.rmask = ld("rw_rmask", D.consts["rmask"][:, :], [128, T])
    X.gneps = P.sb("rw_gneps", [128, 1], F32, st)
    P.V("memset", W=[X.gneps], ap=X.gneps[:], constant=GN_EPS)
    X.kkeps = P.sb("rw_kkeps", [128, 1], F32, st)
    P.V("memset", W=[X.kkeps], ap=X.kkeps[:], constant=1e-12)
    return X


def phase_rwkv(P, l, D, S, b, X, need_ctx):
    zbase = OFF_RW
    SEG = ((0, CTX), (CTX, T))
    for ih in range(2):
        with contextlib.ExitStack() as st:
            A = lambda nm, shp=(128, T): P.sb("rw_" + nm, list(shp), F32, st)
            zt = A("zt")
            r, k, v, kk, kts, g, ysum = A("r"), A("k"), A("v"), A("kk"), A("kts"), A("g"), A("ysum")
            z6 = A("z6")
            t1, t2, t3 = A("t1"), A("t2"), A("t3")
            t2x = g
            KRr = P.sb("rw_KR", [128, NCH, 2, RC], F32R, st)
            BKr = P.sb("rw_BK", [128, NCH, 2, RC], F32R, st)
            KR = _View(KRr, F32)
            BK = _View(BKr, F32)
            wtot = A("wtot", (128, NCH))

            def conv(dst, tile):
                P.dma("sp", R=[D.uT.s(b)], W=[zt], out=zt[:], in_=D.uT[b, zbase + tile * 128:zbase + (tile + 1) * 128, :])
                _ts(P, dst[:], zt[:], X.cw[:, 8 + tile:9 + tile], ALU.mult, [zt, X.cw], [dst])
                for (s0, s1) in SEG:
                    P.V("scalar_tensor_tensor", R=[zt, X.cw, dst], W=[dst], out=dst[:, s0 + 1:s1], in0=zt[:, s0:s1 - 1],
                        scalar=X.cw[:, tile:tile + 1], in1=dst[:, s0 + 1:s1], op0=ALU.mult, op1=ALU.add)
                    P.V("scalar_tensor_tensor", R=[zt, X.cw, dst], W=[dst], out=dst[:, s0:s1 - 1], in0=zt[:, s0 + 1:s1],
                        scalar=X.cw[:, 16 + tile:17 + tile], in1=dst[:, s0:s1 - 1], op0=ALU.mult, op1=ALU.add)

            conv(r, ih)
            conv(k, 2 + ih)
            conv(v, 4 + ih)
            conv(z6, 6)
            P.A("activation", R=[z6], W=[z6], out=z6[0:64, :], in_=z6[0:64, :], func=AF.Tanh)
            P.V("memset", W=[ysum], ap=ysum[:], constant=0.0)
            P.V("memset", W=[kts], ap=kts[:], constant=0.0)
            _ts(P, kk[:], k[:], X.pv[:, 0 + ih:1 + ih], ALU.mult, [k, X.pv], [kk])
            for (t0, n) in TILES:
                ps = S.psum[0]
                P.A("activation", R=[kk], W=[t1], out=t1[:, t0:t0 + n], in_=kk[:, t0:t0 + n], func=AF.Square)
                P.mm(ps[:, 0:n], S.bs64[:], t1[:, t0:t0 + n], True, True, R=[S.bs64, t1], W=[ps])
                P.A("activation", R=[ps, X.kkeps], W=[t1], out=t1[:, t0:t0 + n], in_=ps[:, 0:n], func=AF.Sqrt, bias=X.kkeps[:, 0:1], scale=1.0)
                P.V("reciprocal", R=[t1], W=[t1], out=t1[:, t0:t0 + n], in_=t1[:, t0:t0 + n])
                _tt(P, kk[:, t0:t0 + n], kk[:, t0:t0 + n], t1[:, t0:t0 + n], ALU.mult, [kk, t1], [kk])
            for d in range(2):
                MA, MB, MC = X.msk[f"MA{d}"], X.msk[f"MB{d}"], X.msk[f"MC{d}"]
                for (t0, n) in TILES:
                    pw, pa = S.psum[0], S.psum[1]
                    P.mm(pw[:, 0:n], X.w2a2[d][0:64, ih * 128:(ih + 1) * 128], z6[0:64, t0:t0 + n], True, True, R=[X.w2a2[d], z6], W=[pw])
                    P.mm(pa[:, 0:n], X.w2a2[d][64:128, ih * 128:(ih + 1) * 128], z6[64:128, t0:t0 + n], True, True, R=[X.w2a2[d], z6], W=[pa])
                    P.A("activation", R=[pw, X.w0], W=[t1], out=t1[:, t0:t0 + n], in_=pw[:, 0:n], func=AF.Sigmoid,
                        bias=X.w0[:, 2 * d + ih:2 * d + ih + 1], scale=1.0)
                    P.A("activation", R=[pa, X.a0], W=[t2], out=t2[:, t0:t0 + n], in_=pa[:, 0:n], func=AF.Sigmoid,
                        bias=X.a0[:, 2 * d + ih:2 * d + ih + 1], scale=1.0)
                _ts(P, t1[:], t1[:], W_SCALE, ALU.mult, [t1], [t1])
                _ts(P, t3[:], t2[:], -1.0, ALU.add, [t2], [t3], X.pv[:, 2 + ih:3 + ih], ALU.mult)
                P.V("scalar_tensor_tensor", R=[t3, k], W=[t3], out=t3[:], in0=t3[:], scalar=1.0, in1=k[:], op0=ALU.add, op1=ALU.mult)
                _tt(P, kts[:], kts[:], t3[:], ALU.add, [kts, t3], [kts])
                _tt(P, t2[:], t2[:], kk[:], ALU.mult, [t2, kk], [t2])
                P.V("tensor_tensor_scan", R=[X.rmask, t1], W=[zt], out=zt[:], data0=X.rmask[:], data1=t1[:], initial=0.0,
                    op0=ALU.mult, op1=ALU.add)
                zt3 = zt[:].rearrange("p (c j) -> p c j", j=RC)
                if d == 1:
                    P.V("tensor_tensor", R=[zt], W=[t2x], out=t2x[:].rearrange("p (c j) -> p c j", j=RC),
                        in0=zt3[:, :, RC - 1:RC].to_broadcast([128, NCH, RC]), in1=zt3, op=ALU.subtract)
                    _tt(P, zt[:], t2x[:], t1[:], ALU.add, [t2x, t1], [zt])
                last = RC - 1 if d == 0 else 0
                P.A("activation", R=[zt], W=[wtot], out=wtot[:], in_=zt3[:, :, last], func=AF.Exp)
                c3 = lambda tt_: tt_[:].rearrange("p (c j) -> p c j", j=RC)
                P.A("activation", R=[zt], W=[t2x], out=t2x[:], in_=zt[:], func=AF.Exp)
                _tt(P, KRr[:, :, 1, :], c3(t2x), c3(r), ALU.mult, [t2x, r], [KRr])
                P.A("activation", R=[zt], W=[t2x], out=t2x[:], in_=zt[:], func=AF.Exp, scale=-1.0)
                _tt(P, BKr[:, :, 1, :], c3(t2x), c3(t3), ALU.mult, [t2x, t3], [BKr])
                _tt(P, BKr[:, :, 0, :], c3(t2x), c3(t2), ALU.mult, [t2x, t2], [BKr])
                _tt(P, zt[:], zt[:], t1[:], ALU.subtract, [zt, t1], [zt])
                P.A("activation", R=[zt], W=[t2x], out=t2x[:], in_=zt[:], func=AF.Exp)
                _tt(P, KRr[:, :, 0, :], c3(t2x), c3(kk), ALU.mult, [t2x, kk], [KRr])
                rwkv_scan(P, S, X, d, st, KRr, BKr, wtot, v, ysum, MA, MB, MC)
            z7 = t3
            conv(z7, 7)
            P.A("activation", R=[z7], W=[z7], out=z7[:], in_=z7[:], func=AF.Sigmoid)
            for (t0, n) in TILES:
                ps2 = S.psum[1]
                P.mm(ps2[:, 0:n], X.g2[:, ih * 128:(ih + 1) * 128], z7[:, t0:t0 + n], True, True, R=[X.g2, z7], W=[ps2])
                P.A("activation", R=[ps2], W=[g], out=g[:, t0:t0 + n], in_=ps2[:, 0:n], func=AF.Identity)
            for (t0, n) in TILES:
                if t0 < CTX and not need_ctx:
                    continue
                sl = slice(t0, t0 + n)
                pm, pv_, pb = S.psum[0], S.psum[1], S.psum[2]
                P.mm(pm[:, 0:n], S.bd64[:], ysum[:, sl], True, True, R=[S.bd64, ysum], W=[pm])
                _tt(P, t1[:, sl], ysum[:, sl], pm[:, 0:n], ALU.subtract, [ysum, pm], [t1])
                P.A("activation", R=[t1], W=[t2], out=t2[:, sl], in_=t1[:, sl], func=AF.Square)
                P.mm(pv_[:, 0:n], S.bd64[:], t2[:, sl], True, True, R=[S.bd64, t2], W=[pv_])
                P.A("activation", R=[pv_, X.gneps], W=[t2], out=t2[:, sl], in_=pv_[:, 0:n], func=AF.Sqrt, bias=X.gneps[:, 0:1], scale=1.0)
                P.V("reciprocal", R=[t2], W=[t2], out=t2[:, sl], in_=t2[:, sl])
                _tt(P, t1[:, sl], t1[:, sl], t2[:, sl], ALU.mult, [t1, t2], [t1])
                _ts(P, t1[:, sl], t1[:, sl], X.pv[:, 6 + ih:7 + ih], ALU.mult, [t1, X.pv], [t1], X.pv[:, 8 + ih:9 + ih], ALU.add)
                P.V("scalar_tensor_tensor", R=[r, kts, X.pv], W=[t2], out=t2[:, sl], in0=r[:, sl], scalar=X.pv[:, 4 + ih:5 + ih],
                    in1=kts[:, sl], op0=ALU.mult, op1=ALU.mult)
                P.mm(pb[:, 0:n], S.bs64[:], t2[:, sl], True, True, R=[S.bs64, t2], W=[pb])
                _tt(P, t2[:, sl], pb[:, 0:n], v[:, sl], ALU.mult, [pb, v], [t2])
                _tt(P, t1[:, sl], t1[:, sl], t2[:, sl], ALU.add, [t1, t2], [t1])
                _tt(P, t1[:, sl], t1[:, sl], g[:, sl], ALU.mult, [t1, g], [t1])
            c0 = 0 if need_ctx else CTX
            P.dma("sp", R=[t1], W=[D.orw.s(b)], out=D.orw[b, ih * 128:(ih + 1) * 128, c0:T], in_=t1[:, c0:T])
        P.barrier()


def rwkv_scan(P, S, X, d, st0, KR, BK, wtot, v, ysum, MA, MB, MC):
    G = 2
    with contextlib.ExitStack() as st:
        B2 = lambda nm, shp: [P.sb(f"rs_{nm}{i}", list(shp), F32R, st) for i in range(2)]
        ST = P.sb("rs_ST", [128, 128], F32R, st)
        P.V("tensor_scalar", R=[S.ident], W=[ST], out=ST[:], in0=S.ident[:], scalar1=0.0, scalar2=None, op0=ALU.mult)
        identR = P.sb("rs_identR", [128, 128], F32R, st)
        P.V("tensor_copy", R=[S.ident], W=[identR], out=identR[:], in_=S.ident[:])
        bf = lambda ap: ap.bitcast(F32)
        YA, AB = B2("YA", (64, G, 2, 128)), B2("AB", (64, G, 2, 128))
        YX = B2("YX", (64, G, 2, 2, RC))
        YT = B2("YT", (64, G, 2, RC))
        X6 = B2("X6", (64, G, 2, RC))
        BKt, Vt = B2("BKt", (64, G, 2, 128)), B2("Vt", (64, G, 128))
        RHS, Ps = B2("RHS", (64, 128)), B2("Ps", (64, 128))
        if d == 0:
            order = list(range(NCH))
        else:
            nc_ctx = CTX // RC
            order = list(range(nc_ctx - 1, -1, -1)) + list(range(NCH - 1, nc_ctx - 1, -1))
        pairs = [order[i:i + G] for i in range(0, NCH, G)]

        def pre_stages(pi):
            cs = pairs[pi]
            q = pi % 2
            ya, ab, bkt, vt, x6 = YA[q], AB[q], BKt[q], Vt[q], X6[q]
            stages = []

            def s_init():
                pA, pB, pC = S.psum[0], S.psum[1], S.psum[2]
                for gi, c in enumerate(cs):
                    for hp in range(2):
                        lo = hp * 64
                        o = (gi * 2 + hp)
                        krc = KR[lo:lo + 64, c, :, :].rearrange("p a b -> p (a b)")
                        P.mm(pA[0:64, o * 128:(o + 1) * 128], BK[lo:lo + 64, c, 0, :], krc, True, True, R=[BK, KR], W=[pA])
                        P.mm(pB[0:64, o * 128:(o + 1) * 128], BK[lo:lo + 64, c, 1, :], krc, True, True, R=[BK, KR], W=[pB])
                        P.mm(pC[0:64, o * 64:(o + 1) * 64], KR[lo:lo + 64, c, 0, :], BK[lo:lo + 64, c, 0, :], True, True, R=[BK, KR], W=[pC])
                _tt(P, ya[:].rearrange("p g a b -> p (g a b)"), pA[0:64, 0:512], MA[:], ALU.mult, [pA, MA], [ya])
                _tt(P, ab[:].rearrange("p g a b -> p (g a b)"), pB[0:64, 0:512], MB[:], ALU.mult, [pB, MB], [ab])
                yx, yt = YX[0], YT[0]
                _tt(P, yt[:].rearrange("p g a b -> p (g a b)"), pC[0:64, 0:256], MC[:], ALU.mult, [pC, MC], [yt])
                P.A("activation", R=[X.msk["YXI"]], W=[yx], out=yx[:].rearrange("p g a b c -> p (g a b c)"), in_=X.msk["YXI"][:],
                    func=AF.Identity)
                P.V("tensor_copy", R=[ya], W=[yx], out=yx[:, :, :, 0, :], in_=bf(ya[:, :, :, 0:RC]))
            stages.append(s_init)

            def mk_step(kstep):
                def s_step():
                    yx, yt = YX[kstep % 2], YT[kstep % 2]
                    yxn, ytn = YX[(kstep + 1) % 2], YT[(kstep + 1) % 2]
                    pa_, pc_ = S.psum[3], S.psum[4]
                    for gi in range(G):
                        for hp in range(2):
                            o = gi * 2 + hp
                            P.mm(pa_[0:64, o * 128:(o + 1) * 128], yt[:, gi, hp, :], yx[:, gi, hp, :, :].rearrange("p a b -> p (a b)"),
                                 True, True, R=[yt, yx], W=[pa_])
                            if kstep < 5:
                                P.mm(pc_[0:64, o * 64:(o + 1) * 64], yx[:, gi, hp, 0, :], yt[:, gi, hp, :], True, True, R=[yt, yx], W=[pc_])
                    pa4 = pa_[0:64, 0:512].rearrange("p (g a b c) -> p g a b c", g=G, a=2, b=2)
                    if kstep < 5:
                        P.A("activation", R=[pa_], W=[yxn], out=yxn[:, :, :, 0, :], in_=pa4[:, :, :, 0, :], func=AF.Identity)
                        _tt(P, yxn[:, :, :, 1, :], bf(yx[:, :, :, 1, :]), pa4[:, :, :, 1, :], ALU.add, [yx, pa_], [yxn])
                        P.A("activation", R=[pc_], W=[ytn], out=ytn[:].rearrange("p g a b -> p (g a b)"), in_=pc_[0:64, 0:256],
                            func=AF.Identity)
                    else:
                        _tt(P, x6[:], bf(yx[:, :, :, 1, :]), pa4[:, :, :, 1, :], ALU.add, [yx, pa_], [x6])
                return s_step
            for kstep in range(6):
                stages.append(mk_step(kstep))

            def s_tr():
                pT, pV = S.psum[5], S.psum[2]
                for gi, c in enumerate(cs):
                    P.PE("transpose", R=[BK, S.ident], W=[pT], out=pT[0:64, gi * 256:gi * 256 + 128], in_=BK[:, c, 0, :].bitcast(F32), identity=S.ident[:])
                    P.PE("transpose", R=[BK, S.ident], W=[pT], out=pT[0:64, gi * 256 + 128:gi * 256 + 256], in_=BK[:, c, 1, :].bitcast(F32),
                         identity=S.ident[:])
                    P.PE("transpose", R=[v, S.ident], W=[pV], out=pV[0:64, 256 + gi * 128:256 + (gi + 1) * 128], in_=v[:, c * RC:(c + 1) * RC],
                         identity=S.ident[:])
                pT4 = pT[0:64, 0:512].rearrange("p (g a b) -> p g a b", g=G, a=2)
                _ts(P, bkt[:, :, 0, :], pT4[:, :, 0, :], -1.0, ALU.mult, [pT], [bkt])
                P.A("activation", R=[pT], W=[bkt], out=bkt[:, :, 1, :], in_=pT4[:, :, 1, :], func=AF.Identity)
                P.A("activation", R=[pV], W=[vt], out=vt[:].rearrange("p g b -> p (g b)"), in_=pV[0:64, 256:512], func=AF.Identity)
            stages.append(s_tr)
            return stages

        def seq_stages(pi):
            cs = pairs[pi]
            q = pi % 2
            ya, ab, bkt, vt, x6 = YA[q], AB[q], BKt[q], Vt[q], X6[q]
            stages = []
            for gi, c in enumerate(cs):
                rhs, ps_ = RHS[gi], Ps[gi]
                pR, pP, pY, pS = S.psum[6], S.psum[7], S.psum[6], S.psum[7]

                def s1(gi=gi, c=c, rhs=rhs):
                    for hp in range(2):
                        lo = hp * 64
                        P.mm(pR[0:64, lo:lo + 64], KR[lo:lo + 64, c, 0, :], ST[lo:lo + 64, lo:lo + 64], True, False, R=[KR, ST], W=[pR])
                        P.mm(pR[0:64, lo:lo + 64], ab[:, gi, hp, 0:RC], vt[:, gi, lo:lo + 64], False, True, R=[ab, vt], W=[pR])
                    P.V("tensor_copy", R=[pR], W=[rhs], out=rhs[:], in_=pR[0:64, 0:128])

                def s2(gi=gi, c=c, rhs=rhs, ps_=ps_):
                    for hp in range(2):
                        lo = hp * 64
                        P.mm(pP[0:64, lo:lo + 64], x6[:, gi, hp, :], rhs[:, lo:lo + 64], True, True, R=[x6, rhs], W=[pP])
                    P.V("tensor_copy", R=[pP], W=[ps_], out=ps_[:], in_=pP[0:64, 0:128])

                def s3(gi=gi, c=c, ps_=ps_):
                    P.mm(pS[:, 256:384], bkt[:, gi, 0, :], ps_[:], True, False, R=[bkt, ps_], W=[pS])
                    P.mm(pS[:, 256:384], bkt[:, gi, 1, :], vt[:, gi, :], False, False, R=[bkt, vt], W=[pS])
                    P.mm(pS[:, 256:384], identR[:], ST[:], False, True, R=[identR, ST], W=[pS])
                    for hp2 in range(2):
                        P.mm(pY[:, 256 + hp2 * 64:256 + (hp2 + 1) * 64], ST[:], KR[:, c, 1, :], hp2 == 0, False, R=[ST, KR], W=[pY])
                    P.mm(pY[:, 256:384], ps_[:], ya[:, gi, :, RC:2 * RC], False, False, R=[ps_, ya], W=[pY])
                    P.mm(pY[:, 256:384], vt[:, gi, :], ab[:, gi, :, RC:2 * RC], False, True, R=[vt, ab], W=[pY])
                    P.V("scalar_tensor_tensor", R=[pS, wtot, S.bs64], W=[ST], out=ST[:], in0=pS[:, 256:384], scalar=wtot[:, c:c + 1],
                        in1=S.bs64[:], op0=ALU.mult, op1=ALU.mult)
                    for hp in range(2):
                        lo = hp * 64
                        _tt(P, ysum[lo:lo + 64, c * RC:(c + 1) * RC], ysum[lo:lo + 64, c * RC:(c + 1) * RC],
                            pY[lo:lo + 64, 256 + lo:256 + lo + 64], ALU.add, [ysum, pY], [ysum])
                stages += [s1, s2, s3]
            return stages

        for f in pre_stages(0):
            f()
        for pi in range(len(pairs)):
            sq = seq_stages(pi)
            pr = pre_stages(pi + 1) if pi + 1 < len(pairs) else []
            n = max(len(sq), len(pr))
            for i in range(n):
                if i < len(pr):
                    pr[i]()
                if i < len(sq):
                    sq[i]()
    P.barrier()


def build(ncores_debug=False, nbc=NBC, phases=("mod", "inproj", "attn", "s5", "rwkv", "moe"), depth=DEPTH):
    nc = bass.Bass("TRN2", target_bir_lowering=False)
    P = Prog(nc, debug=ncores_debug)
    D = NS()
    S = NS()

    def ext(name, shape, dtype=F32):
        return P.dram(name, shape, dtype, kind="ExternalInput")

    D.xinT = ext("xinT", [nbc, DM, T])
    D.cT = ext("cT", [128, 8, 8])
    D.w_mod = ext("w_mod", [DEPTH, DM, 6 * DM])
    D.w_in = ext("w_in", [DEPTH, DM, INW])
    D.bmodT = ext("bmodT", [128, DEPTH, 48])
    D.norm1T = ext("norm1T", [128, DEPTH, 8])
    D.norm2T = ext("norm2T", [128, DEPTH, 8])
    D.qg = ext("qg", [128, DEPTH])
    D.kg = ext("kg", [128, DEPTH])
    for nm, shp in (("s5_A_rep_re", [128, DEPTH, 256]), ("s5_A_rep_im", [128, DEPTH, 256]), ("s5_ldt_rep", [128, DEPTH, 256]),
                    ("s5_Bt_re", [128, DEPTH, 256]), ("s5_Bt_im", [128, DEPTH, 256]), ("s5_A_st_re", [128, DEPTH, 16]),
                    ("s5_A_st_im", [128, DEPTH, 16]), ("s5_ldt_st", [128, DEPTH, 16]), ("s5_C_st_re", [128, DEPTH, 1024]),
                    ("s5_C_st_im", [128, DEPTH, 1024]), ("s5_dT", [128, DEPTH, 2]), ("s5_w_glu", [DEPTH, 256, 512])):
        setattr(D, nm, ext(nm, shp))
    D.os5 = P.dram("os5", [nbc, 256, T])
    for nm, shp in (("rw_conv", [128, DEPTH, 24]), ("rw_w0", [128, DEPTH, 4]), ("rw_a0", [128, DEPTH, 4]), ("rw_pv", [128, DEPTH, 10]),
                    ("rw_w2a2", [DEPTH, 2, 128, 256]), ("rw_g2", [DEPTH, 128, 256])):
        setattr(D, nm, ext(nm, shp))
    D.orw = P.dram("orw", [nbc, 256, T])
    D.x1T = P.dram("x1T", [nbc, DM, T])
    D.x2T = P.dram("x2T", [nbc, DM, T])
    D.Xe = P.dram("Xe", [NEXP * CAP, DM])
    D.Ye = P.dram("Ye", [NEXP * CAP, DM])
    for nm, shp in (("proj_att", [DEPTH, 512, DM]), ("proj_s5", [DEPTH, 256, DM]), ("proj_rwkv", [DEPTH, 256, DM]),
                    ("w_out", [DEPTH, DM, DM]), ("router_w", [DEPTH, DM, 36]), ("router_b", [128, DEPTH, 36]),
                    ("exp_w_gate", [DEPTH, NEXP, DM, DEXP]), ("exp_w_up", [DEPTH, NEXP, DM, DEXP]), ("exp_w_down", [DEPTH, NEXP, DEXP, DM])):
        setattr(D, nm, ext(nm, shp))
    consts = make_consts()
    D.consts = {k: ext("c_" + k, list(v.shape)) for k, v in consts.items()}
    D.out = P.dram("out", [nbc, DM, LAT], kind="ExternalOutput")
    D.uT = P.dram("uT", [nbc, INW, T])
    D.oatt = P.dram("oatt", [nbc, 512, T])

    S.psum = [P.ps(f"ps{i}", [128, 512]) for i in range(8)]
    for k in ("ident", "ones", "bd64", "bs64", "rot", "sh0", "sh1", "tri"):
        t = P.sb("k_" + k, [128, 128])
        P.dma("sp", R=[D.consts[k]], W=[t], out=t[:], in_=D.consts[k][:, :])
        setattr(S, k, t)
    S.maskG = P.sb("k_maskG", [128, 8])
    P.dma("sp", R=[D.consts["maskG"]], W=[S.maskG], out=S.maskG[:], in_=D.consts["maskG"][:, :])
    S.modT = P.sb("modT", [128, 48, 8])
    S.epsb = P.sb("epsb", [128, 1])
    P.V("memset", W=[S.epsb], ap=S.epsb[:], constant=EPS)
    for k, shp in (("bmodT", [128, DEPTH, 48]), ("norm1T", [128, DEPTH, 8]), ("norm2T", [128, DEPTH, 8])):
        t = P.sb(k, shp)
        P.dma("sp", R=[getattr(D, k)], W=[t], out=t[:], in_=getattr(D, k)[:, :, :])
        setattr(S, k, t)
    S.qg = []
    S.kg = []
    qgt = P.sb("qgt", [128, DEPTH])
    kgt = P.sb("kgt", [128, DEPTH])
    P.dma("sp", R=[D.qg], W=[qgt], out=qgt[:], in_=D.qg[:, :])
    P.dma("sp", R=[D.kg], W=[kgt], out=kgt[:], in_=D.kg[:, :])
    for l in range(DEPTH):
        a = NS.__new__(NS)
        S.qg.append(_ColView(qgt, l))
        S.kg.append(_ColView(kgt, l))

    xsrc = D.xinT
    for l in range(depth):
        need_ctx = l < DEPTH - 1
        if "mod" in phases:
            phase_mod(P, l, D, S)
        if "inproj" in phases:
            for b in range(nbc):
                phase_inproj(P, l, D, S, b, xsrc)
        if "attn" in phases:
            with contextlib.ExitStack() as lst:
                for k in ("cos", "sin"):
                    t = P.sb("k_" + k, [128, LAT], F32, lst)
                    P.dma("sp", R=[D.consts[k]], W=[t], out=t[:], in_=D.consts[k][:, :])
                    setattr(S, k, t)
                P.barrier()
                for b in range(nbc):
                    phase_attn(P, l, D, S, b, need_ctx)
        if "s5" in phases:
            with contextlib.ExitStack() as lst:
                X5 = s5_setup(P, l, D, S, lst)
                P.barrier()
                for b in range(nbc):
                    phase_s5(P, l, D, S, b, X5, need_ctx)
        if "rwkv" in phases:
            with contextlib.ExitStack() as lst:
                XR = rwkv_setup(P, l, D, S, lst)
                P.barrier()
                for b in range(nbc):
                    phase_rwkv(P, l, D, S, b, XR, need_ctx)
        if "rwkv0" in phases:
            with contextlib.ExitStack() as lst:
                z = P.sb("zrw", [128, T], F32, lst)
                P.V("memset", W=[z], ap=z[:], constant=0.0)
                for b in range(nbc):
                    for m in range(2):
                        P.dma("sp", R=[z], W=[D.orw.s(b)], out=D.orw[b, m * 128:(m + 1) * 128, :], in_=z[:])
            P.barrier()
        if "moe" in phases:
            with contextlib.ExitStack() as lst:
                G = NS()
                G.next = 0
                G.tiles = []
                G.slot = P.sb("g_slot", [128, 72, 2], U32, lst)
                G.gate = P.sb("g_gate", [128, 72, 2], F32, lst)
                with contextlib.ExitStack() as lst2:
                    XM = merge_setup(P, l, D, S, lst2)
                    P.barrier()
                    for b in range(nbc):
                        phase_merge(P, l, D, S, b, XM, need_ctx, xsrc, G)
                P.barrier()
                if "noexp" not in phases:
                    phase_experts(P, l, D, S)
                if "nocomb" not in phases:
                    phase_combine(P, l, D, S, G, D.x2T, l == DEPTH - 1)
            xsrc = D.x2T
    P.barrier()
    P.finish(getattr(P, "dbg_out", []))
    P.finish([D.out])
    P.finish([D.uT.s(b) for b in range(nbc)] + [D.oatt.s(b) for b in range(nbc)] + [D.os5.s(b) for b in range(nbc)] + [D.orw.s(b) for b in range(nbc)])
    info = (P.ninstr, P.nwait)
    P.close()
    return nc, consts, info


class _View:
    def __init__(self, tt, dt):
        self.tt = tt
        self.dt = dt
        self.tok = tt.tok

    def __getitem__(self, idx):
        return self.tt[idx].bitcast(self.dt)

    def s(self, k):
        return self.tt.s(k)


class _ColView:
    def __init__(self, tt, l):
        self.tt = tt
        self.l = l
        self.tok = tt.tok

    def __getitem__(self, idx):
        return self.tt[:, self.l:self.l + 1]


def _tok(x):
    return x.tok if hasattr(x, "tok") else x


def host_inputs(inp, core, nbc=NBC):
    b0 = core * nbc
    x = np.asarray(inp["x"][b0:b0 + nbc], np.float32)
    ctx = np.asarray(inp["ctx"][b0:b0 + nbc], np.float32)
    m = {}
    m["xinT"] = np.ascontiguousarray(np.concatenate([ctx, x], axis=1).transpose(0, 2, 1))
    call = np.zeros((8, DM), np.float32)
    call[:nbc] = inp["c"][b0:b0 + nbc]
    call[4] = inp["c_ctx"]
    m["cT"] = np.ascontiguousarray(call.reshape(8, 8, 128).transpose(2, 1, 0))
    m["w_mod"] = np.asarray(inp["w_mod"], np.float32)
    m["w_in"] = np.asarray(inp["w_in"], np.float32)
    m["bmodT"] = fm(inp["b_mod"])
    m["norm1T"] = fm(inp["norm1"])
    m["norm2T"] = fm(inp["norm2"])
    L = DEPTH
    f32 = lambda k: np.asarray(inp[k], np.float32)
    for nm, key in (("re", "s5_a_re"), ("im", "s5_a_im")):
        a = f32(key)
        m["s5_A_rep_" + nm] = np.ascontiguousarray(np.repeat(a.reshape(L, 2, 2, 8, 64).transpose(3, 0, 1, 2, 4), 16, axis=0).reshape(128, L, 256))
        m["s5_A_st_" + nm] = np.ascontiguousarray(a.reshape(L, 2, 8, 2, 64).transpose(3, 4, 0, 1, 2).reshape(128, L, 16))
    ldt = f32("s5_log_dt")
    r = np.repeat(ldt.reshape(L, 2, 2, 8).transpose(3, 0, 1, 2), 16, axis=0)
    m["s5_ldt_rep"] = np.ascontiguousarray(np.broadcast_to(r[..., None], (128, L, 2, 2, 64)).reshape(128, L, 256))
    r = ldt.reshape(L, 2, 8, 2).transpose(3, 0, 1, 2)
    m["s5_ldt_st"] = np.ascontiguousarray(np.repeat(r[:, None], 64, axis=1).reshape(128, L, 16))
    for nm, key in (("re", "s5_b_re"), ("im", "s5_b_im")):
        bb = f32(key)
        m["s5_Bt_" + nm] = np.ascontiguousarray(bb.reshape(L, 2, 2, 8, 64, 16).transpose(3, 5, 0, 1, 2, 4).reshape(128, L, 256))
    for nm, key in (("re", "s5_c_re"), ("im", "s5_c_im")):
        cc = f32(key).reshape(L, 2, 8, 2, 16, 64)
        o = np.zeros((2, 64, L, 2, 8, 2, 2, 16), np.float32)
        for e in range(2):
            for pr in range(8):
                o[e, :, :, :, pr, pr % 2, e, :] = cc[:, :, pr, e].transpose(3, 0, 1, 2)
        m["s5_C_st_" + nm] = np.ascontiguousarray(o.reshape(128, L, 1024))
    m["s5_dT"] = fm(inp["s5_d"])
    m["s5_w_glu"] = f32("s5_w_glu")
    m["rw_conv"] = np.ascontiguousarray(fm(inp["rwkv_conv"]).reshape(128, L, 24))
    m["rw_w0"] = np.ascontiguousarray(fm(inp["rwkv_w0"]).reshape(128, L, 4))
    m["rw_a0"] = np.ascontiguousarray(fm(inp["rwkv_a0"]).reshape(128, L, 4))
    pv = np.stack([fm(inp[k_]) for k_ in ("rwkv_k_k", "rwkv_k_a", "rwkv_r_k", "rwkv_ln_w", "rwkv_ln_b")], axis=2)
    m["rw_pv"] = np.ascontiguousarray(pv.reshape(128, L, 10))
    m["rw_w2a2"] = np.ascontiguousarray(np.concatenate([f32("rwkv_w2"), f32("rwkv_a2")], axis=2))
    m["rw_g2"] = f32("rwkv_g2")
    for k_ in ("proj_att", "proj_s5", "proj_rwkv", "w_out", "exp_w_gate", "exp_w_up", "exp_w_down"):
        m[k_] = f32(k_)
    m["router_w"] = np.ascontiguousarray(np.concatenate([f32("router_g_w"), f32("router_e_w")], axis=-1))
    rb = np.concatenate([f32("router_g_b"), f32("router_e_b")], axis=-1)
    m["router_b"] = np.ascontiguousarray(np.broadcast_to(rb[None], (128, L, 36)))
    m["qg"] = np.ascontiguousarray(np.tile(np.asarray(inp["q_gain"], np.float32), (1, 2)).T)
    m["kg"] = np.ascontiguousarray(np.tile(np.asarray(inp["k_gain"], np.float32), (1, 2)).T)
    return m


def kernel(**inp):
    nc, consts, info = build()
    in_maps = []
    for core in range(NCORES):
        m = host_inputs(inp, core)
        for k, v in consts.items():
            m["c_" + k] = v
        in_maps.append(m)
    res = run_bass_kernel_spmd(nc, in_maps, core_ids=list(range(NCORES)))
    outs = [r["out"] for r in res.results]
    o = np.concatenate(outs, axis=0)
    return np.ascontiguousarray(o.transpose(0, 2, 1)).astype(np.float32)
```

```python
import contextlib
import math
import numpy as np
import concourse.bass as bass
import concourse.mybir as mybir
from concourse.bass_utils import run_bass_kernel_spmd

F32 = mybir.dt.float32
BF16 = mybir.dt.bfloat16
F32R = mybir.dt.float32r
I32 = mybir.dt.int32
U32 = mybir.dt.uint32
ALU = mybir.AluOpType
AF = mybir.ActivationFunctionType
AX = mybir.AxisListType

NCORES = 8
NBC = 4
DM = 1024
LAT = 2048
CTX = 256
T = LAT + CTX
DEPTH = 2
INW = 5376
OFF_S5 = 1024
OFF_RW = 1280
OFF_GATE = 2304
EPS = 1e-6
NEXP = 32
DEXP = 512
CAP = 1536


class Tok:
    __slots__ = ("lw", "rd", "name")

    def __init__(self, name=""):
        self.lw = None
        self.rd = {}
        self.name = name


class TT:
    def __init__(self, t, name):
        self.t = t
        self.name = name
        self.tok = Tok(name)
        self.slots = {}

    def __getitem__(self, idx):
        return self.t[idx]

    def s(self, k):
        if k not in self.slots:
            self.slots[k] = Tok(f"{self.name}.{k}")
        return self.slots[k]


def _tok(x):
    return x.tok if isinstance(x, TT) else x


class Prog:
    NDMA = 6

    def __init__(self, nc, debug=False):
        self.nc = nc
        self.debug = debug
        self.es = contextlib.ExitStack()
        self.engs = {"pe": nc.tensor, "act": nc.scalar, "dve": nc.vector, "pool": nc.gpsimd, "sp": nc.sync}
        self.sems = []
        self.vals = []
        self.esem = {}
        for n in self.engs:
            self.esem[n] = self._newsem("e_" + n)
        self.dsem = {}
        self.dnext = {}
        for q in ("sp", "act", "pool"):
            self.dsem[q] = [self._newsem(f"d_{q}{i}") for i in range(self.NDMA)]
            self.dnext[q] = 0
        self.seen = {n: {} for n in self.engs}
        self.ninstr = 0
        self.nwait = 0
        self.uid = 0

    def _newsem(self, name):
        s = self.es.enter_context(self.nc.semaphore(name))
        self.sems.append(s)
        self.vals.append(0)
        return len(self.sems) - 1

    def sb(self, name, shape, dtype=F32, stack=None):
        self.uid += 1
        t = (stack if stack is not None else self.es).enter_context(
            self.nc.sbuf_tensor(f"{name}_{self.uid}", list(shape), dtype))
        return TT(t, name)

    def ps(self, name, shape, dtype=F32, stack=None):
        self.uid += 1
        t = (stack if stack is not None else self.es).enter_context(
            self.nc.psum_tensor(f"{name}_{self.uid}", list(shape), dtype))
        return TT(t, name)

    def dram(self, name, shape, dtype=F32, kind="Internal"):
        if kind == "Internal" and self.debug:
            kind = "ExternalOutput"
        t = self.nc.dram_tensor(name, list(shape), dtype, kind=kind)
        return TT(t.ap(), name)

    def _need(self, eng, ev):
        if ev is None:
            return
        s, v = ev
        if self.seen[eng].get(s, 0) >= v:
            return
        self.engs[eng].wait_ge(self.sems[s], v)
        self.seen[eng][s] = v
        self.nwait += 1

    def _deps(self, eng, R, W):
        for x in R:
            self._need(eng, _tok(x).lw)
        for x in W:
            tk = _tok(x)
            self._need(eng, tk.lw)
            for s, v in tk.rd.items():
                self._need(eng, (s, v))

    def _mark(self, ev, R, W):
        for x in R:
            _tok(x).rd[ev[0]] = ev[1]
        for x in W:
            tk = _tok(x)
            tk.lw = ev
            tk.rd = {}

    def op(self, eng, meth, R=(), W=(), **kw):
        self._deps(eng, R, W)
        ins = getattr(self.engs[eng], meth)(**kw)
        s = self.esem[eng]
        self.vals[s] += 1
        ins.then_inc(self.sems[s], 1)
        self._mark((s, self.vals[s]), R, W)
        self.ninstr += 1
        return ins

    def V(self, meth, **kw):
        return self.op("dve", meth, **kw)

    def A(self, meth, **kw):
        return self.op("act", meth, **kw)

    def G(self, meth, **kw):
        return self.op("pool", meth, **kw)

    def PE(self, meth, **kw):
        return self.op("pe", meth, **kw)

    def mm(self, out, lhsT, rhs, start, stop, R, W):
        return self.op("pe", "matmul", R=R, W=W, out=out, lhsT=lhsT, rhs=rhs, start=start, stop=stop)

    def dma(self, q, R=(), W=(), meth="dma_start", **kw):
        self._deps(q, R, W)
        k = self.dnext[q]
        self.dnext[q] = (k + 1) % self.NDMA
        s = self.dsem[q][k]
        if self.vals[s] > 0:
            self._need(q, (s, self.vals[s]))
        ins = getattr(self.engs[q], meth)(**kw)
        self.vals[s] += 16
        ins.then_inc(self.sems[s], 16)
        self._mark((s, self.vals[s]), R, W)
        self.ninstr += 1
        return ins

    def barrier(self):
        for e in self.engs:
            for s in range(len(self.sems)):
                if self.vals[s] > 0:
                    self._need(e, (s, self.vals[s]))

    def finish(self, toks):
        for x in toks:
            self._need("sp", _tok(x).lw)

    def close(self):
        self.es.close()


class NS:
    pass


def fm(v):
    v = np.asarray(v, np.float32)
    lead = v.shape[:-1]
    n = v.shape[-1] // 128
    r = v.reshape(lead + (n, 128))
    return np.ascontiguousarray(np.moveaxis(r, -1, 0))


def make_consts():
    c = {}
    c["ident"] = np.eye(128, dtype=np.float32)
    c["ones"] = np.ones((128, 128), np.float32)
    bd = np.zeros((128, 128), np.float32)
    bd[:64, :64] = 1.0 / 64
    bd[64:, 64:] = 1.0 / 64
    c["bd64"] = bd
    bd1 = np.zeros((128, 128), np.float32)
    bd1[:64, :64] = 1.0
    bd1[64:, 64:] = 1.0
    c["bs64"] = bd1
    rot = np.zeros((128, 128), np.float32)
    for hh in range(2):
        for blk in range(2):
            base = hh * 64 + blk * 32
            for i in range(16):
                rot[base + 16 + i, base + i] = -1.0
                rot[base + i, base + 16 + i] = 1.0
    c["rot"] = rot
    pos = np.arange(LAT)
    row = pos // 64
    col = pos % 64
    inv = 10000.0 ** (-np.arange(16, dtype=np.float32) / 16.0)
    cos = np.zeros((64, LAT), np.float32)
    sin = np.zeros((64, LAT), np.float32)
    for blk, pp in enumerate((row, col)):
        ang = pp[None, :].astype(np.float32) * inv[:, None]
        cc = np.cos(ang).astype(np.float32)
        ss = np.sin(ang).astype(np.float32)
        cos[blk * 32:blk * 32 + 16] = cc
        cos[blk * 32 + 16:blk * 32 + 32] = cc
        sin[blk * 32:blk * 32 + 16] = ss
        sin[blk * 32 + 16:blk * 32 + 32] = ss
    c["cos"] = np.concatenate([cos, cos], 0)
    c["sin"] = np.concatenate([sin, sin], 0)
    sh0 = np.zeros((128, 128), np.float32)
    sh1 = np.zeros((128, 128), np.float32)
    for j in range(64):
        sh0[64 + j, j] = 1.0
        sh1[j, 64 + j] = 1.0
    c["sh0"] = sh0
    c["sh1"] = sh1
    ss = np.arange(64)[:, None]
    tt = np.arange(64)[None, :]
    for d in range(2):
        before = (ss < tt) if d == 0 else (ss > tt)
        beq = (ss <= tt) if d == 0 else (ss >= tt)
        st_ = before.astype(np.float32)
        inc = beq.astype(np.float32)
        c[f"MA{d}"] = np.tile(np.concatenate([-st_, -inc], 1), (1, 4))
        c[f"MB{d}"] = np.tile(np.concatenate([st_, inc], 1), (1, 4))
        c[f"MC{d}"] = np.tile(-(st_.T), (1, 4))
    c["YXI"] = np.tile(np.concatenate([np.zeros((64, 64), np.float32), np.eye(64, dtype=np.float32)], 1), (1, 4))
    rm = np.ones((128, T), np.float32)
    rm[:, ::64] = 0.0
    c["rmask"] = rm
    c["eoff"] = np.tile((np.arange(NEXP, dtype=np.float32) * CAP)[None, :], (128, 1))
    mg = np.zeros((128, 8), np.float32)
    for p in range(128):
        mg[p, p // 16] = 1.0
    c["maskG"] = mg
    tri = np.zeros((128, 128), np.float32)
    for k in range(128):
        tri[k, k + 1:] = 1.0
    c["tri"] = tri
    return c


def phase_mod(P, l, D, S):
    modT = S.modT
    with contextlib.ExitStack() as st:
        wm = [P.sb(f"wm{i}", [128, 8, 1024], F32, st) for i in range(2)]
        ct = P.sb("ct", [128, 8, 8], F32, st)
        sct = P.sb("sct", [128, 8, 8], F32, st)
        psA = S.psum[0]
        P.dma("sp", R=[D.cT], W=[ct], out=ct[:], in_=D.cT[:, :, :])
        P.A("activation", R=[ct], W=[sct], out=sct[:], in_=ct[:], func=AF.Silu)
        for j in range(6):
            w = wm[j % 2]
            P.dma("sp", R=[D.w_mod], W=[w], out=w[:],
                  in_=D.w_mod[l, :, j * 1024:(j + 1) * 1024].rearrange("(kt p) n -> p kt n", p=128))
            for m in range(8):
                o = (j * 8 + m) * 8
                for kt in range(8):
                    P.mm(psA[:, o:o + 8], w[:, kt, m * 128:(m + 1) * 128], sct[:, kt, :], kt == 0, kt == 7,
                         R=[w, sct], W=[psA])
        P.V("tensor_tensor", R=[psA, S.bmodT], W=[modT],
            out=modT[:], in0=psA[:, 0:384].rearrange("p (a b) -> p a b", b=8),
            in1=S.bmodT[:, l, :].unsqueeze(2).to_broadcast([128, 48, 8]), op=ALU.add)
        for j, nt in ((1, S.norm1T), (4, S.norm2T)):
            P.V("tensor_scalar", R=[modT], W=[modT], out=modT[:, j * 8:(j + 1) * 8, :], in0=modT[:, j * 8:(j + 1) * 8, :],
                scalar1=1.0, scalar2=None, op0=ALU.add)
            P.V("tensor_tensor", R=[modT, nt], W=[modT], out=modT[:, j * 8:(j + 1) * 8, :],
                in0=modT[:, j * 8:(j + 1) * 8, :],
                in1=nt[:, l, :].unsqueeze(2).to_broadcast([128, 8, 8]), op=ALU.mult)
    P.barrier()


TILES = [(0, 256)] + [(256 + i * 512, 512) for i in range(4)]


def rms_mod(P, S, x_t, n, col, jsc, jsh, out_fn, sq, rstd, ps):
    P.A("activation", R=[x_t], W=[sq], out=sq[:, :, 0:n], in_=x_t[:, :, 0:n], func=AF.Square)
    for kt in range(8):
        P.mm(ps[:, 0:n], S.ones[:], sq[:, kt, 0:n], kt == 0, kt == 7, R=[S.ones, sq], W=[ps])
    P.A("activation", R=[ps], W=[rstd], out=rstd[:, 0:n], in_=ps[:, 0:n], func=AF.Sqrt, scale=1.0 / DM,
        bias=S.epsb[:, 0:1])
    P.V("reciprocal", R=[rstd], W=[rstd], out=rstd[:, 0:n], in_=rstd[:, 0:n])
    for kt in range(8):
        P.V("tensor_tensor", R=[x_t, rstd], W=[sq], out=sq[:, kt, 0:n], in0=x_t[:, kt, 0:n], in1=rstd[:, 0:n],
            op=ALU.mult)
        o, wt = out_fn(kt)
        P.V("tensor_scalar", R=[sq, S.modT], W=wt, out=o, in0=sq[:, kt, 0:n],
            scalar1=S.modT[:, jsc * 8 + kt, col:col + 1], scalar2=S.modT[:, jsh * 8 + kt, col:col + 1],
            op0=ALU.mult, op1=ALU.add)


def phase_inproj(P, l, D, S, b, xsrc):
    with contextlib.ExitStack() as st:
        hT = P.sb("hT", [128, 8, T], BF16, st)
        xt = [P.sb(f"xt{i}", [128, 8, 512], F32, st) for i in range(2)]
        sq = P.sb("sq", [128, 8, 512], F32, st)
        rstd = P.sb("rstd", [128, 512], F32, st)
        wb = [P.sb(f"wb{i}", [128, 8, 768], BF16, st) for i in range(2)]
        stg = [P.sb(f"stg{i}", [128, T], F32, st) for i in range(2)]
        for ti, (t0, n) in enumerate(TILES):
            col = 4 if t0 < CTX else b
            x_t = xt[ti % 2]
            P.dma("sp", R=[xsrc], W=[x_t], out=x_t[:, :, 0:n],
                  in_=xsrc[b, :, t0:t0 + n].rearrange("(kt p) t -> p kt t", p=128))
            rms_mod(P, S, x_t, n, col, 1, 0, lambda kt: (hT[:, kt, t0:t0 + n], [hT.s(ti)]), sq, rstd, S.psum[0])
        hall = [hT.s(ti) for ti in range(len(TILES))]
        ci = 0
        for cb in range(7):
            w = wb[cb % 2]
            P.dma("pool", R=[D.w_in], W=[w], out=w[:],
                  in_=D.w_in[l, :, cb * 768:(cb + 1) * 768].rearrange("(kt p) n -> p kt n", p=128))
            for m in range(6):
                chunk = cb * 6 + m
                sg = stg[chunk % 2]
                for ti, (t0, n) in enumerate(TILES):
                    ps = S.psum[1 + (ci % 4)]
                    ci += 1
                    for kt in range(8):
                        P.mm(ps[:, 0:n], w[:, kt, m * 128:(m + 1) * 128], hT[:, kt, t0:t0 + n], kt == 0, kt == 7,
                             R=[w, hT.s(ti)], W=[ps])
                    if chunk * 128 >= OFF_GATE:
                        P.A("activation", R=[ps], W=[sg], out=sg[:, t0:t0 + n], in_=ps[:, 0:n], func=AF.Sigmoid)
                    elif ci % 2 == 0:
                        P.A("activation", R=[ps], W=[sg], out=sg[:, t0:t0 + n], in_=ps[:, 0:n], func=AF.Identity)
                    else:
                        P.V("tensor_copy", R=[ps], W=[sg], out=sg[:, t0:t0 + n], in_=ps[:, 0:n])
                P.dma("sp", R=[sg], W=[D.uT.s(b)], out=D.uT[b, chunk * 128:(chunk + 1) * 128, :], in_=sg[:])
    P.barrier()


def qk_norm_rope(P, S, load_fn, dst, nt, gain, stg, tmps):
    tmp, tmp2 = tmps
    for i in range(nt):
        sg = stg.s(i % 2)
        load_fn(i, i % 2)
        for ti, (t0, n) in enumerate(TILES):
            ps = S.psum[0]
            x = stg[:, i % 2, t0:t0 + n]
            d_ = dst[:, i, t0:t0 + n]
            P.A("activation", R=[sg], W=[tmp], out=tmp[:, 0:n], in_=x, func=AF.Square)
            P.mm(ps[:, 0:n], S.bd64[:], tmp[:, 0:n], True, True, R=[S.bd64, tmp], W=[ps])
            P.A("activation", R=[ps], W=[tmp], out=tmp[:, 0:n], in_=ps[:, 0:n], func=AF.Sqrt, scale=1.0,
                bias=S.epsb[:, 0:1])
            P.V("reciprocal", R=[tmp], W=[tmp], out=tmp[:, 0:n], in_=tmp[:, 0:n])
            if t0 < CTX:
                P.V("scalar_tensor_tensor", R=[sg, tmp, gain], W=[dst.s(i)], out=d_, in0=x, scalar=gain[:, 0:1], in1=tmp[:, 0:n],
                    op0=ALU.mult, op1=ALU.mult)
            else:
                P.V("scalar_tensor_tensor", R=[sg, tmp, gain], W=[sg], out=x, in0=x, scalar=gain[:, 0:1], in1=tmp[:, 0:n],
                    op0=ALU.mult, op1=ALU.mult)
                p0 = t0 - CTX
                ps2 = S.psum[1]
                P.mm(ps2[:, 0:n], S.rot[:], x, True, True, R=[S.rot, sg], W=[ps2])
                P.V("tensor_tensor", R=[ps2, S.sin], W=[tmp2], out=tmp2[:, 0:n], in0=ps2[:, 0:n],
                    in1=S.sin[:, p0:p0 + n], op=ALU.mult)
                P.V("tensor_tensor", R=[sg, S.cos], W=[tmp], out=tmp[:, 0:n], in0=x,
                    in1=S.cos[:, p0:p0 + n], op=ALU.mult)
                P.V("tensor_tensor", R=[tmp, tmp2], W=[dst.s(i)], out=d_, in0=tmp[:, 0:n],
                    in1=tmp2[:, 0:n], op=ALU.add)


def phase_attn(P, l, D, S, b, need_ctx):
    with contextlib.ExitStack() as st:
        qT = P.sb("qT", [128, 4, T], F32R, st)
        kTa = P.sb("kTa", [128, 2, T], F32R, st)
        kTb = P.sb("kTb", [128, 2, T], F32R, st)
        vaug = P.sb("vaug", [128, 18, 4, 192], BF16, st)
        oT = P.sb("oT", [128, 4, T], F32, st)
        pT = [P.sb(f"pT{i}", [128, 512], BF16, st) for i in range(3)]
        oacc = P.sb("oacc", [128, 512], F32, st)
        rden = P.sb("rden", [128, 512], F32, st)
        with contextlib.ExitStack() as st2:
            vT = P.sb("vT", [128, 2, T], F32, st2)
            stg = P.sb("qkstg", [128, 2, T], F32, st2)
            for i in range(2):
                P.dma("sp", R=[D.uT.s(b)], W=[vT.s(i)], out=vT[:, i, :], in_=D.uT[b, 768 + i * 128:768 + (i + 1) * 128, :])
            P.G("memset", W=[vaug], ap=vaug[:], constant=1.0)

            def ld_q(i, k):
                P.dma("sp", R=[D.uT.s(b)], W=[stg.s(k)], out=stg[:, k, :], in_=D.uT[b, i * 128:(i + 1) * 128, :])

            def ld_ka(i, k):
                P.dma("sp", R=[D.uT.s(b)], W=[stg.s(k)], out=stg[:, k, :], in_=D.uT[b, 512 + i * 128:512 + (i + 1) * 128, :])

            def ld_kb(i, k):
                P.dma("sp", R=[D.uT.s(b)], W=[stg.s(k)], out=stg[0:64, k, :], in_=D.uT[b, 512 + i * 128 + 64:512 + (i + 1) * 128, :])
                P.dma("sp", R=[D.uT.s(b)], W=[stg.s(k)], out=stg[64:128, k, :], in_=D.uT[b, 512 + i * 128:512 + i * 128 + 64, :])

            tmps = (P.sb("qk_tmp", [128, 512], F32, st2), P.sb("qk_tmp2", [128, 512], F32, st2))
            qk_norm_rope(P, S, ld_q, qT, 4, S.qg[l], stg, tmps)
            qk_norm_rope(P, S, ld_ka, kTa, 2, S.kg[l], stg, tmps)
            qk_norm_rope(P, S, ld_kb, kTb, 2, S.kg[l], stg, tmps)
            cnt = 0
            for i in range(2):
                for kt in range(18):
                    ps = S.psum[2 + cnt % 2]
                    cnt += 1
                    P.PE("transpose", R=[vT.s(i), S.ident], W=[ps], out=ps[:, 0:128], in_=vT[:, i, kt * 128:(kt + 1) * 128],
                         identity=S.ident[:])
                    for e in range(2):
                        P.V("tensor_copy", R=[ps], W=[vaug], out=vaug[:, kt, 2 * i + e, 64:128],
                            in_=ps[:, e * 64:(e + 1) * 64])
        P.barrier()
        qsegs = [(CTX + qc * 512, 512, list(range(18))) for qc in range(4)]
        if need_ctx:
            qsegs.append((0, CTX, [0, 1]))
        gi_ = 0
        for h in range(4):
            i, e = h // 2, h % 2
            for g in range(2):
                ksrc = kTa if e == g else kTb
                shm = S.sh0 if g == 0 else S.sh1
                lo = g * 64
                for (q0, qn, kts) in qsegs:
                    acc = S.psum[4 + (gi_ % 2)]
                    gi_ += 1
                    va_of = (lambda kt: vaug[:, kt, h, 64:192]) if g == 0 else (lambda kt: vaug[:, kt, h, 0:128])

                    def score(ki):
                        kt = kts[ki]
                        pss = S.psum[6 + (ki % 2)]
                        P.mm(pss[:, 0:qn], ksrc[lo:lo + 64, i, kt * 128:(kt + 1) * 128], qT[lo:lo + 64, h, q0:q0 + qn],
                             True, True, R=[ksrc.s(i), qT.s(h)], W=[pss])

                    score(0)
                    for ki, kt in enumerate(kts):
                        pss = S.psum[6 + (ki % 2)]
                        pt = pT[ki % 3]
                        P.A("activation", R=[pss], W=[pt], out=pt[:, 0:qn], in_=pss[:, 0:qn], func=AF.Exp, scale=0.125)
                        if ki + 1 < len(kts):
                            score(ki + 1)
                        P.mm(acc[:, 0:qn], va_of(kt), pt[:, 0:qn], ki == 0, ki == len(kts) - 1, R=[vaug, pt], W=[acc])
                    P.A("activation", R=[acc], W=[oacc], out=oacc[:, 0:qn], in_=acc[:, 0:qn], func=AF.Identity)
                    psd = S.psum[0]
                    P.mm(psd[:, 0:qn], shm[:], oacc[:, 0:qn], True, True, R=[shm, oacc], W=[psd])
                    P.V("reciprocal", R=[psd], W=[rden], out=rden[lo:lo + 64, 0:qn], in_=psd[lo:lo + 64, 0:qn])
                    P.V("tensor_tensor", R=[oacc, rden], W=[oT.s(h)], out=oT[lo:lo + 64, h, q0:q0 + qn],
                        in0=oacc[lo:lo + 64, 0:qn], in1=rden[lo:lo + 64, 0:qn], op=ALU.mult)
        c0 = 0 if need_ctx else CTX
        for h in range(4):
            P.dma("sp", R=[oT.s(h)], W=[D.oatt.s(b)], out=D.oatt[b, h * 128:(h + 1) * 128, c0:T], in_=oT[:, h, c0:T])
    P.barrier()


TWO_PI = 2.0 * math.pi


def _tt(P, out, a, b, op, R, W):
    P.V("tensor_tensor", R=R, W=W, out=out, in0=a, in1=b, op=op)


def _ts(P, out, a, s1, op0, R, W, s2=None, op1=None):
    if op1 is None:
        P.V("tensor_scalar", R=R, W=W, out=out, in0=a, scalar1=s1, scalar2=None, op0=op0)
    else:
        P.V("tensor_scalar", R=R, W=W, out=out, in0=a, scalar1=s1, scalar2=s2, op0=op0, op1=op1)


def sin_turns(P, st, r, out, F, name):
    ki = P.sb(name + "_ki", [128, F], I32, st)
    kf = P.sb(name + "_kf", [128, F], F32, st)
    m = P.sb(name + "_m", [128, F], F32, st)
    P.V("tensor_copy", R=[r], W=[ki], out=ki[:], in_=r[:])
    P.V("tensor_copy", R=[ki], W=[kf], out=kf[:], in_=ki[:])
    _tt(P, kf[:], r[:], kf[:], ALU.subtract, [r, kf], [kf])
    _ts(P, m[:], kf[:], 0.5, ALU.is_gt, [kf], [m])
    _tt(P, kf[:], kf[:], m[:], ALU.subtract, [kf, m], [kf])
    _ts(P, m[:], kf[:], -0.5, ALU.is_lt, [kf], [m])
    _tt(P, kf[:], kf[:], m[:], ALU.add, [kf, m], [kf])
    P.A("activation", R=[kf], W=[out], out=out[:], in_=kf[:], func=AF.Sin, scale=TWO_PI)


def s5_derive(P, st, are, aim, ldt, F, name, need_z):
    mk = lambda n: P.sb(f"{name}_{n}", [128, F], F32, st)
    dt, rho, th, r, r2, cth, sth = mk("dt"), mk("rho"), mk("th"), mk("r"), mk("r2"), mk("cth"), mk("sth")
    P.A("activation", R=[ldt], W=[dt], out=dt[:], in_=ldt[:], func=AF.Exp)
    _tt(P, rho[:], are[:], dt[:], ALU.mult, [are, dt], [rho])
    P.A("activation", R=[rho], W=[rho], out=rho[:], in_=rho[:], func=AF.Exp)
    _tt(P, th[:], aim[:], dt[:], ALU.mult, [aim, dt], [th])
    _ts(P, r[:], th[:], 1.0 / TWO_PI, ALU.mult, [th], [r])
    _ts(P, r2[:], r[:], 0.25, ALU.add, [r], [r2])
    sin_turns(P, st, r, sth, F, name + "_s")
    sin_turns(P, st, r2, cth, F, name + "_c")
    res = dict(rho=rho, cth=cth, sth=sth)
    if need_z:
        abr, abi, den, nr, zre, zim, t1 = mk("abr"), mk("abi"), mk("den"), mk("nr"), mk("zre"), mk("zim"), mk("t1")
        _tt(P, abr[:], rho[:], cth[:], ALU.mult, [rho, cth], [abr])
        _tt(P, abi[:], rho[:], sth[:], ALU.mult, [rho, sth], [abi])
        _tt(P, den[:], are[:], are[:], ALU.mult, [are], [den])
        _tt(P, t1[:], aim[:], aim[:], ALU.mult, [aim], [t1])
        _tt(P, den[:], den[:], t1[:], ALU.add, [den, t1], [den])
        P.V("reciprocal", R=[den], W=[den], out=den[:], in_=den[:])
        _ts(P, nr[:], abr[:], -1.0, ALU.add, [abr], [nr])
        _tt(P, zre[:], nr[:], are[:], ALU.mult, [nr, are], [zre])
        _tt(P, t1[:], abi[:], aim[:], ALU.mult, [abi, aim], [t1])
        _tt(P, zre[:], zre[:], t1[:], ALU.add, [zre, t1], [zre])
        _tt(P, zre[:], zre[:], den[:], ALU.mult, [zre, den], [zre])
        _tt(P, zim[:], abi[:], are[:], ALU.mult, [abi, are], [zim])
        _tt(P, t1[:], nr[:], aim[:], ALU.mult, [nr, aim], [t1])
        _tt(P, zim[:], zim[:], t1[:], ALU.subtract, [zim, t1], [zim])
        _tt(P, zim[:], zim[:], den[:], ALU.mult, [zim, den], [zim])
        res.update(zre=zre, zim=zim)
    return res


def s5_setup(P, l, D, S, st):
    X = NS()
    ld = lambda nm, src, shp, dt=F32: _load(P, st, nm, src, shp, dt)
    are = ld("s5are", D.s5_A_rep_re[:, l, :], [128, 256])
    aim = ld("s5aim", D.s5_A_rep_im[:, l, :], [128, 256])
    ldt = ld("s5ldt", D.s5_ldt_rep[:, l, :], [128, 256])
    bre = ld("s5bre", D.s5_Bt_re[:, l, :], [128, 256])
    bim = ld("s5bim", D.s5_Bt_im[:, l, :], [128, 256])
    X.BTr = P.sb("BTr", [128, 4, 8, 64], BF16, st)
    X.BTi = P.sb("BTi", [128, 4, 8, 64], BF16, st)
    with contextlib.ExitStack() as st2:
        r = s5_derive(P, st2, are, aim, ldt, 256, "dr", True)
        bbr = P.sb("bbr", [128, 256], F32, st2)
        bbi = P.sb("bbi", [128, 256], F32, st2)
        t1 = P.sb("bt1", [128, 256], F32, st2)
        _tt(P, bbr[:], r["zre"][:], bre[:], ALU.mult, [r["zre"], bre], [bbr])
        _tt(P, t1[:], r["zim"][:], bim[:], ALU.mult, [r["zim"], bim], [t1])
        _tt(P, bbr[:], bbr[:], t1[:], ALU.subtract, [bbr, t1], [bbr])
        _tt(P, bbi[:], r["zre"][:], bim[:], ALU.mult, [r["zre"], bim], [bbi])
        _tt(P, t1[:], r["zim"][:], bre[:], ALU.mult, [r["zim"], bre], [t1])
        _tt(P, bbi[:], bbi[:], t1[:], ALU.add, [bbi, t1], [bbi])
        for dst, src in ((X.BTr, bbr), (X.BTi, bbi)):
            for q in range(4):
                P.V("tensor_tensor", R=[src, S.maskG], W=[dst], out=dst[:, q, :, :],
                    in0=src[:, q * 64:(q + 1) * 64].unsqueeze(1).to_broadcast([128, 8, 64]),
                    in1=S.maskG[:, :].unsqueeze(2).to_broadcast([128, 8, 64]), op=ALU.mult)
    P.barrier()
    sare = ld("s5sare", D.s5_A_st_re[:, l, :], [128, 16])
    saim = ld("s5saim", D.s5_A_st_im[:, l, :], [128, 16])
    sldt = ld("s5sldt", D.s5_ldt_st[:, l, :], [128, 16])
    dbg(P, "sare", sare, sare[:], [128, 16])
    dbg(P, "sldt", sldt, sldt[:], [128, 16])
    r2 = s5_derive(P, st, sare, saim, sldt, 16, "ds", False)
    X.rho = r2["rho"]
    dbg(P, "rho", X.rho, X.rho[:], [128, 16])
    dbg(P, "cth", r2["cth"], r2["cth"][:], [128, 16])
    dbg(P, "sth", r2["sth"], r2["sth"][:], [128, 16])
    X.cosT = P.sb("s5cos", [128, 16, 256], F32, st)
    X.sinT = P.sb("s5sin", [128, 16, 256], F32, st)
    nsin = P.sb("s5nsin", [128, 16, 256], F32, st)
    X.nsinT = nsin
    P.V("memset", W=[X.cosT], ap=X.cosT[:, :, 0:1], constant=1.0)
    P.V("memset", W=[X.sinT], ap=X.sinT[:, :, 0:1], constant=0.0)
    P.V("tensor_copy", R=[r2["cth"]], W=[X.cosT], out=X.cosT[:, :, 1], in_=r2["cth"][:])
    P.V("tensor_copy", R=[r2["sth"]], W=[X.sinT], out=X.sinT[:, :, 1], in_=r2["sth"][:])
    tmpa = P.sb("s5tmpa", [128, 16, 128], F32, st)
    m = 2
    while m < 256:
        cm, sm = X.cosT[:, :, m], X.sinT[:, :, m]
        c1, s1 = X.cosT[:, :, 1], X.sinT[:, :, 1]
        cp, sp_ = X.cosT[:, :, m - 1], X.sinT[:, :, m - 1]
        ta = tmpa[:, :, 0]
        tb = tmpa[:, :, 1]
        _tt(P, ta, cp, c1, ALU.mult, [X.cosT], [tmpa])
        _tt(P, tb, sp_, s1, ALU.mult, [X.sinT], [tmpa])
        _tt(P, cm, ta, tb, ALU.subtract, [tmpa], [X.cosT])
        _tt(P, ta, cp, s1, ALU.mult, [X.cosT, X.sinT], [tmpa])
        _tt(P, tb, sp_, c1, ALU.mult, [X.cosT, X.sinT], [tmpa])
        _tt(P, sm, ta, tb, ALU.add, [tmpa], [X.sinT])
        n = m - 1
        cmb = X.cosT[:, :, m:m + 1].to_broadcast([128, 16, n])
        smb = X.sinT[:, :, m:m + 1].to_broadcast([128, 16, n])
        cj, sj = X.cosT[:, :, 1:m], X.sinT[:, :, 1:m]
        co, so = X.cosT[:, :, m + 1:2 * m], X.sinT[:, :, m + 1:2 * m]
        ta = tmpa[:, :, 0:n]
        _tt(P, ta, sj, smb, ALU.mult, [X.sinT], [tmpa])
        _tt(P, co, cj, cmb, ALU.mult, [X.cosT], [X.cosT])
        _tt(P, co, co, ta, ALU.subtract, [X.cosT, tmpa], [X.cosT])
        _tt(P, ta, cj, smb, ALU.mult, [X.cosT, X.sinT], [tmpa])
        _tt(P, so, sj, cmb, ALU.mult, [X.sinT, X.cosT], [X.sinT])
        _tt(P, so, so, ta, ALU.add, [X.sinT, tmpa], [X.sinT])
        m *= 2
    _ts(P, nsin[:], X.sinT[:], -1.0, ALU.mult, [X.sinT], [nsin])
    dbg(P, "cosT", X.cosT, X.cosT[:], [128, 16, 256])
    dbg(P, "sinT", X.sinT, X.sinT[:], [128, 16, 256])
    dbg(P, "BTr", X.BTr, X.BTr[:], [128, 4, 8, 64], BF16)
    X.rhoT = P.sb("s5rhoT", [128, 16, 256], F32, st)
    P.V("tensor_copy", R=[X.rho], W=[X.rhoT], out=X.rhoT[:], in_=X.rho[:].unsqueeze(2).to_broadcast([128, 16, 256]))
    cre = ld("s5cre", D.s5_C_st_re[:, l, :], [128, 16 * 64])
    cim = ld("s5cim", D.s5_C_st_im[:, l, :], [128, 16 * 64])
    X.Cr = P.sb("s5Cr", [128, 16, 64], BF16, st)
    X.Ci = P.sb("s5Ci", [128, 16, 64], BF16, st)
    P.V("tensor_copy", R=[cre], W=[X.Cr], out=X.Cr[:], in_=cre[:].rearrange("p (a b) -> p a b", b=64))
    _ts(P, X.Ci[:], cim[:].rearrange("p (a b) -> p a b", b=64), -1.0, ALU.mult, [cim], [X.Ci])
    X.dT = ld("s5dT", D.s5_dT[:, l, :], [128, 2])
    X.wglu = P.sb("s5wglu", [128, 2, 512], BF16, st)
    P.dma("pool", R=[D.s5_w_glu], W=[X.wglu], out=X.wglu[:], in_=D.s5_w_glu[l].rearrange("(kt p) n -> p kt n", p=128))
    return X


def dbg(P, name, tt, ap, shape, dt=F32):
    if not P.debug:
        return
    d = P.dram("dbg_" + name, shape, dt, kind="ExternalOutput")
    P.dma("sp", R=[tt], W=[d], out=d[tuple(slice(None) for _ in shape)], in_=ap)
    P.dbg_out = getattr(P, "dbg_out", []) + [d]


def _load(P, st, nm, src_ap, shp, dt=F32):
    t = P.sb(nm, shp, dt, st)
    P.dma("sp", R=[], W=[t], out=t[:], in_=src_ap)
    return t


S5CH = 256


def phase_s5(P, l, D, S, b, X, need_ctx):
    nch = T // S5CH
    with contextlib.ExitStack() as st:
        sT = P.sb("s5sT", [128, 2, T], F32, st)
        sT16 = P.sb("s5sT16", [128, 2, T], BF16, st)
        yacc = P.sb("s5yacc", [128, 2, T], F32, st)
        for ft in range(2):
            P.dma("sp", R=[D.uT.s(b)], W=[sT], out=sT[:, ft, :], in_=D.uT[b, OFF_S5 + ft * 128:OFF_S5 + (ft + 1) * 128, :])
        P.A("activation", R=[sT], W=[sT16], out=sT16[:], in_=sT[:], func=AF.Identity)
        for ft in range(2):
            _ts(P, yacc[:, ft, :], sT[:, ft, :], X.dT[:, ft:ft + 1], ALU.mult, [sT, X.dT], [yacc.s((ft, 0)), yacc.s((ft, 1))])
        NCHAIN = 4
        mk = lambda n, dt=F32: [P.sb(f"s5{n}{i}", [128, S5CH], dt, st) for i in range(2 * NCHAIN)]
        xr_re, xr_im, g_re, g_im = mk("xrr"), mk("xri"), mk("gr"), mk("gi")
        h_re, h_im = mk("hr", BF16), mk("hi", BF16)
        hps = [P.sb(f"s5hp{i}", [128, 4], F32, st) for i in range(NCHAIN)]
        tAs = [P.sb(f"s5tA{i}", [128, S5CH], F32, st) for i in range(NCHAIN)]
        tBs = [P.sb(f"s5tB{i}", [128, S5CH], F32, st) for i in range(NCHAIN)]
        tCs = [P.sb(f"s5tC{i}", [128, S5CH], F32, st) for i in range(NCHAIN)]

        def chain(ci, d, pr):
            ft, pp = pr // 4, pr % 4
            dp = d * 8 + pr
            hp, tA, tB, tC = hps[ci], tAs[ci], tBs[ci], tCs[ci]
            cT_, sT_, nsT_ = X.cosT[:, dp, :], X.sinT[:, dp, :], X.nsinT[:, dp, :]
            order = list(range(nch)) if d == 0 else [0] + list(range(nch - 1, 0, -1))
            rv = (lambda ap: ap) if d == 0 else (lambda ap: ap[:, ::-1])
            ytok = yacc.s((ft, pp // 2))
            for oi, ch in enumerate(order):
                c0 = ch * S5CH
                k = ci * 2 + (oi % 2)
                ps_r, ps_i, ps_yb = S.psum[2 * ci], S.psum[2 * ci + 1], S.psum[2 * ci]
                lr = X.BTr[:, d * 2 + ft, 2 * pp:2 * pp + 2, :].rearrange("p a b -> p (a b)")
                li = X.BTi[:, d * 2 + ft, 2 * pp:2 * pp + 2, :].rearrange("p a b -> p (a b)")
                P.mm(ps_r[:, 0:S5CH], lr, sT16[:, ft, c0:c0 + S5CH], True, True, R=[X.BTr, sT16], W=[ps_r])
                P.mm(ps_i[:, 0:S5CH], li, sT16[:, ft, c0:c0 + S5CH], True, True, R=[X.BTi, sT16], W=[ps_i])
                yield
                xrr, xri, gr, gi, hr, hi = xr_re[k], xr_im[k], g_re[k], g_im[k], h_re[k], h_im[k]
                pr_, pi_ = ps_r[:, 0:S5CH], ps_i[:, 0:S5CH]
                _tt(P, rv(tA[:]), pr_, rv(cT_), ALU.mult, [ps_r, X.cosT], [tA])
                yield
                _tt(P, rv(tB[:]), pi_, rv(sT_), ALU.mult, [ps_i, X.sinT], [tB])
                yield
                _tt(P, xrr[:], tA[:], tB[:], ALU.add, [tA, tB], [xrr])
                yield
                _tt(P, rv(tA[:]), pi_, rv(cT_), ALU.mult, [ps_i, X.cosT], [tA])
                yield
                _tt(P, rv(tB[:]), pr_, rv(nsT_), ALU.mult, [ps_r, X.nsinT], [tB])
                yield
                _tt(P, xri[:], tA[:], tB[:], ALU.add, [tA, tB], [xri])
                yield
                if oi == 0:
                    ini_r, ini_i = 0.0, 0.0
                    Rini = []
                else:
                    c1, s1 = X.cosT[:, dp, 1:2], X.sinT[:, dp, 1:2]
                    ns1 = X.nsinT[:, dp, 1:2]
                    _tt(P, hp[:, 2:3], hp[:, 0:1], c1, ALU.mult, [hp, X.cosT], [hp])
                    yield
                    P.V("scalar_tensor_tensor", R=[hp, X.nsinT], W=[hp], out=hp[:, 2:3], in0=hp[:, 1:2], scalar=ns1,
                        in1=hp[:, 2:3], op0=ALU.mult, op1=ALU.add)
                    yield
                    _tt(P, hp[:, 3:4], hp[:, 0:1], s1, ALU.mult, [hp, X.sinT], [hp])
                    yield
                    P.V("scalar_tensor_tensor", R=[hp, X.cosT], W=[hp], out=hp[:, 3:4], in0=hp[:, 1:2], scalar=c1,
                        in1=hp[:, 3:4], op0=ALU.mult, op1=ALU.add)
                    yield
                    ini_r, ini_i = hp[:, 2:3], hp[:, 3:4]
                    Rini = [hp]
                P.V("tensor_tensor_scan", R=[X.rhoT, xrr] + Rini, W=[gr], out=gr[:], data0=X.rhoT[:, dp, :], data1=xrr[:],
                    initial=ini_r, op0=ALU.mult, op1=ALU.add)
                yield
                P.V("tensor_tensor_scan", R=[X.rhoT, xri] + Rini, W=[gi], out=gi[:], data0=X.rhoT[:, dp, :], data1=xri[:],
                    initial=ini_i, op0=ALU.mult, op1=ALU.add)
                yield
                _tt(P, tA[:], gr[:], cT_, ALU.mult, [gr, X.cosT], [tA])
                yield
                _tt(P, tB[:], gi[:], nsT_, ALU.mult, [gi, X.nsinT], [tB])
                yield
                _tt(P, tA[:], tA[:], tB[:], ALU.add, [tA, tB], [tA])
                yield
                P.A("activation", R=[tA], W=[hr], out=rv(hr[:]), in_=tA[:], func=AF.Identity)
                P.A("activation", R=[tA], W=[hp], out=hp[:, 0:1], in_=tA[:, S5CH - 1:S5CH], func=AF.Identity)
                _tt(P, tC[:], gi[:], cT_, ALU.mult, [gi, X.cosT], [tC])
                yield
                _tt(P, tB[:], gr[:], sT_, ALU.mult, [gr, X.sinT], [tB])
                yield
                _tt(P, tC[:], tC[:], tB[:], ALU.add, [tC, tB], [tC])
                yield
                P.A("activation", R=[tC], W=[hi], out=rv(hi[:]), in_=tC[:], func=AF.Identity)
                P.A("activation", R=[tC], W=[hp], out=hp[:, 1:2], in_=tC[:, S5CH - 1:S5CH], func=AF.Identity)
                pb = (pp // 2) * 64
                P.mm(ps_yb[pb:pb + 64, 256:256 + S5CH], X.Cr[:, dp, :], hr[:], True, False, R=[X.Cr, hr], W=[ps_yb])
                P.mm(ps_yb[pb:pb + 64, 256:256 + S5CH], X.Ci[:, dp, :], hi[:], False, True, R=[X.Ci, hi], W=[ps_yb])
                yield
                _tt(P, yacc[pb:pb + 64, ft, c0:c0 + S5CH], yacc[pb:pb + 64, ft, c0:c0 + S5CH],
                    ps_yb[pb:pb + 64, 256:256 + S5CH], ALU.add, [ytok, ps_yb], [ytok])
                yield

        for pr in range(0, 8, 2):
            gens = [chain(0, 0, pr), chain(1, 1, pr), chain(2, 0, pr + 1), chain(3, 1, pr + 1)]
            alive = [True] * 4
            while any(alive):
                for gi_, gq in enumerate(gens):
                    if alive[gi_]:
                        try:
                            next(gq)
                        except StopIteration:
                            alive[gi_] = False
        P.barrier()
        dbg(P, f"yacc{b}", yacc, yacc[:], [128, 2, T])
        ge = sT16
        gt = P.sb("s5gt", [128, 512], F32, st)
        for ft in range(2):
            for (t0, n) in TILES:
                y = yacc[:, ft, t0:t0 + n]
                P.A("activation", R=[yacc], W=[gt], out=gt[:, 0:n], in_=y, func=AF.Square)
                _ts(P, gt[:, 0:n], gt[:, 0:n], 0.044715, ALU.mult, [gt], [gt], 1.0, ALU.add)
                _tt(P, gt[:, 0:n], gt[:, 0:n], y, ALU.mult, [gt, yacc], [gt])
                P.A("activation", R=[gt], W=[gt], out=gt[:, 0:n], in_=gt[:, 0:n], func=AF.Tanh,
                    scale=math.sqrt(2.0 / math.pi))
                _ts(P, gt[:, 0:n], gt[:, 0:n], 1.0, ALU.add, [gt], [gt], 0.5, ALU.mult)
                _tt(P, ge[:, ft, t0:t0 + n], gt[:, 0:n], y, ALU.mult, [gt, yacc], [ge])
        osb = sT
        for m in range(2):
            for (t0, n) in TILES:
                p1, p2 = S.psum[0], S.psum[1]
                for kt in range(2):
                    P.mm(p1[:, 0:n], X.wglu[:, kt, m * 128:(m + 1) * 128], ge[:, kt, t0:t0 + n], kt == 0, kt == 1, R=[X.wglu, ge], W=[p1])
                for kt in range(2):
                    P.mm(p2[:, 0:n], X.wglu[:, kt, 256 + m * 128:256 + (m + 1) * 128], ge[:, kt, t0:t0 + n], kt == 0, kt == 1,
                         R=[X.wglu, ge], W=[p2])
                P.A("activation", R=[p2], W=[gt], out=gt[:, 0:n], in_=p2[:, 0:n], func=AF.Sigmoid)
                _tt(P, osb[:, m, t0:t0 + n], p1[:, 0:n], gt[:, 0:n], ALU.mult, [p1, gt], [osb])
        c0 = 0 if need_ctx else CTX
        for m in range(2):
            P.dma("sp", R=[osb], W=[D.os5.s(b)], out=D.os5[b, m * 128:(m + 1) * 128, c0:T], in_=osb[:, m, c0:T])
    P.barrier()


def merge_setup(P, l, D, S, st):
    X = NS()
    X.pa = P.sb("m_pa", [128, 4, DM], BF16, st)
    X.p5 = P.sb("m_p5", [128, 2, DM], BF16, st)
    X.pr = P.sb("m_pr", [128, 2, DM], BF16, st)
    X.wo = P.sb("m_wo", [128, 8, DM], BF16, st)
    for t, src in ((X.pa, D.proj_att), (X.p5, D.proj_s5), (X.pr, D.proj_rwkv), (X.wo, D.w_out)):
        P.dma("pool", R=[src], W=[t], out=t[:], in_=src[l].rearrange("(kt p) n -> p kt n", p=128))
    X.wr = P.sb("m_wr", [128, 8, 36], F32, st)
    P.dma("sp", R=[D.router_w], W=[X.wr], out=X.wr[:], in_=D.router_w[l].rearrange("(kt p) n -> p kt n", p=128))
    X.rb = P.sb("m_rb", [128, 36], F32, st)
    P.dma("sp", R=[D.router_b], W=[X.rb], out=X.rb[:], in_=D.router_b[:, l, :])
    X.carry = P.sb("m_carry", [128, NEXP], F32, st)
    P.V("memset", W=[X.carry], ap=X.carry[:], constant=0.0)
    X.eoff = P.sb("m_eoff", [128, NEXP], F32, st)
    P.dma("sp", R=[D.consts["eoff"]], W=[X.eoff], out=X.eoff[:], in_=D.consts["eoff"][:, :])
    return X


def phase_merge(P, l, D, S, b, X, need_ctx, xsrc, G):
    tiles = TILES if need_ctx else TILES[1:]
    with contextlib.ExitStack() as st:
        oa = P.sb("mg_oa", [128, 4, 512], BF16, st)
        o5 = P.sb("mg_o5", [128, 2, 512], BF16, st)
        orw = P.sb("mg_or", [128, 2, 512], BF16, st)
        gt = [P.sb(f"mg_g{i}", [128, 3, 512], F32, st) for i in range(2)]
        xt = P.sb("mg_x", [128, 8, 512], F32, st)
        mg = P.sb("mg_m", [128, 8, 512], BF16, st)
        t1 = P.sb("mg_t1", [128, 512], F32, st)
        t2 = P.sb("mg_t2", [128, 512], F32, st)
        sq = P.sb("mg_sq", [128, 8, 512], F32, st)
        rstd = P.sb("mg_rstd", [128, 512], F32, st)
        h2 = P.sb("mg_h2", [128, 8, 512], F32, st)
        htm = P.sb("mg_htm", [128, DM], F32, st)
        rt = {k: P.sb("mg_r" + k, shp, dt, st) for k, shp, dt in (
            ("lg", [128, 36], F32), ("mx", [128, 8], F32), ("ohg", [128, 4], F32), ("el", [128, 8], F32),
            ("ee", [128, 8], F32), ("t8", [128, 8], F32), ("oh1", [128, 8], F32), ("oh2", [128, 8], F32),
            ("M1", [128, 4, 8], F32), ("M2", [128, 4, 8], F32), ("M", [128, NEXP], F32), ("pos", [128, NEXP], F32),
            ("s1", [128, 4], F32), ("gs", [128, 4], F32), ("si", [128, 2], I32))}
        for (t0, n) in tiles:
            col = 4 if t0 < CTX else b
            P.dma("pool", R=[D.oatt.s(b)], W=[oa], out=oa[:, :, 0:n], in_=D.oatt[b, :, t0:t0 + n].rearrange("(k p) t -> p k t", p=128))
            P.dma("pool", R=[D.os5.s(b)], W=[o5], out=o5[:, :, 0:n], in_=D.os5[b, :, t0:t0 + n].rearrange("(k p) t -> p k t", p=128))
            P.dma("pool", R=[D.orw.s(b)], W=[orw], out=orw[:, :, 0:n], in_=D.orw[b, :, t0:t0 + n].rearrange("(k p) t -> p k t", p=128))
            P.dma("sp", R=[xsrc], W=[xt], out=xt[:, :, 0:n], in_=xsrc[b, :, t0:t0 + n].rearrange("(kt p) t -> p kt t", p=128))
            for m in range(8):
                g = gt[m % 2]
                P.dma("sp", R=[D.uT.s(b)], W=[g], out=g[:, :, 0:n],
                      in_=D.uT[b, OFF_GATE:OFF_GATE + 3 * DM, t0:t0 + n].rearrange("(j m p) t -> p j m t", j=3, m=8, p=128)[:, :, m, :])
                pa_, p5_, pr_ = S.psum[1], S.psum[2], S.psum[3]
                for k in range(4):
                    P.mm(pa_[:, 0:n], X.pa[:, k, m * 128:(m + 1) * 128], oa[:, k, 0:n], k == 0, k == 3, R=[X.pa, oa], W=[pa_])
                for k in range(2):
                    P.mm(p5_[:, 0:n], X.p5[:, k, m * 128:(m + 1) * 128], o5[:, k, 0:n], k == 0, k == 1, R=[X.p5, o5], W=[p5_])
                for k in range(2):
                    P.mm(pr_[:, 0:n], X.pr[:, k, m * 128:(m + 1) * 128], orw[:, k, 0:n], k == 0, k == 1, R=[X.pr, orw], W=[pr_])
                _tt(P, t1[:, 0:n], pa_[:, 0:n], g[:, 0, 0:n], ALU.mult, [pa_, g], [t1])
                _tt(P, t2[:, 0:n], p5_[:, 0:n], g[:, 1, 0:n], ALU.mult, [p5_, g], [t2])
                _tt(P, t1[:, 0:n], t1[:, 0:n], t2[:, 0:n], ALU.add, [t1, t2], [t1])
                _tt(P, t2[:, 0:n], pr_[:, 0:n], g[:, 2, 0:n], ALU.mult, [pr_, g], [t2])
                _tt(P, mg[:, m, 0:n], t1[:, 0:n], t2[:, 0:n], ALU.add, [t1, t2], [mg])
            for m in range(8):
                po = S.psum[4 + m % 2]
                for k in range(8):
                    P.mm(po[:, 0:n], X.wo[:, k, m * 128:(m + 1) * 128], mg[:, k, 0:n], k == 0, k == 7, R=[X.wo, mg], W=[po])
                P.V("scalar_tensor_tensor", R=[po, xt, S.modT], W=[xt], out=xt[:, m, 0:n], in0=po[:, 0:n],
                    scalar=S.modT[:, 2 * 8 + m, col:col + 1], in1=xt[:, m, 0:n], op0=ALU.mult, op1=ALU.add)
            P.dma("sp", R=[xt], W=[D.x1T.s(b)], out=D.x1T[b, :, t0:t0 + n].rearrange("(kt p) t -> p kt t", p=128), in_=xt[:, :, 0:n])
            rms_mod(P, S, xt, n, col, 4, 3, lambda kt: (h2[:, kt, 0:n], [h2]), sq, rstd, S.psum[0])
            for sti in range(n // 128):
                tok = slice(sti * 128, (sti + 1) * 128)
                gi = G.next
                G.next += 1
                G.tiles.append((b, t0 + sti * 128))
                pl = S.psum[6]
                for kt in range(8):
                    P.mm(pl[:, 0:36], h2[:, kt, tok], X.wr[:, kt, :], kt == 0, kt == 7, R=[h2, X.wr], W=[pl])
                lg = rt["lg"]
                _tt(P, lg[:], pl[:, 0:36], X.rb[:], ALU.add, [pl, X.rb], [lg])
                mx, ohg, el, ee, t8, oh1, oh2 = rt["mx"], rt["ohg"], rt["el"], rt["ee"], rt["t8"], rt["oh1"], rt["oh2"]
                s1, gs = rt["s1"], rt["gs"]
                P.V("tensor_reduce", R=[lg], W=[s1], out=s1[:, 0:1], in_=lg[:, 0:4], axis=AX.X, op=ALU.max)
                _ts(P, ohg[:], lg[:, 0:4], s1[:, 0:1], ALU.is_equal, [lg, s1], [ohg])
                _ts(P, gs[:], lg[:, 0:4], s1[:, 0:1], ALU.subtract, [lg, s1], [gs])
                P.A("activation", R=[gs], W=[gs], out=gs[:], in_=gs[:], func=AF.Exp)
                P.V("tensor_reduce", R=[gs], W=[s1], out=s1[:, 1:2], in_=gs[:], axis=AX.X, op=ALU.add)
                _ts(P, el[:], lg[:, 4:12], ohg[:, 0:1], ALU.mult, [lg, ohg], [el])
                for j in range(1, 4):
                    P.V("scalar_tensor_tensor", R=[lg, ohg, el], W=[el], out=el[:], in0=lg[:, 4 + 8 * j:12 + 8 * j],
                        scalar=ohg[:, j:j + 1], in1=el[:], op0=ALU.mult, op1=ALU.add)
                P.V("max", R=[el], W=[mx], out=mx[:], in_=el[:])
                _ts(P, oh1[:], el[:], mx[:, 0:1], ALU.is_equal, [el, mx], [oh1])
                _ts(P, oh2[:], el[:], mx[:, 1:2], ALU.is_equal, [el, mx], [oh2])
                _tt(P, s1[:, 2:3], mx[:, 1:2], mx[:, 0:1], ALU.subtract, [mx], [s1])
                P.A("activation", R=[s1], W=[s1], out=s1[:, 2:3], in_=s1[:, 2:3], func=AF.Exp)
                _ts(P, s1[:, 3:4], s1[:, 2:3], 1.0, ALU.add, [s1], [s1])
                _tt(P, s1[:, 3:4], s1[:, 3:4], s1[:, 1:2], ALU.mult, [s1], [s1])
                P.V("reciprocal", R=[s1], W=[s1], out=s1[:, 3:4], in_=s1[:, 3:4])
                P.V("tensor_copy", R=[s1], W=[G.gate], out=G.gate[:, gi, 0:1], in_=s1[:, 3:4])
                _tt(P, G.gate[:, gi, 1:2], s1[:, 3:4], s1[:, 2:3], ALU.mult, [s1], [G.gate])
                for Mk, oh in ((rt["M1"], oh1), (rt["M2"], oh2)):
                    P.V("tensor_tensor", R=[ohg, oh], W=[Mk], out=Mk[:], in0=ohg[:].unsqueeze(2).to_broadcast([128, 4, 8]),
                        in1=oh[:].unsqueeze(1).to_broadcast([128, 4, 8]), op=ALU.mult)
                M = rt["M"]
                _tt(P, M[:], rt["M1"][:].rearrange("p a b -> p (a b)"), rt["M2"][:].rearrange("p a b -> p (a b)"), ALU.add,
                    [rt["M1"], rt["M2"]], [M])
                pp = S.psum[7]
                P.mm(pp[:, 0:NEXP], S.tri[:], M[:], True, True, R=[S.tri, M], W=[pp])
                pos = rt["pos"]
                _tt(P, pos[:], pp[:, 0:NEXP], X.carry[:], ALU.add, [pp, X.carry], [pos])
                _ts(P, pos[:], pos[:], float(CAP - 1), ALU.min, [pos], [pos])
                _tt(P, pos[:], pos[:], X.eoff[:], ALU.add, [pos, X.eoff], [pos])
                P.mm(pp[:, 0:NEXP], S.ones[:], M[:], True, True, R=[S.ones, M], W=[pp])
                _tt(P, X.carry[:], X.carry[:], pp[:, 0:NEXP], ALU.add, [pp, X.carry], [X.carry])
                for k, Mk in enumerate((rt["M1"], rt["M2"])):
                    _tt(P, M[:], Mk[:].rearrange("p a b -> p (a b)"), pos[:], ALU.mult, [Mk, pos], [M])
                    P.V("tensor_reduce", R=[M], W=[s1], out=s1[:, 0:1], in_=M[:], axis=AX.X, op=ALU.add)
                    P.V("tensor_copy", R=[s1], W=[G.slot], out=G.slot[:, gi, k:k + 1], in_=s1[:, 0:1])
                for half in range(2):
                    ph = S.psum[2 + half]
                    for q in range(4):
                        kt = half * 4 + q
                        P.PE("transpose", R=[h2, S.ident], W=[ph], out=ph[:, q * 128:(q + 1) * 128], in_=h2[:, kt, tok], identity=S.ident[:])
                    P.A("activation", R=[ph], W=[htm], out=htm[:, half * 512:(half + 1) * 512], in_=ph[:], func=AF.Identity)
                for k in range(2):
                    P.dma("pool", R=[htm, G.slot], W=[D.Xe], meth="indirect_dma_start", out=D.Xe[:, :],
                          out_offset=bass.IndirectOffsetOnAxis(ap=G.slot[:, gi, k:k + 1], axis=0), in_=htm[:], in_offset=None)
    P.barrier()


def phase_experts(P, l, D, S):
    with contextlib.ExitStack() as st:
        wg = [P.sb(f"e_wg{i}", [128, 8, DEXP], BF16, st) for i in range(2)]
        wu = [P.sb(f"e_wu{i}", [128, 8, DEXP], BF16, st) for i in range(2)]
        wd = [P.sb(f"e_wd{i}", [128, 4, DM], BF16, st) for i in range(2)]
        xtm2 = [P.sb(f"e_xtm{i}", [128, 4, DM], F32, st) for i in range(2)]
        xbT2 = [P.sb(f"e_xbT{i}", [128, 8, 512], BF16, st) for i in range(2)]
        sg = [P.sb(f"e_sg{i}", [128, 512], F32, st) for i in range(2)]
        act = P.sb("e_act", [128, 4, 512], BF16, st)
        yb2 = [P.sb(f"e_yb{i}", [128, 4, DM], F32, st) for i in range(2)]
        blocks = [(e, blk) for e in range(NEXP) for blk in range(CAP // 512)]
        cnt = {"ci": 0}

        def load_w(e):
            k = e % 2
            P.dma("pool", R=[D.exp_w_gate], W=[wg[k]], out=wg[k][:], in_=D.exp_w_gate[l, e].rearrange("(kt p) n -> p kt n", p=128))
            P.dma("pool", R=[D.exp_w_up], W=[wu[k]], out=wu[k][:], in_=D.exp_w_up[l, e].rearrange("(kt p) n -> p kt n", p=128))
            P.dma("pool", R=[D.exp_w_down], W=[wd[k]], out=wd[k][:], in_=D.exp_w_down[l, e].rearrange("(kt p) n -> p kt n", p=128))

        def stage_a(bi):
            e, blk = blocks[bi]
            r0 = e * CAP + blk * 512
            xtm, xbT = xtm2[bi % 2], xbT2[bi % 2]
            P.dma("sp", R=[D.Xe], W=[xtm], out=xtm[:], in_=D.Xe[r0:r0 + 512, :].rearrange("(s p) f -> p s f", p=128))
            for kt in range(8):
                ph = S.psum[cnt["ci"] % 2]
                cnt["ci"] += 1
                for s_ in range(4):
                    P.PE("transpose", R=[xtm, S.ident], W=[ph], out=ph[:, s_ * 128:(s_ + 1) * 128],
                         in_=xtm[:, s_, kt * 128:(kt + 1) * 128], identity=S.ident[:])
                if kt % 2 == 0:
                    P.A("activation", R=[ph], W=[xbT], out=xbT[:, kt, :], in_=ph[:], func=AF.Identity)
                else:
                    P.V("tensor_copy", R=[ph], W=[xbT], out=xbT[:, kt, :], in_=ph[:])

        def stage_bc(bi):
            e, blk = blocks[bi]
            k = e % 2
            r0 = e * CAP + blk * 512
            xbT, yb = xbT2[bi % 2], yb2[bi % 2]
            for hm in range(4):
                pg, pu = S.psum[2 + 2 * (hm % 2)], S.psum[3 + 2 * (hm % 2)]
                for kt in range(8):
                    P.mm(pg[:], wg[k][:, kt, hm * 128:(hm + 1) * 128], xbT[:, kt, :], kt == 0, kt == 7, R=[wg[k], xbT], W=[pg])
                for kt in range(8):
                    P.mm(pu[:], wu[k][:, kt, hm * 128:(hm + 1) * 128], xbT[:, kt, :], kt == 0, kt == 7, R=[wu[k], xbT], W=[pu])
                P.A("activation", R=[pg], W=[sg[hm % 2]], out=sg[hm % 2][:], in_=pg[:], func=AF.Silu)
                _tt(P, act[:, hm, :], pu[:], sg[hm % 2][:], ALU.mult, [pu, sg[hm % 2]], [act])
            for s_ in range(4):
                for half in range(2):
                    pd = S.psum[6 + half]
                    for hm in range(4):
                        P.mm(pd[:], act[:, hm, s_ * 128:(s_ + 1) * 128], wd[k][:, hm, half * 512:(half + 1) * 512], hm == 0, hm == 3,
                             R=[act, wd[k]], W=[pd])
                    if half == 0:
                        P.A("activation", R=[pd], W=[yb], out=yb[:, s_, 0:512], in_=pd[:], func=AF.Identity)
                    else:
                        P.V("tensor_copy", R=[pd], W=[yb], out=yb[:, s_, 512:1024], in_=pd[:])
            P.dma("sp", R=[yb], W=[D.Ye], out=D.Ye[r0:r0 + 512, :].rearrange("(s p) f -> p s f", p=128), in_=yb[:])

        load_w(0)
        stage_a(0)
        for bi in range(len(blocks)):
            e, blk = blocks[bi]
            if blk == 0 and e + 1 < NEXP:
                load_w(e + 1)
            if bi + 1 < len(blocks):
                stage_a(bi + 1)
            stage_bc(bi)
    P.barrier()


def phase_combine(P, l, D, S, G, dst, last):
    with contextlib.ExitStack() as st:
        y1 = [P.sb(f"c_y1{i}", [128, DM], F32, st) for i in range(2)]
        y2 = [P.sb(f"c_y2{i}", [128, DM], F32, st) for i in range(2)]
        xt = [P.sb(f"c_x{i}", [128, 8, 128], F32, st) for i in range(2)]
        for gi, (b, t0) in enumerate(G.tiles):
            k = gi % 2
            col = 4 if t0 < CTX else b
            for yy, kk in ((y1[k], 0), (y2[k], 1)):
                P.dma("pool", R=[D.Ye, G.slot], W=[yy], meth="indirect_dma_start", out=yy[:], out_offset=None, in_=D.Ye[:, :],
                      in_offset=bass.IndirectOffsetOnAxis(ap=G.slot[:, gi, kk:kk + 1], axis=0))
            P.dma("sp", R=[D.x1T.s(b)], W=[xt[k]], out=xt[k][:], in_=D.x1T[b, :, t0:t0 + 128].rearrange("(kt p) t -> p kt t", p=128))
            _ts(P, y1[k][:], y1[k][:], G.gate[:, gi, 0:1], ALU.mult, [y1[k], G.gate], [y1[k]])
            P.V("scalar_tensor_tensor", R=[y1[k], y2[k], G.gate], W=[y1[k]], out=y1[k][:], in0=y2[k][:], scalar=G.gate[:, gi, 1:2],
                in1=y1[k][:], op0=ALU.mult, op1=ALU.add)
            for half in range(2):
                ph = S.psum[2 * k + half]
                for q in range(4):
                    m = half * 4 + q
                    P.PE("transpose", R=[y1[k], S.ident], W=[ph], out=ph[:, q * 128:(q + 1) * 128], in_=y1[k][:, m * 128:(m + 1) * 128],
                         identity=S.ident[:])
                for q in range(4):
                    m = half * 4 + q
                    P.V("scalar_tensor_tensor", R=[ph, xt[k], S.modT], W=[xt[k]], out=xt[k][:, m, :], in0=ph[:, q * 128:(q + 1) * 128],
                        scalar=S.modT[:, 5 * 8 + m, col:col + 1], in1=xt[k][:, m, :], op0=ALU.mult, op1=ALU.add)
            if last:
                P.dma("sp", R=[xt[k]], W=[D.out], out=D.out[b, :, t0 - CTX:t0 - CTX + 128].rearrange("(kt p) t -> p kt t", p=128), in_=xt[k][:])
            else:
                P.dma("sp", R=[xt[k]], W=[dst.s(b)], out=dst[b, :, t0:t0 + 128].rearrange("(kt p) t -> p kt t", p=128), in_=xt[k][:])
    P.barrier()


RC = 64
NCH = T // RC
GN_EPS = 64e-5
W_SCALE = -math.exp(-0.5)


def rwkv_setup(P, l, D, S, st):
    X = NS()
    ld = lambda nm, src, shp: _load(P, st, nm, src, shp)
    X.cw = ld("rw_cw", D.rw_conv[:, l, :], [128, 24])
    X.w0 = ld("rw_w0", D.rw_w0[:, l, :], [128, 4])
    X.a0 = ld("rw_a0", D.rw_a0[:, l, :], [128, 4])
    X.pv = ld("rw_pv", D.rw_pv[:, l, :], [128, 10])
    X.w2a2 = [ld(f"rw_w2a2{d}", D.rw_w2a2[l, d], [128, 256]) for d in range(2)]
    X.g2 = ld("rw_g2", D.rw_g2[l], [128, 256])
    X.msk = {k: ld("rw_" + k, D.consts[k][:, :], list(D.consts[k].t.shape)) for k in ("MA0", "MA1", "MB0", "MB1", "MC0", "MC1", "YXI")}
    X.rmask = ld("rw_rmask", D.consts["rmask"][:, :], [128, T])
    X.gneps = P.sb("rw_gneps", [128, 1], F32, st)
    P.V("memset", W=[X.gneps], ap=X.gneps[:], constant=GN_EPS)
    X.kkeps = P.sb("rw_kkeps", [128, 1], F32, st)
    P.V("memset", W=[X.kkeps], ap=X.kkeps[:], constant=1e-12)
    return X


def phase_rwkv(P, l, D, S, b, X, need_ctx):
    zbase = OFF_RW
    SEG = ((0, CTX), (CTX, T))
    for ih in range(2):
        with contextlib.ExitStack() as st:
            A = lambda nm, shp=(128, T): P.sb("rw_" + nm, list(shp), F32, st)
            zt = A("zt")
            r, k, v, kk, kts, g, ysum = A("r"), A("k"), A("v"), A("kk"), A("kts"), A("g"), A("ysum")
            z6 = A("z6")
            t1, t2, t3 = A("t1"), A("t2"), A("t3")
            t2x = g
            KRr = P.sb("rw_KR", [128, NCH, 2, RC], F32R, st)
            BKr = P.sb("rw_BK", [128, NCH, 2, RC], F32R, st)
            KR = _View(KRr, F32)
            BK = _View(BKr, F32)
            wtot = A("wtot", (128, NCH))

            def conv(dst, tile):
                P.dma("sp", R=[D.uT.s(b)], W=[zt], out=zt[:], in_=D.uT[b, zbase + tile * 128:zbase + (tile + 1) * 128, :])
                _ts(P, dst[:], zt[:], X.cw[:, 8 + tile:9 + tile], ALU.mult, [zt, X.cw], [dst])
                for (s0, s1) in SEG:
                    P.V("scalar_tensor_tensor", R=[zt, X.cw, dst], W=[dst], out=dst[:, s0 + 1:s1], in0=zt[:, s0:s1 - 1],
                        scalar=X.cw[:, tile:tile + 1], in1=dst[:, s0 + 1:s1], op0=ALU.mult, op1=ALU.add)
                    P.V("scalar_tensor_tensor", R=[zt, X.cw, dst], W=[dst], out=dst[:, s0:s1 - 1], in0=zt[:, s0 + 1:s1],
                        scalar=X.cw[:, 16 + tile:17 + tile], in1=dst[:, s0:s1 - 1], op0=ALU.mult, op1=ALU.add)

            conv(r, ih)
            conv(k, 2 + ih)
            conv(v, 4 + ih)
            conv(z6, 6)
            P.A("activation", R=[z6], W=[z6], out=z6[0:64, :], in_=z6[0:64, :], func=AF.Tanh)
            P.V("memset", W=[ysum], ap=ysum[:], constant=0.0)
            P.V("memset", W=[kts], ap=kts[:], constant=0.0)
            _ts(P, kk[:], k[:], X.pv[:, 0 + ih:1 + ih], ALU.mult, [k, X.pv], [kk])
            for (t0, n) in TILES:
                ps = S.psum[0]
                P.A("activation", R=[kk], W=[t1], out=t1[:, t0:t0 + n], in_=kk[:, t0:t0 + n], func=AF.Square)
                P.mm(ps[:, 0:n], S.bs64[:], t1[:, t0:t0 + n], True, True, R=[S.bs64, t1], W=[ps])
                P.A("activation", R=[ps, X.kkeps], W=[t1], out=t1[:, t0:t0 + n], in_=ps[:, 0:n], func=AF.Sqrt, bias=X.kkeps[:, 0:1], scale=1.0)
                P.V("reciprocal", R=[t1], W=[t1], out=t1[:, t0:t0 + n], in_=t1[:, t0:t0 + n])
                _tt(P, kk[:, t0:t0 + n], kk[:, t0:t0 + n], t1[:, t0:t0 + n], ALU.mult, [kk, t1], [kk])
            for d in range(2):
                MA, MB, MC = X.msk[f"MA{d}"], X.msk[f"MB{d}"], X.msk[f"MC{d}"]
                for (t0, n) in TILES:
                    pw, pa = S.psum[0], S.psum[1]
                    P.mm(pw[:, 0:n], X.w2a2[d][0:64, ih * 128:(ih + 1) * 128], z6[0:64, t0:t0 + n], True, True, R=[X.w2a2[d], z6], W=[pw])
                    P.mm(pa[:, 0:n], X.w2a2[d][64:128, ih * 128:(ih + 1) * 128], z6[64:128, t0:t0 + n], True, True, R=[X.w2a2[d], z6], W=[pa])
                    P.A("activation", R=[pw, X.w0], W=[t1], out=t1[:, t0:t0 + n], in_=pw[:, 0:n], func=AF.Sigmoid,
                        bias=X.w0[:, 2 * d + ih:2 * d + ih + 1], scale=1.0)
                    P.A("activation", R=[pa, X.a0], W=[t2], out=t2[:, t0:t0 + n], in_=pa[:, 0:n], func=AF.Sigmoid,
                        bias=X.a0[:, 2 * d + ih:2 * d + ih + 1], scale=1.0)
                _ts(P, t1[:], t1[:], W_SCALE, ALU.mult, [t1], [t1])
                _ts(P, t3[:], t2[:], -1.0, ALU.add, [t2], [t3], X.pv[:, 2 + ih:3 + ih], ALU.mult)
                P.V("scalar_tensor_tensor", R=[t3, k], W=[t3], out=t3[:], in0=t3[:], scalar=1.0, in1=k[:], op0=ALU.add, op1=ALU.mult)
                _tt(P, kts[:], kts[:], t3[:], ALU.add, [kts, t3], [kts])
                _tt(P, t2[:], t2[:], kk[:], ALU.mult, [t2, kk], [t2])
                P.V("tensor_tensor_scan", R=[X.rmask, t1], W=[zt], out=zt[:], data0=X.rmask[:], data1=t1[:], initial=0.0,
                    op0=ALU.mult, op1=ALU.add)
                zt3 = zt[:].rearrange("p (c j) -> p c j", j=RC)
                if d == 1:
                    P.V("tensor_tensor", R=[zt], W=[t2x], out=t2x[:].rearrange("p (c j) -> p c j", j=RC),
                        in0=zt3[:, :, RC - 1:RC].to_broadcast([128, NCH, RC]), in1=zt3, op=ALU.subtract)
                    _tt(P, zt[:], t2x[:], t1[:], ALU.add, [t2x, t1], [zt])
                last = RC - 1 if d == 0 else 0
                P.A("activation", R=[zt], W=[wtot], out=wtot[:], in_=zt3[:, :, last], func=AF.Exp)
                c3 = lambda tt_: tt_[:].rearrange("p (c j) -> p c j", j=RC)
                P.A("activation", R=[zt], W=[t2x], out=t2x[:], in_=zt[:], func=AF.Exp)
                _tt(P, KRr[:, :, 1, :], c3(t2x), c3(r), ALU.mult, [t2x, r], [KRr])
                P.A("activation", R=[zt], W=[t2x], out=t2x[:], in_=zt[:], func=AF.Exp, scale=-1.0)
                _tt(P, BKr[:, :, 1, :], c3(t2x), c3(t3), ALU.mult, [t2x, t3], [BKr])
                _tt(P, BKr[:, :, 0, :], c3(t2x), c3(t2), ALU.mult, [t2x, t2], [BKr])
                _tt(P, zt[:], zt[:], t1[:], ALU.subtract, [zt, t1], [zt])
                P.A("activation", R=[zt], W=[t2x], out=t2x[:], in_=zt[:], func=AF.Exp)
                _tt(P, KRr[:, :, 0, :], c3(t2x), c3(kk), ALU.mult, [t2x, kk], [KRr])
                rwkv_scan(P, S, X, d, st, KRr, BKr, wtot, v, ysum, MA, MB, MC)
            z7 = t3
            conv(z7, 7)
            P.A("activation", R=[z7], W=[z7], out=z7[:], in_=z7[:], func=AF.Sigmoid)
            for (t0, n) in TILES:
                ps2 = S.psum[1]
                P.mm(ps2[:, 0:n], X.g2[:, ih * 128:(ih + 1) * 128], z7[:, t0:t0 + n], True, True, R=[X.g2, z7], W=[ps2])
                P.A("activation", R=[ps2], W=[g], out=g[:, t0:t0 + n], in_=ps2[:, 0:n], func=AF.Identity)
            for (t0, n) in TILES:
                if t0 < CTX and not need_ctx:
                    continue
                sl = slice(t0, t0 + n)
                pm, pv_, pb = S.psum[0], S.psum[1], S.psum[2]
                P.mm(pm[:, 0:n], S.bd64[:], ysum[:, sl], True, True, R=[S.bd64, ysum], W=[pm])
                _tt(P, t1[:, sl], ysum[:, sl], pm[:, 0:n], ALU.subtract, [ysum, pm], [t1])
                P.A("activation", R=[t1], W=[t2], out=t2[:, sl], in_=t1[:, sl], func=AF.Square)
                P.mm(pv_[:, 0:n], S.bd64[:], t2[:, sl], True, True, R=[S.bd64, t2], W=[pv_])
                P.A("activation", R=[pv_, X.gneps], W=[t2], out=t2[:, sl], in_=pv_[:, 0:n], func=AF.Sqrt, bias=X.gneps[:, 0:1], scale=1.0)
                P.V("reciprocal", R=[t2], W=[t2], out=t2[:, sl], in_=t2[:, sl])
                _tt(P, t1[:, sl], t1[:, sl], t2[:, sl], ALU.mult, [t1, t2], [t1])
                _ts(P, t1[:, sl], t1[:, sl], X.pv[:, 6 + ih:7 + ih], ALU.mult, [t1, X.pv], [t1], X.pv[:, 8 + ih:9 + ih], ALU.add)
                P.V("scalar_tensor_tensor", R=[r, kts, X.pv], W=[t2], out=t2[:, sl], in0=r[:, sl], scalar=X.pv[:, 4 + ih:5 + ih],
                    in1=kts[:, sl], op0=ALU.mult, op1=ALU.mult)
                P.mm(pb[:, 0:n], S.bs64[:], t2[:, sl], True, True, R=[S.bs64, t2], W=[pb])
                _tt(P, t2[:, sl], pb[:, 0:n], v[:, sl], ALU.mult, [pb, v], [t2])
                _tt(P, t1[:, sl], t1[:, sl], t2[:, sl], ALU.add, [t1, t2], [t1])
                _tt(P, t1[:, sl], t1[:, sl], g[:, sl], ALU.mult, [t1, g], [t1])
            c0 = 0 if need_ctx else CTX
            P.dma("sp", R=[t1], W=[D.orw.s(b)], out=D.orw[b, ih * 128:(ih + 1) * 128, c0:T], in_=t1[:, c0:T])
        P.barrier()


def rwkv_scan(P, S, X, d, st0, KR, BK, wtot, v, ysum, MA, MB, MC):
    G = 2
    with contextlib.ExitStack() as st:
        B2 = lambda nm, shp: [P.sb(f"rs_{nm}{i}", list(shp), F32R, st) for i in range(2)]
        ST = P.sb("rs_ST", [128, 128], F32R, st)
        P.V("tensor_scalar", R=[S.ident], W=[ST], out=ST[:], in0=S.ident[:], scalar1=0.0, scalar2=None, op0=ALU.mult)
        identR = P.sb("rs_identR", [128, 128], F32R, st)
        P.V("tensor_copy", R=[S.ident], W=[identR], out=identR[:], in_=S.ident[:])
        bf = lambda ap: ap.bitcast(F32)
        YA, AB = B2("YA", (64, G, 2, 128)), B2("AB", (64, G, 2, 128))
        YX = B2("YX", (64, G, 2, 2, RC))
        YT = B2("YT", (64, G, 2, RC))
        X6 = B2("X6", (64, G, 2, RC))
        BKt, Vt = B2("BKt", (64, G, 2, 128)), B2("Vt", (64, G, 128))
        RHS, Ps = B2("RHS", (64, 128)), B2("Ps", (64, 128))
        if d == 0:
            order = list(range(NCH))
        else:
            nc_ctx = CTX // RC
            order = list(range(nc_ctx - 1, -1, -1)) + list(range(NCH - 1, nc_ctx - 1, -1))
        pairs = [order[i:i + G] for i in range(0, NCH, G)]

        def pre_stages(pi):
            cs = pairs[pi]
            q = pi % 2
            ya, ab, bkt, vt, x6 = YA[q], AB[q], BKt[q], Vt[q], X6[q]
            stages = []

            def s_init():
                pA, pB, pC = S.psum[0], S.psum[1], S.psum[2]
                for gi, c in enumerate(cs):
                    for hp in range(2):
                        lo = hp * 64
                        o = (gi * 2 + hp)
                        krc = KR[lo:lo + 64, c, :, :].rearrange("p a b -> p (a b)")
                        P.mm(pA[0:64, o * 128:(o + 1) * 128], BK[lo:lo + 64, c, 0, :], krc, True, True, R=[BK, KR], W=[pA])
                        P.mm(pB[0:64, o * 128:(o + 1) * 128], BK[lo:lo + 64, c, 1, :], krc, True, True, R=[BK, KR], W=[pB])
                        P.mm(pC[0:64, o * 64:(o + 1) * 64], KR[lo:lo + 64, c, 0, :], BK[lo:lo + 64, c, 0, :], True, True, R=[BK, KR], W=[pC])
                _tt(P, ya[:].rearrange("p g a b -> p (g a b)"), pA[0:64, 0:512], MA[:], ALU.mult, [pA, MA], [ya])
                _tt(P, ab[:].rearrange("p g a b -> p (g a b)"), pB[0:64, 0:512], MB[:], ALU.mult, [pB, MB], [ab])
                yx, yt = YX[0], YT[0]
                _tt(P, yt[:].rearrange("p g a b -> p (g a b)"), pC[0:64, 0:256], MC[:], ALU.mult, [pC, MC], [yt])
                P.A("activation", R=[X.msk["YXI"]], W=[yx], out=yx[:].rearrange("p g a b c -> p (g a b c)"), in_=X.msk["YXI"][:],
                    func=AF.Identity)
                P.V("tensor_copy", R=[ya], W=[yx], out=yx[:, :, :, 0, :], in_=bf(ya[:, :, :, 0:RC]))
            stages.append(s_init)

            def mk_step(kstep):
                def s_step():
                    yx, yt = YX[kstep % 2], YT[kstep % 2]
                    yxn, ytn = YX[(kstep + 1) % 2], YT[(kstep + 1) % 2]
                    pa_, pc_ = S.psum[3], S.psum[4]
                    for gi in range(G):
                        for hp in range(2):
                            o = gi * 2 + hp
                            P.mm(pa_[0:64, o * 128:(o + 1) * 128], yt[:, gi, hp, :], yx[:, gi, hp, :, :].rearrange("p a b -> p (a b)"),
                                 True, True, R=[yt, yx], W=[pa_])
                            if kstep < 5:
                                P.mm(pc_[0:64, o * 64:(o + 1) * 64], yx[:, gi, hp, 0, :], yt[:, gi, hp, :], True, True, R=[yt, yx], W=[pc_])
                    pa4 = pa_[0:64, 0:512].rearrange("p (g a b c) -> p g a b c", g=G, a=2, b=2)
                    if kstep < 5:
                        P.A("activation", R=[pa_], W=[yxn], out=yxn[:, :, :, 0, :], in_=pa4[:, :, :, 0, :], func=AF.Identity)
                        _tt(P, yxn[:, :, :, 1, :], bf(yx[:, :, :, 1, :]), pa4[:, :, :, 1, :], ALU.add, [yx, pa_], [yxn])
                        P.A("activation", R=[pc_], W=[ytn], out=ytn[:].rearrange("p g a b -> p (g a b)"), in_=pc_[0:64, 0:256],
                            func=AF.Identity)
                    else:
                        _tt(P, x6[:], bf(yx[:, :, :, 1, :]), pa4[:, :, :, 1, :], ALU.add, [yx, pa_], [x6])
                return s_step
            for kstep in range(6):
                stages.append(mk_step(kstep))

            def s_tr():
                pT, pV = S.psum[5], S.psum[2]
                for gi, c in enumerate(cs):
                    P.PE("transpose", R=[BK, S.ident], W=[pT], out=pT[0:64, gi * 256:gi * 256 + 128], in_=BK[:, c, 0, :].bitcast(F32), identity=S.ident[:])
                    P.PE("transpose", R=[BK, S.ident], W=[pT], out=pT[0:64, gi * 256 + 128:gi * 256 + 256], in_=BK[:, c, 1, :].bitcast(F32),
                         identity=S.ident[:])
                    P.PE("transpose", R=[v, S.ident], W=[pV], out=pV[0:64, 256 + gi * 128:256 + (gi + 1) * 128], in_=v[:, c * RC:(c + 1) * RC],
                         identity=S.ident[:])
                pT4 = pT[0:64, 0:512].rearrange("p (g a b) -> p g a b", g=G, a=2)
                _ts(P, bkt[:, :, 0, :], pT4[:, :, 0, :], -1.0, ALU.mult, [pT], [bkt])
                P.A("activation", R=[pT], W=[bkt], out=bkt[:, :, 1, :], in_=pT4[:, :, 1, :], func=AF.Identity)
                P.A("activation", R=[pV], W=[vt], out=vt[:].rearrange("p g b -> p (g b)"), in_=pV[0:64, 256:512], func=AF.Identity)
            stages.append(s_tr)
            return stages

        def seq_stages(pi):
            cs = pairs[pi]
            q = pi % 2
            ya, ab, bkt, vt, x6 = YA[q], AB[q], BKt[q], Vt[q], X6[q]
            stages = []
            for gi, c in enumerate(cs):
                rhs, ps_ = RHS[gi], Ps[gi]
                pR, pP, pY, pS = S.psum[6], S.psum[7], S.psum[6], S.psum[7]

                def s1(gi=gi, c=c, rhs=rhs):
                    for hp in range(2):
                        lo = hp * 64
                        P.mm(pR[0:64, lo:lo + 64], KR[lo:lo + 64, c, 0, :], ST[lo:lo + 64, lo:lo + 64], True, False, R=[KR, ST], W=[pR])
                        P.mm(pR[0:64, lo:lo + 64], ab[:, gi, hp, 0:RC], vt[:, gi, lo:lo + 64], False, True, R=[ab, vt], W=[pR])
                    P.V("tensor_copy", R=[pR], W=[rhs], out=rhs[:], in_=pR[0:64, 0:128])

                def s2(gi=gi, c=c, rhs=rhs, ps_=ps_):
                    for hp in range(2):
                        lo = hp * 64
                        P.mm(pP[0:64, lo:lo + 64], x6[:, gi, hp, :], rhs[:, lo:lo + 64], True, True, R=[x6, rhs], W=[pP])
                    P.V("tensor_copy", R=[pP], W=[ps_], out=ps_[:], in_=pP[0:64, 0:128])

                def s3(gi=gi, c=c, ps_=ps_):
                    P.mm(pS[:, 256:384], bkt[:, gi, 0, :], ps_[:], True, False, R=[bkt, ps_], W=[pS])
                    P.mm(pS[:, 256:384], bkt[:, gi, 1, :], vt[:, gi, :], False, False, R=[bkt, vt], W=[pS])
                    P.mm(pS[:, 256:384], identR[:], ST[:], False, True, R=[identR, ST], W=[pS])
                    for hp2 in range(2):
                        P.mm(pY[:, 256 + hp2 * 64:256 + (hp2 + 1) * 64], ST[:], KR[:, c, 1, :], hp2 == 0, False, R=[ST, KR], W=[pY])
                    P.mm(pY[:, 256:384], ps_[:], ya[:, gi, :, RC:2 * RC], False, False, R=[ps_, ya], W=[pY])
                    P.mm(pY[:, 256:384], vt[:, gi, :], ab[:, gi, :, RC:2 * RC], False, True, R=[vt, ab], W=[pY])
                    P.V("scalar_tensor_tensor", R=[pS, wtot, S.bs64], W=[ST], out=ST[:], in0=pS[:, 256:384], scalar=wtot[:, c:c + 1],
                        in1=S.bs64[:], op0=ALU.mult, op1=ALU.mult)
                    for hp in range(2):
                        lo = hp * 64
                        _tt(P, ysum[lo:lo + 64, c * RC:(c + 1) * RC], ysum[lo:lo + 64, c * RC:(c + 1) * RC],
                            pY[lo:lo + 64, 256 + lo:256 + lo + 64], ALU.add, [ysum, pY], [ysum])
                stages += [s1, s2, s3]
            return stages

        for f in pre_stages(0):
            f()
        for pi in range(len(pairs)):
            sq = seq_stages(pi)
            pr = pre_stages(pi + 1) if pi + 1 < len(pairs) else []
            n = max(len(sq), len(pr))
            for i in range(n):
                if i < len(pr):
                    pr[i]()
                if i < len(sq):
                    sq[i]()
    P.barrier()


def build(ncores_debug=False, nbc=NBC, phases=("mod", "inproj", "attn", "s5", "rwkv", "moe"), depth=DEPTH):
    nc = bass.Bass("TRN2", target_bir_lowering=False)
    P = Prog(nc, debug=ncores_debug)
    D = NS()
    S = NS()

    def ext(name, shape, dtype=F32):
        return P.dram(name, shape, dtype, kind="ExternalInput")

    D.xinT = ext("xinT", [nbc, DM, T])
    D.cT = ext("cT", [128, 8, 8])
    D.w_mod = ext("w_mod", [DEPTH, DM, 6 * DM])
    D.w_in = ext("w_in", [DEPTH, DM, INW])
    D.bmodT = ext("bmodT", [128, DEPTH, 48])
    D.norm1T = ext("norm1T", [128, DEPTH, 8])
    D.norm2T = ext("norm2T", [128, DEPTH, 8])
    D.qg = ext("qg", [128, DEPTH])
    D.kg = ext("kg", [128, DEPTH])
    for nm, shp in (("s5_A_rep_re", [128, DEPTH, 256]), ("s5_A_rep_im", [128, DEPTH, 256]), ("s5_ldt_rep", [128, DEPTH, 256]),
                    ("s5_Bt_re", [128, DEPTH, 256]), ("s5_Bt_im", [128, DEPTH, 256]), ("s5_A_st_re", [128, DEPTH, 16]),
                    ("s5_A_st_im", [128, DEPTH, 16]), ("s5_ldt_st", [128, DEPTH, 16]), ("s5_C_st_re", [128, DEPTH, 1024]),
                    ("s5_C_st_im", [128, DEPTH, 1024]), ("s5_dT", [128, DEPTH, 2]), ("s5_w_glu", [DEPTH, 256, 512])):
        setattr(D, nm, ext(nm, shp))
    D.os5 = P.dram("os5", [nbc, 256, T])
    for nm, shp in (("rw_conv", [128, DEPTH, 24]), ("rw_w0", [128, DEPTH, 4]), ("rw_a0", [128, DEPTH, 4]), ("rw_pv", [128, DEPTH, 10]),
                    ("rw_w2a2", [DEPTH, 2, 128, 256]), ("rw_g2", [DEPTH, 128, 256])):
        setattr(D, nm, ext(nm, shp))
    D.orw = P.dram("orw", [nbc, 256, T])
    D.x1T = P.dram("x1T", [nbc, DM, T])
    D.x2T = P.dram("x2T", [nbc, DM, T])
    D.Xe = P.dram("Xe", [NEXP * CAP, DM])
    D.Ye = P.dram("Ye", [NEXP * CAP, DM])
    for nm, shp in (("proj_att", [DEPTH, 512, DM]), ("proj_s5", [DEPTH, 256, DM]), ("proj_rwkv", [DEPTH, 256, DM]),
                    ("w_out", [DEPTH, DM, DM]), ("router_w", [DEPTH, DM, 36]), ("router_b", [128, DEPTH, 36]),
                    ("exp_w_gate", [DEPTH, NEXP, DM, DEXP]), ("exp_w_up", [DEPTH, NEXP, DM, DEXP]), ("exp_w_down", [DEPTH, NEXP, DEXP, DM])):
        setattr(D, nm, ext(nm, shp))
    consts = make_consts()
    D.consts = {k: ext("c_" + k, list(v.shape)) for k, v in consts.items()}
    D.out = P.dram("out", [nbc, DM, LAT], kind="ExternalOutput")
    D.uT = P.dram("uT", [nbc, INW, T])
    D.oatt = P.dram("oatt", [nbc, 512, T])

    S.psum = [P.ps(f"ps{i}", [128, 512]) for i in range(8)]
    for k in ("ident", "ones", "bd64", "bs64", "rot", "sh0", "sh1", "tri"):
        t = P.sb("k_" + k, [128, 128])
        P.dma("sp", R=[D.consts[k]], W=[t], out=t[:], in_=D.consts[k][:, :])
        setattr(S, k, t)
    S.maskG = P.sb("k_maskG", [128, 8])
    P.dma("sp", R=[D.consts["maskG"]], W=[S.maskG], out=S.maskG[:], in_=D.consts["maskG"][:, :])
    S.modT = P.sb("modT", [128, 48, 8])
    S.epsb = P.sb("epsb", [128, 1])
    P.V("memset", W=[S.epsb], ap=S.epsb[:], constant=EPS)
    for k, shp in (("bmodT", [128, DEPTH, 48]), ("norm1T", [128, DEPTH, 8]), ("norm2T", [128, DEPTH, 8])):
        t = P.sb(k, shp)
        P.dma("sp", R=[getattr(D, k)], W=[t], out=t[:], in_=getattr(D, k)[:, :, :])
        setattr(S, k, t)
    S.qg = []
    S.kg = []
    qgt = P.sb("qgt", [128, DEPTH])
    kgt = P.sb("kgt", [128, DEPTH])
    P.dma("sp", R=[D.qg], W=[qgt], out=qgt[:], in_=D.qg[:, :])
    P.dma("sp", R=[D.kg], W=[kgt], out=kgt[:], in_=D.kg[:, :])
    for l in range(DEPTH):
        a = NS.__new__(NS)
        S.qg.append(_ColView(qgt, l))
        S.kg.append(_ColView(kgt, l))

    xsrc = D.xinT
    for l in range(depth):
        need_ctx = l < DEPTH - 1
        if "mod" in phases:
            phase_mod(P, l, D, S)
        if "inproj" in phases:
            for b in range(nbc):
                phase_inproj(P, l, D, S, b, xsrc)
        if "attn" in phases:
            with contextlib.ExitStack() as lst:
                for k in ("cos", "sin"):
                    t = P.sb("k_" + k, [128, LAT], F32, lst)
                    P.dma("sp", R=[D.consts[k]], W=[t], out=t[:], in_=D.consts[k][:, :])
                    setattr(S, k, t)
                P.barrier()
                for b in range(nbc):
                    phase_attn(P, l, D, S, b, need_ctx)
        if "s5" in phases:
            with contextlib.ExitStack() as lst:
                X5 = s5_setup(P, l, D, S, lst)
                P.barrier()
                for b in range(nbc):
                    phase_s5(P, l, D, S, b, X5, need_ctx)
        if "rwkv" in phases:
            with contextlib.ExitStack() as lst:
                XR = rwkv_setup(P, l, D, S, lst)
                P.barrier()
                for b in range(nbc):
                    phase_rwkv(P, l, D, S, b, XR, need_ctx)
        if "rwkv0" in phases:
            with contextlib.ExitStack() as lst:
                z = P.sb("zrw", [128, T], F32, lst)
                P.V("memset", W=[z], ap=z[:], constant=0.0)
                for b in range(nbc):
                    for m in range(2):
                        P.dma("sp", R=[z], W=[D.orw.s(b)], out=D.orw[b, m * 128:(m + 1) * 128, :], in_=z[:])
            P.barrier()
        if "moe" in phases:
            with contextlib.ExitStack() as lst:
                G = NS()
                G.next = 0
                G.tiles = []
                G.slot = P.sb("g_slot", [128, 72, 2], U32, lst)
                G.gate = P.sb("g_gate", [128, 72, 2], F32, lst)
                with contextlib.ExitStack() as lst2:
                    XM = merge_setup(P, l, D, S, lst2)
                    P.barrier()
                    for b in range(nbc):
                        phase_merge(P, l, D, S, b, XM, need_ctx, xsrc, G)
                P.barrier()
                if "noexp" not in phases:
                    phase_experts(P, l, D, S)
                if "nocomb" not in phases:
                    phase_combine(P, l, D, S, G, D.x2T, l == DEPTH - 1)
            xsrc = D.x2T
    P.barrier()
    P.finish(getattr(P, "dbg_out", []))
    P.finish([D.out])
    P.finish([D.uT.s(b) for b in range(nbc)] + [D.oatt.s(b) for b in range(nbc)] + [D.os5.s(b) for b in range(nbc)] + [D.orw.s(b) for b in range(nbc)])
    info = (P.ninstr, P.nwait)
    P.close()
    return nc, consts, info


class _View:
    def __init__(self, tt, dt):
        self.tt = tt
        self.dt = dt
        self.tok = tt.tok

    def __getitem__(self, idx):
        return self.tt[idx].bitcast(self.dt)

    def s(self, k):
        return self.tt.s(k)


class _ColView:
    def __init__(self, tt, l):
        self.tt = tt
        self.l = l
        self.tok = tt.tok

    def __getitem__(self, idx):
        return self.tt[:, self.l:self.l + 1]


def _tok(x):
    return x.tok if hasattr(x, "tok") else x


def host_inputs(inp, core, nbc=NBC):
    b0 = core * nbc
    x = np.asarray(inp["x"][b0:b0 + nbc], np.float32)
    ctx = np.asarray(inp["ctx"][b0:b0 + nbc], np.float32)
    m = {}
    m["xinT"] = np.ascontiguousarray(np.concatenate([ctx, x], axis=1).transpose(0, 2, 1))
    call = np.zeros((8, DM), np.float32)
    call[:nbc] = inp["c"][b0:b0 + nbc]
    call[4] = inp["c_ctx"]
    m["cT"] = np.ascontiguousarray(call.reshape(8, 8, 128).transpose(2, 1, 0))
    m["w_mod"] = np.asarray(inp["w_mod"], np.float32)
    m["w_in"] = np.asarray(inp["w_in"], np.float32)
    m["bmodT"] = fm(inp["b_mod"])
    m["norm1T"] = fm(inp["norm1"])
    m["norm2T"] = fm(inp["norm2"])
    L = DEPTH
    f32 = lambda k: np.asarray(inp[k], np.float32)
    for nm, key in (("re", "s5_a_re"), ("im", "s5_a_im")):
        a = f32(key)
        m["s5_A_rep_" + nm] = np.ascontiguousarray(np.repeat(a.reshape(L, 2, 2, 8, 64).transpose(3, 0, 1, 2, 4), 16, axis=0).reshape(128, L, 256))
        m["s5_A_st_" + nm] = np.ascontiguousarray(a.reshape(L, 2, 8, 2, 64).transpose(3, 4, 0, 1, 2).reshape(128, L, 16))
    ldt = f32("s5_log_dt")
    r = np.repeat(ldt.reshape(L, 2, 2, 8).transpose(3, 0, 1, 2), 16, axis=0)
    m["s5_ldt_rep"] = np.ascontiguousarray(np.broadcast_to(r[..., None], (128, L, 2, 2, 64)).reshape(128, L, 256))
    r = ldt.reshape(L, 2, 8, 2).transpose(3, 0, 1, 2)
    m["s5_ldt_st"] = np.ascontiguousarray(np.repeat(r[:, None], 64, axis=1).reshape(128, L, 16))
    for nm, key in (("re", "s5_b_re"), ("im", "s5_b_im")):
        bb = f32(key)
        m["s5_Bt_" + nm] = np.ascontiguousarray(bb.reshape(L, 2, 2, 8, 64, 16).transpose(3, 5, 0, 1, 2, 4).reshape(128, L, 256))
    for nm, key in (("re", "s5_c_re"), ("im", "s5_c_im")):
        cc = f32(key).reshape(L, 2, 8, 2, 16, 64)
        o = np.zeros((2, 64, L, 2, 8, 2, 2, 16), np.float32)
        for e in range(2):
            for pr in range(8):
                o[e, :, :, :, pr, pr % 2, e, :] = cc[:, :, pr, e].transpose(3, 0, 1, 2)
        m["s5_C_st_" + nm] = np.ascontiguousarray(o.reshape(128, L, 1024))
    m["s5_dT"] = fm(inp["s5_d"])
    m["s5_w_glu"] = f32("s5_w_glu")
    m["rw_conv"] = np.ascontiguousarray(fm(inp["rwkv_conv"]).reshape(128, L, 24))
    m["rw_w0"] = np.ascontiguousarray(fm(inp["rwkv_w0"]).reshape(128, L, 4))
    m["rw_a0"] = np.ascontiguousarray(fm(inp["rwkv_a0"]).reshape(128, L, 4))
    pv = np.stack([fm(inp[k_]) for k_ in ("rwkv_k_k", "rwkv_k_a", "rwkv_r_k", "rwkv_ln_w", "rwkv_ln_b")], axis=2)
    m["rw_pv"] = np.ascontiguousarray(pv.reshape(128, L, 10))
    m["rw_w2a2"] = np.ascontiguousarray(np.concatenate([f32("rwkv_w2"), f32("rwkv_a2")], axis=2))
    m["rw_g2"] = f32("rwkv_g2")
    for k_ in ("proj_att", "proj_s5", "proj_rwkv", "w_out", "exp_w_gate", "exp_w_up", "exp_w_down"):
        m[k_] = f32(k_)
    m["router_w"] = np.ascontiguousarray(np.concatenate([f32("router_g_w"), f32("router_e_w")], axis=-1))
    rb = np.concatenate([f32("router_g_b"), f32("router_e_b")], axis=-1)
    m["router_b"] = np.ascontiguousarray(np.broadcast_to(rb[None], (128, L, 36)))
    m["qg"] = np.ascontiguousarray(np.tile(np.asarray(inp["q_gain"], np.float32), (1, 2)).T)
    m["kg"] = np.ascontiguousarray(np.tile(np.asarray(inp["k_gain"], np.float32), (1, 2)).T)
    return m


def kernel(**inp):
    nc, consts, info = build()
    in_maps = []
    for core in range(NCORES):
        m = host_inputs(inp, core)
        for k, v in consts.items():
            m["c_" + k] = v
        in_maps.append(m)
    res = run_bass_kernel_spmd(nc, in_maps, core_ids=list(range(NCORES)))
    outs = [r["out"] for r in res.results]
    o = np.concatenate(outs, axis=0)
    return np.ascontiguousarray(o.transpose(0, 2, 1)).astype(np.float32)
```
